# Optimizing a Trainium2 kernel written in Bass

```python
import jax
import jax.numpy as jnp
from jax import lax
import numpy as np

D_MODEL = 1024
BATCH = 16
SEQ = 4096
DEPTH = 4

GRID_W = 64
CTX_LEN = 256
N_MIXERS = 3

DN_ALPHA = (2 * DEPTH) ** 0.25
DN_BETA = (8 * DEPTH) ** -0.25
LN_EPS = 1e-5
RMS_EPS = 1e-6
ROPE_THETA = 10000.0
NEG_INF = -1e30

D_RNN = 3 * D_MODEL // 2
RG_BLOCKS = 6
RG_BW = D_RNN // RG_BLOCKS
CONV_W = 4
RG_C = 8.0

GQA_HEADS = 16
GQA_KV = 2
GQA_GROUP = GQA_HEADS // GQA_KV
GQA_HD = 64
WINDOW = 128
Q_BLOCK = 128

MLA_HEADS = 16
Q_LORA = 256
KV_LORA = 128
QK_NOPE = 64
QK_ROPE = 32
V_HD = 64

N_EXPERTS = 32
TOP_K = 4
D_FF = 1024
SWIGLU_LIMIT = 7.0
SWIGLU_ALPHA = 1.702
MOE_BLOCK = 256

N_RG = (DEPTH + 2) // 3
N_GQA = (DEPTH + 1) // 3
N_MLA = DEPTH // 3

kernel_name = 'hybrid_rglru_swa_mla_moe_flow_backbone'


def layer_norm(x, g, b):
    xf = x.astype(jnp.float32)
    mu = jnp.mean(xf, axis=-1, keepdims=True)
    var = jnp.mean(jnp.square(xf - mu), axis=-1, keepdims=True)
    return ((xf - mu) * lax.rsqrt(var + LN_EPS)).astype(x.dtype) * g + b


def rms_norm(x, g):
    xf = x.astype(jnp.float32)
    return (xf * lax.rsqrt(jnp.mean(xf * xf, axis=-1, keepdims=True) + RMS_EPS)).astype(x.dtype) * g


def modulate(h, shift, scale):
    return h * (1 + scale) + shift


def axial_rope_tables(n_tok, rot_dim):
    rows = n_tok // GRID_W
    row = jnp.repeat(jnp.arange(rows, dtype=jnp.float32), GRID_W)
    col = jnp.tile(jnp.arange(GRID_W, dtype=jnp.float32), rows)
    n_freq = rot_dim // 4
    inv_freq = ROPE_THETA ** (-jnp.arange(n_freq, dtype=jnp.float32) / n_freq)
    ang = jnp.concatenate([row[:, None] * inv_freq, col[:, None] * inv_freq], axis=-1)
    return jnp.cos(ang), jnp.sin(ang)


def apply_rope(x, cos, sin):
    half = x.shape[-1] // 2
    xf = x.astype(jnp.float32)
    x1, x2 = xf[..., :half], xf[..., half:]
    cs, sn = cos[None, :, None, :], sin[None, :, None, :]
    return jnp.concatenate([x1 * cs - x2 * sn, x2 * cs + x1 * sn], axis=-1).astype(x.dtype)


def depthwise_conv(x, w, b):
    y = lax.conv_general_dilated(x, w[:, None, :], window_strides=(1,), padding=[(1, 2)],
                                 dimension_numbers=('NWC', 'WIO', 'NWC'), feature_group_count=x.shape[-1])
    return y + b


def rglru_gates(xt, wa, ba, wx, bx, lam):
    L, B, C = xt.shape
    xb = xt.reshape(L, B, RG_BLOCKS, RG_BW)
    r = jax.nn.sigmoid(jnp.einsum('lbnc,ncd->lbnd', xb, wa.astype(jnp.float32)).reshape(L, B, C) + ba)
    i = jax.nn.sigmoid(jnp.einsum('lbnc,ncd->lbnd', xb, wx.astype(jnp.float32)).reshape(L, B, C) + bx)
    log_a = -RG_C * r * jax.nn.softplus(-lam.astype(jnp.float32))
    a = jnp.exp(log_a)
    u = jnp.sqrt(-jnp.expm1(2.0 * log_a)) * (i * xt)
    return a, u


def linear_scan(a, u, h0, reverse, emit):
    def step(h, au):
        h = au[0] * h + au[1]
        return h, (h if emit else None)
    return lax.scan(step, h0, (a, u), reverse=reverse)


def rglru_mixer(hc, hl, w_in, conv_w, conv_b, gate_a_w, gate_a_b, gate_x_w, gate_x_b, lam, w_out, need_ctx):
    B = hl.shape[0]
    pl = hl @ w_in
    pc = hc @ w_in
    xl_t = jnp.swapaxes(depthwise_conv(pl[..., D_RNN:], conv_w, conv_b), 0, 1).astype(jnp.float32)
    xc_t = jnp.swapaxes(depthwise_conv(pc[..., D_RNN:], conv_w, conv_b), 0, 1).astype(jnp.float32)
    h_lat = 0.0
    h_ctx = 0.0
    for d, rev in ((0, False), (1, True)):
        prm = (gate_a_w[d], gate_a_b[d], gate_x_w[d], gate_x_b[d], lam[d])
        ac, uc = rglru_gates(xc_t, *prm)
        hc_final, hc_seq = linear_scan(ac, uc, jnp.zeros((B, D_RNN), jnp.float32), rev, need_ctx)
        al, ul = rglru_gates(xl_t, *prm)
        _, hl_seq = linear_scan(al, ul, hc_final, rev, True)
        h_lat = h_lat + hl_seq
        if need_ctx:
            h_ctx = h_ctx + hc_seq
    yl = (jnp.swapaxes(h_lat, 0, 1).astype(hl.dtype) * jax.nn.gelu(pl[..., :D_RNN])) @ w_out
    yc = None
    if need_ctx:
        yc = (jnp.swapaxes(h_ctx, 0, 1).astype(hc.dtype) * jax.nn.gelu(pc[..., :D_RNN])) @ w_out
    return yc, yl


def gqa_window_mixer(hc, hl, w_qkv, b_qkv, sinks, w_o, b_o, need_ctx):
    B, S, _ = hl.shape
    n_ctx = hc.shape[1]
    q_dim, kv_dim = GQA_HEADS * GQA_HD, GQA_KV * GQA_HD

    def project(h):
        L = h.shape[1]
        p = h @ w_qkv + b_qkv
        q = p[..., :q_dim].reshape(B, L, GQA_HEADS, GQA_HD)
        k = p[..., q_dim:q_dim + kv_dim].reshape(B, L, GQA_KV, GQA_HD)
        v = p[..., q_dim + kv_dim:].reshape(B, L, GQA_KV, GQA_HD)
        return q, k, v

    qc, kc, vc = project(hc)
    ql, kl, vl = project(hl)
    cos, sin = axial_rope_tables(S, GQA_HD)
    ql = apply_rope(ql, cos, sin).reshape(B, S, GQA_KV, GQA_GROUP, GQA_HD)
    kl = apply_rope(kl, cos, sin)
    sink = sinks.astype(jnp.float32).reshape(GQA_KV, GQA_GROUP)
    scale = GQA_HD ** -0.5

    def attend(q, k, v, mask):
        s = jnp.einsum('bqkgd,bskd->bkgqs', q, k).astype(jnp.float32) * scale
        if mask is not None:
            s = jnp.where(mask, s, NEG_INF)
        s_sink = jnp.broadcast_to(sink[None, :, :, None, None], s.shape[:-1] + (1,))
        p = jax.nn.softmax(jnp.concatenate([s, s_sink], axis=-1), axis=-1)[..., :-1]
        return jnp.einsum('bkgqs,bskd->bqkgd', p.astype(v.dtype), v)

    span = Q_BLOCK + 2 * WINDOW
    k_pad = jnp.pad(kl, ((0, 0), (WINDOW, WINDOW), (0, 0), (0, 0)))
    v_pad = jnp.pad(vl, ((0, 0), (WINDOW, WINDOW), (0, 0), (0, 0)))
    rel = jnp.arange(span)[None, :] - WINDOW - jnp.arange(Q_BLOCK)[:, None]
    band = jnp.abs(rel) <= WINDOW
    ctx_mask = jnp.ones((Q_BLOCK, n_ctx), dtype=bool)

    def block(j):
        start = j * Q_BLOCK
        q_j = lax.dynamic_slice_in_dim(ql, start, Q_BLOCK, axis=1)
        k_j = jnp.concatenate([lax.dynamic_slice_in_dim(k_pad, start, span, axis=1), kc], axis=1)
        v_j = jnp.concatenate([lax.dynamic_slice_in_dim(v_pad, start, span, axis=1), vc], axis=1)
        key_pos = start - WINDOW + jnp.arange(span)
        lat_mask = band & ((key_pos >= 0) & (key_pos < S))[None, :]
        return attend(q_j, k_j, v_j, jnp.concatenate([lat_mask, ctx_mask], axis=-1))

    o = lax.map(block, jnp.arange(S // Q_BLOCK))
    o = jnp.moveaxis(o, 0, 1).reshape(B, S, q_dim)
    yl = o @ w_o + b_o
    yc = None
    if need_ctx:
        oc = attend(qc.reshape(B, n_ctx, GQA_KV, GQA_GROUP, GQA_HD), kc, vc, None)
        yc = oc.reshape(B, n_ctx, q_dim) @ w_o + b_o
    return yc, yl


def mla_mixer(hc, hl, w_down, q_norm, kv_norm, w_uq, w_ukv, w_o, need_ctx):
    B, S, _ = hl.shape
    n_ctx = hc.shape[1]
    cos, sin = axial_rope_tables(S, QK_ROPE)

    def project(h, rotate):
        L = h.shape[1]
        p = h @ w_down
        cq = rms_norm(p[..., :Q_LORA], q_norm)
        ckv = rms_norm(p[..., Q_LORA:Q_LORA + KV_LORA], kv_norm)
        k_rope = p[..., Q_LORA + KV_LORA:][:, :, None, :]
        q = (cq @ w_uq).reshape(B, L, MLA_HEADS, QK_NOPE + QK_ROPE)
        kv = (ckv @ w_ukv).reshape(B, L, MLA_HEADS, QK_NOPE + V_HD)
        q_nope, q_rope = q[..., :QK_NOPE], q[..., QK_NOPE:]
        if rotate:
            q_rope = apply_rope(q_rope, cos, sin)
            k_rope = apply_rope(k_rope, cos, sin)
        q = jnp.concatenate([q_nope, q_rope], axis=-1)
        k = jnp.concatenate([kv[..., :QK_NOPE], jnp.broadcast_to(k_rope, (B, L, MLA_HEADS, QK_ROPE))], axis=-1)
        return q, k, kv[..., QK_NOPE:]

    qc, kc, vc = project(hc, False)
    ql, kl, vl = project(hl, True)
    k_all = jnp.concatenate([kc, kl], axis=1)
    v_all = jnp.concatenate([vc, vl], axis=1)
    scale = (QK_NOPE + QK_ROPE) ** -0.5

    def attend(q, k, v):
        s = jnp.einsum('bqhd,bshd->bhqs', q, k).astype(jnp.float32) * scale
        p = jax.nn.softmax(s, axis=-1)
        return jnp.einsum('bhqs,bshd->bqhd', p.astype(v.dtype), v)

    def block(j):
        return attend(lax.dynamic_slice_in_dim(ql, j * Q_BLOCK, Q_BLOCK, axis=1), k_all, v_all)

    o = lax.map(block, jnp.arange(S // Q_BLOCK))
    yl = jnp.moveaxis(o, 0, 1).reshape(B, S, MLA_HEADS * V_HD) @ w_o
    yc = None
    if need_ctx:
        yc = attend(qc, kc, vc).reshape(B, n_ctx, MLA_HEADS * V_HD) @ w_o
    return yc, yl


def moe_ffn(h, router_w, router_b, w_gu, b_gu, w_down, b_down):
    n_tok, d = h.shape
    logits = (h @ router_w + router_b).astype(jnp.float32)
    top_logit, top_idx = lax.top_k(logits, TOP_K)
    gate = jax.nn.softmax(top_logit, axis=-1)
    n_asg = n_tok * TOP_K
    e_flat = top_idx.reshape(-1)
    order = jnp.argsort(e_flat)
    e_sorted = e_flat[order]
    tok_sorted = (order // TOP_K).astype(jnp.int32)
    gate_sorted = gate.reshape(-1)[order]
    count = jnp.bincount(e_flat, length=N_EXPERTS)
    start = jnp.cumsum(count) - count
    padded = (count + MOE_BLOCK - 1) // MOE_BLOCK * MOE_BLOCK
    pad_end = jnp.cumsum(padded)
    dest = pad_end[e_sorted] - padded[e_sorted] + jnp.arange(n_asg) - start[e_sorted]
    n_blocks = (n_asg + N_EXPERTS * (MOE_BLOCK - 1) + MOE_BLOCK - 1) // MOE_BLOCK
    n_slots = n_blocks * MOE_BLOCK
    slot_tok = jnp.full((n_slots,), n_tok, jnp.int32).at[dest].set(tok_sorted)
    slot_gate = jnp.zeros((n_slots,), jnp.float32).at[dest].set(gate_sorted)
    block_expert = jnp.minimum(jnp.searchsorted(pad_end, jnp.arange(n_blocks) * MOE_BLOCK, side='right'),
                               N_EXPERTS - 1)
    h_pad = jnp.concatenate([h, jnp.zeros((1, d), h.dtype)], axis=0)

    def expert_block(args):
        idx, e = args
        hu = h_pad[idx] @ w_gu[e] + b_gu[e]
        g = jnp.minimum(hu[:, 0::2], SWIGLU_LIMIT)
        u = jnp.clip(hu[:, 1::2], -SWIGLU_LIMIT, SWIGLU_LIMIT)
        return ((u + 1) * (g * jax.nn.sigmoid(SWIGLU_ALPHA * g))) @ w_down[e] + b_down[e]

    y = lax.map(expert_block, (slot_tok.reshape(n_blocks, MOE_BLOCK), block_expert))
    y = y.reshape(n_slots, d) * slot_gate[:, None]
    return jax.ops.segment_sum(y, slot_tok, num_segments=n_tok + 1)[:n_tok].astype(h.dtype)


def setup_inputs(seed: int = 0) -> dict:
    key = jax.random.key(seed)
    ks = iter(jax.random.split(key, 40))
    f32 = jnp.float32
    D = D_MODEL

    def nrm(shape, scale):
        return jax.random.normal(next(ks), shape, f32) * scale

    a0 = jax.random.uniform(next(ks), (N_RG, 2, D_RNN), f32, 0.9, 0.999)
    rg_lambda = jnp.log(a0) - jnp.log1p(-a0)
    return {
        'x': nrm((BATCH, SEQ, D), 1.0),
        'c': nrm((BATCH, D), 1.0),
        'ctx': nrm((BATCH, CTX_LEN, D), 1.0),
        'c_ctx': nrm((D,), 1.0),
        'ada_w': nrm((DEPTH, D, 6 * D), 0.5 * D ** -0.5),
        'ada_b': nrm((DEPTH, 6 * D), 0.02),
        'ln1_g': 1.0 + nrm((DEPTH, D), 0.02),
        'ln1_b': nrm((DEPTH, D), 0.02),
        'ln2_g': 1.0 + nrm((DEPTH, D), 0.02),
        'ln2_b': nrm((DEPTH, D), 0.02),
        'router_w': nrm((DEPTH, D, N_EXPERTS), D ** -0.5),
        'router_b': nrm((DEPTH, N_EXPERTS), 0.01),
        'exp_gu_w': nrm((DEPTH, N_EXPERTS, D, 2 * D_FF), D ** -0.5),
        'exp_gu_b': nrm((DEPTH, N_EXPERTS, 2 * D_FF), 0.02),
        'exp_down_w': nrm((DEPTH, N_EXPERTS, D_FF, D), D_FF ** -0.5 * DN_BETA),
        'exp_down_b': nrm((DEPTH, N_EXPERTS, D), 0.02),
        'rg_w_in': nrm((N_RG, D, 2 * D_RNN), D ** -0.5),
        'rg_conv_w': nrm((N_RG, CONV_W, D_RNN), CONV_W ** -0.5),
        'rg_conv_b': nrm((N_RG, D_RNN), 0.02),
        'rg_gate_a_w': nrm((N_RG, 2, RG_BLOCKS, RG_BW, RG_BW), RG_BW ** -0.5),
        'rg_gate_a_b': nrm((N_RG, 2, D_RNN), 0.02),
        'rg_gate_x_w': nrm((N_RG, 2, RG_BLOCKS, RG_BW, RG_BW), RG_BW ** -0.5),
        'rg_gate_x_b': nrm((N_RG, 2, D_RNN), 0.02),
        'rg_lambda': rg_lambda,
        'rg_w_out': nrm((N_RG, D_RNN, D), D_RNN ** -0.5 * DN_BETA),
        'gqa_w_qkv': nrm((N_GQA, D, (GQA_HEADS + 2 * GQA_KV) * GQA_HD), D ** -0.5),
        'gqa_b_qkv': nrm((N_GQA, (GQA_HEADS + 2 * GQA_KV) * GQA_HD), 0.02),
        'gqa_sinks': nrm((N_GQA, GQA_HEADS), 1.0),
        'gqa_w_o': nrm((N_GQA, GQA_HEADS * GQA_HD, D), (GQA_HEADS * GQA_HD) ** -0.5 * DN_BETA),
        'gqa_b_o': nrm((N_GQA, D), 0.02),
        'mla_w_down': nrm((N_MLA, D, Q_LORA + KV_LORA + QK_ROPE), D ** -0.5),
        'mla_q_norm': 1.0 + nrm((N_MLA, Q_LORA), 0.02),
        'mla_kv_norm': 1.0 + nrm((N_MLA, KV_LORA), 0.02),
        'mla_w_uq': nrm((N_MLA, Q_LORA, MLA_HEADS * (QK_NOPE + QK_ROPE)), Q_LORA ** -0.5),
        'mla_w_ukv': nrm((N_MLA, KV_LORA, MLA_HEADS * (QK_NOPE + V_HD)), KV_LORA ** -0.5),
        'mla_w_o': nrm((N_MLA, MLA_HEADS * V_HD, D), (MLA_HEADS * V_HD) ** -0.5 * DN_BETA),
    }


def reference(x, c, ctx, c_ctx, ada_w, ada_b, ln1_g, ln1_b, ln2_g, ln2_b, router_w, router_b,
              exp_gu_w, exp_gu_b, exp_down_w, exp_down_b, rg_w_in, rg_conv_w, rg_conv_b,
              rg_gate_a_w, rg_gate_a_b, rg_gate_x_w, rg_gate_x_b, rg_lambda, rg_w_out,
              gqa_w_qkv, gqa_b_qkv, gqa_sinks, gqa_w_o, gqa_b_o,
              mla_w_down, mla_q_norm, mla_kv_norm, mla_w_uq, mla_w_ukv, mla_w_o):
    B, S, D = x.shape
    n_ctx = ctx.shape[1]
    xl, xc = x, ctx
    silu_c = jax.nn.silu(c)
    silu_cc = jax.nn.silu(c_ctx)
    for i in range(DEPTH):
        need_ctx = i < DEPTH - 1
        sh1, sc1, g1, sh2, sc2, g2 = jnp.split((silu_c @ ada_w[i] + ada_b[i])[:, None, :], 6, axis=-1)
        csh1, csc1, cg1, csh2, csc2, cg2 = jnp.split(silu_cc @ ada_w[i] + ada_b[i], 6, axis=-1)
        hl = modulate(xl, sh1, sc1)
        hc = modulate(xc, csh1, csc1)
        kind, j = i % N_MIXERS, i // N_MIXERS
        if kind == 0:
            yc, yl = rglru_mixer(hc, hl, rg_w_in[j], rg_conv_w[j], rg_conv_b[j], rg_gate_a_w[j], rg_gate_a_b[j],
                                 rg_gate_x_w[j], rg_gate_x_b[j], rg_lambda[j], rg_w_out[j], need_ctx)
        elif kind == 1:
            yc, yl = gqa_window_mixer(hc, hl, gqa_w_qkv[j], gqa_b_qkv[j], gqa_sinks[j], gqa_w_o[j], gqa_b_o[j],
                                      need_ctx)
        else:
            yc, yl = mla_mixer(hc, hl, mla_w_down[j], mla_q_norm[j], mla_kv_norm[j], mla_w_uq[j], mla_w_ukv[j],
                               mla_w_o[j], need_ctx)
        xl = layer_norm(DN_ALPHA * xl + g1 * yl, ln1_g[i], ln1_b[i])
        hl2 = modulate(xl, sh2, sc2).reshape(B * S, D)
        if need_ctx:
            xc = layer_norm(DN_ALPHA * xc + cg1 * yc, ln1_g[i], ln1_b[i])
            hc2 = modulate(xc, csh2, csc2).reshape(B * n_ctx, D)
            tokens = jnp.concatenate([hc2, hl2], axis=0)
        else:
            tokens = hl2
        y = moe_ffn(tokens, router_w[i], router_b[i], exp_gu_w[i], exp_gu_b[i], exp_down_w[i], exp_down_b[i])
        xl = layer_norm(DN_ALPHA * xl + g2 * y[y.shape[0] - B * S:].reshape(B, S, D), ln2_g[i], ln2_b[i])
        if need_ctx:
            xc = layer_norm(DN_ALPHA * xc + cg2 * y[:B * n_ctx].reshape(B, n_ctx, D), ln2_g[i], ln2_b[i])
    return xl
```

```python
import numpy as np
from contextlib import ExitStack
import concourse.bass as bass
import concourse.mybir as mybir
from concourse.bass_utils import run_bass_kernel_spmd

F32 = mybir.dt.float32
I32 = mybir.dt.int32
U32 = mybir.dt.uint32
AF = mybir.ActivationFunctionType
ALU = mybir.AluOpType
AX = mybir.AxisListType

D = 1024
C = 256
E = 32
FF = 1024
D_RNN = 1536
SB = 512
ALPHA = 8.0 ** 0.25
LN_EPS = 1e-5


class Res:
    __slots__ = ("w", "r")

    def __init__(self):
        self.w = None
        self.r = {}


class T:
    def __init__(self, h):
        self.h = h
        self.res = [Res()]

    def __getitem__(self, k):
        return self.h[k]


class KB:
    def __init__(self, n_dma_sems=48):
        self.nc = bass.Bass("TRN2", target_bir_lowering=False)
        nc = self.nc
        self.es = ExitStack()
        self.stacks = []
        self.eng = {"pe": nc.tensor, "act": nc.scalar, "dve": nc.vector, "pool": nc.gpsimd, "sp": nc.sync}
        self.sems = []
        self.esem = {}
        self.cnt = {}
        self.waited = {e: {} for e in self.eng}
        for e in self.eng:
            self.esem[e] = self._newsem("s_" + e)
            self.cnt[e] = 0
        self.dsem = [self._newsem("d%d" % i) for i in range(n_dma_sems)]
        self.duses = [0] * n_dma_sems
        self.drr = 0
        self.ninst = 0
        self.uid = 0

    def _newsem(self, name):
        h = self.es.enter_context(self.nc.semaphore(name))
        self.sems.append(h)
        return len(self.sems) - 1

    def push(self):
        self.stacks.append(ExitStack())

    def pop(self):
        self.barrier()
        self.stacks.pop().close()

    def _ctx(self):
        return self.stacks[-1] if self.stacks else self.es

    def sb(self, name, shape, dtype=F32):
        self.uid += 1
        return T(self._ctx().enter_context(self.nc.sbuf_tensor("%s_%d" % (name, self.uid), list(shape), dtype)))

    def ps(self, name, shape, dtype=F32):
        return T(self.es.enter_context(self.nc.psum_tensor(name, list(shape), dtype)))

    def dram(self, name, shape, dtype=F32, kind="Internal"):
        return self.nc.dram_tensor(name, list(shape), dtype, kind=kind)

    def _wait(self, e, deps):
        eng = self.eng[e]
        best = {}
        for d in deps:
            if d is None:
                continue
            s, v, src = d
            if src == e and e == "pe":
                continue
            if self.waited[e].get(s, 0) >= v:
                continue
            if best.get(s, 0) < v:
                best[s] = v
        for s, v in best.items():
            eng.wait_ge(self.sems[s], v)
            self.waited[e][s] = v

    def _collect(self, r, w):
        deps = []
        for x in r:
            for rs in x.res:
                deps.append(rs.w)
        for x in w:
            for rs in x.res:
                deps.append(rs.w)
                deps.extend(rs.r.values())
        return deps

    def _commit(self, r, w, dep, key):
        for x in r:
            for rs in x.res:
                rs.r[key] = dep
        for x in w:
            for rs in x.res:
                rs.w = dep
                rs.r = {}

    def op(self, e, fn, r=(), w=()):
        self._wait(e, self._collect(r, w))
        inst = fn(self.eng[e])
        self.cnt[e] += 1
        inst.then_inc(self.sems[self.esem[e]], 1)
        dep = (self.esem[e], self.cnt[e], e)
        self._commit(r, w, dep, e)
        self.ninst += 1
        return inst

    def dma(self, q, out, in_, r=(), w=(), fn=None):
        i = self.drr
        self.drr = (i + 1) % len(self.dsem)
        s = self.dsem[i]
        deps = self._collect(r, w)
        deps.append((s, self.duses[i] * 16, "dma"))
        self._wait(q, deps)
        eng = self.eng[q]
        if fn is None:
            inst = eng.dma_start(out=out, in_=in_)
        else:
            inst = fn(eng)
        inst.then_inc(self.sems[s], 16)
        self.duses[i] += 1
        dep = (s, self.duses[i] * 16, "dma")
        self._commit(r, w, dep, ("dma", s))
        self.ninst += 1
        return inst

    def barrier(self):
        deps = [(self.esem[e], self.cnt[e], "x") for e in self.eng]
        deps += [(s, self.duses[i] * 16, "x") for i, s in enumerate(self.dsem)]
        for e in self.eng:
            self._wait(e, [d for d in deps if d[0] != self.esem[e]])

    def close(self):
        self.barrier()
        self.es.close()


class Cfg:
    def __init__(self, NB=2, S=4096, kinds=(0, 1, 2, 0), NC=8, last_noctx=True, debug=False):
        self.debug = debug
        self.NB = NB
        self.S = S
        self.kinds = list(kinds)
        self.NC = NC
        self.last_noctx = last_noctx


def build(cfg):
    NB, S, kinds = cfg.NB, cfg.S, cfg.kinds
    L = len(kinds)
    Tn = C + S
    NT = Tn // 128
    NS = NB + 1
    kb = KB()
    nc = kb.nc
    op, dma = kb.op, kb.dma

    def din(name, shape, dt=F32):
        return kb.dram(name, shape, dt, kind="ExternalInput")

    xin = din("xin", [NB * Tn, D])
    cc = din("cc", [NS, D])
    ada_w = din("ada_w", [L * D, 6 * D])
    ada_b = din("ada_b", [L, 6 * D])
    lnp = din("lnp", [L * 4, D])
    router_w = din("router_w", [L * D, E])
    router_b = din("router_b", [L, E])
    wgu = din("wgu", [L * E * D, 2 * FF])
    bgu = din("bgu", [L * E, 2 * FF])
    wdn = din("wdn", [L * E * FF, D])
    bdn = din("bdn", [L * E, D])
    cmat = din("cmat", [128, 384 + 32])
    w_id = din("w_id", [D, D]) if -1 in kinds else None
    n_rg = max(1, kinds.count(0))
    if 2 in kinds:
        n_ml = kinds.count(2)
        ml_wd = din("ml_wd", [n_ml * D, 448])
        ml_norm = din("ml_norm", [n_ml, 384])
        ml_wuq = din("ml_wuq", [n_ml * 256, 2048])
        ml_wukv = din("ml_wukv", [n_ml * 128, 2048])
        ml_w_o = din("ml_w_o", [n_ml * D, D])
        rope32 = din("rope32", [64, S])
        CT = kb.dram("CT", [384, Tn])
        KRT = kb.dram("KRT", [32, Tn])
    if 1 in kinds:
        n_gq = kinds.count(1)
        gq_w = din("gq_w", [n_gq * D, 2432])
        gq_b64 = din("gq_b64", [n_gq * 64, 40])
        gq_bv = din("gq_bv", [n_gq, 128])
        gq_sinks = din("gq_sinks", [n_gq, 16])
        gq_w_o = din("gq_w_o", [n_gq * D, D])
        gq_b_o = din("gq_b_o", [n_gq, D])
        rope64 = din("rope64", [2 * 64, S])
        QT = kb.dram("QT", [64, 16 * Tn])
        KT = kb.dram("KT", [64, 2 * Tn])
        VA = kb.dram("VA", [Tn, 130])
    if 0 in kinds:
        rg_w_in = din("rg_w_in", [n_rg * D, 2 * D_RNN])
        rg_p = din("rg_p", [n_rg * 11, D_RNN])
        rg_ga = din("rg_ga", [n_rg * 2 * 6 * 256, 256])
        rg_gx = din("rg_gx", [n_rg * 2 * 6 * 256, 256])
        rg_w_out = din("rg_w_out", [n_rg * D_RNN, D])
        PR = kb.dram("PR", [2 * D_RNN, Tn])
    out = kb.dram("out", [NB * S, D], F32, kind="ExternalOutput")

    dk = "ExternalOutput" if cfg.debug else "Internal"
    XA = kb.dram("XA", [NB * Tn, D], F32, dk)
    XB = kb.dram("XB", [NB * Tn, D], F32, dk)
    H2 = kb.dram("H2", [NB * Tn, D], F32, dk)
    HT = kb.dram("HT", [D, Tn], F32, dk)
    OT = kb.dram("OT", [D_RNN, Tn], F32, dk)
    MODD = kb.dram("MODD", [NS, 6 * D], F32, dk)
    n_asg_max = NB * Tn * 4
    NBLK = (n_asg_max + E * (SB - 1) + SB - 1) // SB
    NSLOT = NBLK * SB
    XS = kb.dram("XS", [NSLOT, D], F32, dk)
    YS = kb.dram("YS", [NSLOT, D], F32, dk)
    if cfg.debug:
        DBG = {n: kb.dram("dbg_" + n, shp, dt, "ExternalOutput") for n, shp, dt in [
            ("slot4", [128, NB * NT * 4], I32), ("gate4", [128, NB * NT * 4], F32), ("top8", [128, NB * NT * 8], F32),
            ("bef", [128, NBLK], F32), ("runcnt", [128, E], F32), ("excl", [128, E], F32), ("padf", [128, E], F32), ("incl", [128, E], F32), ("posA", [128, NB * NT * E], F32)]}

    cm = kb.sb("cm", [128, 416])
    ident = lambda n=128: cm[0:n, 0:n]
    tri = cm[:, 128:256]
    ones = cm[:, 256:384]
    iota32 = cm[:, 384:416]
    dma("sp", cm[:], cmat[:, :], w=[cm])
    PS = [kb.ps("ps%d" % i, [128, 512]) for i in range(8)]
    psi = [0]
    psn = [8]

    def nps():
        psi[0] = (psi[0] + 1) % psn[0]
        return PS[psi[0]]

    siluT = kb.sb("siluT", [128, 8, NS])
    modT = kb.sb("modT", [128, 48, NS])
    lnrow = kb.sb("lnrow", [128, 4, D])
    epsT = kb.sb("epsT", [128, 1])
    op("dve", lambda e: e.memset(epsT[:], LN_EPS), w=[epsT])

    kb.push()
    ccs = kb.sb("ccs", [NS, D])
    dma("sp", ccs[:], cc[:, :], w=[ccs])
    op("act", lambda e: e.activation(out=ccs[:], in_=ccs[:], func=AF.Silu), r=[ccs], w=[ccs])
    p = nps()
    for c in range(8):
        op("pe", lambda e: e.transpose(p[:, c * NS:(c + 1) * NS], ccs[:, c * 128:(c + 1) * 128], ident(NS)), r=[ccs, cm], w=[p])
    op("dve", lambda e: e.tensor_copy(out=siluT[:].rearrange("p c s -> p (c s)"), in_=p[:, 0:8 * NS]), r=[p], w=[siluT])
    kb.pop()

    def phase_mod(i):
        kb.push()
        aw = [kb.sb("aw", [128, 8, 512]) for _ in range(2)]
        modtm = kb.sb("modtm", [NS, 6 * D])
        adab = kb.sb("adab", [NS, 6 * D])
        for s in range(NS):
            dma("sp", adab[s:s + 1, :], ada_b[i:i + 1, :], w=[adab])
        for g in range(12):
            a = aw[g % 2]
            dma("sp", a[:], ada_w[i * D:(i + 1) * D, g * 512:(g + 1) * 512].rearrange("(c p) n -> p c n", p=128), w=[a])
            p = nps()
            for kc in range(8):
                op("pe", lambda e: e.matmul(p[0:NS, :], siluT[:, kc, :], a[:, kc, :], start=(kc == 0), stop=(kc == 7)),
                   r=[siluT, a], w=[p])
            op("dve", lambda e: e.tensor_tensor(out=modtm[:, g * 512:(g + 1) * 512], in0=p[0:NS, :], in1=adab[:, g * 512:(g + 1) * 512], op=ALU.add),
               r=[p, adab], w=[modtm])
        for k in (1, 4):
            op("dve", lambda e: e.tensor_scalar(out=modtm[:, k * D:(k + 1) * D], in0=modtm[:, k * D:(k + 1) * D], scalar1=1.0, scalar2=None, op0=ALU.add),
               r=[modtm], w=[modtm])
        dma("sp", MODD[:, :], modtm[:], r=[modtm])
        p = nps()
        for j in range(48):
            op("pe", lambda e: e.transpose(p[:, j * NS:(j + 1) * NS], modtm[:, j * 128:(j + 1) * 128], ident(NS)), r=[modtm, cm], w=[p])
        op("dve", lambda e: e.tensor_copy(out=modT[:].rearrange("p c s -> p (c s)"), in_=p[:, 0:48 * NS]), r=[p], w=[modT])
        for k in range(4):
            dma("sp", lnrow[:, k, :], lnp[i * 4 + k, :].partition_broadcast(128), w=[lnrow])
        kb.pop()

    def groups():
        gs = [(0, C)]
        t = C
        while t < Tn:
            gs.append((t, min(512, Tn - t)))
            t += 512
        return gs

    def phase_h(i, b, Xsrc):
        kb.push()
        xt = [kb.sb("xt", [128, D]) for _ in range(8)]
        hT = [kb.sb("hT", [128, 8, 512]) for _ in range(2)]
        n = 0
        for gi, (t0, tl) in enumerate(groups()):
            s = NB if t0 < C else b
            nt = tl // 128
            tiles = []
            for j in range(nt):
                x = xt[n % 8]
                n += 1
                dma("sp", x[:], Xsrc[b * Tn + t0 + j * 128: b * Tn + t0 + (j + 1) * 128, :], w=[x])
                tiles.append(x)
            h = hT[gi % 2]
            for c in range(8):
                p = nps()
                for j in range(nt):
                    x = tiles[j]
                    op("pe", lambda e: e.transpose(p[:, j * 128:(j + 1) * 128], x[:, c * 128:(c + 1) * 128], ident()), r=[x, cm], w=[p])
                op("dve", lambda e: e.tensor_scalar(out=h[:, c, 0:tl], in0=p[:, 0:tl], scalar1=modT[:, 8 + c, s:s + 1], scalar2=modT[:, c, s:s + 1],
                                                    op0=ALU.mult, op1=ALU.add), r=[p, modT], w=[h])
            dma("pool", HT[:, t0:t0 + tl].rearrange("(c p) t -> p c t", p=128), h[:, :, 0:tl], r=[h])
        kb.pop()

    def layernorm(t, k0, st, mv, tmp):
        for h2 in range(2):
            op("dve", lambda e: e.bn_stats(out=st[:, h2, :], in_=t[:, h2 * 512:(h2 + 1) * 512]), r=[t], w=[st])
        op("dve", lambda e: e.bn_aggr(out=mv[:, 0:2], in_=st[:].rearrange("p a b -> p (a b)")), r=[st], w=[mv])
        op("act", lambda e: e.activation(out=mv[:, 2:3], in_=mv[:, 1:2], func=AF.Sqrt, bias=epsT[:, 0:1], scale=1.0), r=[mv, epsT], w=[mv])
        op("dve", lambda e: e.reciprocal(out=mv[:, 3:4], in_=mv[:, 2:3]), r=[mv], w=[mv])
        op("dve", lambda e: e.tensor_scalar(out=t[:], in0=t[:], scalar1=mv[:, 0:1], scalar2=mv[:, 3:4], op0=ALU.subtract, op1=ALU.mult), r=[t, mv], w=[t])
        op("dve", lambda e: e.tensor_tensor(out=t[:], in0=t[:], in1=lnrow[:, k0, :], op=ALU.mult), r=[t, lnrow], w=[t])
        op("dve", lambda e: e.tensor_tensor(out=t[:], in0=t[:], in1=lnrow[:, k0 + 1, :], op=ALU.add), r=[t, lnrow], w=[t])

    NTT = NB * NT
    maskA = kb.sb("maskA", [128, NTT, E])
    top8 = kb.sb("top8", [128, NTT, 8])
    idx8 = kb.sb("idx8", [128, NTT, 8], U32)
    posA = kb.sb("posA", [128, NTT, E])
    runcnt = kb.sb("runcnt", [128, E])
    slot4f = kb.sb("slot4f", [128, NTT, 4])
    slot4 = kb.sb("slot4", [128, NTT, 4], I32)
    gate4 = kb.sb("gate4", [128, NTT, 4])

    def phase_out(i, b, Xsrc, w_o, Dm, b_o, need_ctx):
        kb.push()
        KC = Dm // 128
        wo = kb.sb("wo", [128, KC, D])
        dma("sp", wo[:], w_o.rearrange("(c p) n -> p c n", p=128), w=[wo])
        rw = kb.sb("rw", [128, 8, E])
        dma("sp", rw[:], router_w[i * D:(i + 1) * D, :].rearrange("(c p) n -> p c n", p=128), w=[rw])
        rbrow = kb.sb("rbrow", [128, E])
        dma("sp", rbrow[:], router_b[i, :].partition_broadcast(128), w=[rbrow])
        grow = {}
        for s in (NB, b):
            g = kb.sb("grow", [128, 3, D])
            for k, kind in enumerate((2, 4, 3)):
                dma("sp", g[:, k, :], MODD[s, kind * D:(kind + 1) * D].partition_broadcast(128), w=[g])
            grow[s] = g
        borow = None
        if b_o is not None:
            borow = kb.sb("borow", [128, D])
            dma("sp", borow[:], b_o.partition_broadcast(128), w=[borow])
        oTs = [kb.sb("oT", [128, KC, 128]) for _ in range(2)]
        xts = [kb.sb("xt", [128, D]) for _ in range(2)]
        ts = [kb.sb("tt", [128, D]) for _ in range(2)]
        h2s = [kb.sb("h2", [128, D]) for _ in range(2)]
        h2Ts = [kb.sb("h2T", [128, 8, 128]) for _ in range(2)]
        st = kb.sb("st", [128, 2, 6])
        mv = kb.sb("mv", [128, 4])
        lg = kb.sb("lg", [128, E])
        t_start = 0 if need_ctx else C // 128
        for ti in range(t_start, NT):
            s = NB if ti < C // 128 else b
            g = grow[s]
            r0 = b * Tn + ti * 128
            gi = b * NT + ti
            oT = oTs[ti % 2]; x = xts[ti % 2]; t = ts[ti % 2]; h2 = h2s[ti % 2]; h2T = h2Ts[ti % 2]
            dma("sp", oT[:], OT[0:Dm, ti * 128:(ti + 1) * 128].rearrange("(c p) t -> p c t", p=128), w=[oT])
            dma("sp", x[:], Xsrc[r0:r0 + 128, :], w=[x])
            for hf in range(2):
                p = nps()
                for kc in range(KC):
                    op("pe", lambda e: e.matmul(p[:, :], oT[:, kc, :], wo[:, kc, hf * 512:(hf + 1) * 512], start=(kc == 0), stop=(kc == KC - 1)),
                       r=[oT, wo], w=[p])
                sl = slice(hf * 512, (hf + 1) * 512)
                if borow is not None:
                    op("dve", lambda e: e.tensor_tensor(out=t[:, sl], in0=p[:, :], in1=borow[:, sl], op=ALU.add), r=[p, borow], w=[t])
                    op("dve", lambda e: e.tensor_tensor(out=t[:, sl], in0=t[:, sl], in1=g[:, 0, sl], op=ALU.mult), r=[t, g], w=[t])
                else:
                    op("dve", lambda e: e.tensor_tensor(out=t[:, sl], in0=p[:, :], in1=g[:, 0, sl], op=ALU.mult), r=[p, g], w=[t])
            op("dve", lambda e: e.scalar_tensor_tensor(out=t[:], in0=x[:], scalar=ALPHA, in1=t[:], op0=ALU.mult, op1=ALU.add), r=[x, t], w=[t])
            layernorm(t, 0, st, mv, None)
            dma("pool", XA[r0:r0 + 128, :], t[:], r=[t])
            op("pool", lambda e: e.tensor_tensor(out=h2[:], in0=t[:], in1=g[:, 1, :], op=ALU.mult), r=[t, g], w=[h2])
            op("pool", lambda e: e.tensor_tensor(out=h2[:], in0=h2[:], in1=g[:, 2, :], op=ALU.add), r=[h2, g], w=[h2])
            dma("pool", H2[r0:r0 + 128, :], h2[:], r=[h2])
            for half in range(2):
                p = nps()
                for c4 in range(4):
                    c = half * 4 + c4
                    op("pe", lambda e: e.transpose(p[:, c4 * 128:(c4 + 1) * 128], h2[:, c * 128:(c + 1) * 128], ident()), r=[h2, cm], w=[p])
                op("act", lambda e: e.activation(out=h2T[:, half * 4:(half + 1) * 4, :].rearrange("p c t -> p (c t)"), in_=p[:, :], func=AF.Copy),
                   r=[p], w=[h2T])
            p = nps()
            for c in range(8):
                op("pe", lambda e: e.matmul(p[:, 0:E], h2T[:, c, :], rw[:, c, :], start=(c == 0), stop=(c == 7)), r=[h2T, rw], w=[p])
            op("dve", lambda e: e.tensor_tensor(out=lg[:], in0=p[:, 0:E], in1=rbrow[:], op=ALU.add), r=[p, rbrow], w=[lg])
            op("dve", lambda e: e.max(out=top8[:, gi, :], in_=lg[:]), r=[lg], w=[top8])
            op("dve", lambda e: e.max_index(out=idx8[:, gi, :], in_max=top8[:, gi, :], in_values=lg[:]), r=[lg, top8], w=[idx8])
            op("dve", lambda e: e.tensor_scalar(out=maskA[:, gi, :], in0=lg[:], scalar1=top8[:, gi, 3:4], scalar2=None, op0=ALU.is_ge), r=[lg, top8], w=[maskA])
        kb.pop()

    def phase_moe(i, last):
        tiles = []
        for b in range(NB):
            for ti in range(NT):
                if last and cfg.last_noctx and ti < C // 128:
                    continue
                tiles.append((b, ti))
        kb.push()
        op("dve", lambda e: e.memset(runcnt[:], 0.0), w=[runcnt])
        for (b, ti) in tiles:
            gi = b * NT + ti
            p = nps()
            op("pe", lambda e: e.matmul(p[:, 0:E], tri, maskA[:, gi, :], start=True, stop=True), r=[cm, maskA], w=[p])
            op("pe", lambda e: e.matmul(p[:, E:2 * E], ones, maskA[:, gi, :], start=True, stop=True), r=[cm, maskA], w=[p])
            op("dve", lambda e: e.tensor_tensor(out=posA[:, gi, :], in0=p[:, 0:E], in1=runcnt[:], op=ALU.add), r=[p, runcnt], w=[posA])
            op("dve", lambda e: e.tensor_tensor(out=runcnt[:], in0=p[:, E:2 * E], in1=runcnt[:], op=ALU.add), r=[p, runcnt], w=[runcnt])
        padf = kb.sb("padf", [128, E])
        incl = kb.sb("incl", [128, E])
        excl = kb.sb("excl", [128, E])
        zer = kb.sb("zer", [128, E])
        op("dve", lambda e: e.memset(zer[:], 0.0), w=[zer])
        blkpos = kb.sb("blkpos", [128, NBLK])
        tmpb = kb.sb("tmpb", [128, NBLK])
        op("pool", lambda e: e.iota(blkpos[:], pattern=[[SB, NBLK]], base=0, channel_multiplier=0, allow_small_or_imprecise_dtypes=True), w=[blkpos])
        for ex in range(E):
            op("dve", lambda e: e.tensor_scalar(out=tmpb[:], in0=blkpos[:], scalar1=runcnt[:, ex:ex + 1], scalar2=None, op0=ALU.is_lt, op1=ALU.add,
                                                accum_out=padf[:, ex:ex + 1]), r=[blkpos, runcnt], w=[tmpb, padf])
        op("dve", lambda e: e.tensor_scalar(out=padf[:], in0=padf[:], scalar1=float(SB), scalar2=None, op0=ALU.mult), r=[padf], w=[padf])
        op("dve", lambda e: e.tensor_tensor_scan(out=incl[:], data0=padf[:], data1=zer[:], initial=0.0, op0=ALU.add, op1=ALU.add), r=[padf, zer], w=[incl])
        op("dve", lambda e: e.tensor_tensor(out=excl[:], in0=incl[:], in1=padf[:], op=ALU.subtract), r=[incl, padf], w=[excl])
        bef = kb.sb("bef", [128, NBLK])
        op("dve", lambda e: e.memset(bef[:], 0.0), w=[bef])
        for ex in range(E):
            op("dve", lambda e: e.tensor_scalar(out=tmpb[:], in0=blkpos[:], scalar1=incl[:, ex:ex + 1], scalar2=None, op0=ALU.is_ge), r=[blkpos, incl], w=[tmpb])
            op("dve", lambda e: e.tensor_tensor(out=bef[:], in0=bef[:], in1=tmpb[:], op=ALU.add), r=[bef, tmpb], w=[bef])
        op("dve", lambda e: e.tensor_scalar(out=bef[:], in0=bef[:], scalar1=float(E - 1), scalar2=None, op0=ALU.min), r=[bef], w=[bef])
        idxf = kb.sb("idxf", [128, 4])
        oh = kb.sb("oh", [128, 4, E])
        slt = kb.sb("slt", [128, E])
        den = kb.sb("den", [128, 1])
        nb0 = kb.sb("nb0", [128, 1])
        for (b, ti) in tiles:
            gi = b * NT + ti
            op("dve", lambda e: e.tensor_copy(out=idxf[:], in_=idx8[:, gi, 0:4]), r=[idx8], w=[idxf])
            op("dve", lambda e: e.tensor_tensor(out=slt[:], in0=posA[:, gi, :], in1=excl[:], op=ALU.add), r=[posA, excl], w=[slt])
            for k in range(4):
                op("dve", lambda e: e.tensor_scalar(out=oh[:, k, :], in0=iota32, scalar1=idxf[:, k:k + 1], scalar2=None, op0=ALU.is_equal), r=[cm, idxf], w=[oh])
                op("dve", lambda e: e.tensor_tensor(out=oh[:, k, :], in0=oh[:, k, :], in1=slt[:], op=ALU.mult), r=[oh, slt], w=[oh])
            op("dve", lambda e: e.reduce_sum(out=slot4f[:, gi, :], in_=oh[:], axis=AX.X), r=[oh], w=[slot4f])
            op("dve", lambda e: e.tensor_copy(out=slot4[:, gi, :], in_=slot4f[:, gi, :]), r=[slot4f], w=[slot4])
            op("dve", lambda e: e.tensor_scalar(out=nb0[:], in0=top8[:, gi, 0:1], scalar1=-1.0, scalar2=None, op0=ALU.mult), r=[top8], w=[nb0])
            op("act", lambda e: e.activation(out=gate4[:, gi, :], in_=top8[:, gi, 0:4], func=AF.Exp, bias=nb0[:, 0:1], scale=1.0), r=[top8, nb0], w=[gate4])
            op("dve", lambda e: e.reduce_sum(out=den[:], in_=gate4[:, gi, :], axis=AX.X), r=[gate4], w=[den])
            op("dve", lambda e: e.reciprocal(out=den[:], in_=den[:]), r=[den], w=[den])
            op("dve", lambda e: e.tensor_scalar(out=gate4[:, gi, :], in0=gate4[:, gi, :], scalar1=den[:, 0:1], scalar2=None, op0=ALU.mult), r=[gate4, den], w=[gate4])
        if cfg.debug:
            dma("sp", DBG["slot4"][:, :], slot4[:].rearrange("p a b -> p (a b)"), r=[slot4])
            dma("sp", DBG["gate4"][:, :], gate4[:].rearrange("p a b -> p (a b)"), r=[gate4])
            dma("sp", DBG["top8"][:, :], top8[:].rearrange("p a b -> p (a b)"), r=[top8])
            dma("sp", DBG["posA"][:, :], posA[:].rearrange("p a b -> p (a b)"), r=[posA])
            dma("sp", DBG["bef"][:, :], bef[:], r=[bef])
            dma("sp", DBG["runcnt"][:, :], runcnt[:], r=[runcnt])
            dma("sp", DBG["excl"][:, :], excl[:], r=[excl])
            dma("sp", DBG["padf"][:, :], padf[:], r=[padf])
            dma("sp", DBG["incl"][:, :], incl[:], r=[incl])
        kb.push()
        h2s = [kb.sb("h2d", [128, D]) for _ in range(3)]
        for n, (b, ti) in enumerate(tiles):
            gi = b * NT + ti
            r0 = b * Tn + ti * 128
            h2 = h2s[n % 3]
            dma("sp", h2[:], H2[r0:r0 + 128, :], w=[h2])
            for k in range(4):
                dma("pool", None, None, r=[h2, slot4], fn=lambda e: e.indirect_dma_start(
                    out=XS[:, :], out_offset=bass.IndirectOffsetOnAxis(ap=slot4[:, gi, k:k + 1], axis=0), in_=h2[:], in_offset=None))
        kb.pop()
        kb.push()
        wg = [kb.sb("wg", [128, 8, 2 * FF]) for _ in range(1)]
        wd = [kb.sb("wd", [128, 8, D]) for _ in range(1)]
        brow = [kb.sb("brow", [128, 3 * D]) for _ in range(1)]
        iotap = kb.sb("iotap", [128, 8])
        op("pool", lambda e: e.iota(iotap[:], pattern=[[128, 8]], base=0, channel_multiplier=1, allow_small_or_imprecise_dtypes=True), w=[iotap])
        widx_f = kb.sb("widxf", [128, 9])
        widx = [kb.sb("widx", [128, 9], I32) for _ in range(2)]
        xs = [kb.sb("xs", [128, D]) for _ in range(1)]
        xT = kb.sb("xTm", [128, 8, SB])
        aT = kb.sb("aTm", [128, 8, SB])
        gsb = kb.sb("gsb", [128, SB])
        sgb = kb.sb("sgb", [128, SB])
        usb = kb.sb("usb", [128, SB])
        ysb = [kb.sb("ysb", [128, D]) for _ in range(1)]
        be_l = kb.sb("be_l", [128, NBLK])
        op("dve", lambda e: e.tensor_scalar(out=be_l[:], in0=bef[:], scalar1=float(i * E), scalar2=None, op0=ALU.add), r=[bef], w=[be_l])
        for blk in range(NBLK):
            wi = widx[blk % 2]
            op("dve", lambda e: e.tensor_scalar(out=widx_f[:, 8:9], in0=be_l[:, blk:blk + 1], scalar1=1.0, scalar2=None, op0=ALU.mult), r=[be_l], w=[widx_f])
            op("dve", lambda e: e.scalar_tensor_tensor(out=widx_f[:, 0:8], in0=be_l[:, blk:blk + 1].to_broadcast([128, 8]), scalar=1024.0, in1=iotap[:], op0=ALU.mult, op1=ALU.add),
               r=[be_l, iotap], w=[widx_f])
            op("dve", lambda e: e.tensor_copy(out=wi[:], in_=widx_f[:]), r=[widx_f], w=[wi])
            w1 = wg[0]; w2 = wd[0]; br = brow[0]
            for kc in range(8):
                dma("pool", None, None, r=[wi], w=[w1], fn=lambda e: e.indirect_dma_start(
                    out=w1[:, kc, :], out_offset=None, in_=wgu[:, :], in_offset=bass.IndirectOffsetOnAxis(ap=wi[:, kc:kc + 1], axis=0)))
            for kc in range(8):
                dma("pool", None, None, r=[wi], w=[w2], fn=lambda e: e.indirect_dma_start(
                    out=w2[:, kc, :], out_offset=None, in_=wdn[:, :], in_offset=bass.IndirectOffsetOnAxis(ap=wi[:, kc:kc + 1], axis=0)))
            dma("pool", None, None, r=[wi], w=[br], fn=lambda e: e.indirect_dma_start(
                out=br[:, 0:2 * FF], out_offset=None, in_=bgu[:, :], in_offset=bass.IndirectOffsetOnAxis(ap=wi[:, 8:9], axis=0)))
            dma("pool", None, None, r=[wi], w=[br], fn=lambda e: e.indirect_dma_start(
                out=br[:, 2 * FF:3 * FF], out_offset=None, in_=bdn[:, :], in_offset=bass.IndirectOffsetOnAxis(ap=wi[:, 8:9], axis=0)))
            for j in range(SB // 128):
                x = xs[0]
                dma("sp", x[:], XS[blk * SB + j * 128: blk * SB + (j + 1) * 128, :], w=[x])
                for half in range(2):
                    p = nps()
                    for c4 in range(4):
                        c = half * 4 + c4
                        op("pe", lambda e: e.transpose(p[:, c4 * 128:(c4 + 1) * 128], x[:, c * 128:(c + 1) * 128], ident()), r=[x, cm], w=[p])
                    op("act", lambda e: e.activation(out=xT[:, half * 4:(half + 1) * 4, j * 128:(j + 1) * 128], in_=p[:, :].rearrange("p (c t) -> p c t", c=4), func=AF.Copy),
                       r=[p], w=[xT])
            for fc in range(8):
                pg = nps()
                for kc in range(8):
                    op("pe", lambda e: e.matmul(pg[:, :], w1[:, kc, fc * 128:(fc + 1) * 128], xT[:, kc, :], start=(kc == 0), stop=False), r=[w1, xT], w=[pg])
                op("pe", lambda e: e.matmul(pg[:, :], br[0:1, fc * 128:(fc + 1) * 128], onesrow[0:1, :], start=False, stop=True), r=[br, onesrow_t], w=[pg])
                pu = nps()
                for kc in range(8):
                    op("pe", lambda e: e.matmul(pu[:, :], w1[:, kc, FF + fc * 128:FF + (fc + 1) * 128], xT[:, kc, :], start=(kc == 0), stop=False), r=[w1, xT], w=[pu])
                op("pe", lambda e: e.matmul(pu[:, :], br[0:1, FF + fc * 128:FF + (fc + 1) * 128], onesrow[0:1, :], start=False, stop=True), r=[br, onesrow_t], w=[pu])
                op("dve", lambda e: e.tensor_scalar(out=gsb[:], in0=pg[:, :], scalar1=7.0, scalar2=None, op0=ALU.min), r=[pg], w=[gsb])
                op("act", lambda e: e.activation(out=sgb[:], in_=gsb[:], func=AF.Sigmoid, scale=1.702), r=[gsb], w=[sgb])
                op("dve", lambda e: e.tensor_scalar(out=usb[:], in0=pu[:, :], scalar1=7.0, scalar2=-7.0, op0=ALU.min, op1=ALU.max), r=[pu], w=[usb])
                op("dve", lambda e: e.scalar_tensor_tensor(out=usb[:], in0=usb[:], scalar=1.0, in1=gsb[:], op0=ALU.add, op1=ALU.mult), r=[usb, gsb], w=[usb])
                op("pool", lambda e: e.tensor_tensor(out=aT[:, fc, :], in0=usb[:], in1=sgb[:], op=ALU.mult), r=[usb, sgb], w=[aT])
            for j in range(SB // 128):
                y = ysb[0]
                for hf in range(2):
                    p = nps()
                    for fc in range(8):
                        op("pe", lambda e: e.matmul(p[:, :], aT[:, fc, j * 128:(j + 1) * 128], w2[:, fc, hf * 512:(hf + 1) * 512], start=(fc == 0), stop=False), r=[aT, w2], w=[p])
                    op("pe", lambda e: e.matmul(p[:, :], onesrow[0:1, 0:128], br[0:1, 2 * FF + hf * 512:2 * FF + (hf + 1) * 512], start=False, stop=True), r=[br, onesrow_t], w=[p])
                    op("act", lambda e: e.activation(out=y[:, hf * 512:(hf + 1) * 512], in_=p[:, :], func=AF.Copy), r=[p], w=[y])
                dma("sp", YS[blk * SB + j * 128: blk * SB + (j + 1) * 128, :], y[:], r=[y])
        kb.pop()
        kb.push()
        grow = {}
        for s in range(NS):
            g = kb.sb("g2row", [128, D])
            dma("sp", g[:], MODD[s, 5 * D:6 * D].partition_broadcast(128), w=[g])
            grow[s] = g
        yk = [kb.sb("yk", [128, D]) for _ in range(4)]
        accs = [kb.sb("acc", [128, D]) for _ in range(2)]
        xts = [kb.sb("xt2", [128, D]) for _ in range(2)]
        st = kb.sb("st2", [128, 2, 6])
        mv = kb.sb("mv2", [128, 4])
        for n, (b, ti) in enumerate(tiles):
            gi = b * NT + ti
            r0 = b * Tn + ti * 128
            s = NB if ti < C // 128 else b
            acc = accs[n % 2]; x = xts[n % 2]
            dma("sp", x[:], XA[r0:r0 + 128, :], w=[x])
            for k in range(4):
                y = yk[k]
                dma("pool", None, None, r=[slot4], w=[y], fn=lambda e: e.indirect_dma_start(
                    out=y[:], out_offset=None, in_=YS[:, :], in_offset=bass.IndirectOffsetOnAxis(ap=slot4[:, gi, k:k + 1], axis=0)))
                if k == 0:
                    op("dve", lambda e: e.tensor_scalar(out=acc[:], in0=y[:], scalar1=gate4[:, gi, 0:1], scalar2=None, op0=ALU.mult), r=[y, gate4], w=[acc])
                else:
                    op("dve", lambda e: e.scalar_tensor_tensor(out=acc[:], in0=y[:], scalar=gate4[:, gi, k:k + 1], in1=acc[:], op0=ALU.mult, op1=ALU.add), r=[y, gate4, acc], w=[acc])
            op("dve", lambda e: e.tensor_tensor(out=acc[:], in0=acc[:], in1=grow[s][:], op=ALU.mult), r=[acc, grow[s]], w=[acc])
            op("dve", lambda e: e.scalar_tensor_tensor(out=acc[:], in0=x[:], scalar=ALPHA, in1=acc[:], op0=ALU.mult, op1=ALU.add), r=[x, acc], w=[acc])
            layernorm(acc, 2, st, mv, None)
            if last:
                if ti >= C // 128:
                    ro = b * S + (ti - C // 128) * 128
                    dma("sp", out[ro:ro + 128, :], acc[:], r=[acc])
            else:
                dma("sp", XB[r0:r0 + 128, :], acc[:], r=[acc])
        kb.pop()
        kb.pop()

    onesrow_t = kb.sb("onesrow", [1, SB])
    onesrow = onesrow_t
    op("dve", lambda e: e.memset(onesrow_t[:], 1.0), w=[onesrow_t])

    def mixer_identity(i, b):
        kb.push()
        buf = [kb.sb("cp", [128, 8, 512]) for _ in range(2)]
        for gi, (t0, tl) in enumerate(groups()):
            bb = buf[gi % 2]
            dma("sp", bb[:, :, 0:tl], HT[:, t0:t0 + tl].rearrange("(c p) t -> p c t", p=128), w=[bb])
            dma("sp", OT[0:D, t0:t0 + tl].rearrange("(c p) t -> p c t", p=128), bb[:, :, 0:tl], r=[bb])
        kb.pop()
        return w_id[:, :], D, None

    def mixer_rg(i, b, j, need_ctx):
        kb.push()
        win = kb.sb("win", [128, 8, 2 * D_RNN])
        for q in range(6):
            dma("sp", win[:, :, q * 512:(q + 1) * 512], rg_w_in[j * D:(j + 1) * D, q * 512:(q + 1) * 512].rearrange("(c p) n -> p c n", p=128), w=[win])
        hTs = [kb.sb("hTr", [128, 8, 512]) for _ in range(2)]
        pos_ = [kb.sb("pout", [128, 4, 512]) for _ in range(2)]
        n = 0
        for gi, (t0, tl) in enumerate(groups()):
            h = hTs[gi % 2]
            dma("sp", h[:, :, 0:tl], HT[:, t0:t0 + tl].rearrange("(c p) t -> p c t", p=128), w=[h])
            for q in range(6):
                po = pos_[n % 2]; n += 1
                for jj in range(4):
                    jc = q * 4 + jj
                    p = nps()
                    for kc in range(8):
                        op("pe", lambda e: e.matmul(p[:, 0:tl], win[:, kc, jc * 128:(jc + 1) * 128], h[:, kc, 0:tl], start=(kc == 0), stop=(kc == 7)), r=[win, h], w=[p])
                    if jj % 2 == 0:
                        op("act", lambda e: e.activation(out=po[:, jj, 0:tl], in_=p[:, 0:tl], func=AF.Copy), r=[p], w=[po])
                    else:
                        op("dve", lambda e: e.tensor_copy(out=po[:, jj, 0:tl], in_=p[:, 0:tl]), r=[p], w=[po])
                dma("pool", PR[q * 512:(q + 1) * 512, t0:t0 + tl].rearrange("(c p) t -> p c t", p=128), po[:, :, 0:tl], r=[po])
        kb.pop()
        kb.push()
        prow = kb.sb("prow", [11, D_RNN])
        dma("sp", prow[:], rg_p[j * 11:(j + 1) * 11, :], w=[prow])
        chp = kb.sb("chp", [128, 12, 11])
        p = nps()
        for c in range(12):
            op("pe", lambda e: e.transpose(p[:, c * 11:(c + 1) * 11], prow[:, c * 128:(c + 1) * 128], ident(11)), r=[prow, cm], w=[p])
        op("dve", lambda e: e.tensor_copy(out=chp[:].rearrange("p c k -> p (c k)"), in_=p[:, 0:132]), r=[p], w=[chp])
        cdec = kb.sb("cdec", [128, 12, 4])
        op("act", lambda e: e.activation(out=cdec[:, :, 0:2], in_=chp[:, :, 9:11], func=AF.Exp, scale=-1.0), r=[chp], w=[cdec])
        op("act", lambda e: e.activation(out=cdec[:, :, 0:2], in_=cdec[:, :, 0:2], func=AF.Ln, bias=1.0, scale=1.0), r=[cdec], w=[cdec])
        op("dve", lambda e: e.tensor_scalar(out=cdec[:, :, 2:4], in0=cdec[:, :, 0:2], scalar1=-16.0, scalar2=None, op0=ALU.mult), r=[cdec], w=[cdec])
        op("dve", lambda e: e.tensor_scalar(out=cdec[:, :, 0:2], in0=cdec[:, :, 0:2], scalar1=-8.0, scalar2=None, op0=ALU.mult), r=[cdec], w=[cdec])
        xr = kb.sb("xr", [128, Tn])
        xc = kb.sb("xc", [128, 2, Tn])
        gl = kb.sb("gl", [128, Tn])
        aa = kb.sb("aa", [128, Tn])
        uu = kb.sb("uu", [128, Tn])
        hh = kb.sb("hh", [128, Tn])
        hs = kb.sb("hs", [128, Tn])
        gw = kb.sb("gw", [128, 2, 2, 2, 256])
        rr = [kb.sb("rr", [128, 512]) for _ in range(2)]
        ii = [kb.sb("ii", [128, 512]) for _ in range(2)]
        segs = [(0, C), (C, Tn)]
        for nb_ in range(6):
            for d in range(2):
                base = ((j * 2 + d) * 6 + nb_) * 256
                dma("sp", gw[:, d, 0, :, :], rg_ga[base:base + 256, :].rearrange("(c p) n -> p c n", p=128), w=[gw])
                dma("sp", gw[:, d, 1, :, :], rg_gx[base:base + 256, :].rearrange("(c p) n -> p c n", p=128), w=[gw])
            for cc_ in range(2):
                ch = nb_ * 2 + cc_
                dma("sp", xr[:], PR[D_RNN + ch * 128: D_RNN + (ch + 1) * 128, :], w=[xr])
                for (a0, b0) in segs:
                    op("dve", lambda e: e.tensor_scalar(out=xc[:, cc_, a0:b0], in0=xr[:, a0:b0], scalar1=chp[:, ch, 1:2], scalar2=chp[:, ch, 4:5], op0=ALU.mult, op1=ALU.add), r=[xr, chp], w=[xc])
                    op("dve", lambda e: e.scalar_tensor_tensor(out=xc[:, cc_, a0 + 1:b0], in0=xr[:, a0:b0 - 1], scalar=chp[:, ch, 0:1], in1=xc[:, cc_, a0 + 1:b0], op0=ALU.mult, op1=ALU.add), r=[xr, chp, xc], w=[xc])
                    op("dve", lambda e: e.scalar_tensor_tensor(out=xc[:, cc_, a0:b0 - 1], in0=xr[:, a0 + 1:b0], scalar=chp[:, ch, 2:3], in1=xc[:, cc_, a0:b0 - 1], op0=ALU.mult, op1=ALU.add), r=[xr, chp, xc], w=[xc])
                    op("dve", lambda e: e.scalar_tensor_tensor(out=xc[:, cc_, a0:b0 - 2], in0=xr[:, a0 + 2:b0], scalar=chp[:, ch, 3:4], in1=xc[:, cc_, a0:b0 - 2], op0=ALU.mult, op1=ALU.add), r=[xr, chp, xc], w=[xc])
            for oc in range(2):
                ch = nb_ * 2 + oc
                dma("sp", gl[:], PR[ch * 128:(ch + 1) * 128, :], w=[gl])
                for d in range(2):
                    for gi, (t0, tl) in enumerate(groups()):
                        r_ = rr[gi % 2]; i_ = ii[gi % 2]
                        pa = nps()
                        for kc in range(2):
                            op("pe", lambda e: e.matmul(pa[:, 0:tl], gw[:, d, 0, kc, oc * 128:(oc + 1) * 128], xc[:, kc, t0:t0 + tl], start=(kc == 0), stop=(kc == 1)), r=[gw, xc], w=[pa])
                        px = nps()
                        for kc in range(2):
                            op("pe", lambda e: e.matmul(px[:, 0:tl], gw[:, d, 1, kc, oc * 128:(oc + 1) * 128], xc[:, kc, t0:t0 + tl], start=(kc == 0), stop=(kc == 1)), r=[gw, xc], w=[px])
                        op("act", lambda e: e.activation(out=r_[:, 0:tl], in_=pa[:, 0:tl], func=AF.Sigmoid, bias=chp[:, ch, 5 + d:6 + d], scale=1.0), r=[pa, chp], w=[r_])
                        op("act", lambda e: e.activation(out=i_[:, 0:tl], in_=px[:, 0:tl], func=AF.Sigmoid, bias=chp[:, ch, 7 + d:8 + d], scale=1.0), r=[px, chp], w=[i_])
                        op("act", lambda e: e.activation(out=aa[:, t0:t0 + tl], in_=r_[:, 0:tl], func=AF.Exp, scale=cdec[:, ch, d:d + 1]), r=[r_, cdec], w=[aa])
                        op("act", lambda e: e.activation(out=r_[:, 0:tl], in_=r_[:, 0:tl], func=AF.Exp, scale=cdec[:, ch, 2 + d:3 + d]), r=[r_, cdec], w=[r_])
                        op("act", lambda e: e.activation(out=r_[:, 0:tl], in_=r_[:, 0:tl], func=AF.Sqrt, bias=1.0, scale=-1.0), r=[r_], w=[r_])
                        op("dve", lambda e: e.tensor_tensor(out=i_[:, 0:tl], in0=i_[:, 0:tl], in1=xc[:, oc, t0:t0 + tl], op=ALU.mult), r=[i_, xc], w=[i_])
                        op("dve", lambda e: e.tensor_tensor(out=uu[:, t0:t0 + tl], in0=i_[:, 0:tl], in1=r_[:, 0:tl], op=ALU.mult), r=[i_, r_], w=[uu])
                    if d == 0:
                        op("dve", lambda e: e.tensor_tensor_scan(out=hs[:, :], data0=aa[:, :], data1=uu[:, :], initial=0.0, op0=ALU.mult, op1=ALU.add), r=[aa, uu], w=[hs])
                    else:
                        op("dve", lambda e: e.tensor_tensor_scan(out=hh[:, 0:C][:, ::-1], data0=aa[:, 0:C][:, ::-1], data1=uu[:, 0:C][:, ::-1], initial=0.0, op0=ALU.mult, op1=ALU.add), r=[aa, uu], w=[hh])
                        op("dve", lambda e: e.tensor_tensor_scan(out=hh[:, C:Tn][:, ::-1], data0=aa[:, C:Tn][:, ::-1], data1=uu[:, C:Tn][:, ::-1], initial=hh[:, 0:1], op0=ALU.mult, op1=ALU.add), r=[aa, uu, hh], w=[hh])
                        op("pool", lambda e: e.tensor_tensor(out=hs[:], in0=hs[:], in1=hh[:], op=ALU.add), r=[hs, hh], w=[hs])
                op("pool", lambda e: e.tensor_tensor(out=hh[:], in0=gl[:], in1=gl[:], op=ALU.mult), r=[gl], w=[hh])
                op("dve", lambda e: e.tensor_scalar(out=hh[:], in0=hh[:], scalar1=0.044715, scalar2=1.0, op0=ALU.mult, op1=ALU.add), r=[hh], w=[hh])
                op("pool", lambda e: e.tensor_tensor(out=hh[:], in0=hh[:], in1=gl[:], op=ALU.mult), r=[hh, gl], w=[hh])
                op("act", lambda e: e.activation(out=hh[:], in_=hh[:], func=AF.Sigmoid, scale=1.5957691216057308), r=[hh], w=[hh])
                op("pool", lambda e: e.tensor_tensor(out=hh[:], in0=hh[:], in1=gl[:], op=ALU.mult), r=[hh, gl], w=[hh])
                op("dve", lambda e: e.tensor_tensor(out=hs[:], in0=hs[:], in1=hh[:], op=ALU.mult), r=[hs, hh], w=[hs])
                dma("pool", OT[ch * 128:(ch + 1) * 128, :], hs[:], r=[hs])
        kb.pop()
        return rg_w_out[j * D_RNN:(j + 1) * D_RNN, :], D_RNN, None

    def mixer_gqa(i, b, j, need_ctx):
        QT3 = QT.ap().rearrange("p (h t) -> p h t", h=16)
        KT3 = KT.ap().rearrange("p (h t) -> p h t", h=2)
        kb.push()
        w = kb.sb("gqw", [128, 8, 2432])
        for q in range(0, 2432, 608):
            dma("sp", w[:, :, q:q + 608], gq_w[j * D:(j + 1) * D, q:q + 608].rearrange("(c p) n -> p c n", p=128), w=[w])
        b64 = kb.sb("b64", [64, 40])
        dma("sp", b64[:], gq_b64[j * 64:(j + 1) * 64, :], w=[b64])
        bvrow = kb.sb("bvrow", [128, 128])
        dma("sp", bvrow[:], gq_bv[j, :].partition_broadcast(128), w=[bvrow])
        hTs = [kb.sb("hTg", [128, 8, 512]) for _ in range(2)]
        qo = kb.sb("qo", [64, 16, 512])
        ko = kb.sb("ko", [64, 2, 512])
        cs = kb.sb("cs", [64, 512]); sn = kb.sb("sn", [64, 512])
        ta = [kb.sb("ta", [64, 512]) for _ in range(2)]
        tb = [kb.sb("tb", [64, 512]) for _ in range(2)]
        va = [kb.sb("va", [128, 2, 65]) for _ in range(2)]
        for v_ in va:
            op("dve", lambda e: e.memset(v_[:], 1.0), w=[v_])
        n = 0
        for gi, (t0, tl) in enumerate(groups()):
            h = hTs[gi % 2]
            lat = t0 >= C
            dma("sp", h[:, :, 0:tl], HT[:, t0:t0 + tl].rearrange("(c p) t -> p c t", p=128), w=[h])
            if lat:
                dma("sp", cs[:, 0:tl], rope64[0:64, t0 - C:t0 - C + tl], w=[cs])
                dma("sp", sn[:, 0:tl], rope64[64:128, t0 - C:t0 - C + tl], w=[sn])
            for hq in range(18):
                col = hq * 64 if hq < 16 else 2048 + (hq - 16) * 64
                colsw = 1024 + hq * 64 if hq < 16 else 2176 + (hq - 16) * 64
                dst = qo[:, hq, 0:tl] if hq < 16 else ko[:, hq - 16, 0:tl]
                dstT = qo if hq < 16 else ko
                p = nps()
                for kc in range(8):
                    op("pe", lambda e: e.matmul(p[0:64, 0:tl], w[:, kc, col:col + 64], h[:, kc, 0:tl], start=(kc == 0), stop=(kc == 7)), r=[w, h], w=[p])
                if lat:
                    p2 = nps()
                    for kc in range(8):
                        op("pe", lambda e: e.matmul(p2[0:64, 0:tl], w[:, kc, colsw:colsw + 64], h[:, kc, 0:tl], start=(kc == 0), stop=(kc == 7)), r=[w, h], w=[p2])
                    a_ = ta[n % 2]; b_ = tb[n % 2]; n += 1
                    op("dve", lambda e: e.scalar_tensor_tensor(out=a_[:, 0:tl], in0=p[0:64, 0:tl], scalar=b64[:, hq:hq + 1], in1=cs[:, 0:tl], op0=ALU.add, op1=ALU.mult), r=[p, b64, cs], w=[a_])
                    op("dve", lambda e: e.scalar_tensor_tensor(out=b_[:, 0:tl], in0=p2[0:64, 0:tl], scalar=b64[:, 20 + hq:21 + hq], in1=sn[:, 0:tl], op0=ALU.add, op1=ALU.mult), r=[p2, b64, sn], w=[b_])
                    op("pool", lambda e: e.tensor_tensor(out=dst, in0=a_[:, 0:tl], in1=b_[:, 0:tl], op=ALU.add), r=[a_, b_], w=[dstT])
                else:
                    op("dve", lambda e: e.tensor_scalar(out=dst, in0=p[0:64, 0:tl], scalar1=b64[:, hq:hq + 1], scalar2=None, op0=ALU.add), r=[p, b64], w=[dstT])
            dma("pool", QT3[:, :, t0:t0 + tl], qo[:, :, 0:tl], r=[qo])
            dma("pool", KT3[:, :, t0:t0 + tl], ko[:, :, 0:tl], r=[ko])
            for tt in range(tl // 128):
                v_ = va[tt % 2]
                p = nps()
                for kc in range(8):
                    op("pe", lambda e: e.matmul(p[:, 0:128], h[:, kc, tt * 128:(tt + 1) * 128], w[:, kc, 2304:2432], start=(kc == 0), stop=(kc == 7)), r=[w, h], w=[p])
                op("dve", lambda e: e.tensor_tensor(out=v_[:, :, 0:64], in0=p[:, 0:128].rearrange("p (h d) -> p h d", h=2), in1=bvrow[:].rearrange("p (h d) -> p h d", h=2), op=ALU.add), r=[p, bvrow], w=[v_])
                dma("pool", VA[t0 + tt * 128:t0 + (tt + 1) * 128, :], v_[:].rearrange("p h d -> p (h d)"), r=[v_])
        kb.pop()
        kb.push()
        kT = kb.sb("kT", [64, 2, Tn])
        dma("sp", kT[:], KT3[:, :, :], w=[kT])
        V = kb.sb("Vg", [128, NT, 130])
        dma("sp", V[:], VA.ap().rearrange("(n p) c -> p n c", p=128), w=[V])
        esk = kb.sb("esk", [64, 16])
        dma("sp", esk[:], gq_sinks[j, :].partition_broadcast(64), w=[esk])
        op("act", lambda e: e.activation(out=esk[:], in_=esk[:], func=AF.Exp), r=[esk], w=[esk])
        mge = kb.sb("mge", [128, 128])
        mle = kb.sb("mle", [128, 128])
        op("dve", lambda e: e.tensor_scalar(out=mge[:], in0=tri, scalar1=-1.0, scalar2=1.0, op0=ALU.mult, op1=ALU.add), r=[cm], w=[mge])
        op("dve", lambda e: e.tensor_tensor(out=mle[:], in0=tri, in1=ident(), op=ALU.add), r=[cm], w=[mle])
        sel = kb.sb("sel", [65, 64])
        op("dve", lambda e: e.memset(sel[:], 0.0), w=[sel])
        op("dve", lambda e: e.memset(sel[64:65, :], 1.0), w=[sel])
        qs = [kb.sb("qs", [64, 16, 128]) for _ in range(2)]
        pTs = [kb.sb("pT", [128, 512]) for _ in range(4)]
        Xs = [kb.sb("Xo", [65, 512]) for _ in range(2)]
        rb = [kb.sb("rb", [64, 512]) for _ in range(2)]
        oo = [kb.sb("oo", [64, 512]) for _ in range(2)]
        nq = 0
        blocks = []
        if need_ctx:
            blocks += [(tq, [(0, None), (1, None)]) for tq in range(C // 128)]
        nlb = S // 128
        for jq in range(nlb):
            ch = [(0, None), (1, None)]
            if jq > 0:
                ch.append((C // 128 + jq - 1, mge))
            ch.append((C // 128 + jq, None))
            if jq < nlb - 1:
                ch.append((C // 128 + jq + 1, mle))
            blocks.append((C // 128 + jq, ch))
        npt = 0
        for (tq, chunks) in blocks:
            q = qs[nq % 2]; nq += 1
            dma("sp", q[:], QT3[:, :, tq * 128:(tq + 1) * 128], w=[q])
            for kvh in range(2):
                for half in range(2):
                    h0 = kvh * 8 + half * 4
                    po = PS[4 + (npt % 4)]
                    for ci, (kt, msk) in enumerate(chunks):
                        pT = pTs[npt % 4]; npt += 1
                        p = nps()
                        op("pe", lambda e: e.matmul(p[:, :], kT[:, kvh, kt * 128:(kt + 1) * 128], q[:, h0:h0 + 4, :].rearrange("p h t -> p (h t)"), start=True, stop=True), r=[kT, q], w=[p])
                        op("act", lambda e: e.activation(out=pT[:], in_=p[:, :], func=AF.Exp, scale=0.125), r=[p], w=[pT])
                        if msk is not None:
                            for hh in range(4):
                                op("pool" if hh % 2 else "dve", lambda e: e.tensor_tensor(out=pT[:, hh * 128:(hh + 1) * 128], in0=pT[:, hh * 128:(hh + 1) * 128], in1=msk[:], op=ALU.mult), r=[pT, msk], w=[pT])
                        op("pe", lambda e: e.matmul(po[0:65, :], V[:, kt, kvh * 65:(kvh + 1) * 65], pT[:], start=(ci == 0), stop=(ci == len(chunks) - 1)), r=[V, pT], w=[po])
                    X = Xs[npt % 2]; r_ = rb[npt % 2]; o_ = oo[npt % 2]
                    op("act", lambda e: e.activation(out=X[:], in_=po[0:65, :], func=AF.Copy), r=[po], w=[X])
                    p = nps()
                    op("pe", lambda e: e.matmul(p[0:64, :], sel[:], X[:], start=True, stop=True), r=[sel, X], w=[p])
                    for hh in range(4):
                        op("dve", lambda e: e.tensor_scalar(out=r_[:, hh * 128:(hh + 1) * 128], in0=p[0:64, hh * 128:(hh + 1) * 128], scalar1=esk[:, h0 + hh:h0 + hh + 1], scalar2=None, op0=ALU.add), r=[p, esk], w=[r_])
                    op("dve", lambda e: e.reciprocal(out=r_[:], in_=r_[:]), r=[r_], w=[r_])
                    op("dve", lambda e: e.tensor_tensor(out=o_[:], in0=X[0:64, :], in1=r_[:], op=ALU.mult), r=[X, r_], w=[o_])
                    dma("pool", OT[h0 * 64:(h0 + 4) * 64, tq * 128:(tq + 1) * 128].rearrange("(h p) t -> p h t", p=64), o_[:].rearrange("p (h t) -> p h t", h=4), r=[o_])
        kb.pop()
        return gq_w_o[j * D:(j + 1) * D, :], D, gq_b_o[j, :]

    def mixer_mla(i, b, j, need_ctx):
        RMS_EPS = 1e-6
        kb.push()
        wd = kb.sb("mlwd", [128, 8, 448])
        dma("sp", wd[:], ml_wd[j * D:(j + 1) * D, :].rearrange("(c p) n -> p c n", p=128), w=[wd])
        nrow = kb.sb("nrow", [128, 384])
        dma("sp", nrow[:], ml_norm[j, :].partition_broadcast(128), w=[nrow])
        epsr = kb.sb("epsr", [128, 1])
        op("dve", lambda e: e.memset(epsr[:], RMS_EPS), w=[epsr])
        hTs = [kb.sb("hTm", [128, 8, 512]) for _ in range(2)]
        cTs = [kb.sb("cTm", [128, 3, 512]) for _ in range(2)]
        krs = [kb.sb("krm", [32, 512]) for _ in range(2)]
        cs = kb.sb("csm", [32, 512]); sn = kb.sb("snm", [32, 512])
        t1 = kb.sb("t1m", [32, 512]); t2 = kb.sb("t2m", [32, 512])
        cqs = [kb.sb("cqm", [128, 384]) for _ in range(2)]
        junk = kb.sb("junk", [128, 256])
        ss = kb.sb("ssm", [128, 4])
        for gi, (t0, tl) in enumerate(groups()):
            h = hTs[gi % 2]; cT = cTs[gi % 2]; kr = krs[gi % 2]
            lat = t0 >= C
            dma("sp", h[:, :, 0:tl], HT[:, t0:t0 + tl].rearrange("(c p) t -> p c t", p=128), w=[h])
            for tt in range(tl // 128):
                cq = cqs[tt % 2]
                p = nps()
                for kc in range(8):
                    op("pe", lambda e: e.matmul(p[:, 0:384], h[:, kc, tt * 128:(tt + 1) * 128], wd[:, kc, 0:384], start=(kc == 0), stop=(kc == 7)), r=[h, wd], w=[p])
                op("act", lambda e: e.activation(out=junk[:, 0:256], in_=p[:, 0:256], func=AF.Square, accum_out=ss[:, 0:1]), r=[p], w=[junk, ss])
                op("act", lambda e: e.activation(out=junk[:, 0:128], in_=p[:, 256:384], func=AF.Square, accum_out=ss[:, 1:2]), r=[p], w=[junk, ss])
                op("act", lambda e: e.activation(out=ss[:, 2:3], in_=ss[:, 0:1], func=AF.Sqrt, bias=epsr[:, 0:1], scale=1.0 / 256), r=[ss, epsr], w=[ss])
                op("act", lambda e: e.activation(out=ss[:, 3:4], in_=ss[:, 1:2], func=AF.Sqrt, bias=epsr[:, 0:1], scale=1.0 / 128), r=[ss, epsr], w=[ss])
                op("dve", lambda e: e.reciprocal(out=ss[:, 2:4], in_=ss[:, 2:4]), r=[ss], w=[ss])
                op("dve", lambda e: e.scalar_tensor_tensor(out=cq[:, 0:256], in0=p[:, 0:256], scalar=ss[:, 2:3], in1=nrow[:, 0:256], op0=ALU.mult, op1=ALU.mult), r=[p, ss, nrow], w=[cq])
                op("dve", lambda e: e.scalar_tensor_tensor(out=cq[:, 256:384], in0=p[:, 256:384], scalar=ss[:, 3:4], in1=nrow[:, 256:384], op0=ALU.mult, op1=ALU.mult), r=[p, ss, nrow], w=[cq])
                p2 = nps()
                for c3 in range(3):
                    op("pe", lambda e: e.transpose(p2[:, c3 * 128:(c3 + 1) * 128], cq[:, c3 * 128:(c3 + 1) * 128], ident()), r=[cq, cm], w=[p2])
                op("act", lambda e: e.activation(out=cT[:, :, tt * 128:(tt + 1) * 128], in_=p2[:, 0:384].rearrange("p (c t) -> p c t", c=3), func=AF.Copy), r=[p2], w=[cT])
            dma("pool", CT[:, t0:t0 + tl].rearrange("(c p) t -> p c t", p=128), cT[:, :, 0:tl], r=[cT])
            p = nps()
            for kc in range(8):
                op("pe", lambda e: e.matmul(p[0:32, 0:tl], wd[:, kc, 384:416], h[:, kc, 0:tl], start=(kc == 0), stop=(kc == 7)), r=[h, wd], w=[p])
            if lat:
                p2 = nps()
                for kc in range(8):
                    op("pe", lambda e: e.matmul(p2[0:32, 0:tl], wd[:, kc, 416:448], h[:, kc, 0:tl], start=(kc == 0), stop=(kc == 7)), r=[h, wd], w=[p2])
                dma("sp", cs[:, 0:tl], rope32[0:32, t0 - C:t0 - C + tl], w=[cs])
                dma("sp", sn[:, 0:tl], rope32[32:64, t0 - C:t0 - C + tl], w=[sn])
                op("dve", lambda e: e.tensor_tensor(out=t1[:, 0:tl], in0=p[0:32, 0:tl], in1=cs[:, 0:tl], op=ALU.mult), r=[p, cs], w=[t1])
                op("dve", lambda e: e.tensor_tensor(out=t2[:, 0:tl], in0=p2[0:32, 0:tl], in1=sn[:, 0:tl], op=ALU.mult), r=[p2, sn], w=[t2])
                op("pool", lambda e: e.tensor_tensor(out=kr[:, 0:tl], in0=t1[:, 0:tl], in1=t2[:, 0:tl], op=ALU.add), r=[t1, t2], w=[kr])
            else:
                op("dve", lambda e: e.tensor_copy(out=kr[:, 0:tl], in_=p[0:32, 0:tl]), r=[p], w=[kr])
            dma("pool", KRT[:, t0:t0 + tl], kr[:, 0:tl], r=[kr])
        kb.pop()
        kb.push()
        cT = kb.sb("cTall", [128, 3, Tn])
        dma("sp", cT[:], CT.ap().rearrange("(c p) t -> p c t", p=128), w=[cT])
        krT = kb.sb("krT", [32, Tn])
        dma("sp", krT[:], KRT[:, :], w=[krT])
        csb = [kb.sb("csq", [32, 512]) for _ in range(2)]
        snb = [kb.sb("snq", [32, 512]) for _ in range(2)]
        sel = kb.sb("selm", [65, 64])
        op("dve", lambda e: e.memset(sel[:], 0.0), w=[sel])
        op("dve", lambda e: e.memset(sel[64:65, :], 1.0), w=[sel])
        qn = kb.sb("qn", [64, Tn]); qr = kb.sb("qr", [32, Tn]); kn = kb.sb("kn", [64, Tn])
        Vh = kb.sb("Vh", [128, NT, 65])
        op("dve", lambda e: e.memset(Vh[:], 1.0), w=[Vh])
        wq = kb.sb("wqh", [128, 2, 128]); wkv = kb.sb("wkvh", [128, 128])
        t1 = kb.sb("t1q", [32, 512]); t2 = kb.sb("t2q", [32, 512])
        pTs = [kb.sb("pTm", [128, 512]) for _ in range(4)]
        Xs = [kb.sb("Xm", [65, 512]) for _ in range(2)]
        rb = [kb.sb("rbm", [64, 512]) for _ in range(2)]
        oo = [kb.sb("oom", [64, 512]) for _ in range(2)]
        scale = 96.0 ** -0.5
        npt = 0
        for hd in range(16):
            dma("sp", wq[:, :, 0:96], ml_wuq[j * 256:(j + 1) * 256, hd * 96:(hd + 1) * 96].rearrange("(c p) n -> p c n", p=128), w=[wq])
            dma("sp", wq[:, :, 96:128], ml_wuq[j * 256:(j + 1) * 256, 1536 + hd * 32:1536 + (hd + 1) * 32].rearrange("(c p) n -> p c n", p=128), w=[wq])
            dma("sp", wkv[:], ml_wukv[j * 128:(j + 1) * 128, hd * 128:(hd + 1) * 128], w=[wkv])
            for gi, (t0, tl) in enumerate(groups()):
                lat = t0 >= C
                p = nps()
                for kc in range(2):
                    op("pe", lambda e: e.matmul(p[0:64, 0:tl], wq[:, kc, 0:64], cT[:, kc, t0:t0 + tl], start=(kc == 0), stop=(kc == 1)), r=[wq, cT], w=[p])
                op("act", lambda e: e.activation(out=qn[:, t0:t0 + tl], in_=p[0:64, 0:tl], func=AF.Copy), r=[p], w=[qn])
                p = nps()
                for kc in range(2):
                    op("pe", lambda e: e.matmul(p[0:32, 0:tl], wq[:, kc, 64:96], cT[:, kc, t0:t0 + tl], start=(kc == 0), stop=(kc == 1)), r=[wq, cT], w=[p])
                if lat:
                    cs = csb[gi % 2]; sn = snb[gi % 2]
                    dma("sp", cs[:, 0:tl], rope32[0:32, t0 - C:t0 - C + tl], w=[cs])
                    dma("sp", sn[:, 0:tl], rope32[32:64, t0 - C:t0 - C + tl], w=[sn])
                    p2 = nps()
                    for kc in range(2):
                        op("pe", lambda e: e.matmul(p2[0:32, 0:tl], wq[:, kc, 96:128], cT[:, kc, t0:t0 + tl], start=(kc == 0), stop=(kc == 1)), r=[wq, cT], w=[p2])
                    op("dve", lambda e: e.tensor_tensor(out=t1[:, 0:tl], in0=p[0:32, 0:tl], in1=cs[:, 0:tl], op=ALU.mult), r=[p, cs], w=[t1])
                    op("dve", lambda e: e.tensor_tensor(out=t2[:, 0:tl], in0=p2[0:32, 0:tl], in1=sn[:, 0:tl], op=ALU.mult), r=[p2, sn], w=[t2])
                    op("pool", lambda e: e.tensor_tensor(out=qr[:, t0:t0 + tl], in0=t1[:, 0:tl], in1=t2[:, 0:tl], op=ALU.add), r=[t1, t2], w=[qr])
                else:
                    op("dve", lambda e: e.tensor_copy(out=qr[:, t0:t0 + tl], in_=p[0:32, 0:tl]), r=[p], w=[qr])
                p = nps()
                op("pe", lambda e: e.matmul(p[0:64, 0:tl], wkv[:, 0:64], cT[:, 2, t0:t0 + tl], start=True, stop=True), r=[wkv, cT], w=[p])
                op("act", lambda e: e.activation(out=kn[:, t0:t0 + tl], in_=p[0:64, 0:tl], func=AF.Copy), r=[p], w=[kn])
                p = nps()
                ntl = tl // 128
                for tt in range(ntl):
                    op("pe", lambda e: e.matmul(p[:, tt * 64:(tt + 1) * 64], cT[:, 2, t0 + tt * 128:t0 + (tt + 1) * 128], wkv[:, 64:128], start=True, stop=True), r=[wkv, cT], w=[p])
                op("dve", lambda e: e.tensor_copy(out=Vh[:, t0 // 128:t0 // 128 + ntl, 0:64], in_=p[:, 0:ntl * 64].rearrange("p (n d) -> p n d", d=64)), r=[p], w=[Vh])
            qgroups = [g for g in groups() if g[0] >= C]
            if need_ctx:
                qgroups = [(0, C)] + qgroups
            for (t0, tl) in qgroups:
                kts = list(range(C // 128)) if t0 < C else list(range(NT))
                po = PS[4 + (npt % 4)]
                for ci, kt in enumerate(kts):
                    pT = pTs[npt % 4]; npt += 1
                    p = nps()
                    op("pe", lambda e: e.matmul(p[:, 0:tl], kn[:, kt * 128:(kt + 1) * 128], qn[:, t0:t0 + tl], start=True, stop=False), r=[kn, qn], w=[p])
                    op("pe", lambda e: e.matmul(p[:, 0:tl], krT[:, kt * 128:(kt + 1) * 128], qr[:, t0:t0 + tl], start=False, stop=True), r=[krT, qr], w=[p])
                    op("act", lambda e: e.activation(out=pT[:, 0:tl], in_=p[:, 0:tl], func=AF.Exp, scale=scale), r=[p], w=[pT])
                    op("pe", lambda e: e.matmul(po[0:65, 0:tl], Vh[:, kt, :], pT[:, 0:tl], start=(ci == 0), stop=(ci == len(kts) - 1)), r=[Vh, pT], w=[po])
                X = Xs[npt % 2]; r_ = rb[npt % 2]; o_ = oo[npt % 2]
                op("act", lambda e: e.activation(out=X[:, 0:tl], in_=po[0:65, 0:tl], func=AF.Copy), r=[po], w=[X])
                p = nps()
                op("pe", lambda e: e.matmul(p[0:64, 0:tl], sel[:], X[:, 0:tl], start=True, stop=True), r=[sel, X], w=[p])
                op("dve", lambda e: e.reciprocal(out=r_[:, 0:tl], in_=p[0:64, 0:tl]), r=[p], w=[r_])
                op("dve", lambda e: e.tensor_tensor(out=o_[:, 0:tl], in0=X[0:64, 0:tl], in1=r_[:, 0:tl], op=ALU.mult), r=[X, r_], w=[o_])
                dma("pool", OT[hd * 64:(hd + 1) * 64, t0:t0 + tl], o_[:, 0:tl], r=[o_])
        kb.pop()
        return ml_w_o[j * D:(j + 1) * D, :], D, None

    Xsrc = xin
    kcount = {0: 0, 1: 0, 2: 0, -1: 0}
    for i, kind in enumerate(kinds):
        last = (i == L - 1)
        need_ctx = not (last and cfg.last_noctx)
        phase_mod(i)
        for b in range(NB):
            phase_h(i, b, Xsrc)
            if kind == -1:
                w_o, Dm, b_o = mixer_identity(i, b)
            elif kind == 0:
                w_o, Dm, b_o = mixer_rg(i, b, kcount[0], need_ctx)
            elif kind == 2:
                psn[0] = 4
                w_o, Dm, b_o = mixer_mla(i, b, kcount[2], need_ctx)
                psn[0] = 8
            elif kind == 1:
                psn[0] = 4
                w_o, Dm, b_o = mixer_gqa(i, b, kcount[1], need_ctx)
                psn[0] = 8
            else:
                raise NotImplementedError
            phase_out(i, b, Xsrc, w_o, Dm, b_o, need_ctx)
        phase_moe(i, last)
        kcount[kind] += 1
        Xsrc = XB
    kb.close()
    return kb


def consts():
    cm = np.zeros((128, 416), np.float32)
    cm[:, 0:128] = np.eye(128, dtype=np.float32)
    k = np.arange(128)
    cm[:, 128:256] = (k[:, None] < k[None, :]).astype(np.float32)
    cm[:, 256:384] = 1.0
    cm[:, 384:416] = np.arange(32, dtype=np.float32)[None, :]
    return cm


def rope_table(S, rot_dim):
    rows = S // 64
    row = np.repeat(np.arange(rows, dtype=np.float32), 64)
    col = np.tile(np.arange(64, dtype=np.float32), rows)
    n_freq = rot_dim // 4
    inv = (np.float32(10000.0) ** (-np.arange(n_freq, dtype=np.float32) / np.float32(n_freq))).astype(np.float32)
    ang = np.concatenate([row[:, None] * inv, col[:, None] * inv], axis=-1).astype(np.float32)
    cos = np.cos(ang).astype(np.float32).T
    sin = np.sin(ang).astype(np.float32).T
    return np.ascontiguousarray(np.concatenate([cos, cos, -sin, sin], axis=0))


def prep_inputs(cfg, inp):
    NB, S, kinds = cfg.NB, cfg.S, cfg.kinds
    L = len(kinds)
    f = lambda a: np.ascontiguousarray(np.asarray(a, dtype=np.float32))
    shared = {
        "ada_w": f(inp["ada_w"]).reshape(L * D, 6 * D),
        "ada_b": f(inp["ada_b"]),
        "lnp": f(np.stack([inp["ln1_g"], inp["ln1_b"], inp["ln2_g"], inp["ln2_b"]], axis=1)).reshape(L * 4, D),
        "router_w": f(inp["router_w"]).reshape(L * D, E),
        "router_b": f(inp["router_b"]),
        "wgu": f(np.concatenate([inp["exp_gu_w"][..., 0::2], inp["exp_gu_w"][..., 1::2]], axis=-1)).reshape(L * E * D, 2 * FF),
        "bgu": f(np.concatenate([inp["exp_gu_b"][..., 0::2], inp["exp_gu_b"][..., 1::2]], axis=-1)).reshape(L * E, 2 * FF),
        "wdn": f(inp["exp_down_w"]).reshape(L * E * FF, D),
        "bdn": f(inp["exp_down_b"]).reshape(L * E, D),
        "cmat": consts(),
    }
    if -1 in kinds:
        shared["w_id"] = np.eye(D, dtype=np.float32)
    if 2 in kinds:
        n = kinds.count(2)
        Wd = inp["mla_w_down"][:n]
        def sw16(a):
            sh = a.shape
            return a.reshape(sh[:-1] + (sh[-1] // 32, 2, 16))[..., ::-1, :].reshape(sh)
        shared["ml_wd"] = f(np.concatenate([Wd, sw16(Wd[..., 384:416])], axis=-1)).reshape(n * D, 448)
        shared["ml_norm"] = f(np.concatenate([inp["mla_q_norm"][:n], inp["mla_kv_norm"][:n]], axis=-1))
        Wq = inp["mla_w_uq"][:n].reshape(n, 256, 16, 96)
        shared["ml_wuq"] = f(np.concatenate([Wq.reshape(n, 256, 1536), sw16(np.ascontiguousarray(Wq[..., 64:96])).reshape(n, 256, 512)], axis=-1)).reshape(n * 256, 2048)
        shared["ml_wukv"] = f(inp["mla_w_ukv"][:n]).reshape(n * 128, 2048)
        shared["ml_w_o"] = f(inp["mla_w_o"][:n]).reshape(n * D, D)
        shared["rope32"] = rope_table(S, 32)
    if 1 in kinds:
        n = kinds.count(1)
        W = inp["gqa_w_qkv"][:n]
        Bq = inp["gqa_b_qkv"][:n]
        def sw(a):
            sh = a.shape
            return a.reshape(sh[:-1] + (sh[-1] // 64, 2, 32))[..., ::-1, :].reshape(sh)
        shared["gq_w"] = f(np.concatenate([W[..., 0:1024], sw(W[..., 0:1024]), W[..., 1024:1152], sw(W[..., 1024:1152]), W[..., 1152:1280]], axis=-1)).reshape(n * D, 2432)
        b20 = Bq.reshape(n, 20, 64)
        shared["gq_b64"] = f(np.concatenate([b20.transpose(0, 2, 1), sw(Bq).reshape(n, 20, 64).transpose(0, 2, 1)], axis=-1)).reshape(n * 64, 40)
        shared["gq_bv"] = f(Bq[:, 1152:1280])
        shared["gq_sinks"] = f(inp["gqa_sinks"][:n])
        shared["gq_w_o"] = f(inp["gqa_w_o"][:n]).reshape(n * D, D)
        shared["gq_b_o"] = f(inp["gqa_b_o"][:n])
        shared["rope64"] = rope_table(S, 64)
    if 0 in kinds:
        n_rg = kinds.count(0)
        shared["rg_w_in"] = f(inp["rg_w_in"][:n_rg]).reshape(n_rg * D, 2 * D_RNN)
        shared["rg_p"] = f(np.concatenate([inp["rg_conv_w"][:n_rg], inp["rg_conv_b"][:n_rg, None, :], inp["rg_gate_a_b"][:n_rg], inp["rg_gate_x_b"][:n_rg],
                                           inp["rg_lambda"][:n_rg]], axis=1)).reshape(n_rg * 11, D_RNN)
        shared["rg_ga"] = f(inp["rg_gate_a_w"][:n_rg]).reshape(n_rg * 2 * 6 * 256, 256)
        shared["rg_gx"] = f(inp["rg_gate_x_w"][:n_rg]).reshape(n_rg * 2 * 6 * 256, 256)
        shared["rg_w_out"] = f(inp["rg_w_out"][:n_rg]).reshape(n_rg * D_RNN, D)
    maps = []
    for c in range(cfg.NC):
        bs = slice(c * NB, (c + 1) * NB)
        m = dict(shared)
        m["xin"] = f(np.concatenate([inp["ctx"][bs], inp["x"][bs]], axis=1)).reshape(NB * (C + S), D)
        m["cc"] = f(np.concatenate([inp["c"][bs], inp["c_ctx"][None, :]], axis=0))
        maps.append(m)
    return maps


def run(cfg, inp):
    kb = build(cfg)
    maps = prep_inputs(cfg, inp)
    res = run_bass_kernel_spmd(kb.nc, maps, core_ids=list(range(cfg.NC)))
    outs = [res.results[c]["out"].reshape(cfg.NB, cfg.S, D) for c in range(cfg.NC)]
    return np.concatenate(outs, axis=0)


def kernel(**inputs):
    cfg = Cfg(NB=2, S=4096, kinds=(0, 1, 2, 0), NC=8)
    return run(cfg, inputs).astype(np.float32)
```

```python
import numpy as np
from contextlib import ExitStack
import concourse.bass as bass
import concourse.mybir as mybir
from concourse.bass_utils import run_bass_kernel_spmd

F32 = mybir.dt.float32
I32 = mybir.dt.int32
U32 = mybir.dt.uint32
BF16 = mybir.dt.bfloat16
AF = mybir.ActivationFunctionType
ALU = mybir.AluOpType
AX = mybir.AxisListType

D = 1024
C = 256
E = 32
FF = 1024
D_RNN = 1536
SB = 512
ALPHA = 8.0 ** 0.25
LN_EPS = 1e-5


class Res:
    __slots__ = ("w", "r")

    def __init__(self):
        self.w = None
        self.r = {}


class T:
    def __init__(self, h):
        self.h = h
        self.res = [Res()]

    def __getitem__(self, k):
        return self.h[k]


class KB:
    def __init__(self, n_dma_sems=48):
        self.nc = bass.Bass("TRN2", target_bir_lowering=False)
        nc = self.nc
        self.es = ExitStack()
        self.stacks = []
        self.eng = {"pe": nc.tensor, "act": nc.scalar, "dve": nc.vector, "pool": nc.gpsimd, "sp": nc.sync}
        self.sems = []
        self.esem = {}
        self.cnt = {}
        self.waited = {e: {} for e in self.eng}
        for e in self.eng:
            self.esem[e] = self._newsem("s_" + e)
            self.cnt[e] = 0
        self.dsem = [self._newsem("d%d" % i) for i in range(n_dma_sems)]
        self.duses = [0] * n_dma_sems
        self.drr = 0
        self.ninst = 0
        self.uid = 0

    def _newsem(self, name):
        h = self.es.enter_context(self.nc.semaphore(name))
        self.sems.append(h)
        return len(self.sems) - 1

    def push(self):
        self.stacks.append(ExitStack())

    def pop(self):
        self.barrier()
        self.stacks.pop().close()

    def _ctx(self):
        return self.stacks[-1] if self.stacks else self.es

    def sb(self, name, shape, dtype=F32):
        self.uid += 1
        return T(self._ctx().enter_context(self.nc.sbuf_tensor("%s_%d" % (name, self.uid), list(shape), dtype)))

    def ps(self, name, shape, dtype=F32):
        return T(self.es.enter_context(self.nc.psum_tensor(name, list(shape), dtype)))

    def dram(self, name, shape, dtype=F32, kind="Internal"):
        return self.nc.dram_tensor(name, list(shape), dtype, kind=kind)

    def _wait(self, e, deps):
        eng = self.eng[e]
        best = {}
        for d in deps:
            if d is None:
                continue
            s, v, src = d
            if src == e and e == "pe":
                continue
            if self.waited[e].get(s, 0) >= v:
                continue
            if best.get(s, 0) < v:
                best[s] = v
        for s, v in best.items():
            eng.wait_ge(self.sems[s], v)
            self.waited[e][s] = v

    def _collect(self, r, w):
        deps = []
        for x in r:
            for rs in x.res:
                deps.append(rs.w)
        for x in w:
            for rs in x.res:
                deps.append(rs.w)
                deps.extend(rs.r.values())
        return deps

    def _commit(self, r, w, dep, key):
        for x in r:
            for rs in x.res:
                rs.r[key] = dep
        for x in w:
            for rs in x.res:
                rs.w = dep
                rs.r = {}

    def op(self, e, fn, r=(), w=()):
        self._wait(e, self._collect(r, w))
        inst = fn(self.eng[e])
        self.cnt[e] += 1
        inst.then_inc(self.sems[self.esem[e]], 1)
        dep = (self.esem[e], self.cnt[e], e)
        self._commit(r, w, dep, e)
        self.ninst += 1
        return inst

    def dma(self, q, out, in_, r=(), w=(), fn=None):
        i = self.drr
        self.drr = (i + 1) % len(self.dsem)
        s = self.dsem[i]
        deps = self._collect(r, w)
        deps.append((s, self.duses[i] * 16, "dma"))
        self._wait(q, deps)
        eng = self.eng[q]
        if fn is None:
            inst = eng.dma_start(out=out, in_=in_)
        else:
            inst = fn(eng)
        inst.then_inc(self.sems[s], 16)
        self.duses[i] += 1
        dep = (s, self.duses[i] * 16, "dma")
        self._commit(r, w, dep, ("dma", s))
        self.ninst += 1
        return inst

    def barrier(self):
        deps = [(self.esem[e], self.cnt[e], "x") for e in self.eng]
        deps += [(s, self.duses[i] * 16, "x") for i, s in enumerate(self.dsem)]
        for e in self.eng:
            self._wait(e, [d for d in deps if d[0] != self.esem[e]])

    def close(self):
        self.barrier()
        self.es.close()


class Cfg:
    def __init__(self, NB=2, S=4096, kinds=(0, 1, 2, 0), NC=8, last_noctx=True, debug=False):
        self.debug = debug
        self.NB = NB
        self.S = S
        self.kinds = list(kinds)
        self.NC = NC
        self.last_noctx = last_noctx


def build(cfg):
    NB, S, kinds = cfg.NB, cfg.S, cfg.kinds
    L = len(kinds)
    Tn = C + S
    NT = Tn // 128
    NS = NB + 1
    kb = KB()
    nc = kb.nc
    op, dma = kb.op, kb.dma

    def din(name, shape, dt=F32):
        return kb.dram(name, shape, dt, kind="ExternalInput")

    xin = din("xin", [NB * Tn, D])
    cc = din("cc", [NS, D])
    ada_w = din("ada_w", [L * D, 6 * D])
    ada_b = din("ada_b", [L, 6 * D])
    lnp = din("lnp", [L * 4, D])
    router_w = din("router_w", [L * D, E])
    router_b = din("router_b", [L, E])
    wgu = din("wgu", [L * E * D, 2 * FF])
    bgu = din("bgu", [L * E, 2 * FF])
    wdn = din("wdn", [L * E * FF, D])
    bdn = din("bdn", [L * E, D])
    cmat = din("cmat", [128, 384 + 32])
    w_id = din("w_id", [D, D]) if -1 in kinds else None
    n_rg = max(1, kinds.count(0))
    if 2 in kinds:
        n_ml = kinds.count(2)
        ml_wd = din("ml_wd", [n_ml * D, 448])
        ml_norm = din("ml_norm", [n_ml, 384])
        ml_wuq = din("ml_wuq", [n_ml * 256, 2048])
        ml_wukv = din("ml_wukv", [n_ml * 128, 2048])
        ml_w_o = din("ml_w_o", [n_ml * D, D])
        rope32 = din("rope32", [64, S])
        CT = kb.dram("CT", [384, Tn])
        KRT = kb.dram("KRT", [32, Tn])
    if 1 in kinds:
        n_gq = kinds.count(1)
        gq_w = din("gq_w", [n_gq * D, 2432])
        gq_b64 = din("gq_b64", [n_gq * 64, 40])
        gq_bv = din("gq_bv", [n_gq, 128])
        gq_sinks = din("gq_sinks", [n_gq, 16])
        gq_w_o = din("gq_w_o", [n_gq * D, D])
        gq_b_o = din("gq_b_o", [n_gq, D])
        rope64 = din("rope64", [2 * 64, S])
        QT = kb.dram("QT", [64, 16 * Tn])
        KT = kb.dram("KT", [64, 2 * Tn])
        VA = kb.dram("VA", [Tn, 130])
    if 0 in kinds:
        rg_w_in = din("rg_w_in", [n_rg * D, 2 * D_RNN])
        rg_p = din("rg_p", [n_rg * 11, D_RNN])
        rg_ga = din("rg_ga", [n_rg * 2 * 6 * 256, 256])
        rg_gx = din("rg_gx", [n_rg * 2 * 6 * 256, 256])
        rg_w_out = din("rg_w_out", [n_rg * D_RNN, D])
        PR = kb.dram("PR", [2 * D_RNN, Tn])
    out = kb.dram("out", [NB * S, D], F32, kind="ExternalOutput")

    dk = "ExternalOutput" if cfg.debug else "Internal"
    XA = kb.dram("XA", [NB * Tn, D], F32, dk)
    XB = kb.dram("XB", [NB * Tn, D], F32, dk)
    H2 = kb.dram("H2", [NB * Tn, D], F32, dk)
    HT = kb.dram("HT", [D, Tn], F32, dk)
    OT = kb.dram("OT", [D_RNN, Tn], F32, dk)
    MODD = kb.dram("MODD", [NS, 6 * D], F32, dk)
    n_asg_max = NB * Tn * 4
    NBLK = (n_asg_max + E * (SB - 1) + SB - 1) // SB
    NSLOT = NBLK * SB
    XS = kb.dram("XS", [NSLOT, D], F32, dk)
    YS = kb.dram("YS", [NSLOT, D], F32, dk)
    if cfg.debug:
        DBG = {n: kb.dram("dbg_" + n, shp, dt, "ExternalOutput") for n, shp, dt in [
            ("slot4", [128, NB * NT * 4], I32), ("gate4", [128, NB * NT * 4], F32), ("top8", [128, NB * NT * 8], F32),
            ("bef", [128, NBLK], F32), ("runcnt", [128, E], F32), ("excl", [128, E], F32), ("padf", [128, E], F32), ("incl", [128, E], F32), ("posA", [128, NB * NT * E], F32)]}

    cm = kb.sb("cm", [128, 416])
    ident = lambda n=128: cm[0:n, 0:n]
    tri = cm[:, 128:256]
    ones = cm[:, 256:384]
    iota32 = cm[:, 384:416]
    dma("sp", cm[:], cmat[:, :], w=[cm])
    PS = [kb.ps("ps%d" % i, [128, 512]) for i in range(8)]
    psi = [0]
    psn = [8]

    def nps():
        psi[0] = (psi[0] + 1) % psn[0]
        return PS[psi[0]]

    siluT = kb.sb("siluT", [128, 8, NS])
    modT = kb.sb("modT", [128, 48, NS])
    lnrow = kb.sb("lnrow", [128, 4, D])
    epsT = kb.sb("epsT", [128, 1])
    op("dve", lambda e: e.memset(epsT[:], LN_EPS), w=[epsT])

    kb.push()
    ccs = kb.sb("ccs", [NS, D])
    dma("sp", ccs[:], cc[:, :], w=[ccs])
    op("act", lambda e: e.activation(out=ccs[:], in_=ccs[:], func=AF.Silu), r=[ccs], w=[ccs])
    p = nps()
    for c in range(8):
        op("pe", lambda e: e.transpose(p[:, c * NS:(c + 1) * NS], ccs[:, c * 128:(c + 1) * 128], ident(NS)), r=[ccs, cm], w=[p])
    op("dve", lambda e: e.tensor_copy(out=siluT[:].rearrange("p c s -> p (c s)"), in_=p[:, 0:8 * NS]), r=[p], w=[siluT])
    kb.pop()

    def phase_mod(i):
        kb.push()
        aw = [kb.sb("aw", [128, 8, 512]) for _ in range(2)]
        modtm = kb.sb("modtm", [NS, 6 * D])
        adab = kb.sb("adab", [NS, 6 * D])
        for s in range(NS):
            dma("sp", adab[s:s + 1, :], ada_b[i:i + 1, :], w=[adab])
        for g in range(12):
            a = aw[g % 2]
            dma("sp", a[:], ada_w[i * D:(i + 1) * D, g * 512:(g + 1) * 512].rearrange("(c p) n -> p c n", p=128), w=[a])
            p = nps()
            for kc in range(8):
                op("pe", lambda e: e.matmul(p[0:NS, :], siluT[:, kc, :], a[:, kc, :], start=(kc == 0), stop=(kc == 7)),
                   r=[siluT, a], w=[p])
            op("dve", lambda e: e.tensor_tensor(out=modtm[:, g * 512:(g + 1) * 512], in0=p[0:NS, :], in1=adab[:, g * 512:(g + 1) * 512], op=ALU.add),
               r=[p, adab], w=[modtm])
        for k in (1, 4):
            op("dve", lambda e: e.tensor_scalar(out=modtm[:, k * D:(k + 1) * D], in0=modtm[:, k * D:(k + 1) * D], scalar1=1.0, scalar2=None, op0=ALU.add),
               r=[modtm], w=[modtm])
        dma("sp", MODD[:, :], modtm[:], r=[modtm])
        p = nps()
        for j in range(48):
            op("pe", lambda e: e.transpose(p[:, j * NS:(j + 1) * NS], modtm[:, j * 128:(j + 1) * 128], ident(NS)), r=[modtm, cm], w=[p])
        op("dve", lambda e: e.tensor_copy(out=modT[:].rearrange("p c s -> p (c s)"), in_=p[:, 0:48 * NS]), r=[p], w=[modT])
        for k in range(4):
            dma("sp", lnrow[:, k, :], lnp[i * 4 + k, :].partition_broadcast(128), w=[lnrow])
        kb.pop()

    def groups():
        gs = [(0, C)]
        t = C
        while t < Tn:
            gs.append((t, min(512, Tn - t)))
            t += 512
        return gs

    def phase_h(i, b, Xsrc):
        kb.push()
        xt = [kb.sb("xt", [128, D]) for _ in range(8)]
        hT = [kb.sb("hT", [128, 8, 512]) for _ in range(2)]
        n = 0
        for gi, (t0, tl) in enumerate(groups()):
            s = NB if t0 < C else b
            nt = tl // 128
            tiles = []
            for j in range(nt):
                x = xt[n % 8]
                n += 1
                dma("sp", x[:], Xsrc[b * Tn + t0 + j * 128: b * Tn + t0 + (j + 1) * 128, :], w=[x])
                tiles.append(x)
            h = hT[gi % 2]
            for c in range(8):
                p = nps()
                for j in range(nt):
                    x = tiles[j]
                    op("pe", lambda e: e.transpose(p[:, j * 128:(j + 1) * 128], x[:, c * 128:(c + 1) * 128], ident()), r=[x, cm], w=[p])
                op("dve", lambda e: e.tensor_scalar(out=h[:, c, 0:tl], in0=p[:, 0:tl], scalar1=modT[:, 8 + c, s:s + 1], scalar2=modT[:, c, s:s + 1],
                                                    op0=ALU.mult, op1=ALU.add), r=[p, modT], w=[h])
            dma("pool", HT[:, t0:t0 + tl].rearrange("(c p) t -> p c t", p=128), h[:, :, 0:tl], r=[h])
        kb.pop()

    def layernorm(t, k0, st, mv, tmp):
        for h2 in range(2):
            op("dve", lambda e: e.bn_stats(out=st[:, h2, :], in_=t[:, h2 * 512:(h2 + 1) * 512]), r=[t], w=[st])
        op("dve", lambda e: e.bn_aggr(out=mv[:, 0:2], in_=st[:].rearrange("p a b -> p (a b)")), r=[st], w=[mv])
        op("act", lambda e: e.activation(out=mv[:, 2:3], in_=mv[:, 1:2], func=AF.Sqrt, bias=epsT[:, 0:1], scale=1.0), r=[mv, epsT], w=[mv])
        op("dve", lambda e: e.reciprocal(out=mv[:, 3:4], in_=mv[:, 2:3]), r=[mv], w=[mv])
        op("dve", lambda e: e.tensor_scalar(out=t[:], in0=t[:], scalar1=mv[:, 0:1], scalar2=mv[:, 3:4], op0=ALU.subtract, op1=ALU.mult), r=[t, mv], w=[t])
        op("dve", lambda e: e.tensor_tensor(out=t[:], in0=t[:], in1=lnrow[:, k0, :], op=ALU.mult), r=[t, lnrow], w=[t])
        op("dve", lambda e: e.tensor_tensor(out=t[:], in0=t[:], in1=lnrow[:, k0 + 1, :], op=ALU.add), r=[t, lnrow], w=[t])

    NTT = NB * NT
    maskA = kb.sb("maskA", [128, NTT, E])
    top8 = kb.sb("top8", [128, NTT, 8])
    idx8 = kb.sb("idx8", [128, NTT, 8], U32)
    posA = kb.sb("posA", [128, NTT, E])
    runcnt = kb.sb("runcnt", [128, E])
    slot4f = kb.sb("slot4f", [128, NTT, 4])
    slot4 = kb.sb("slot4", [128, NTT, 4], I32)
    gate4 = kb.sb("gate4", [128, NTT, 4])

    def phase_out(i, b, Xsrc, w_o, Dm, b_o, need_ctx):
        kb.push()
        KC = Dm // 128
        wo = kb.sb("wo", [128, KC, D])
        dma("sp", wo[:], w_o.rearrange("(c p) n -> p c n", p=128), w=[wo])
        rw = kb.sb("rw", [128, 8, E])
        dma("sp", rw[:], router_w[i * D:(i + 1) * D, :].rearrange("(c p) n -> p c n", p=128), w=[rw])
        rbrow = kb.sb("rbrow", [128, E])
        dma("sp", rbrow[:], router_b[i, :].partition_broadcast(128), w=[rbrow])
        grow = {}
        for s in (NB, b):
            g = kb.sb("grow", [128, 3, D])
            for k, kind in enumerate((2, 4, 3)):
                dma("sp", g[:, k, :], MODD[s, kind * D:(kind + 1) * D].partition_broadcast(128), w=[g])
            grow[s] = g
        borow = None
        if b_o is not None:
            borow = kb.sb("borow", [128, D])
            dma("sp", borow[:], b_o.partition_broadcast(128), w=[borow])
        oTs = [kb.sb("oT", [128, KC, 128]) for _ in range(2)]
        xts = [kb.sb("xt", [128, D]) for _ in range(2)]
        ts = [kb.sb("tt", [128, D]) for _ in range(2)]
        h2s = [kb.sb("h2", [128, D]) for _ in range(2)]
        h2Ts = [kb.sb("h2T", [128, 8, 128]) for _ in range(2)]
        st = kb.sb("st", [128, 2, 6])
        mv = kb.sb("mv", [128, 4])
        lg = kb.sb("lg", [128, E])
        t_start = 0 if need_ctx else C // 128
        for ti in range(t_start, NT):
            s = NB if ti < C // 128 else b
            g = grow[s]
            r0 = b * Tn + ti * 128
            gi = b * NT + ti
            oT = oTs[ti % 2]; x = xts[ti % 2]; t = ts[ti % 2]; h2 = h2s[ti % 2]; h2T = h2Ts[ti % 2]
            dma("sp", oT[:], OT[0:Dm, ti * 128:(ti + 1) * 128].rearrange("(c p) t -> p c t", p=128), w=[oT])
            dma("sp", x[:], Xsrc[r0:r0 + 128, :], w=[x])
            for hf in range(2):
                p = nps()
                for kc in range(KC):
                    op("pe", lambda e: e.matmul(p[:, :], oT[:, kc, :], wo[:, kc, hf * 512:(hf + 1) * 512], start=(kc == 0), stop=(kc == KC - 1)),
                       r=[oT, wo], w=[p])
                sl = slice(hf * 512, (hf + 1) * 512)
                if borow is not None:
                    op("dve", lambda e: e.tensor_tensor(out=t[:, sl], in0=p[:, :], in1=borow[:, sl], op=ALU.add), r=[p, borow], w=[t])
                    op("dve", lambda e: e.tensor_tensor(out=t[:, sl], in0=t[:, sl], in1=g[:, 0, sl], op=ALU.mult), r=[t, g], w=[t])
                else:
                    op("dve", lambda e: e.tensor_tensor(out=t[:, sl], in0=p[:, :], in1=g[:, 0, sl], op=ALU.mult), r=[p, g], w=[t])
            op("dve", lambda e: e.scalar_tensor_tensor(out=t[:], in0=x[:], scalar=ALPHA, in1=t[:], op0=ALU.mult, op1=ALU.add), r=[x, t], w=[t])
            layernorm(t, 0, st, mv, None)
            dma("pool", XA[r0:r0 + 128, :], t[:], r=[t])
            op("pool", lambda e: e.tensor_tensor(out=h2[:], in0=t[:], in1=g[:, 1, :], op=ALU.mult), r=[t, g], w=[h2])
            op("pool", lambda e: e.tensor_tensor(out=h2[:], in0=h2[:], in1=g[:, 2, :], op=ALU.add), r=[h2, g], w=[h2])
            dma("pool", H2[r0:r0 + 128, :], h2[:], r=[h2])
            for half in range(2):
                p = nps()
                for c4 in range(4):
                    c = half * 4 + c4
                    op("pe", lambda e: e.transpose(p[:, c4 * 128:(c4 + 1) * 128], h2[:, c * 128:(c + 1) * 128], ident()), r=[h2, cm], w=[p])
                op("act", lambda e: e.activation(out=h2T[:, half * 4:(half + 1) * 4, :].rearrange("p c t -> p (c t)"), in_=p[:, :], func=AF.Copy),
                   r=[p], w=[h2T])
            p = nps()
            for c in range(8):
                op("pe", lambda e: e.matmul(p[:, 0:E], h2T[:, c, :], rw[:, c, :], start=(c == 0), stop=(c == 7)), r=[h2T, rw], w=[p])
            op("dve", lambda e: e.tensor_tensor(out=lg[:], in0=p[:, 0:E], in1=rbrow[:], op=ALU.add), r=[p, rbrow], w=[lg])
            op("dve", lambda e: e.max(out=top8[:, gi, :], in_=lg[:]), r=[lg], w=[top8])
            op("dve", lambda e: e.max_index(out=idx8[:, gi, :], in_max=top8[:, gi, :], in_values=lg[:]), r=[lg, top8], w=[idx8])
            op("dve", lambda e: e.tensor_scalar(out=maskA[:, gi, :], in0=lg[:], scalar1=top8[:, gi, 3:4], scalar2=None, op0=ALU.is_ge), r=[lg, top8], w=[maskA])
        kb.pop()

    def phase_moe(i, last):
        tiles = []
        for b in range(NB):
            for ti in range(NT):
                if last and cfg.last_noctx and ti < C // 128:
                    continue
                tiles.append((b, ti))
        kb.push()
        op("dve", lambda e: e.memset(runcnt[:], 0.0), w=[runcnt])
        for (b, ti) in tiles:
            gi = b * NT + ti
            p = nps()
            op("pe", lambda e: e.matmul(p[:, 0:E], tri, maskA[:, gi, :], start=True, stop=True), r=[cm, maskA], w=[p])
            op("pe", lambda e: e.matmul(p[:, E:2 * E], ones, maskA[:, gi, :], start=True, stop=True), r=[cm, maskA], w=[p])
            op("dve", lambda e: e.tensor_tensor(out=posA[:, gi, :], in0=p[:, 0:E], in1=runcnt[:], op=ALU.add), r=[p, runcnt], w=[posA])
            op("dve", lambda e: e.tensor_tensor(out=runcnt[:], in0=p[:, E:2 * E], in1=runcnt[:], op=ALU.add), r=[p, runcnt], w=[runcnt])
        padf = kb.sb("padf", [128, E])
        incl = kb.sb("incl", [128, E])
        excl = kb.sb("excl", [128, E])
        zer = kb.sb("zer", [128, E])
        op("dve", lambda e: e.memset(zer[:], 0.0), w=[zer])
        blkpos = kb.sb("blkpos", [128, NBLK])
        tmpb = kb.sb("tmpb", [128, NBLK])
        op("pool", lambda e: e.iota(blkpos[:], pattern=[[SB, NBLK]], base=0, channel_multiplier=0, allow_small_or_imprecise_dtypes=True), w=[blkpos])
        for ex in range(E):
            op("dve", lambda e: e.tensor_scalar(out=tmpb[:], in0=blkpos[:], scalar1=runcnt[:, ex:ex + 1], scalar2=None, op0=ALU.is_lt, op1=ALU.add,
                                                accum_out=padf[:, ex:ex + 1]), r=[blkpos, runcnt], w=[tmpb, padf])
        op("dve", lambda e: e.tensor_scalar(out=padf[:], in0=padf[:], scalar1=float(SB), scalar2=None, op0=ALU.mult), r=[padf], w=[padf])
        op("dve", lambda e: e.tensor_tensor_scan(out=incl[:], data0=padf[:], data1=zer[:], initial=0.0, op0=ALU.add, op1=ALU.add), r=[padf, zer], w=[incl])
        op("dve", lambda e: e.tensor_tensor(out=excl[:], in0=incl[:], in1=padf[:], op=ALU.subtract), r=[incl, padf], w=[excl])
        bef = kb.sb("bef", [128, NBLK])
        op("dve", lambda e: e.memset(bef[:], 0.0), w=[bef])
        for ex in range(E):
            op("dve", lambda e: e.tensor_scalar(out=tmpb[:], in0=blkpos[:], scalar1=incl[:, ex:ex + 1], scalar2=None, op0=ALU.is_ge), r=[blkpos, incl], w=[tmpb])
            op("dve", lambda e: e.tensor_tensor(out=bef[:], in0=bef[:], in1=tmpb[:], op=ALU.add), r=[bef, tmpb], w=[bef])
        op("dve", lambda e: e.tensor_scalar(out=bef[:], in0=bef[:], scalar1=float(E - 1), scalar2=None, op0=ALU.min), r=[bef], w=[bef])
        idxf = kb.sb("idxf", [128, 4])
        oh = kb.sb("oh", [128, 4, E])
        slt = kb.sb("slt", [128, E])
        den = kb.sb("den", [128, 1])
        nb0 = kb.sb("nb0", [128, 1])
        for (b, ti) in tiles:
            gi = b * NT + ti
            op("dve", lambda e: e.tensor_copy(out=idxf[:], in_=idx8[:, gi, 0:4]), r=[idx8], w=[idxf])
            op("dve", lambda e: e.tensor_tensor(out=slt[:], in0=posA[:, gi, :], in1=excl[:], op=ALU.add), r=[posA, excl], w=[slt])
            for k in range(4):
                op("dve", lambda e: e.tensor_scalar(out=oh[:, k, :], in0=iota32, scalar1=idxf[:, k:k + 1], scalar2=None, op0=ALU.is_equal), r=[cm, idxf], w=[oh])
                op("dve", lambda e: e.tensor_tensor(out=oh[:, k, :], in0=oh[:, k, :], in1=slt[:], op=ALU.mult), r=[oh, slt], w=[oh])
            op("dve", lambda e: e.reduce_sum(out=slot4f[:, gi, :], in_=oh[:], axis=AX.X), r=[oh], w=[slot4f])
            op("dve", lambda e: e.tensor_copy(out=slot4[:, gi, :], in_=slot4f[:, gi, :]), r=[slot4f], w=[slot4])
            op("dve", lambda e: e.tensor_scalar(out=nb0[:], in0=top8[:, gi, 0:1], scalar1=-1.0, scalar2=None, op0=ALU.mult), r=[top8], w=[nb0])
            op("act", lambda e: e.activation(out=gate4[:, gi, :], in_=top8[:, gi, 0:4], func=AF.Exp, bias=nb0[:, 0:1], scale=1.0), r=[top8, nb0], w=[gate4])
            op("dve", lambda e: e.reduce_sum(out=den[:], in_=gate4[:, gi, :], axis=AX.X), r=[gate4], w=[den])
            op("dve", lambda e: e.reciprocal(out=den[:], in_=den[:]), r=[den], w=[den])
            op("dve", lambda e: e.tensor_scalar(out=gate4[:, gi, :], in0=gate4[:, gi, :], scalar1=den[:, 0:1], scalar2=None, op0=ALU.mult), r=[gate4, den], w=[gate4])
        if cfg.debug:
            dma("sp", DBG["slot4"][:, :], slot4[:].rearrange("p a b -> p (a b)"), r=[slot4])
            dma("sp", DBG["gate4"][:, :], gate4[:].rearrange("p a b -> p (a b)"), r=[gate4])
            dma("sp", DBG["top8"][:, :], top8[:].rearrange("p a b -> p (a b)"), r=[top8])
            dma("sp", DBG["posA"][:, :], posA[:].rearrange("p a b -> p (a b)"), r=[posA])
            dma("sp", DBG["bef"][:, :], bef[:], r=[bef])
            dma("sp", DBG["runcnt"][:, :], runcnt[:], r=[runcnt])
            dma("sp", DBG["excl"][:, :], excl[:], r=[excl])
            dma("sp", DBG["padf"][:, :], padf[:], r=[padf])
            dma("sp", DBG["incl"][:, :], incl[:], r=[incl])
        kb.push()
        h2s = [kb.sb("h2d", [128, D]) for _ in range(3)]
        for n, (b, ti) in enumerate(tiles):
            gi = b * NT + ti
            r0 = b * Tn + ti * 128
            h2 = h2s[n % 3]
            dma("sp", h2[:], H2[r0:r0 + 128, :], w=[h2])
            for k in range(4):
                dma("pool", None, None, r=[h2, slot4], fn=lambda e: e.indirect_dma_start(
                    out=XS[:, :], out_offset=bass.IndirectOffsetOnAxis(ap=slot4[:, gi, k:k + 1], axis=0), in_=h2[:], in_offset=None))
        kb.pop()
        kb.push()
        wg = [kb.sb("wg", [128, 8, 2 * FF], BF16) for _ in range(2)]
        wd = [kb.sb("wd", [128, 8, D], BF16) for _ in range(2)]
        brow = [kb.sb("brow", [128, 3 * D], BF16) for _ in range(2)]
        onesb = kb.sb("onesb", [1, SB], BF16)
        op("dve", lambda e: e.memset(onesb[:], 1.0), w=[onesb])
        iotap = kb.sb("iotap", [128, 8])
        op("pool", lambda e: e.iota(iotap[:], pattern=[[128, 8]], base=0, channel_multiplier=1, allow_small_or_imprecise_dtypes=True), w=[iotap])
        widx_f = kb.sb("widxf", [128, 9])
        widx = [kb.sb("widx", [128, 9], I32) for _ in range(3)]
        xs = [kb.sb("xs", [128, D]) for _ in range(2)]
        xT = kb.sb("xTm", [128, 8, SB], BF16)
        aT = kb.sb("aTm", [128, 8, SB], BF16)
        gsb = [kb.sb("gsb", [128, SB]) for _ in range(2)]
        sgb = [kb.sb("sgb", [128, SB]) for _ in range(2)]
        usb = [kb.sb("usb", [128, SB]) for _ in range(2)]
        ysb = [kb.sb("ysb", [128, D]) for _ in range(2)]
        be_l = kb.sb("be_l", [128, NBLK])
        op("dve", lambda e: e.tensor_scalar(out=be_l[:], in0=bef[:], scalar1=float(i * E), scalar2=None, op0=ALU.add), r=[bef], w=[be_l])

        def load_w(blk):
            wi = widx[blk % 3]
            op("dve", lambda e: e.tensor_scalar(out=widx_f[:, 8:9], in0=be_l[:, blk:blk + 1], scalar1=1.0, scalar2=None, op0=ALU.mult), r=[be_l], w=[widx_f])
            op("dve", lambda e: e.scalar_tensor_tensor(out=widx_f[:, 0:8], in0=be_l[:, blk:blk + 1].to_broadcast([128, 8]), scalar=1024.0, in1=iotap[:], op0=ALU.mult, op1=ALU.add),
               r=[be_l, iotap], w=[widx_f])
            op("dve", lambda e: e.tensor_copy(out=wi[:], in_=widx_f[:]), r=[widx_f], w=[wi])
            w1 = wg[blk % 2]; w2 = wd[blk % 2]; br = brow[blk % 2]
            for kc in range(8):
                dma("pool", None, None, r=[wi], w=[w1], fn=lambda e: e.indirect_dma_start(
                    out=w1[:, kc, :], out_offset=None, in_=wgu[:, :], in_offset=bass.IndirectOffsetOnAxis(ap=wi[:, kc:kc + 1], axis=0)))
            for kc in range(8):
                dma("pool", None, None, r=[wi], w=[w2], fn=lambda e: e.indirect_dma_start(
                    out=w2[:, kc, :], out_offset=None, in_=wdn[:, :], in_offset=bass.IndirectOffsetOnAxis(ap=wi[:, kc:kc + 1], axis=0)))
            dma("pool", None, None, r=[wi], w=[br], fn=lambda e: e.indirect_dma_start(
                out=br[:, 0:2 * FF], out_offset=None, in_=bgu[:, :], in_offset=bass.IndirectOffsetOnAxis(ap=wi[:, 8:9], axis=0)))
            dma("pool", None, None, r=[wi], w=[br], fn=lambda e: e.indirect_dma_start(
                out=br[:, 2 * FF:3 * FF], out_offset=None, in_=bdn[:, :], in_offset=bass.IndirectOffsetOnAxis(ap=wi[:, 8:9], axis=0)))

        load_w(0)
        nxs = 0
        for blk in range(NBLK):
            if blk + 1 < NBLK:
                load_w(blk + 1)
            w1 = wg[blk % 2]; w2 = wd[blk % 2]; br = brow[blk % 2]
            for j in range(SB // 128):
                x = xs[nxs % 2]; nxs += 1
                dma("sp", x[:], XS[blk * SB + j * 128: blk * SB + (j + 1) * 128, :], w=[x])
                for half in range(2):
                    p = nps()
                    for c4 in range(4):
                        c = half * 4 + c4
                        op("pe", lambda e: e.transpose(p[:, c4 * 128:(c4 + 1) * 128], x[:, c * 128:(c + 1) * 128], ident()), r=[x, cm], w=[p])
                    op("act", lambda e: e.activation(out=xT[:, half * 4:(half + 1) * 4, j * 128:(j + 1) * 128], in_=p[:, :].rearrange("p (c t) -> p c t", c=4), func=AF.Copy),
                       r=[p], w=[xT])
            for fc in range(8):
                g_ = gsb[fc % 2]; s_ = sgb[fc % 2]; u_ = usb[fc % 2]
                pg = nps()
                for kc in range(8):
                    op("pe", lambda e: e.matmul(pg[:, :], w1[:, kc, fc * 128:(fc + 1) * 128], xT[:, kc, :], start=(kc == 0), stop=False), r=[w1, xT], w=[pg])
                op("pe", lambda e: e.matmul(pg[:, :], br[0:1, fc * 128:(fc + 1) * 128], onesb[0:1, :], start=False, stop=True), r=[br, onesb], w=[pg])
                pu = nps()
                for kc in range(8):
                    op("pe", lambda e: e.matmul(pu[:, :], w1[:, kc, FF + fc * 128:FF + (fc + 1) * 128], xT[:, kc, :], start=(kc == 0), stop=False), r=[w1, xT], w=[pu])
                op("pe", lambda e: e.matmul(pu[:, :], br[0:1, FF + fc * 128:FF + (fc + 1) * 128], onesb[0:1, :], start=False, stop=True), r=[br, onesb], w=[pu])
                op("dve", lambda e: e.tensor_scalar(out=g_[:], in0=pg[:, :], scalar1=7.0, scalar2=None, op0=ALU.min), r=[pg], w=[g_])
                op("act", lambda e: e.activation(out=s_[:], in_=g_[:], func=AF.Sigmoid, scale=1.702), r=[g_], w=[s_])
                op("dve", lambda e: e.tensor_scalar(out=u_[:], in0=pu[:, :], scalar1=7.0, scalar2=-7.0, op0=ALU.min, op1=ALU.max), r=[pu], w=[u_])
                op("dve", lambda e: e.scalar_tensor_tensor(out=u_[:], in0=u_[:], scalar=1.0, in1=g_[:], op0=ALU.add, op1=ALU.mult), r=[u_, g_], w=[u_])
                op("pool", lambda e: e.tensor_tensor(out=aT[:, fc, :], in0=u_[:], in1=s_[:], op=ALU.mult), r=[u_, s_], w=[aT])
            for j in range(SB // 128):
                y = ysb[j % 2]
                for hf in range(2):
                    p = nps()
                    for fc in range(8):
                        op("pe", lambda e: e.matmul(p[:, :], aT[:, fc, j * 128:(j + 1) * 128], w2[:, fc, hf * 512:(hf + 1) * 512], start=(fc == 0), stop=False), r=[aT, w2], w=[p])
                    op("pe", lambda e: e.matmul(p[:, :], onesb[0:1, 0:128], br[0:1, 2 * FF + hf * 512:2 * FF + (hf + 1) * 512], start=False, stop=True), r=[br, onesb], w=[p])
                    op("act", lambda e: e.activation(out=y[:, hf * 512:(hf + 1) * 512], in_=p[:, :], func=AF.Copy), r=[p], w=[y])
                dma("sp", YS[blk * SB + j * 128: blk * SB + (j + 1) * 128, :], y[:], r=[y])
        kb.pop()
        kb.push()
        grow = {}
        for s in range(NS):
            g = kb.sb("g2row", [128, D])
            dma("sp", g[:], MODD[s, 5 * D:6 * D].partition_broadcast(128), w=[g])
            grow[s] = g
        yk = [kb.sb("yk", [128, D]) for _ in range(4)]
        accs = [kb.sb("acc", [128, D]) for _ in range(2)]
        xts = [kb.sb("xt2", [128, D]) for _ in range(2)]
        st = kb.sb("st2", [128, 2, 6])
        mv = kb.sb("mv2", [128, 4])
        for n, (b, ti) in enumerate(tiles):
            gi = b * NT + ti
            r0 = b * Tn + ti * 128
            s = NB if ti < C // 128 else b
            acc = accs[n % 2]; x = xts[n % 2]
            dma("sp", x[:], XA[r0:r0 + 128, :], w=[x])
            for k in range(4):
                y = yk[k]
                dma("pool", None, None, r=[slot4], w=[y], fn=lambda e: e.indirect_dma_start(
                    out=y[:], out_offset=None, in_=YS[:, :], in_offset=bass.IndirectOffsetOnAxis(ap=slot4[:, gi, k:k + 1], axis=0)))
                if k == 0:
                    op("dve", lambda e: e.tensor_scalar(out=acc[:], in0=y[:], scalar1=gate4[:, gi, 0:1], scalar2=None, op0=ALU.mult), r=[y, gate4], w=[acc])
                else:
                    op("dve", lambda e: e.scalar_tensor_tensor(out=acc[:], in0=y[:], scalar=gate4[:, gi, k:k + 1], in1=acc[:], op0=ALU.mult, op1=ALU.add), r=[y, gate4, acc], w=[acc])
            op("dve", lambda e: e.tensor_tensor(out=acc[:], in0=acc[:], in1=grow[s][:], op=ALU.mult), r=[acc, grow[s]], w=[acc])
            op("dve", lambda e: e.scalar_tensor_tensor(out=acc[:], in0=x[:], scalar=ALPHA, in1=acc[:], op0=ALU.mult, op1=ALU.add), r=[x, acc], w=[acc])
            layernorm(acc, 2, st, mv, None)
            if last:
                if ti >= C // 128:
                    ro = b * S + (ti - C // 128) * 128
                    dma("sp", out[ro:ro + 128, :], acc[:], r=[acc])
            else:
                dma("sp", XB[r0:r0 + 128, :], acc[:], r=[acc])
        kb.pop()
        kb.pop()

    onesrow_t = kb.sb("onesrow", [1, SB])
    onesrow = onesrow_t
    op("dve", lambda e: e.memset(onesrow_t[:], 1.0), w=[onesrow_t])

    def mixer_identity(i, b):
        kb.push()
        buf = [kb.sb("cp", [128, 8, 512]) for _ in range(2)]
        for gi, (t0, tl) in enumerate(groups()):
            bb = buf[gi % 2]
            dma("sp", bb[:, :, 0:tl], HT[:, t0:t0 + tl].rearrange("(c p) t -> p c t", p=128), w=[bb])
            dma("sp", OT[0:D, t0:t0 + tl].rearrange("(c p) t -> p c t", p=128), bb[:, :, 0:tl], r=[bb])
        kb.pop()
        return w_id[:, :], D, None

    def mixer_rg(i, b, j, need_ctx):
        kb.push()
        win = kb.sb("win", [128, 8, 2 * D_RNN])
        for q in range(6):
            dma("sp", win[:, :, q * 512:(q + 1) * 512], rg_w_in[j * D:(j + 1) * D, q * 512:(q + 1) * 512].rearrange("(c p) n -> p c n", p=128), w=[win])
        hTs = [kb.sb("hTr", [128, 8, 512]) for _ in range(2)]
        pos_ = [kb.sb("pout", [128, 4, 512]) for _ in range(2)]
        n = 0
        for gi, (t0, tl) in enumerate(groups()):
            h = hTs[gi % 2]
            dma("sp", h[:, :, 0:tl], HT[:, t0:t0 + tl].rearrange("(c p) t -> p c t", p=128), w=[h])
            for q in range(6):
                po = pos_[n % 2]; n += 1
                for jj in range(4):
                    jc = q * 4 + jj
                    p = nps()
                    for kc in range(8):
                        op("pe", lambda e: e.matmul(p[:, 0:tl], win[:, kc, jc * 128:(jc + 1) * 128], h[:, kc, 0:tl], start=(kc == 0), stop=(kc == 7)), r=[win, h], w=[p])
                    if jj % 2 == 0:
                        op("act", lambda e: e.activation(out=po[:, jj, 0:tl], in_=p[:, 0:tl], func=AF.Copy), r=[p], w=[po])
                    else:
                        op("dve", lambda e: e.tensor_copy(out=po[:, jj, 0:tl], in_=p[:, 0:tl]), r=[p], w=[po])
                dma("pool", PR[q * 512:(q + 1) * 512, t0:t0 + tl].rearrange("(c p) t -> p c t", p=128), po[:, :, 0:tl], r=[po])
        kb.pop()
        kb.push()
        prow = kb.sb("prow", [11, D_RNN])
        dma("sp", prow[:], rg_p[j * 11:(j + 1) * 11, :], w=[prow])
        chp = kb.sb("chp", [128, 12, 11])
        p = nps()
        for c in range(12):
            op("pe", lambda e: e.transpose(p[:, c * 11:(c + 1) * 11], prow[:, c * 128:(c + 1) * 128], ident(11)), r=[prow, cm], w=[p])
        op("dve", lambda e: e.tensor_copy(out=chp[:].rearrange("p c k -> p (c k)"), in_=p[:, 0:132]), r=[p], w=[chp])
        cdec = kb.sb("cdec", [128, 12, 4])
        op("act", lambda e: e.activation(out=cdec[:, :, 0:2], in_=chp[:, :, 9:11], func=AF.Exp, scale=-1.0), r=[chp], w=[cdec])
        op("act", lambda e: e.activation(out=cdec[:, :, 0:2], in_=cdec[:, :, 0:2], func=AF.Ln, bias=1.0, scale=1.0), r=[cdec], w=[cdec])
        op("dve", lambda e: e.tensor_scalar(out=cdec[:, :, 2:4], in0=cdec[:, :, 0:2], scalar1=-16.0, scalar2=None, op0=ALU.mult), r=[cdec], w=[cdec])
        op("dve", lambda e: e.tensor_scalar(out=cdec[:, :, 0:2], in0=cdec[:, :, 0:2], scalar1=-8.0, scalar2=None, op0=ALU.mult), r=[cdec], w=[cdec])
        xr = kb.sb("xr", [128, Tn])
        xc = kb.sb("xc", [128, 2, Tn])
        gl = kb.sb("gl", [128, Tn])
        aa = kb.sb("aa", [128, Tn])
        uu = kb.sb("uu", [128, Tn])
        hh = kb.sb("hh", [128, Tn])
        hs = kb.sb("hs", [128, Tn])
        gw = kb.sb("gw", [128, 2, 2, 2, 256])
        rr = [kb.sb("rr", [128, 512]) for _ in range(2)]
        ii = [kb.sb("ii", [128, 512]) for _ in range(2)]
        segs = [(0, C), (C, Tn)]
        for nb_ in range(6):
            for d in range(2):
                base = ((j * 2 + d) * 6 + nb_) * 256
                dma("sp", gw[:, d, 0, :, :], rg_ga[base:base + 256, :].rearrange("(c p) n -> p c n", p=128), w=[gw])
                dma("sp", gw[:, d, 1, :, :], rg_gx[base:base + 256, :].rearrange("(c p) n -> p c n", p=128), w=[gw])
            for cc_ in range(2):
                ch = nb_ * 2 + cc_
                dma("sp", xr[:], PR[D_RNN + ch * 128: D_RNN + (ch + 1) * 128, :], w=[xr])
                for (a0, b0) in segs:
                    op("dve", lambda e: e.tensor_scalar(out=xc[:, cc_, a0:b0], in0=xr[:, a0:b0], scalar1=chp[:, ch, 1:2], scalar2=chp[:, ch, 4:5], op0=ALU.mult, op1=ALU.add), r=[xr, chp], w=[xc])
                    op("dve", lambda e: e.scalar_tensor_tensor(out=xc[:, cc_, a0 + 1:b0], in0=xr[:, a0:b0 - 1], scalar=chp[:, ch, 0:1], in1=xc[:, cc_, a0 + 1:b0], op0=ALU.mult, op1=ALU.add), r=[xr, chp, xc], w=[xc])
                    op("dve", lambda e: e.scalar_tensor_tensor(out=xc[:, cc_, a0:b0 - 1], in0=xr[:, a0 + 1:b0], scalar=chp[:, ch, 2:3], in1=xc[:, cc_, a0:b0 - 1], op0=ALU.mult, op1=ALU.add), r=[xr, chp, xc], w=[xc])
                    op("dve", lambda e: e.scalar_tensor_tensor(out=xc[:, cc_, a0:b0 - 2], in0=xr[:, a0 + 2:b0], scalar=chp[:, ch, 3:4], in1=xc[:, cc_, a0:b0 - 2], op0=ALU.mult, op1=ALU.add), r=[xr, chp, xc], w=[xc])
            for oc in range(2):
                ch = nb_ * 2 + oc
                dma("sp", gl[:], PR[ch * 128:(ch + 1) * 128, :], w=[gl])
                for d in range(2):
                    for gi, (t0, tl) in enumerate(groups()):
                        r_ = rr[gi % 2]; i_ = ii[gi % 2]
                        pa = nps()
                        for kc in range(2):
                            op("pe", lambda e: e.matmul(pa[:, 0:tl], gw[:, d, 0, kc, oc * 128:(oc + 1) * 128], xc[:, kc, t0:t0 + tl], start=(kc == 0), stop=(kc == 1)), r=[gw, xc], w=[pa])
                        px = nps()
                        for kc in range(2):
                            op("pe", lambda e: e.matmul(px[:, 0:tl], gw[:, d, 1, kc, oc * 128:(oc + 1) * 128], xc[:, kc, t0:t0 + tl], start=(kc == 0), stop=(kc == 1)), r=[gw, xc], w=[px])
                        op("act", lambda e: e.activation(out=r_[:, 0:tl], in_=pa[:, 0:tl], func=AF.Sigmoid, bias=chp[:, ch, 5 + d:6 + d], scale=1.0), r=[pa, chp], w=[r_])
                        op("act", lambda e: e.activation(out=i_[:, 0:tl], in_=px[:, 0:tl], func=AF.Sigmoid, bias=chp[:, ch, 7 + d:8 + d], scale=1.0), r=[px, chp], w=[i_])
                        op("act", lambda e: e.activation(out=aa[:, t0:t0 + tl], in_=r_[:, 0:tl], func=AF.Exp, scale=cdec[:, ch, d:d + 1]), r=[r_, cdec], w=[aa])
                        op("act", lambda e: e.activation(out=r_[:, 0:tl], in_=r_[:, 0:tl], func=AF.Exp, scale=cdec[:, ch, 2 + d:3 + d]), r=[r_, cdec], w=[r_])
                        op("act", lambda e: e.activation(out=r_[:, 0:tl], in_=r_[:, 0:tl], func=AF.Sqrt, bias=1.0, scale=-1.0), r=[r_], w=[r_])
                        op("dve", lambda e: e.tensor_tensor(out=i_[:, 0:tl], in0=i_[:, 0:tl], in1=xc[:, oc, t0:t0 + tl], op=ALU.mult), r=[i_, xc], w=[i_])
                        op("dve", lambda e: e.tensor_tensor(out=uu[:, t0:t0 + tl], in0=i_[:, 0:tl], in1=r_[:, 0:tl], op=ALU.mult), r=[i_, r_], w=[uu])
                    if d == 0:
                        op("dve", lambda e: e.tensor_tensor_scan(out=hs[:, :], data0=aa[:, :], data1=uu[:, :], initial=0.0, op0=ALU.mult, op1=ALU.add), r=[aa, uu], w=[hs])
                    else:
                        op("dve", lambda e: e.tensor_tensor_scan(out=hh[:, 0:C][:, ::-1], data0=aa[:, 0:C][:, ::-1], data1=uu[:, 0:C][:, ::-1], initial=0.0, op0=ALU.mult, op1=ALU.add), r=[aa, uu], w=[hh])
                        op("dve", lambda e: e.tensor_tensor_scan(out=hh[:, C:Tn][:, ::-1], data0=aa[:, C:Tn][:, ::-1], data1=uu[:, C:Tn][:, ::-1], initial=hh[:, 0:1], op0=ALU.mult, op1=ALU.add), r=[aa, uu, hh], w=[hh])
                        op("pool", lambda e: e.tensor_tensor(out=hs[:], in0=hs[:], in1=hh[:], op=ALU.add), r=[hs, hh], w=[hs])
                op("pool", lambda e: e.tensor_tensor(out=hh[:], in0=gl[:], in1=gl[:], op=ALU.mult), r=[gl], w=[hh])
                op("dve", lambda e: e.tensor_scalar(out=hh[:], in0=hh[:], scalar1=0.044715, scalar2=1.0, op0=ALU.mult, op1=ALU.add), r=[hh], w=[hh])
                op("pool", lambda e: e.tensor_tensor(out=hh[:], in0=hh[:], in1=gl[:], op=ALU.mult), r=[hh, gl], w=[hh])
                op("act", lambda e: e.activation(out=hh[:], in_=hh[:], func=AF.Sigmoid, scale=1.5957691216057308), r=[hh], w=[hh])
                op("pool", lambda e: e.tensor_tensor(out=hh[:], in0=hh[:], in1=gl[:], op=ALU.mult), r=[hh, gl], w=[hh])
                op("dve", lambda e: e.tensor_tensor(out=hs[:], in0=hs[:], in1=hh[:], op=ALU.mult), r=[hs, hh], w=[hs])
                dma("pool", OT[ch * 128:(ch + 1) * 128, :], hs[:], r=[hs])
        kb.pop()
        return rg_w_out[j * D_RNN:(j + 1) * D_RNN, :], D_RNN, None

    def mixer_gqa(i, b, j, need_ctx):
        QT3 = QT.ap().rearrange("p (h t) -> p h t", h=16)
        KT3 = KT.ap().rearrange("p (h t) -> p h t", h=2)
        kb.push()
        w = kb.sb("gqw", [128, 8, 2432])
        for q in range(0, 2432, 608):
            dma("sp", w[:, :, q:q + 608], gq_w[j * D:(j + 1) * D, q:q + 608].rearrange("(c p) n -> p c n", p=128), w=[w])
        b64 = kb.sb("b64", [64, 40])
        dma("sp", b64[:], gq_b64[j * 64:(j + 1) * 64, :], w=[b64])
        bvrow = kb.sb("bvrow", [128, 128])
        dma("sp", bvrow[:], gq_bv[j, :].partition_broadcast(128), w=[bvrow])
        hTs = [kb.sb("hTg", [128, 8, 512]) for _ in range(2)]
        qo = kb.sb("qo", [64, 16, 512])
        ko = kb.sb("ko", [64, 2, 512])
        cs = kb.sb("cs", [64, 512]); sn = kb.sb("sn", [64, 512])
        ta = [kb.sb("ta", [64, 512]) for _ in range(2)]
        tb = [kb.sb("tb", [64, 512]) for _ in range(2)]
        va = [kb.sb("va", [128, 2, 65]) for _ in range(2)]
        for v_ in va:
            op("dve", lambda e: e.memset(v_[:], 1.0), w=[v_])
        n = 0
        for gi, (t0, tl) in enumerate(groups()):
            h = hTs[gi % 2]
            lat = t0 >= C
            dma("sp", h[:, :, 0:tl], HT[:, t0:t0 + tl].rearrange("(c p) t -> p c t", p=128), w=[h])
            if lat:
                dma("sp", cs[:, 0:tl], rope64[0:64, t0 - C:t0 - C + tl], w=[cs])
                dma("sp", sn[:, 0:tl], rope64[64:128, t0 - C:t0 - C + tl], w=[sn])
            for hq in range(18):
                col = hq * 64 if hq < 16 else 2048 + (hq - 16) * 64
                colsw = 1024 + hq * 64 if hq < 16 else 2176 + (hq - 16) * 64
                dst = qo[:, hq, 0:tl] if hq < 16 else ko[:, hq - 16, 0:tl]
                dstT = qo if hq < 16 else ko
                p = nps()
                for kc in range(8):
                    op("pe", lambda e: e.matmul(p[0:64, 0:tl], w[:, kc, col:col + 64], h[:, kc, 0:tl], start=(kc == 0), stop=(kc == 7)), r=[w, h], w=[p])
                if lat:
                    p2 = nps()
                    for kc in range(8):
                        op("pe", lambda e: e.matmul(p2[0:64, 0:tl], w[:, kc, colsw:colsw + 64], h[:, kc, 0:tl], start=(kc == 0), stop=(kc == 7)), r=[w, h], w=[p2])
                    a_ = ta[n % 2]; b_ = tb[n % 2]; n += 1
                    op("dve", lambda e: e.scalar_tensor_tensor(out=a_[:, 0:tl], in0=p[0:64, 0:tl], scalar=b64[:, hq:hq + 1], in1=cs[:, 0:tl], op0=ALU.add, op1=ALU.mult), r=[p, b64, cs], w=[a_])
                    op("dve", lambda e: e.scalar_tensor_tensor(out=b_[:, 0:tl], in0=p2[0:64, 0:tl], scalar=b64[:, 20 + hq:21 + hq], in1=sn[:, 0:tl], op0=ALU.add, op1=ALU.mult), r=[p2, b64, sn], w=[b_])
                    op("pool", lambda e: e.tensor_tensor(out=dst, in0=a_[:, 0:tl], in1=b_[:, 0:tl], op=ALU.add), r=[a_, b_], w=[dstT])
                else:
                    op("dve", lambda e: e.tensor_scalar(out=dst, in0=p[0:64, 0:tl], scalar1=b64[:, hq:hq + 1], scalar2=None, op0=ALU.add), r=[p, b64], w=[dstT])
            dma("pool", QT3[:, :, t0:t0 + tl], qo[:, :, 0:tl], r=[qo])
            dma("pool", KT3[:, :, t0:t0 + tl], ko[:, :, 0:tl], r=[ko])
            for tt in range(tl // 128):
                v_ = va[tt % 2]
                p = nps()
                for kc in range(8):
                    op("pe", lambda e: e.matmul(p[:, 0:128], h[:, kc, tt * 128:(tt + 1) * 128], w[:, kc, 2304:2432], start=(kc == 0), stop=(kc == 7)), r=[w, h], w=[p])
                op("dve", lambda e: e.tensor_tensor(out=v_[:, :, 0:64], in0=p[:, 0:128].rearrange("p (h d) -> p h d", h=2), in1=bvrow[:].rearrange("p (h d) -> p h d", h=2), op=ALU.add), r=[p, bvrow], w=[v_])
                dma("pool", VA[t0 + tt * 128:t0 + (tt + 1) * 128, :], v_[:].rearrange("p h d -> p (h d)"), r=[v_])
        kb.pop()
        kb.push()
        kT = kb.sb("kT", [64, 2, Tn])
        dma("sp", kT[:], KT3[:, :, :], w=[kT])
        V = kb.sb("Vg", [128, NT, 130])
        dma("sp", V[:], VA.ap().rearrange("(n p) c -> p n c", p=128), w=[V])
        esk = kb.sb("esk", [64, 16])
        dma("sp", esk[:], gq_sinks[j, :].partition_broadcast(64), w=[esk])
        op("act", lambda e: e.activation(out=esk[:], in_=esk[:], func=AF.Exp), r=[esk], w=[esk])
        mge = kb.sb("mge", [128, 128])
        mle = kb.sb("mle", [128, 128])
        op("dve", lambda e: e.tensor_scalar(out=mge[:], in0=tri, scalar1=-1.0, scalar2=1.0, op0=ALU.mult, op1=ALU.add), r=[cm], w=[mge])
        op("dve", lambda e: e.tensor_tensor(out=mle[:], in0=tri, in1=ident(), op=ALU.add), r=[cm], w=[mle])
        sel = kb.sb("sel", [65, 64])
        op("dve", lambda e: e.memset(sel[:], 0.0), w=[sel])
        op("dve", lambda e: e.memset(sel[64:65, :], 1.0), w=[sel])
        qs = [kb.sb("qs", [64, 16, 128]) for _ in range(2)]
        pTs = [kb.sb("pT", [128, 512]) for _ in range(4)]
        Xs = [kb.sb("Xo", [65, 512]) for _ in range(2)]
        rb = [kb.sb("rb", [64, 512]) for _ in range(2)]
        oo = [kb.sb("oo", [64, 512]) for _ in range(2)]
        nq = 0
        blocks = []
        if need_ctx:
            blocks += [(tq, [(0, None), (1, None)]) for tq in range(C // 128)]
        nlb = S // 128
        for jq in range(nlb):
            ch = [(0, None), (1, None)]
            if jq > 0:
                ch.append((C // 128 + jq - 1, mge))
            ch.append((C // 128 + jq, None))
            if jq < nlb - 1:
                ch.append((C // 128 + jq + 1, mle))
            blocks.append((C // 128 + jq, ch))
        npt = 0
        for (tq, chunks) in blocks:
            q = qs[nq % 2]; nq += 1
            dma("sp", q[:], QT3[:, :, tq * 128:(tq + 1) * 128], w=[q])
            for kvh in range(2):
                for half in range(2):
                    h0 = kvh * 8 + half * 4
                    po = PS[4 + (npt % 4)]
                    for ci, (kt, msk) in enumerate(chunks):
                        pT = pTs[npt % 4]; npt += 1
                        p = nps()
                        op("pe", lambda e: e.matmul(p[:, :], kT[:, kvh, kt * 128:(kt + 1) * 128], q[:, h0:h0 + 4, :].rearrange("p h t -> p (h t)"), start=True, stop=True), r=[kT, q], w=[p])
                        op("act", lambda e: e.activation(out=pT[:], in_=p[:, :], func=AF.Exp, scale=0.125), r=[p], w=[pT])
                        if msk is not None:
                            for hh in range(4):
                                op("pool" if hh % 2 else "dve", lambda e: e.tensor_tensor(out=pT[:, hh * 128:(hh + 1) * 128], in0=pT[:, hh * 128:(hh + 1) * 128], in1=msk[:], op=ALU.mult), r=[pT, msk], w=[pT])
                        op("pe", lambda e: e.matmul(po[0:65, :], V[:, kt, kvh * 65:(kvh + 1) * 65], pT[:], start=(ci == 0), stop=(ci == len(chunks) - 1)), r=[V, pT], w=[po])
                    X = Xs[npt % 2]; r_ = rb[npt % 2]; o_ = oo[npt % 2]
                    op("act", lambda e: e.activation(out=X[:], in_=po[0:65, :], func=AF.Copy), r=[po], w=[X])
                    p = nps()
                    op("pe", lambda e: e.matmul(p[0:64, :], sel[:], X[:], start=True, stop=True), r=[sel, X], w=[p])
                    for hh in range(4):
                        op("dve", lambda e: e.tensor_scalar(out=r_[:, hh * 128:(hh + 1) * 128], in0=p[0:64, hh * 128:(hh + 1) * 128], scalar1=esk[:, h0 + hh:h0 + hh + 1], scalar2=None, op0=ALU.add), r=[p, esk], w=[r_])
                    op("dve", lambda e: e.reciprocal(out=r_[:], in_=r_[:]), r=[r_], w=[r_])
                    op("dve", lambda e: e.tensor_tensor(out=o_[:], in0=X[0:64, :], in1=r_[:], op=ALU.mult), r=[X, r_], w=[o_])
                    dma("pool", OT[h0 * 64:(h0 + 4) * 64, tq * 128:(tq + 1) * 128].rearrange("(h p) t -> p h t", p=64), o_[:].rearrange("p (h t) -> p h t", h=4), r=[o_])
        kb.pop()
        return gq_w_o[j * D:(j + 1) * D, :], D, gq_b_o[j, :]

    def mixer_mla(i, b, j, need_ctx):
        RMS_EPS = 1e-6
        kb.push()
        wd = kb.sb("mlwd", [128, 8, 448])
        dma("sp", wd[:], ml_wd[j * D:(j + 1) * D, :].rearrange("(c p) n -> p c n", p=128), w=[wd])
        nrow = kb.sb("nrow", [128, 384])
        dma("sp", nrow[:], ml_norm[j, :].partition_broadcast(128), w=[nrow])
        epsr = kb.sb("epsr", [128, 1])
        op("dve", lambda e: e.memset(epsr[:], RMS_EPS), w=[epsr])
        hTs = [kb.sb("hTm", [128, 8, 512]) for _ in range(2)]
        cTs = [kb.sb("cTm", [128, 3, 512]) for _ in range(2)]
        krs = [kb.sb("krm", [32, 512]) for _ in range(2)]
        cs = kb.sb("csm", [32, 512]); sn = kb.sb("snm", [32, 512])
        t1 = kb.sb("t1m", [32, 512]); t2 = kb.sb("t2m", [32, 512])
        cqs = [kb.sb("cqm", [128, 384]) for _ in range(2)]
        junk = kb.sb("junk", [128, 256])
        ss = kb.sb("ssm", [128, 4])
        for gi, (t0, tl) in enumerate(groups()):
            h = hTs[gi % 2]; cT = cTs[gi % 2]; kr = krs[gi % 2]
            lat = t0 >= C
            dma("sp", h[:, :, 0:tl], HT[:, t0:t0 + tl].rearrange("(c p) t -> p c t", p=128), w=[h])
            for tt in range(tl // 128):
                cq = cqs[tt % 2]
                p = nps()
                for kc in range(8):
                    op("pe", lambda e: e.matmul(p[:, 0:384], h[:, kc, tt * 128:(tt + 1) * 128], wd[:, kc, 0:384], start=(kc == 0), stop=(kc == 7)), r=[h, wd], w=[p])
                op("act", lambda e: e.activation(out=junk[:, 0:256], in_=p[:, 0:256], func=AF.Square, accum_out=ss[:, 0:1]), r=[p], w=[junk, ss])
                op("act", lambda e: e.activation(out=junk[:, 0:128], in_=p[:, 256:384], func=AF.Square, accum_out=ss[:, 1:2]), r=[p], w=[junk, ss])
                op("act", lambda e: e.activation(out=ss[:, 2:3], in_=ss[:, 0:1], func=AF.Sqrt, bias=epsr[:, 0:1], scale=1.0 / 256), r=[ss, epsr], w=[ss])
                op("act", lambda e: e.activation(out=ss[:, 3:4], in_=ss[:, 1:2], func=AF.Sqrt, bias=epsr[:, 0:1], scale=1.0 / 128), r=[ss, epsr], w=[ss])
                op("dve", lambda e: e.reciprocal(out=ss[:, 2:4], in_=ss[:, 2:4]), r=[ss], w=[ss])
                op("dve", lambda e: e.scalar_tensor_tensor(out=cq[:, 0:256], in0=p[:, 0:256], scalar=ss[:, 2:3], in1=nrow[:, 0:256], op0=ALU.mult, op1=ALU.mult), r=[p, ss, nrow], w=[cq])
                op("dve", lambda e: e.scalar_tensor_tensor(out=cq[:, 256:384], in0=p[:, 256:384], scalar=ss[:, 3:4], in1=nrow[:, 256:384], op0=ALU.mult, op1=ALU.mult), r=[p, ss, nrow], w=[cq])
                p2 = nps()
                for c3 in range(3):
                    op("pe", lambda e: e.transpose(p2[:, c3 * 128:(c3 + 1) * 128], cq[:, c3 * 128:(c3 + 1) * 128], ident()), r=[cq, cm], w=[p2])
                op("act", lambda e: e.activation(out=cT[:, :, tt * 128:(tt + 1) * 128], in_=p2[:, 0:384].rearrange("p (c t) -> p c t", c=3), func=AF.Copy), r=[p2], w=[cT])
            dma("pool", CT[:, t0:t0 + tl].rearrange("(c p) t -> p c t", p=128), cT[:, :, 0:tl], r=[cT])
            p = nps()
            for kc in range(8):
                op("pe", lambda e: e.matmul(p[0:32, 0:tl], wd[:, kc, 384:416], h[:, kc, 0:tl], start=(kc == 0), stop=(kc == 7)), r=[h, wd], w=[p])
            if lat:
                p2 = nps()
                for kc in range(8):
                    op("pe", lambda e: e.matmul(p2[0:32, 0:tl], wd[:, kc, 416:448], h[:, kc, 0:tl], start=(kc == 0), stop=(kc == 7)), r=[h, wd], w=[p2])
                dma("sp", cs[:, 0:tl], rope32[0:32, t0 - C:t0 - C + tl], w=[cs])
                dma("sp", sn[:, 0:tl], rope32[32:64, t0 - C:t0 - C + tl], w=[sn])
                op("dve", lambda e: e.tensor_tensor(out=t1[:, 0:tl], in0=p[0:32, 0:tl], in1=cs[:, 0:tl], op=ALU.mult), r=[p, cs], w=[t1])
                op("dve", lambda e: e.tensor_tensor(out=t2[:, 0:tl], in0=p2[0:32, 0:tl], in1=sn[:, 0:tl], op=ALU.mult), r=[p2, sn], w=[t2])
                op("pool", lambda e: e.tensor_tensor(out=kr[:, 0:tl], in0=t1[:, 0:tl], in1=t2[:, 0:tl], op=ALU.add), r=[t1, t2], w=[kr])
            else:
                op("dve", lambda e: e.tensor_copy(out=kr[:, 0:tl], in_=p[0:32, 0:tl]), r=[p], w=[kr])
            dma("pool", KRT[:, t0:t0 + tl], kr[:, 0:tl], r=[kr])
        kb.pop()
        kb.push()
        cT = kb.sb("cTall", [128, 3, Tn])
        dma("sp", cT[:], CT.ap().rearrange("(c p) t -> p c t", p=128), w=[cT])
        krT = kb.sb("krT", [32, Tn], BF16)
        dma("pool", krT[:], KRT[:, :], w=[krT])
        csb = [kb.sb("csq", [32, 512]) for _ in range(2)]
        snb = [kb.sb("snq", [32, 512]) for _ in range(2)]
        sel = kb.sb("selm", [65, 64])
        op("dve", lambda e: e.memset(sel[:], 0.0), w=[sel])
        op("dve", lambda e: e.memset(sel[64:65, :], 1.0), w=[sel])
        qn = kb.sb("qn", [64, Tn], BF16); qr = kb.sb("qr", [32, Tn], BF16); kn = kb.sb("kn", [64, Tn], BF16)
        Vh = kb.sb("Vh", [128, NT, 65], BF16)
        op("dve", lambda e: e.memset(Vh[:], 1.0), w=[Vh])
        wq = kb.sb("wqh", [128, 2, 128]); wkv = kb.sb("wkvh", [128, 128])
        t1 = kb.sb("t1q", [32, 512]); t2 = kb.sb("t2q", [32, 512])
        pTs = [kb.sb("pTm", [128, 512], BF16) for _ in range(4)]
        Xs = [kb.sb("Xm", [65, 512]) for _ in range(2)]
        rb = [kb.sb("rbm", [64, 512]) for _ in range(2)]
        oo = [kb.sb("oom", [64, 512]) for _ in range(2)]
        scale = 96.0 ** -0.5
        npt = 0
        for hd in range(16):
            dma("sp", wq[:, :, 0:96], ml_wuq[j * 256:(j + 1) * 256, hd * 96:(hd + 1) * 96].rearrange("(c p) n -> p c n", p=128), w=[wq])
            dma("sp", wq[:, :, 96:128], ml_wuq[j * 256:(j + 1) * 256, 1536 + hd * 32:1536 + (hd + 1) * 32].rearrange("(c p) n -> p c n", p=128), w=[wq])
            dma("sp", wkv[:], ml_wukv[j * 128:(j + 1) * 128, hd * 128:(hd + 1) * 128], w=[wkv])
            for gi, (t0, tl) in enumerate(groups()):
                lat = t0 >= C
                p = nps()
                for kc in range(2):
                    op("pe", lambda e: e.matmul(p[0:64, 0:tl], wq[:, kc, 0:64], cT[:, kc, t0:t0 + tl], start=(kc == 0), stop=(kc == 1)), r=[wq, cT], w=[p])
                op("act", lambda e: e.activation(out=qn[:, t0:t0 + tl], in_=p[0:64, 0:tl], func=AF.Copy), r=[p], w=[qn])
                p = nps()
                for kc in range(2):
                    op("pe", lambda e: e.matmul(p[0:32, 0:tl], wq[:, kc, 64:96], cT[:, kc, t0:t0 + tl], start=(kc == 0), stop=(kc == 1)), r=[wq, cT], w=[p])
                if lat:
                    cs = csb[gi % 2]; sn = snb[gi % 2]
                    dma("sp", cs[:, 0:tl], rope32[0:32, t0 - C:t0 - C + tl], w=[cs])
                    dma("sp", sn[:, 0:tl], rope32[32:64, t0 - C:t0 - C + tl], w=[sn])
                    p2 = nps()
                    for kc in range(2):
                        op("pe", lambda e: e.matmul(p2[0:32, 0:tl], wq[:, kc, 96:128], cT[:, kc, t0:t0 + tl], start=(kc == 0), stop=(kc == 1)), r=[wq, cT], w=[p2])
                    op("dve", lambda e: e.tensor_tensor(out=t1[:, 0:tl], in0=p[0:32, 0:tl], in1=cs[:, 0:tl], op=ALU.mult), r=[p, cs], w=[t1])
                    op("dve", lambda e: e.tensor_tensor(out=t2[:, 0:tl], in0=p2[0:32, 0:tl], in1=sn[:, 0:tl], op=ALU.mult), r=[p2, sn], w=[t2])
                    op("pool", lambda e: e.tensor_tensor(out=qr[:, t0:t0 + tl], in0=t1[:, 0:tl], in1=t2[:, 0:tl], op=ALU.add), r=[t1, t2], w=[qr])
                else:
                    op("dve", lambda e: e.tensor_copy(out=qr[:, t0:t0 + tl], in_=p[0:32, 0:tl]), r=[p], w=[qr])
                p = nps()
                op("pe", lambda e: e.matmul(p[0:64, 0:tl], wkv[:, 0:64], cT[:, 2, t0:t0 + tl], start=True, stop=True), r=[wkv, cT], w=[p])
                op("act", lambda e: e.activation(out=kn[:, t0:t0 + tl], in_=p[0:64, 0:tl], func=AF.Copy), r=[p], w=[kn])
                p = nps()
                ntl = tl // 128
                for tt in range(ntl):
                    op("pe", lambda e: e.matmul(p[:, tt * 64:(tt + 1) * 64], cT[:, 2, t0 + tt * 128:t0 + (tt + 1) * 128], wkv[:, 64:128], start=True, stop=True), r=[wkv, cT], w=[p])
                op("dve", lambda e: e.tensor_copy(out=Vh[:, t0 // 128:t0 // 128 + ntl, 0:64], in_=p[:, 0:ntl * 64].rearrange("p (n d) -> p n d", d=64)), r=[p], w=[Vh])
            qgroups = [g for g in groups() if g[0] >= C]
            if need_ctx:
                qgroups = [(0, C)] + qgroups
            for (t0, tl) in qgroups:
                kts = list(range(C // 128)) if t0 < C else list(range(NT))
                po = PS[4 + (npt % 4)]
                for ci, kt in enumerate(kts):
                    pT = pTs[npt % 4]; npt += 1
                    p = nps()
                    op("pe", lambda e: e.matmul(p[:, 0:tl], kn[:, kt * 128:(kt + 1) * 128], qn[:, t0:t0 + tl], start=True, stop=False), r=[kn, qn], w=[p])
                    op("pe", lambda e: e.matmul(p[:, 0:tl], krT[:, kt * 128:(kt + 1) * 128], qr[:, t0:t0 + tl], start=False, stop=True), r=[krT, qr], w=[p])
                    op("act", lambda e: e.activation(out=pT[:, 0:tl], in_=p[:, 0:tl], func=AF.Exp, scale=scale), r=[p], w=[pT])
                    op("pe", lambda e: e.matmul(po[0:65, 0:tl], Vh[:, kt, :], pT[:, 0:tl], start=(ci == 0), stop=(ci == len(kts) - 1)), r=[Vh, pT], w=[po])
                X = Xs[npt % 2]; r_ = rb[npt % 2]; o_ = oo[npt % 2]
                op("act", lambda e: e.activation(out=X[:, 0:tl], in_=po[0:65, 0:tl], func=AF.Copy), r=[po], w=[X])
                p = nps()
                op("pe", lambda e: e.matmul(p[0:64, 0:tl], sel[:], X[:, 0:tl], start=True, stop=True), r=[sel, X], w=[p])
                op("dve", lambda e: e.reciprocal(out=r_[:, 0:tl], in_=p[0:64, 0:tl]), r=[p], w=[r_])
                op("dve", lambda e: e.tensor_tensor(out=o_[:, 0:tl], in0=X[0:64, 0:tl], in1=r_[:, 0:tl], op=ALU.mult), r=[X, r_], w=[o_])
                dma("pool", OT[hd * 64:(hd + 1) * 64, t0:t0 + tl], o_[:, 0:tl], r=[o_])
        kb.pop()
        return ml_w_o[j * D:(j + 1) * D, :], D, None

    Xsrc = xin
    kcount = {0: 0, 1: 0, 2: 0, -1: 0}
    for i, kind in enumerate(kinds):
        last = (i == L - 1)
        need_ctx = not (last and cfg.last_noctx)
        phase_mod(i)
        for b in range(NB):
            phase_h(i, b, Xsrc)
            if kind == -1:
                w_o, Dm, b_o = mixer_identity(i, b)
            elif kind == 0:
                w_o, Dm, b_o = mixer_rg(i, b, kcount[0], need_ctx)
            elif kind == 2:
                psn[0] = 4
                w_o, Dm, b_o = mixer_mla(i, b, kcount[2], need_ctx)
                psn[0] = 8
            elif kind == 1:
                psn[0] = 4
                w_o, Dm, b_o = mixer_gqa(i, b, kcount[1], need_ctx)
                psn[0] = 8
            else:
                raise NotImplementedError
            phase_out(i, b, Xsrc, w_o, Dm, b_o, need_ctx)
        phase_moe(i, last)
        kcount[kind] += 1
        Xsrc = XB
    kb.close()
    return kb


def consts():
    cm = np.zeros((128, 416), np.float32)
    cm[:, 0:128] = np.eye(128, dtype=np.float32)
    k = np.arange(128)
    cm[:, 128:256] = (k[:, None] < k[None, :]).astype(np.float32)
    cm[:, 256:384] = 1.0
    cm[:, 384:416] = np.arange(32, dtype=np.float32)[None, :]
    return cm


def rope_table(S, rot_dim):
    rows = S // 64
    row = np.repeat(np.arange(rows, dtype=np.float32), 64)
    col = np.tile(np.arange(64, dtype=np.float32), rows)
    n_freq = rot_dim // 4
    inv = (np.float32(10000.0) ** (-np.arange(n_freq, dtype=np.float32) / np.float32(n_freq))).astype(np.float32)
    ang = np.concatenate([row[:, None] * inv, col[:, None] * inv], axis=-1).astype(np.float32)
    cos = np.cos(ang).astype(np.float32).T
    sin = np.sin(ang).astype(np.float32).T
    return np.ascontiguousarray(np.concatenate([cos, cos, -sin, sin], axis=0))


def prep_inputs(cfg, inp):
    NB, S, kinds = cfg.NB, cfg.S, cfg.kinds
    L = len(kinds)
    f = lambda a: np.ascontiguousarray(np.asarray(a, dtype=np.float32))
    shared = {
        "ada_w": f(inp["ada_w"]).reshape(L * D, 6 * D),
        "ada_b": f(inp["ada_b"]),
        "lnp": f(np.stack([inp["ln1_g"], inp["ln1_b"], inp["ln2_g"], inp["ln2_b"]], axis=1)).reshape(L * 4, D),
        "router_w": f(inp["router_w"]).reshape(L * D, E),
        "router_b": f(inp["router_b"]),
        "wgu": f(np.concatenate([inp["exp_gu_w"][..., 0::2], inp["exp_gu_w"][..., 1::2]], axis=-1)).reshape(L * E * D, 2 * FF),
        "bgu": f(np.concatenate([inp["exp_gu_b"][..., 0::2], inp["exp_gu_b"][..., 1::2]], axis=-1)).reshape(L * E, 2 * FF),
        "wdn": f(inp["exp_down_w"]).reshape(L * E * FF, D),
        "bdn": f(inp["exp_down_b"]).reshape(L * E, D),
        "cmat": consts(),
    }
    if -1 in kinds:
        shared["w_id"] = np.eye(D, dtype=np.float32)
    if 2 in kinds:
        n = kinds.count(2)
        Wd = inp["mla_w_down"][:n]
        def sw16(a):
            sh = a.shape
            return a.reshape(sh[:-1] + (sh[-1] // 32, 2, 16))[..., ::-1, :].reshape(sh)
        shared["ml_wd"] = f(np.concatenate([Wd, sw16(Wd[..., 384:416])], axis=-1)).reshape(n * D, 448)
        shared["ml_norm"] = f(np.concatenate([inp["mla_q_norm"][:n], inp["mla_kv_norm"][:n]], axis=-1))
        Wq = inp["mla_w_uq"][:n].reshape(n, 256, 16, 96)
        shared["ml_wuq"] = f(np.concatenate([Wq.reshape(n, 256, 1536), sw16(np.ascontiguousarray(Wq[..., 64:96])).reshape(n, 256, 512)], axis=-1)).reshape(n * 256, 2048)
        shared["ml_wukv"] = f(inp["mla_w_ukv"][:n]).reshape(n * 128, 2048)
        shared["ml_w_o"] = f(inp["mla_w_o"][:n]).reshape(n * D, D)
        shared["rope32"] = rope_table(S, 32)
    if 1 in kinds:
        n = kinds.count(1)
        W = inp["gqa_w_qkv"][:n]
        Bq = inp["gqa_b_qkv"][:n]
        def sw(a):
            sh = a.shape
            return a.reshape(sh[:-1] + (sh[-1] // 64, 2, 32))[..., ::-1, :].reshape(sh)
        shared["gq_w"] = f(np.concatenate([W[..., 0:1024], sw(W[..., 0:1024]), W[..., 1024:1152], sw(W[..., 1024:1152]), W[..., 1152:1280]], axis=-1)).reshape(n * D, 2432)
        b20 = Bq.reshape(n, 20, 64)
        shared["gq_b64"] = f(np.concatenate([b20.transpose(0, 2, 1), sw(Bq).reshape(n, 20, 64).transpose(0, 2, 1)], axis=-1)).reshape(n * 64, 40)
        shared["gq_bv"] = f(Bq[:, 1152:1280])
        shared["gq_sinks"] = f(inp["gqa_sinks"][:n])
        shared["gq_w_o"] = f(inp["gqa_w_o"][:n]).reshape(n * D, D)
        shared["gq_b_o"] = f(inp["gqa_b_o"][:n])
        shared["rope64"] = rope_table(S, 64)
    if 0 in kinds:
        n_rg = kinds.count(0)
        shared["rg_w_in"] = f(inp["rg_w_in"][:n_rg]).reshape(n_rg * D, 2 * D_RNN)
        shared["rg_p"] = f(np.concatenate([inp["rg_conv_w"][:n_rg], inp["rg_conv_b"][:n_rg, None, :], inp["rg_gate_a_b"][:n_rg], inp["rg_gate_x_b"][:n_rg],
                                           inp["rg_lambda"][:n_rg]], axis=1)).reshape(n_rg * 11, D_RNN)
        shared["rg_ga"] = f(inp["rg_gate_a_w"][:n_rg]).reshape(n_rg * 2 * 6 * 256, 256)
        shared["rg_gx"] = f(inp["rg_gate_x_w"][:n_rg]).reshape(n_rg * 2 * 6 * 256, 256)
        shared["rg_w_out"] = f(inp["rg_w_out"][:n_rg]).reshape(n_rg * D_RNN, D)
    maps = []
    for c in range(cfg.NC):
        bs = slice(c * NB, (c + 1) * NB)
        m = dict(shared)
        m["xin"] = f(np.concatenate([inp["ctx"][bs], inp["x"][bs]], axis=1)).reshape(NB * (C + S), D)
        m["cc"] = f(np.concatenate([inp["c"][bs], inp["c_ctx"][None, :]], axis=0))
        maps.append(m)
    return maps


def run(cfg, inp):
    kb = build(cfg)
    maps = prep_inputs(cfg, inp)
    res = run_bass_kernel_spmd(kb.nc, maps, core_ids=list(range(cfg.NC)))
    outs = [res.results[c]["out"].reshape(cfg.NB, cfg.S, D) for c in range(cfg.NC)]
    return np.concatenate(outs, axis=0)


def kernel(**inputs):
    cfg = Cfg(NB=2, S=4096, kinds=(0, 1, 2, 0), NC=8)
    return run(cfg, inputs).astype(np.float32)
```

```python
import numpy as np
from contextlib import ExitStack
import concourse.bass as bass
import concourse.mybir as mybir
from concourse.bass_utils import run_bass_kernel_spmd

F32 = mybir.dt.float32
I32 = mybir.dt.int32
U32 = mybir.dt.uint32
BF16 = mybir.dt.bfloat16
AF = mybir.ActivationFunctionType
ALU = mybir.AluOpType
AX = mybir.AxisListType

D = 1024
C = 256
E = 32
FF = 1024
D_RNN = 1536
SB = 512
ALPHA = 8.0 ** 0.25
LN_EPS = 1e-5


class Res:
    __slots__ = ("w", "r")

    def __init__(self):
        self.w = None
        self.r = {}


class T:
    def __init__(self, h):
        self.h = h
        self.res = [Res()]

    def __getitem__(self, k):
        return self.h[k]


class KB:
    def __init__(self, n_dma_sems=48):
        self.nc = bass.Bass("TRN2", target_bir_lowering=False)
        nc = self.nc
        self.es = ExitStack()
        self.stacks = []
        self.eng = {"pe": nc.tensor, "act": nc.scalar, "dve": nc.vector, "pool": nc.gpsimd, "sp": nc.sync}
        self.sems = []
        self.esem = {}
        self.cnt = {}
        self.waited = {e: {} for e in self.eng}
        for e in self.eng:
            self.esem[e] = self._newsem("s_" + e)
            self.cnt[e] = 0
        self.dsem = [self._newsem("d%d" % i) for i in range(n_dma_sems)]
        self.duses = [0] * n_dma_sems
        self.drr = 0
        self.ninst = 0
        self.uid = 0
        self.bgsem = [self._newsem("bg%d" % i) for i in range(8)]
        self.bguses = [0] * 8
        self.bgrr = 0

    def _newsem(self, name):
        h = self.es.enter_context(self.nc.semaphore(name))
        self.sems.append(h)
        return len(self.sems) - 1

    def push(self):
        self.stacks.append(ExitStack())

    def pop(self):
        self.barrier()
        self.stacks.pop().close()

    def _ctx(self):
        return self.stacks[-1] if self.stacks else self.es

    def sb(self, name, shape, dtype=F32):
        self.uid += 1
        return T(self._ctx().enter_context(self.nc.sbuf_tensor("%s_%d" % (name, self.uid), list(shape), dtype)))

    def ps(self, name, shape, dtype=F32):
        return T(self.es.enter_context(self.nc.psum_tensor(name, list(shape), dtype)))

    def dram(self, name, shape, dtype=F32, kind="Internal"):
        return self.nc.dram_tensor(name, list(shape), dtype, kind=kind)

    def _wait(self, e, deps):
        eng = self.eng[e]
        best = {}
        for d in deps:
            if d is None:
                continue
            s, v, src = d
            if src == e and e == "pe":
                continue
            if self.waited[e].get(s, 0) >= v:
                continue
            if best.get(s, 0) < v:
                best[s] = v
        for s, v in best.items():
            eng.wait_ge(self.sems[s], v)
            self.waited[e][s] = v

    def _collect(self, r, w):
        deps = []
        for x in r:
            for rs in x.res:
                deps.append(rs.w)
        for x in w:
            for rs in x.res:
                deps.append(rs.w)
                deps.extend(rs.r.values())
        return deps

    def _commit(self, r, w, dep, key):
        for x in r:
            for rs in x.res:
                rs.r[key] = dep
        for x in w:
            for rs in x.res:
                rs.w = dep
                rs.r = {}

    def op(self, e, fn, r=(), w=()):
        self._wait(e, self._collect(r, w))
        inst = fn(self.eng[e])
        self.cnt[e] += 1
        inst.then_inc(self.sems[self.esem[e]], 1)
        dep = (self.esem[e], self.cnt[e], e)
        self._commit(r, w, dep, e)
        self.ninst += 1
        return inst

    def dma(self, q, out, in_, r=(), w=(), fn=None):
        i = self.drr
        self.drr = (i + 1) % len(self.dsem)
        s = self.dsem[i]
        deps = self._collect(r, w)
        deps.append((s, self.duses[i] * 16, "dma"))
        self._wait(q, deps)
        eng = self.eng[q]
        if fn is None:
            inst = eng.dma_start(out=out, in_=in_)
        else:
            inst = fn(eng)
        inst.then_inc(self.sems[s], 16)
        self.duses[i] += 1
        dep = (s, self.duses[i] * 16, "dma")
        self._commit(r, w, dep, ("dma", s))
        self.ninst += 1
        return inst

    def dma_bg(self, q, out, in_):
        i = self.bgrr
        self.bgrr = (i + 1) % len(self.bgsem)
        s = self.bgsem[i]
        self._wait(q, [(s, self.bguses[i] * 16, "dma")])
        self.eng[q].dma_start(out=out, in_=in_).then_inc(self.sems[s], 16)
        self.bguses[i] += 1
        self.ninst += 1

    def wait_bg(self):
        deps = [(s, self.bguses[i] * 16, "dma") for i, s in enumerate(self.bgsem)]
        for e in self.eng:
            self._wait(e, deps)

    def barrier(self):
        deps = [(self.esem[e], self.cnt[e], "x") for e in self.eng]
        deps += [(s, self.duses[i] * 16, "x") for i, s in enumerate(self.dsem)]
        for e in self.eng:
            self._wait(e, [d for d in deps if d[0] != self.esem[e]])

    def close(self):
        self.barrier()
        self.es.close()


class Cfg:
    def __init__(self, NB=2, S=4096, kinds=(0, 1, 2, 0), NC=8, last_noctx=True, debug=False):
        self.debug = debug
        self.NB = NB
        self.S = S
        self.kinds = list(kinds)
        self.NC = NC
        self.last_noctx = last_noctx


def build(cfg):
    NB, S, kinds = cfg.NB, cfg.S, cfg.kinds
    L = len(kinds)
    Tn = C + S
    NT = Tn // 128
    NS = NB + 1
    kb = KB()
    nc = kb.nc
    op, dma = kb.op, kb.dma

    def din(name, shape, dt=F32):
        return kb.dram(name, shape, dt, kind="ExternalInput")

    xin = din("xin", [NB * Tn, D])
    cc = din("cc", [NS, D])
    ada_w = din("ada_w", [L * D, 6 * D])
    ada_b = din("ada_b", [L, 6 * D])
    lnp = din("lnp", [L * 4, D])
    router_w = din("router_w", [L * D, E])
    router_b = din("router_b", [L, E])
    wgu = din("wgu", [L * E * D, 2 * FF])
    bgu = din("bgu", [L * E, 2 * FF])
    wdn = din("wdn", [L * E * FF, D])
    bdn = din("bdn", [L * E, D])
    cmat = din("cmat", [128, 384 + 32])
    w_id = din("w_id", [D, D]) if -1 in kinds else None
    n_rg = max(1, kinds.count(0))
    if 2 in kinds:
        n_ml = kinds.count(2)
        ml_wd = din("ml_wd", [n_ml * D, 448])
        ml_norm = din("ml_norm", [n_ml, 384])
        ml_wuq = din("ml_wuq", [n_ml * 256, 2048])
        ml_wukv = din("ml_wukv", [n_ml * 128, 2048])
        ml_w_o = din("ml_w_o", [n_ml * D, D])
        rope32 = din("rope32", [64, S])
        CT = kb.dram("CT", [384, Tn])
        KRT = kb.dram("KRT", [32, Tn])
    if 1 in kinds:
        n_gq = kinds.count(1)
        gq_w = din("gq_w", [n_gq * D, 2432])
        gq_b64 = din("gq_b64", [n_gq * 64, 40])
        gq_bv = din("gq_bv", [n_gq, 128])
        gq_sinks = din("gq_sinks", [n_gq, 16])
        gq_w_o = din("gq_w_o", [n_gq * D, D])
        gq_b_o = din("gq_b_o", [n_gq, D])
        rope64 = din("rope64", [2 * 64, S])
        QT = kb.dram("QT", [64, 16 * Tn])
        KT = kb.dram("KT", [64, 2 * Tn])
        VA = kb.dram("VA", [Tn, 130])
    if 0 in kinds:
        rg_w_in = din("rg_w_in", [n_rg * D, 2 * D_RNN])
        rg_p = din("rg_p", [n_rg * 11, D_RNN])
        rg_ga = din("rg_ga", [n_rg * 2 * 6 * 256, 256])
        rg_gx = din("rg_gx", [n_rg * 2 * 6 * 256, 256])
        rg_w_out = din("rg_w_out", [n_rg * D_RNN, D])
        PR = kb.dram("PR", [2 * D_RNN, Tn])
    out = kb.dram("out", [NB * S, D], F32, kind="ExternalOutput")

    dk = "ExternalOutput" if cfg.debug else "Internal"
    XA = kb.dram("XA", [NB * Tn, D], F32, dk)
    XB = kb.dram("XB", [NB * Tn, D], F32, dk)
    H2 = kb.dram("H2", [NB * Tn, D], F32, dk)
    HT = kb.dram("HT", [D, Tn], BF16, dk)
    OT = kb.dram("OT", [D_RNN, Tn], BF16, dk)
    MODD = kb.dram("MODD", [NS, 6 * D], F32, dk)
    n_asg_max = NB * Tn * 4
    NBLK = (n_asg_max + E * (SB - 1) + SB - 1) // SB
    NSLOT = NBLK * SB
    WGU16 = kb.dram("WGU16", [E * D, 2 * FF], BF16)
    WDN16 = kb.dram("WDN16", [E * FF, D], BF16)
    BGU16 = kb.dram("BGU16", [E, 2 * FF], BF16)
    BDN16 = kb.dram("BDN16", [E, D], BF16)
    XS = kb.dram("XS", [NSLOT, D], F32, dk)
    YS = kb.dram("YS", [NSLOT, D], F32, dk)
    if cfg.debug:
        DBG = {n: kb.dram("dbg_" + n, shp, dt, "ExternalOutput") for n, shp, dt in [
            ("slot4", [128, NB * NT * 4], I32), ("gate4", [128, NB * NT * 4], F32), ("top8", [128, NB * NT * 8], F32),
            ("bef", [128, NBLK], F32), ("runcnt", [128, E], F32), ("excl", [128, E], F32), ("padf", [128, E], F32), ("incl", [128, E], F32), ("posA", [128, NB * NT * E], F32)]}

    cm = kb.sb("cm", [128, 416])
    ident = lambda n=128: cm[0:n, 0:n]
    tri = cm[:, 128:256]
    ones = cm[:, 256:384]
    iota32 = cm[:, 384:416]
    dma("sp", cm[:], cmat[:, :], w=[cm])
    PS = [kb.ps("ps%d" % i, [128, 512]) for i in range(8)]
    psi = [0]
    psn = [8]

    def nps():
        psi[0] = (psi[0] + 1) % psn[0]
        return PS[psi[0]]

    siluT = kb.sb("siluT", [128, 8, NS])
    modT = kb.sb("modT", [128, 48, NS])
    lnrow = kb.sb("lnrow", [128, 4, D])
    epsT = kb.sb("epsT", [128, 1])
    op("dve", lambda e: e.memset(epsT[:], LN_EPS), w=[epsT])

    kb.push()
    ccs = kb.sb("ccs", [NS, D])
    dma("sp", ccs[:], cc[:, :], w=[ccs])
    op("act", lambda e: e.activation(out=ccs[:], in_=ccs[:], func=AF.Silu), r=[ccs], w=[ccs])
    p = nps()
    for c in range(8):
        op("pe", lambda e: e.transpose(p[:, c * NS:(c + 1) * NS], ccs[:, c * 128:(c + 1) * 128], ident(NS)), r=[ccs, cm], w=[p])
    op("dve", lambda e: e.tensor_copy(out=siluT[:].rearrange("p c s -> p (c s)"), in_=p[:, 0:8 * NS]), r=[p], w=[siluT])
    kb.pop()

    def phase_mod(i):
        kb.push()
        aw = [kb.sb("aw", [128, 8, 512]) for _ in range(2)]
        modtm = kb.sb("modtm", [NS, 6 * D])
        adab = kb.sb("adab", [NS, 6 * D])
        for s in range(NS):
            dma("sp", adab[s:s + 1, :], ada_b[i:i + 1, :], w=[adab])
        for g in range(12):
            a = aw[g % 2]
            dma("sp", a[:], ada_w[i * D:(i + 1) * D, g * 512:(g + 1) * 512].rearrange("(c p) n -> p c n", p=128), w=[a])
            p = nps()
            for kc in range(8):
                op("pe", lambda e: e.matmul(p[0:NS, :], siluT[:, kc, :], a[:, kc, :], start=(kc == 0), stop=(kc == 7)),
                   r=[siluT, a], w=[p])
            op("dve", lambda e: e.tensor_tensor(out=modtm[:, g * 512:(g + 1) * 512], in0=p[0:NS, :], in1=adab[:, g * 512:(g + 1) * 512], op=ALU.add),
               r=[p, adab], w=[modtm])
        for k in (1, 4):
            op("dve", lambda e: e.tensor_scalar(out=modtm[:, k * D:(k + 1) * D], in0=modtm[:, k * D:(k + 1) * D], scalar1=1.0, scalar2=None, op0=ALU.add),
               r=[modtm], w=[modtm])
        dma("sp", MODD[:, :], modtm[:], r=[modtm])
        p = nps()
        for j in range(48):
            op("pe", lambda e: e.transpose(p[:, j * NS:(j + 1) * NS], modtm[:, j * 128:(j + 1) * 128], ident(NS)), r=[modtm, cm], w=[p])
        op("dve", lambda e: e.tensor_copy(out=modT[:].rearrange("p c s -> p (c s)"), in_=p[:, 0:48 * NS]), r=[p], w=[modT])
        for k in range(4):
            dma("sp", lnrow[:, k, :], lnp[i * 4 + k, :].partition_broadcast(128), w=[lnrow])
        kb.pop()

    def groups():
        gs = [(0, C)]
        t = C
        while t < Tn:
            gs.append((t, min(512, Tn - t)))
            t += 512
        return gs

    def phase_h(i, b, Xsrc):
        kb.push()
        xt = [kb.sb("xt", [128, D]) for _ in range(8)]
        hT = [kb.sb("hT", [128, 8, 512], BF16) for _ in range(2)]
        n = 0
        for gi, (t0, tl) in enumerate(groups()):
            s = NB if t0 < C else b
            nt = tl // 128
            tiles = []
            for j in range(nt):
                x = xt[n % 8]
                n += 1
                dma("sp", x[:], Xsrc[b * Tn + t0 + j * 128: b * Tn + t0 + (j + 1) * 128, :], w=[x])
                tiles.append(x)
            h = hT[gi % 2]
            for c in range(8):
                p = nps()
                for j in range(nt):
                    x = tiles[j]
                    op("pe", lambda e: e.transpose(p[:, j * 128:(j + 1) * 128], x[:, c * 128:(c + 1) * 128], ident()), r=[x, cm], w=[p])
                op("dve", lambda e: e.tensor_scalar(out=h[:, c, 0:tl], in0=p[:, 0:tl], scalar1=modT[:, 8 + c, s:s + 1], scalar2=modT[:, c, s:s + 1],
                                                    op0=ALU.mult, op1=ALU.add), r=[p, modT], w=[h])
            dma("pool", HT[:, t0:t0 + tl].rearrange("(c p) t -> p c t", p=128), h[:, :, 0:tl], r=[h])
        kb.pop()

    def layernorm(t, k0, st, mv, tmp):
        for h2 in range(2):
            op("dve", lambda e: e.bn_stats(out=st[:, h2, :], in_=t[:, h2 * 512:(h2 + 1) * 512]), r=[t], w=[st])
        op("dve", lambda e: e.bn_aggr(out=mv[:, 0:2], in_=st[:].rearrange("p a b -> p (a b)")), r=[st], w=[mv])
        op("act", lambda e: e.activation(out=mv[:, 2:3], in_=mv[:, 1:2], func=AF.Sqrt, bias=epsT[:, 0:1], scale=1.0), r=[mv, epsT], w=[mv])
        op("dve", lambda e: e.reciprocal(out=mv[:, 3:4], in_=mv[:, 2:3]), r=[mv], w=[mv])
        op("dve", lambda e: e.tensor_scalar(out=t[:], in0=t[:], scalar1=mv[:, 0:1], scalar2=mv[:, 3:4], op0=ALU.subtract, op1=ALU.mult), r=[t, mv], w=[t])
        op("dve", lambda e: e.tensor_tensor(out=t[:], in0=t[:], in1=lnrow[:, k0, :], op=ALU.mult), r=[t, lnrow], w=[t])
        op("dve", lambda e: e.tensor_tensor(out=t[:], in0=t[:], in1=lnrow[:, k0 + 1, :], op=ALU.add), r=[t, lnrow], w=[t])

    NTT = NB * NT
    maskA = kb.sb("maskA", [128, NTT, E])
    top8 = kb.sb("top8", [128, NTT, 8])
    idx8 = kb.sb("idx8", [128, NTT, 8], U32)
    posA = kb.sb("posA", [128, NTT, E])
    runcnt = kb.sb("runcnt", [128, E])
    slot4f = kb.sb("slot4f", [128, NTT, 4])
    slot4 = kb.sb("slot4", [128, NTT, 4], I32)
    gate4 = kb.sb("gate4", [128, NTT, 4])

    def phase_out(i, b, Xsrc, w_o, Dm, b_o, need_ctx):
        kb.push()
        KC = Dm // 128
        wo = kb.sb("wo", [128, KC, D], BF16)
        for kc_ in range(KC):
            dma("pool", wo[:, kc_, :], w_o[kc_ * 128:(kc_ + 1) * 128, :], w=[wo])
        rw = kb.sb("rw", [128, 8, E])
        dma("sp", rw[:], router_w[i * D:(i + 1) * D, :].rearrange("(c p) n -> p c n", p=128), w=[rw])
        rbrow = kb.sb("rbrow", [128, E])
        dma("sp", rbrow[:], router_b[i, :].partition_broadcast(128), w=[rbrow])
        grow = {}
        for s in (NB, b):
            g = kb.sb("grow", [128, 3, D])
            for k, kind in enumerate((2, 4, 3)):
                dma("sp", g[:, k, :], MODD[s, kind * D:(kind + 1) * D].partition_broadcast(128), w=[g])
            grow[s] = g
        borow = None
        if b_o is not None:
            borow = kb.sb("borow", [128, D])
            dma("sp", borow[:], b_o.partition_broadcast(128), w=[borow])
        oTs = [kb.sb("oT", [128, KC, 128], BF16) for _ in range(2)]
        xts = [kb.sb("xt", [128, D]) for _ in range(2)]
        ts = [kb.sb("tt", [128, D]) for _ in range(2)]
        h2s = [kb.sb("h2", [128, D]) for _ in range(2)]
        h2Ts = [kb.sb("h2T", [128, 8, 128]) for _ in range(2)]
        st = kb.sb("st", [128, 2, 6])
        mv = kb.sb("mv", [128, 4])
        lg = kb.sb("lg", [128, E])
        t_start = 0 if need_ctx else C // 128
        for ti in range(t_start, NT):
            s = NB if ti < C // 128 else b
            g = grow[s]
            r0 = b * Tn + ti * 128
            gi = b * NT + ti
            oT = oTs[ti % 2]; x = xts[ti % 2]; t = ts[ti % 2]; h2 = h2s[ti % 2]; h2T = h2Ts[ti % 2]
            dma("sp", oT[:], OT[0:Dm, ti * 128:(ti + 1) * 128].rearrange("(c p) t -> p c t", p=128), w=[oT])
            dma("sp", x[:], Xsrc[r0:r0 + 128, :], w=[x])
            for hf in range(2):
                p = nps()
                for kc in range(KC):
                    op("pe", lambda e: e.matmul(p[:, :], oT[:, kc, :], wo[:, kc, hf * 512:(hf + 1) * 512], start=(kc == 0), stop=(kc == KC - 1)),
                       r=[oT, wo], w=[p])
                sl = slice(hf * 512, (hf + 1) * 512)
                if borow is not None:
                    op("dve", lambda e: e.tensor_tensor(out=t[:, sl], in0=p[:, :], in1=borow[:, sl], op=ALU.add), r=[p, borow], w=[t])
                    op("dve", lambda e: e.tensor_tensor(out=t[:, sl], in0=t[:, sl], in1=g[:, 0, sl], op=ALU.mult), r=[t, g], w=[t])
                else:
                    op("dve", lambda e: e.tensor_tensor(out=t[:, sl], in0=p[:, :], in1=g[:, 0, sl], op=ALU.mult), r=[p, g], w=[t])
            op("dve", lambda e: e.scalar_tensor_tensor(out=t[:], in0=x[:], scalar=ALPHA, in1=t[:], op0=ALU.mult, op1=ALU.add), r=[x, t], w=[t])
            layernorm(t, 0, st, mv, None)
            dma("pool", XA[r0:r0 + 128, :], t[:], r=[t])
            op("pool", lambda e: e.tensor_tensor(out=h2[:], in0=t[:], in1=g[:, 1, :], op=ALU.mult), r=[t, g], w=[h2])
            op("pool", lambda e: e.tensor_tensor(out=h2[:], in0=h2[:], in1=g[:, 2, :], op=ALU.add), r=[h2, g], w=[h2])
            dma("pool", H2[r0:r0 + 128, :], h2[:], r=[h2])
            for half in range(2):
                p = nps()
                for c4 in range(4):
                    c = half * 4 + c4
                    op("pe", lambda e: e.transpose(p[:, c4 * 128:(c4 + 1) * 128], h2[:, c * 128:(c + 1) * 128], ident()), r=[h2, cm], w=[p])
                op("act", lambda e: e.activation(out=h2T[:, half * 4:(half + 1) * 4, :].rearrange("p c t -> p (c t)"), in_=p[:, :], func=AF.Copy),
                   r=[p], w=[h2T])
            p = nps()
            for c in range(8):
                op("pe", lambda e: e.matmul(p[:, 0:E], h2T[:, c, :], rw[:, c, :], start=(c == 0), stop=(c == 7)), r=[h2T, rw], w=[p])
            op("dve", lambda e: e.tensor_tensor(out=lg[:], in0=p[:, 0:E], in1=rbrow[:], op=ALU.add), r=[p, rbrow], w=[lg])
            op("dve", lambda e: e.max(out=top8[:, gi, :], in_=lg[:]), r=[lg], w=[top8])
            op("dve", lambda e: e.max_index(out=idx8[:, gi, :], in_max=top8[:, gi, :], in_values=lg[:]), r=[lg, top8], w=[idx8])
            op("dve", lambda e: e.tensor_scalar(out=maskA[:, gi, :], in0=lg[:], scalar1=top8[:, gi, 3:4], scalar2=None, op0=ALU.is_ge), r=[lg, top8], w=[maskA])
        kb.pop()

    def phase_moe(i, last):
        tiles = []
        for b in range(NB):
            for ti in range(NT):
                if last and cfg.last_noctx and ti < C // 128:
                    continue
                tiles.append((b, ti))
        kb.push()
        op("dve", lambda e: e.memset(runcnt[:], 0.0), w=[runcnt])
        for (b, ti) in tiles:
            gi = b * NT + ti
            p = nps()
            op("pe", lambda e: e.matmul(p[:, 0:E], tri, maskA[:, gi, :], start=True, stop=True), r=[cm, maskA], w=[p])
            op("pe", lambda e: e.matmul(p[:, E:2 * E], ones, maskA[:, gi, :], start=True, stop=True), r=[cm, maskA], w=[p])
            op("dve", lambda e: e.tensor_tensor(out=posA[:, gi, :], in0=p[:, 0:E], in1=runcnt[:], op=ALU.add), r=[p, runcnt], w=[posA])
            op("dve", lambda e: e.tensor_tensor(out=runcnt[:], in0=p[:, E:2 * E], in1=runcnt[:], op=ALU.add), r=[p, runcnt], w=[runcnt])
        padf = kb.sb("padf", [128, E])
        incl = kb.sb("incl", [128, E])
        excl = kb.sb("excl", [128, E])
        zer = kb.sb("zer", [128, E])
        op("dve", lambda e: e.memset(zer[:], 0.0), w=[zer])
        blkpos = kb.sb("blkpos", [128, NBLK])
        tmpb = kb.sb("tmpb", [128, NBLK])
        op("pool", lambda e: e.iota(blkpos[:], pattern=[[SB, NBLK]], base=0, channel_multiplier=0, allow_small_or_imprecise_dtypes=True), w=[blkpos])
        for ex in range(E):
            op("dve", lambda e: e.tensor_scalar(out=tmpb[:], in0=blkpos[:], scalar1=runcnt[:, ex:ex + 1], scalar2=None, op0=ALU.is_lt, op1=ALU.add,
                                                accum_out=padf[:, ex:ex + 1]), r=[blkpos, runcnt], w=[tmpb, padf])
        op("dve", lambda e: e.tensor_scalar(out=padf[:], in0=padf[:], scalar1=float(SB), scalar2=None, op0=ALU.mult), r=[padf], w=[padf])
        op("dve", lambda e: e.tensor_tensor_scan(out=incl[:], data0=padf[:], data1=zer[:], initial=0.0, op0=ALU.add, op1=ALU.add), r=[padf, zer], w=[incl])
        op("dve", lambda e: e.tensor_tensor(out=excl[:], in0=incl[:], in1=padf[:], op=ALU.subtract), r=[incl, padf], w=[excl])
        bef = kb.sb("bef", [128, NBLK])
        op("dve", lambda e: e.memset(bef[:], 0.0), w=[bef])
        for ex in range(E):
            op("dve", lambda e: e.tensor_scalar(out=tmpb[:], in0=blkpos[:], scalar1=incl[:, ex:ex + 1], scalar2=None, op0=ALU.is_ge), r=[blkpos, incl], w=[tmpb])
            op("dve", lambda e: e.tensor_tensor(out=bef[:], in0=bef[:], in1=tmpb[:], op=ALU.add), r=[bef, tmpb], w=[bef])
        op("dve", lambda e: e.tensor_scalar(out=bef[:], in0=bef[:], scalar1=float(E - 1), scalar2=None, op0=ALU.min), r=[bef], w=[bef])
        idxf = kb.sb("idxf", [128, 4])
        oh = kb.sb("oh", [128, 4, E])
        slt = kb.sb("slt", [128, E])
        den = kb.sb("den", [128, 1])
        nb0 = kb.sb("nb0", [128, 1])
        for (b, ti) in tiles:
            gi = b * NT + ti
            op("dve", lambda e: e.tensor_copy(out=idxf[:], in_=idx8[:, gi, 0:4]), r=[idx8], w=[idxf])
            op("dve", lambda e: e.tensor_tensor(out=slt[:], in0=posA[:, gi, :], in1=excl[:], op=ALU.add), r=[posA, excl], w=[slt])
            for k in range(4):
                op("dve", lambda e: e.tensor_scalar(out=oh[:, k, :], in0=iota32, scalar1=idxf[:, k:k + 1], scalar2=None, op0=ALU.is_equal), r=[cm, idxf], w=[oh])
                op("dve", lambda e: e.tensor_tensor(out=oh[:, k, :], in0=oh[:, k, :], in1=slt[:], op=ALU.mult), r=[oh, slt], w=[oh])
            op("dve", lambda e: e.reduce_sum(out=slot4f[:, gi, :], in_=oh[:], axis=AX.X), r=[oh], w=[slot4f])
            op("dve", lambda e: e.tensor_copy(out=slot4[:, gi, :], in_=slot4f[:, gi, :]), r=[slot4f], w=[slot4])
            op("dve", lambda e: e.tensor_scalar(out=nb0[:], in0=top8[:, gi, 0:1], scalar1=-1.0, scalar2=None, op0=ALU.mult), r=[top8], w=[nb0])
            op("act", lambda e: e.activation(out=gate4[:, gi, :], in_=top8[:, gi, 0:4], func=AF.Exp, bias=nb0[:, 0:1], scale=1.0), r=[top8, nb0], w=[gate4])
            op("dve", lambda e: e.reduce_sum(out=den[:], in_=gate4[:, gi, :], axis=AX.X), r=[gate4], w=[den])
            op("dve", lambda e: e.reciprocal(out=den[:], in_=den[:]), r=[den], w=[den])
            op("dve", lambda e: e.tensor_scalar(out=gate4[:, gi, :], in0=gate4[:, gi, :], scalar1=den[:, 0:1], scalar2=None, op0=ALU.mult), r=[gate4, den], w=[gate4])
        if cfg.debug:
            dma("sp", DBG["slot4"][:, :], slot4[:].rearrange("p a b -> p (a b)"), r=[slot4])
            dma("sp", DBG["gate4"][:, :], gate4[:].rearrange("p a b -> p (a b)"), r=[gate4])
            dma("sp", DBG["top8"][:, :], top8[:].rearrange("p a b -> p (a b)"), r=[top8])
            dma("sp", DBG["posA"][:, :], posA[:].rearrange("p a b -> p (a b)"), r=[posA])
            dma("sp", DBG["bef"][:, :], bef[:], r=[bef])
            dma("sp", DBG["runcnt"][:, :], runcnt[:], r=[runcnt])
            dma("sp", DBG["excl"][:, :], excl[:], r=[excl])
            dma("sp", DBG["padf"][:, :], padf[:], r=[padf])
            dma("sp", DBG["incl"][:, :], incl[:], r=[incl])
        kb.push()
        h2s = [kb.sb("h2d", [128, D]) for _ in range(3)]
        for n, (b, ti) in enumerate(tiles):
            gi = b * NT + ti
            r0 = b * Tn + ti * 128
            h2 = h2s[n % 3]
            dma("sp", h2[:], H2[r0:r0 + 128, :], w=[h2])
            for k in range(4):
                dma("pool", None, None, r=[h2, slot4], fn=lambda e: e.indirect_dma_start(
                    out=XS[:, :], out_offset=bass.IndirectOffsetOnAxis(ap=slot4[:, gi, k:k + 1], axis=0), in_=h2[:], in_offset=None))
        kb.pop()
        kb.push()
        wg = [kb.sb("wg", [128, 8, 2 * FF], BF16) for _ in range(2)]
        wd = [kb.sb("wd", [128, 8, D], BF16) for _ in range(2)]
        brow = [kb.sb("brow", [128, 3 * D], BF16) for _ in range(2)]
        onesb = kb.sb("onesb", [1, SB], BF16)
        op("dve", lambda e: e.memset(onesb[:], 1.0), w=[onesb])
        iotap = kb.sb("iotap", [128, 8])
        op("pool", lambda e: e.iota(iotap[:], pattern=[[128, 8]], base=0, channel_multiplier=1, allow_small_or_imprecise_dtypes=True), w=[iotap])
        widx_f = kb.sb("widxf", [128, 9])
        widx = [kb.sb("widx", [128, 9], I32) for _ in range(3)]
        xs = [kb.sb("xs", [128, D]) for _ in range(2)]
        xT = kb.sb("xTm", [128, 8, SB], BF16)
        aT = kb.sb("aTm", [128, 8, SB], BF16)
        gsb = [kb.sb("gsb", [128, SB]) for _ in range(2)]
        sgb = [kb.sb("sgb", [128, SB]) for _ in range(2)]
        usb = [kb.sb("usb", [128, SB]) for _ in range(2)]
        ysb = [kb.sb("ysb", [128, D]) for _ in range(2)]
        be_l = kb.sb("be_l", [128, NBLK])
        op("dve", lambda e: e.tensor_copy(out=be_l[:], in_=bef[:]), r=[bef], w=[be_l])
        kb.wait_bg()

        def load_w(blk):
            wi = widx[blk % 3]
            op("dve", lambda e: e.tensor_scalar(out=widx_f[:, 8:9], in0=be_l[:, blk:blk + 1], scalar1=1.0, scalar2=None, op0=ALU.mult), r=[be_l], w=[widx_f])
            op("dve", lambda e: e.scalar_tensor_tensor(out=widx_f[:, 0:8], in0=be_l[:, blk:blk + 1].to_broadcast([128, 8]), scalar=1024.0, in1=iotap[:], op0=ALU.mult, op1=ALU.add),
               r=[be_l, iotap], w=[widx_f])
            op("dve", lambda e: e.tensor_copy(out=wi[:], in_=widx_f[:]), r=[widx_f], w=[wi])
            w1 = wg[blk % 2]; w2 = wd[blk % 2]; br = brow[blk % 2]
            for kc in range(8):
                dma("pool", None, None, r=[wi], w=[w1], fn=lambda e: e.indirect_dma_start(
                    out=w1[:, kc, :], out_offset=None, in_=WGU16[:, :], in_offset=bass.IndirectOffsetOnAxis(ap=wi[:, kc:kc + 1], axis=0)))
            for kc in range(8):
                dma("pool", None, None, r=[wi], w=[w2], fn=lambda e: e.indirect_dma_start(
                    out=w2[:, kc, :], out_offset=None, in_=WDN16[:, :], in_offset=bass.IndirectOffsetOnAxis(ap=wi[:, kc:kc + 1], axis=0)))
            dma("pool", None, None, r=[wi], w=[br], fn=lambda e: e.indirect_dma_start(
                out=br[:, 0:2 * FF], out_offset=None, in_=BGU16[:, :], in_offset=bass.IndirectOffsetOnAxis(ap=wi[:, 8:9], axis=0)))
            dma("pool", None, None, r=[wi], w=[br], fn=lambda e: e.indirect_dma_start(
                out=br[:, 2 * FF:3 * FF], out_offset=None, in_=BDN16[:, :], in_offset=bass.IndirectOffsetOnAxis(ap=wi[:, 8:9], axis=0)))

        load_w(0)
        nxs = 0
        for blk in range(NBLK):
            if blk + 1 < NBLK:
                load_w(blk + 1)
            w1 = wg[blk % 2]; w2 = wd[blk % 2]; br = brow[blk % 2]
            for j in range(SB // 128):
                x = xs[nxs % 2]; nxs += 1
                dma("sp", x[:], XS[blk * SB + j * 128: blk * SB + (j + 1) * 128, :], w=[x])
                for half in range(2):
                    p = nps()
                    for c4 in range(4):
                        c = half * 4 + c4
                        op("pe", lambda e: e.transpose(p[:, c4 * 128:(c4 + 1) * 128], x[:, c * 128:(c + 1) * 128], ident()), r=[x, cm], w=[p])
                    op("act", lambda e: e.activation(out=xT[:, half * 4:(half + 1) * 4, j * 128:(j + 1) * 128], in_=p[:, :].rearrange("p (c t) -> p c t", c=4), func=AF.Copy),
                       r=[p], w=[xT])
            for fc in range(8):
                g_ = gsb[fc % 2]; s_ = sgb[fc % 2]; u_ = usb[fc % 2]
                pg = nps()
                for kc in range(8):
                    op("pe", lambda e: e.matmul(pg[:, :], w1[:, kc, fc * 128:(fc + 1) * 128], xT[:, kc, :], start=(kc == 0), stop=False), r=[w1, xT], w=[pg])
                op("pe", lambda e: e.matmul(pg[:, :], br[0:1, fc * 128:(fc + 1) * 128], onesb[0:1, :], start=False, stop=True), r=[br, onesb], w=[pg])
                pu = nps()
                for kc in range(8):
                    op("pe", lambda e: e.matmul(pu[:, :], w1[:, kc, FF + fc * 128:FF + (fc + 1) * 128], xT[:, kc, :], start=(kc == 0), stop=False), r=[w1, xT], w=[pu])
                op("pe", lambda e: e.matmul(pu[:, :], br[0:1, FF + fc * 128:FF + (fc + 1) * 128], onesb[0:1, :], start=False, stop=True), r=[br, onesb], w=[pu])
                op("dve", lambda e: e.tensor_scalar(out=g_[:], in0=pg[:, :], scalar1=7.0, scalar2=None, op0=ALU.min), r=[pg], w=[g_])
                op("act", lambda e: e.activation(out=s_[:], in_=g_[:], func=AF.Sigmoid, scale=1.702), r=[g_], w=[s_])
                op("dve", lambda e: e.tensor_scalar(out=u_[:], in0=pu[:, :], scalar1=7.0, scalar2=-7.0, op0=ALU.min, op1=ALU.max), r=[pu], w=[u_])
                op("dve", lambda e: e.scalar_tensor_tensor(out=u_[:], in0=u_[:], scalar=1.0, in1=g_[:], op0=ALU.add, op1=ALU.mult), r=[u_, g_], w=[u_])
                op("pool", lambda e: e.tensor_tensor(out=aT[:, fc, :], in0=u_[:], in1=s_[:], op=ALU.mult), r=[u_, s_], w=[aT])
            for j in range(SB // 128):
                y = ysb[j % 2]
                for hf in range(2):
                    p = nps()
                    for fc in range(8):
                        op("pe", lambda e: e.matmul(p[:, :], aT[:, fc, j * 128:(j + 1) * 128], w2[:, fc, hf * 512:(hf + 1) * 512], start=(fc == 0), stop=False), r=[aT, w2], w=[p])
                    op("pe", lambda e: e.matmul(p[:, :], onesb[0:1, 0:128], br[0:1, 2 * FF + hf * 512:2 * FF + (hf + 1) * 512], start=False, stop=True), r=[br, onesb], w=[p])
                    op("act", lambda e: e.activation(out=y[:, hf * 512:(hf + 1) * 512], in_=p[:, :], func=AF.Copy), r=[p], w=[y])
                dma("sp", YS[blk * SB + j * 128: blk * SB + (j + 1) * 128, :], y[:], r=[y])
        kb.pop()
        kb.push()
        grow = {}
        for s in range(NS):
            g = kb.sb("g2row", [128, D])
            dma("sp", g[:], MODD[s, 5 * D:6 * D].partition_broadcast(128), w=[g])
            grow[s] = g
        yk = [kb.sb("yk", [128, D]) for _ in range(4)]
        accs = [kb.sb("acc", [128, D]) for _ in range(2)]
        xts = [kb.sb("xt2", [128, D]) for _ in range(2)]
        st = kb.sb("st2", [128, 2, 6])
        mv = kb.sb("mv2", [128, 4])
        for n, (b, ti) in enumerate(tiles):
            gi = b * NT + ti
            r0 = b * Tn + ti * 128
            s = NB if ti < C // 128 else b
            acc = accs[n % 2]; x = xts[n % 2]
            dma("sp", x[:], XA[r0:r0 + 128, :], w=[x])
            for k in range(4):
                y = yk[k]
                dma("pool", None, None, r=[slot4], w=[y], fn=lambda e: e.indirect_dma_start(
                    out=y[:], out_offset=None, in_=YS[:, :], in_offset=bass.IndirectOffsetOnAxis(ap=slot4[:, gi, k:k + 1], axis=0)))
                if k == 0:
                    op("dve", lambda e: e.tensor_scalar(out=acc[:], in0=y[:], scalar1=gate4[:, gi, 0:1], scalar2=None, op0=ALU.mult), r=[y, gate4], w=[acc])
                else:
                    op("dve", lambda e: e.scalar_tensor_tensor(out=acc[:], in0=y[:], scalar=gate4[:, gi, k:k + 1], in1=acc[:], op0=ALU.mult, op1=ALU.add), r=[y, gate4, acc], w=[acc])
            op("dve", lambda e: e.tensor_tensor(out=acc[:], in0=acc[:], in1=grow[s][:], op=ALU.mult), r=[acc, grow[s]], w=[acc])
            op("dve", lambda e: e.scalar_tensor_tensor(out=acc[:], in0=x[:], scalar=ALPHA, in1=acc[:], op0=ALU.mult, op1=ALU.add), r=[x, acc], w=[acc])
            layernorm(acc, 2, st, mv, None)
            if last:
                if ti >= C // 128:
                    ro = b * S + (ti - C // 128) * 128
                    dma("sp", out[ro:ro + 128, :], acc[:], r=[acc])
            else:
                dma("sp", XB[r0:r0 + 128, :], acc[:], r=[acc])
        kb.pop()
        kb.pop()

    onesrow_t = kb.sb("onesrow", [1, SB])
    onesrow = onesrow_t
    op("dve", lambda e: e.memset(onesrow_t[:], 1.0), w=[onesrow_t])

    def mixer_identity(i, b):
        kb.push()
        buf = [kb.sb("cp", [128, 8, 512], BF16) for _ in range(2)]
        for gi, (t0, tl) in enumerate(groups()):
            bb = buf[gi % 2]
            dma("sp", bb[:, :, 0:tl], HT[:, t0:t0 + tl].rearrange("(c p) t -> p c t", p=128), w=[bb])
            dma("sp", OT[0:D, t0:t0 + tl].rearrange("(c p) t -> p c t", p=128), bb[:, :, 0:tl], r=[bb])
        kb.pop()
        return w_id[:, :], D, None

    def mixer_rg(i, b, j, need_ctx):
        kb.push()
        win = kb.sb("win", [128, 8, 2 * D_RNN], BF16)
        for kc_ in range(8):
            dma("pool", win[:, kc_, :], rg_w_in[j * D + kc_ * 128:j * D + (kc_ + 1) * 128, :], w=[win])
        hTs = [kb.sb("hTr", [128, 8, 512], BF16) for _ in range(2)]
        pos_ = [kb.sb("pout", [128, 4, 512]) for _ in range(2)]
        n = 0
        for gi, (t0, tl) in enumerate(groups()):
            h = hTs[gi % 2]
            dma("sp", h[:, :, 0:tl], HT[:, t0:t0 + tl].rearrange("(c p) t -> p c t", p=128), w=[h])
            for q in range(6):
                po = pos_[n % 2]; n += 1
                for jj in range(4):
                    jc = q * 4 + jj
                    p = nps()
                    for kc in range(8):
                        op("pe", lambda e: e.matmul(p[:, 0:tl], win[:, kc, jc * 128:(jc + 1) * 128], h[:, kc, 0:tl], start=(kc == 0), stop=(kc == 7)), r=[win, h], w=[p])
                    if jj % 2 == 0:
                        op("act", lambda e: e.activation(out=po[:, jj, 0:tl], in_=p[:, 0:tl], func=AF.Copy), r=[p], w=[po])
                    else:
                        op("dve", lambda e: e.tensor_copy(out=po[:, jj, 0:tl], in_=p[:, 0:tl]), r=[p], w=[po])
                dma("pool", PR[q * 512:(q + 1) * 512, t0:t0 + tl].rearrange("(c p) t -> p c t", p=128), po[:, :, 0:tl], r=[po])
        kb.pop()
        kb.push()
        prow = kb.sb("prow", [11, D_RNN])
        dma("sp", prow[:], rg_p[j * 11:(j + 1) * 11, :], w=[prow])
        chp = kb.sb("chp", [128, 12, 11])
        p = nps()
        for c in range(12):
            op("pe", lambda e: e.transpose(p[:, c * 11:(c + 1) * 11], prow[:, c * 128:(c + 1) * 128], ident(11)), r=[prow, cm], w=[p])
        op("dve", lambda e: e.tensor_copy(out=chp[:].rearrange("p c k -> p (c k)"), in_=p[:, 0:132]), r=[p], w=[chp])
        cdec = kb.sb("cdec", [128, 12, 4])
        op("act", lambda e: e.activation(out=cdec[:, :, 0:2], in_=chp[:, :, 9:11], func=AF.Exp, scale=-1.0), r=[chp], w=[cdec])
        op("act", lambda e: e.activation(out=cdec[:, :, 0:2], in_=cdec[:, :, 0:2], func=AF.Ln, bias=1.0, scale=1.0), r=[cdec], w=[cdec])
        op("dve", lambda e: e.tensor_scalar(out=cdec[:, :, 2:4], in0=cdec[:, :, 0:2], scalar1=-16.0, scalar2=None, op0=ALU.mult), r=[cdec], w=[cdec])
        op("dve", lambda e: e.tensor_scalar(out=cdec[:, :, 0:2], in0=cdec[:, :, 0:2], scalar1=-8.0, scalar2=None, op0=ALU.mult), r=[cdec], w=[cdec])
        xr = kb.sb("xr", [128, Tn])
        xc = kb.sb("xc", [128, 2, Tn])
        gl = kb.sb("gl", [128, Tn])
        aa = kb.sb("aa", [128, Tn])
        uu = kb.sb("uu", [128, Tn])
        hh = kb.sb("hh", [128, Tn])
        hs = kb.sb("hs", [128, Tn])
        gw = kb.sb("gw", [128, 2, 2, 2, 256])
        rr = [kb.sb("rr", [128, 512]) for _ in range(2)]
        ii = [kb.sb("ii", [128, 512]) for _ in range(2)]
        segs = [(0, C), (C, Tn)]
        for nb_ in range(6):
            for d in range(2):
                base = ((j * 2 + d) * 6 + nb_) * 256
                dma("sp", gw[:, d, 0, :, :], rg_ga[base:base + 256, :].rearrange("(c p) n -> p c n", p=128), w=[gw])
                dma("sp", gw[:, d, 1, :, :], rg_gx[base:base + 256, :].rearrange("(c p) n -> p c n", p=128), w=[gw])
            for cc_ in range(2):
                ch = nb_ * 2 + cc_
                dma("sp", xr[:], PR[D_RNN + ch * 128: D_RNN + (ch + 1) * 128, :], w=[xr])
                for (a0, b0) in segs:
                    op("dve", lambda e: e.tensor_scalar(out=xc[:, cc_, a0:b0], in0=xr[:, a0:b0], scalar1=chp[:, ch, 1:2], scalar2=chp[:, ch, 4:5], op0=ALU.mult, op1=ALU.add), r=[xr, chp], w=[xc])
                    op("dve", lambda e: e.scalar_tensor_tensor(out=xc[:, cc_, a0 + 1:b0], in0=xr[:, a0:b0 - 1], scalar=chp[:, ch, 0:1], in1=xc[:, cc_, a0 + 1:b0], op0=ALU.mult, op1=ALU.add), r=[xr, chp, xc], w=[xc])
                    op("dve", lambda e: e.scalar_tensor_tensor(out=xc[:, cc_, a0:b0 - 1], in0=xr[:, a0 + 1:b0], scalar=chp[:, ch, 2:3], in1=xc[:, cc_, a0:b0 - 1], op0=ALU.mult, op1=ALU.add), r=[xr, chp, xc], w=[xc])
                    op("dve", lambda e: e.scalar_tensor_tensor(out=xc[:, cc_, a0:b0 - 2], in0=xr[:, a0 + 2:b0], scalar=chp[:, ch, 3:4], in1=xc[:, cc_, a0:b0 - 2], op0=ALU.mult, op1=ALU.add), r=[xr, chp, xc], w=[xc])
            for oc in range(2):
                ch = nb_ * 2 + oc
                dma("sp", gl[:], PR[ch * 128:(ch + 1) * 128, :], w=[gl])
                for d in range(2):
                    for gi, (t0, tl) in enumerate(groups()):
                        r_ = rr[gi % 2]; i_ = ii[gi % 2]
                        pa = nps()
                        for kc in range(2):
                            op("pe", lambda e: e.matmul(pa[:, 0:tl], gw[:, d, 0, kc, oc * 128:(oc + 1) * 128], xc[:, kc, t0:t0 + tl], start=(kc == 0), stop=(kc == 1)), r=[gw, xc], w=[pa])
                        px = nps()
                        for kc in range(2):
                            op("pe", lambda e: e.matmul(px[:, 0:tl], gw[:, d, 1, kc, oc * 128:(oc + 1) * 128], xc[:, kc, t0:t0 + tl], start=(kc == 0), stop=(kc == 1)), r=[gw, xc], w=[px])
                        op("act", lambda e: e.activation(out=r_[:, 0:tl], in_=pa[:, 0:tl], func=AF.Sigmoid, bias=chp[:, ch, 5 + d:6 + d], scale=1.0), r=[pa, chp], w=[r_])
                        op("act", lambda e: e.activation(out=i_[:, 0:tl], in_=px[:, 0:tl], func=AF.Sigmoid, bias=chp[:, ch, 7 + d:8 + d], scale=1.0), r=[px, chp], w=[i_])
                        op("act", lambda e: e.activation(out=aa[:, t0:t0 + tl], in_=r_[:, 0:tl], func=AF.Exp, scale=cdec[:, ch, d:d + 1]), r=[r_, cdec], w=[aa])
                        op("act", lambda e: e.activation(out=r_[:, 0:tl], in_=r_[:, 0:tl], func=AF.Exp, scale=cdec[:, ch, 2 + d:3 + d]), r=[r_, cdec], w=[r_])
                        op("act", lambda e: e.activation(out=r_[:, 0:tl], in_=r_[:, 0:tl], func=AF.Sqrt, bias=1.0, scale=-1.0), r=[r_], w=[r_])
                        op("dve", lambda e: e.tensor_tensor(out=i_[:, 0:tl], in0=i_[:, 0:tl], in1=xc[:, oc, t0:t0 + tl], op=ALU.mult), r=[i_, xc], w=[i_])
                        op("dve", lambda e: e.tensor_tensor(out=uu[:, t0:t0 + tl], in0=i_[:, 0:tl], in1=r_[:, 0:tl], op=ALU.mult), r=[i_, r_], w=[uu])
                    if d == 0:
                        op("dve", lambda e: e.tensor_tensor_scan(out=hs[:, :], data0=aa[:, :], data1=uu[:, :], initial=0.0, op0=ALU.mult, op1=ALU.add), r=[aa, uu], w=[hs])
                    else:
                        op("dve", lambda e: e.tensor_tensor_scan(out=hh[:, 0:C][:, ::-1], data0=aa[:, 0:C][:, ::-1], data1=uu[:, 0:C][:, ::-1], initial=0.0, op0=ALU.mult, op1=ALU.add), r=[aa, uu], w=[hh])
                        op("dve", lambda e: e.tensor_tensor_scan(out=hh[:, C:Tn][:, ::-1], data0=aa[:, C:Tn][:, ::-1], data1=uu[:, C:Tn][:, ::-1], initial=hh[:, 0:1], op0=ALU.mult, op1=ALU.add), r=[aa, uu, hh], w=[hh])
                        op("pool", lambda e: e.tensor_tensor(out=hs[:], in0=hs[:], in1=hh[:], op=ALU.add), r=[hs, hh], w=[hs])
                op("pool", lambda e: e.tensor_tensor(out=hh[:], in0=gl[:], in1=gl[:], op=ALU.mult), r=[gl], w=[hh])
                op("dve", lambda e: e.tensor_scalar(out=hh[:], in0=hh[:], scalar1=0.044715, scalar2=1.0, op0=ALU.mult, op1=ALU.add), r=[hh], w=[hh])
                op("pool", lambda e: e.tensor_tensor(out=hh[:], in0=hh[:], in1=gl[:], op=ALU.mult), r=[hh, gl], w=[hh])
                op("act", lambda e: e.activation(out=hh[:], in_=hh[:], func=AF.Sigmoid, scale=1.5957691216057308), r=[hh], w=[hh])
                op("pool", lambda e: e.tensor_tensor(out=hh[:], in0=hh[:], in1=gl[:], op=ALU.mult), r=[hh, gl], w=[hh])
                op("dve", lambda e: e.tensor_tensor(out=hs[:], in0=hs[:], in1=hh[:], op=ALU.mult), r=[hs, hh], w=[hs])
                dma("pool", OT[ch * 128:(ch + 1) * 128, :], hs[:], r=[hs])
        kb.pop()
        return rg_w_out[j * D_RNN:(j + 1) * D_RNN, :], D_RNN, None

    def mixer_gqa(i, b, j, need_ctx):
        QT3 = QT.ap().rearrange("p (h t) -> p h t", h=16)
        KT3 = KT.ap().rearrange("p (h t) -> p h t", h=2)
        kb.push()
        w = kb.sb("gqw", [128, 8, 2432], BF16)
        for kc_ in range(8):
            dma("pool", w[:, kc_, :], gq_w[j * D + kc_ * 128:j * D + (kc_ + 1) * 128, :], w=[w])
        b64 = kb.sb("b64", [64, 40])
        dma("sp", b64[:], gq_b64[j * 64:(j + 1) * 64, :], w=[b64])
        bvrow = kb.sb("bvrow", [128, 128])
        dma("sp", bvrow[:], gq_bv[j, :].partition_broadcast(128), w=[bvrow])
        hTs = [kb.sb("hTg", [128, 8, 512], BF16) for _ in range(2)]
        qo = kb.sb("qo", [64, 16, 512])
        ko = kb.sb("ko", [64, 2, 512])
        cs = kb.sb("cs", [64, 512]); sn = kb.sb("sn", [64, 512])
        ta = [kb.sb("ta", [64, 512]) for _ in range(2)]
        tb = [kb.sb("tb", [64, 512]) for _ in range(2)]
        va = [kb.sb("va", [128, 2, 65]) for _ in range(2)]
        for v_ in va:
            op("dve", lambda e: e.memset(v_[:], 1.0), w=[v_])
        n = 0
        for gi, (t0, tl) in enumerate(groups()):
            h = hTs[gi % 2]
            lat = t0 >= C
            dma("sp", h[:, :, 0:tl], HT[:, t0:t0 + tl].rearrange("(c p) t -> p c t", p=128), w=[h])
            if lat:
                dma("sp", cs[:, 0:tl], rope64[0:64, t0 - C:t0 - C + tl], w=[cs])
                dma("sp", sn[:, 0:tl], rope64[64:128, t0 - C:t0 - C + tl], w=[sn])
            for hq in range(18):
                col = hq * 64 if hq < 16 else 2048 + (hq - 16) * 64
                colsw = 1024 + hq * 64 if hq < 16 else 2176 + (hq - 16) * 64
                dst = qo[:, hq, 0:tl] if hq < 16 else ko[:, hq - 16, 0:tl]
                dstT = qo if hq < 16 else ko
                p = nps()
                for kc in range(8):
                    op("pe", lambda e: e.matmul(p[0:64, 0:tl], w[:, kc, col:col + 64], h[:, kc, 0:tl], start=(kc == 0), stop=(kc == 7)), r=[w, h], w=[p])
                if lat:
                    p2 = nps()
                    for kc in range(8):
                        op("pe", lambda e: e.matmul(p2[0:64, 0:tl], w[:, kc, colsw:colsw + 64], h[:, kc, 0:tl], start=(kc == 0), stop=(kc == 7)), r=[w, h], w=[p2])
                    a_ = ta[n % 2]; b_ = tb[n % 2]; n += 1
                    op("dve", lambda e: e.scalar_tensor_tensor(out=a_[:, 0:tl], in0=p[0:64, 0:tl], scalar=b64[:, hq:hq + 1], in1=cs[:, 0:tl], op0=ALU.add, op1=ALU.mult), r=[p, b64, cs], w=[a_])
                    op("dve", lambda e: e.scalar_tensor_tensor(out=b_[:, 0:tl], in0=p2[0:64, 0:tl], scalar=b64[:, 20 + hq:21 + hq], in1=sn[:, 0:tl], op0=ALU.add, op1=ALU.mult), r=[p2, b64, sn], w=[b_])
                    op("pool", lambda e: e.tensor_tensor(out=dst, in0=a_[:, 0:tl], in1=b_[:, 0:tl], op=ALU.add), r=[a_, b_], w=[dstT])
                else:
                    op("dve", lambda e: e.tensor_scalar(out=dst, in0=p[0:64, 0:tl], scalar1=b64[:, hq:hq + 1], scalar2=None, op0=ALU.add), r=[p, b64], w=[dstT])
            dma("pool", QT3[:, :, t0:t0 + tl], qo[:, :, 0:tl], r=[qo])
            dma("pool", KT3[:, :, t0:t0 + tl], ko[:, :, 0:tl], r=[ko])
            for tt in range(tl // 128):
                v_ = va[tt % 2]
                p = nps()
                for kc in range(8):
                    op("pe", lambda e: e.matmul(p[:, 0:128], h[:, kc, tt * 128:(tt + 1) * 128], w[:, kc, 2304:2432], start=(kc == 0), stop=(kc == 7)), r=[w, h], w=[p])
                op("dve", lambda e: e.tensor_tensor(out=v_[:, :, 0:64], in0=p[:, 0:128].rearrange("p (h d) -> p h d", h=2), in1=bvrow[:].rearrange("p (h d) -> p h d", h=2), op=ALU.add), r=[p, bvrow], w=[v_])
                dma("pool", VA[t0 + tt * 128:t0 + (tt + 1) * 128, :], v_[:].rearrange("p h d -> p (h d)"), r=[v_])
        kb.pop()
        kb.push()
        kT = kb.sb("kT", [64, 2, Tn])
        dma("sp", kT[:], KT3[:, :, :], w=[kT])
        V = kb.sb("Vg", [128, NT, 130])
        dma("sp", V[:], VA.ap().rearrange("(n p) c -> p n c", p=128), w=[V])
        esk = kb.sb("esk", [64, 16])
        dma("sp", esk[:], gq_sinks[j, :].partition_broadcast(64), w=[esk])
        op("act", lambda e: e.activation(out=esk[:], in_=esk[:], func=AF.Exp), r=[esk], w=[esk])
        mge = kb.sb("mge", [128, 128])
        mle = kb.sb("mle", [128, 128])
        op("dve", lambda e: e.tensor_scalar(out=mge[:], in0=tri, scalar1=-1.0, scalar2=1.0, op0=ALU.mult, op1=ALU.add), r=[cm], w=[mge])
        op("dve", lambda e: e.tensor_tensor(out=mle[:], in0=tri, in1=ident(), op=ALU.add), r=[cm], w=[mle])
        sel = kb.sb("sel", [65, 64])
        op("dve", lambda e: e.memset(sel[:], 0.0), w=[sel])
        op("dve", lambda e: e.memset(sel[64:65, :], 1.0), w=[sel])
        qs = [kb.sb("qs", [64, 16, 128]) for _ in range(2)]
        pTs = [kb.sb("pT", [128, 512]) for _ in range(4)]
        Xs = [kb.sb("Xo", [65, 512]) for _ in range(2)]
        rb = [kb.sb("rb", [64, 512]) for _ in range(2)]
        oo = [kb.sb("oo", [64, 512]) for _ in range(2)]
        nq = 0
        blocks = []
        if need_ctx:
            blocks += [(tq, [(0, None), (1, None)]) for tq in range(C // 128)]
        nlb = S // 128
        for jq in range(nlb):
            ch = [(0, None), (1, None)]
            if jq > 0:
                ch.append((C // 128 + jq - 1, mge))
            ch.append((C // 128 + jq, None))
            if jq < nlb - 1:
                ch.append((C // 128 + jq + 1, mle))
            blocks.append((C // 128 + jq, ch))
        npt = 0
        for (tq, chunks) in blocks:
            q = qs[nq % 2]; nq += 1
            dma("sp", q[:], QT3[:, :, tq * 128:(tq + 1) * 128], w=[q])
            for kvh in range(2):
                for half in range(2):
                    h0 = kvh * 8 + half * 4
                    po = PS[4 + (npt % 4)]
                    for ci, (kt, msk) in enumerate(chunks):
                        pT = pTs[npt % 4]; npt += 1
                        p = nps()
                        op("pe", lambda e: e.matmul(p[:, :], kT[:, kvh, kt * 128:(kt + 1) * 128], q[:, h0:h0 + 4, :].rearrange("p h t -> p (h t)"), start=True, stop=True), r=[kT, q], w=[p])
                        op("act", lambda e: e.activation(out=pT[:], in_=p[:, :], func=AF.Exp, scale=0.125), r=[p], w=[pT])
                        if msk is not None:
                            for hh in range(4):
                                op("pool" if hh % 2 else "dve", lambda e: e.tensor_tensor(out=pT[:, hh * 128:(hh + 1) * 128], in0=pT[:, hh * 128:(hh + 1) * 128], in1=msk[:], op=ALU.mult), r=[pT, msk], w=[pT])
                        op("pe", lambda e: e.matmul(po[0:65, :], V[:, kt, kvh * 65:(kvh + 1) * 65], pT[:], start=(ci == 0), stop=(ci == len(chunks) - 1)), r=[V, pT], w=[po])
                    X = Xs[npt % 2]; r_ = rb[npt % 2]; o_ = oo[npt % 2]
                    op("act", lambda e: e.activation(out=X[:], in_=po[0:65, :], func=AF.Copy), r=[po], w=[X])
                    p = nps()
                    op("pe", lambda e: e.matmul(p[0:64, :], sel[:], X[:], start=True, stop=True), r=[sel, X], w=[p])
                    for hh in range(4):
                        op("dve", lambda e: e.tensor_scalar(out=r_[:, hh * 128:(hh + 1) * 128], in0=p[0:64, hh * 128:(hh + 1) * 128], scalar1=esk[:, h0 + hh:h0 + hh + 1], scalar2=None, op0=ALU.add), r=[p, esk], w=[r_])
                    op("dve", lambda e: e.reciprocal(out=r_[:], in_=r_[:]), r=[r_], w=[r_])
                    op("dve", lambda e: e.tensor_tensor(out=o_[:], in0=X[0:64, :], in1=r_[:], op=ALU.mult), r=[X, r_], w=[o_])
                    dma("pool", OT[h0 * 64:(h0 + 4) * 64, tq * 128:(tq + 1) * 128].rearrange("(h p) t -> p h t", p=64), o_[:].rearrange("p (h t) -> p h t", h=4), r=[o_])
        kb.pop()
        return gq_w_o[j * D:(j + 1) * D, :], D, gq_b_o[j, :]

    def mixer_mla(i, b, j, need_ctx):
        RMS_EPS = 1e-6
        kb.push()
        wd = kb.sb("mlwd", [128, 8, 448], BF16)
        for kc_ in range(8):
            dma("pool", wd[:, kc_, :], ml_wd[j * D + kc_ * 128:j * D + (kc_ + 1) * 128, :], w=[wd])
        nrow = kb.sb("nrow", [128, 384])
        dma("sp", nrow[:], ml_norm[j, :].partition_broadcast(128), w=[nrow])
        epsr = kb.sb("epsr", [128, 1])
        op("dve", lambda e: e.memset(epsr[:], RMS_EPS), w=[epsr])
        hTs = [kb.sb("hTm", [128, 8, 512], BF16) for _ in range(2)]
        cTs = [kb.sb("cTm", [128, 3, 512]) for _ in range(2)]
        krs = [kb.sb("krm", [32, 512]) for _ in range(2)]
        cs = kb.sb("csm", [32, 512]); sn = kb.sb("snm", [32, 512])
        t1 = kb.sb("t1m", [32, 512]); t2 = kb.sb("t2m", [32, 512])
        cqs = [kb.sb("cqm", [128, 384]) for _ in range(2)]
        junk = kb.sb("junk", [128, 256])
        ss = kb.sb("ssm", [128, 4])
        for gi, (t0, tl) in enumerate(groups()):
            h = hTs[gi % 2]; cT = cTs[gi % 2]; kr = krs[gi % 2]
            lat = t0 >= C
            dma("sp", h[:, :, 0:tl], HT[:, t0:t0 + tl].rearrange("(c p) t -> p c t", p=128), w=[h])
            for tt in range(tl // 128):
                cq = cqs[tt % 2]
                p = nps()
                for kc in range(8):
                    op("pe", lambda e: e.matmul(p[:, 0:384], h[:, kc, tt * 128:(tt + 1) * 128], wd[:, kc, 0:384], start=(kc == 0), stop=(kc == 7)), r=[h, wd], w=[p])
                op("act", lambda e: e.activation(out=junk[:, 0:256], in_=p[:, 0:256], func=AF.Square, accum_out=ss[:, 0:1]), r=[p], w=[junk, ss])
                op("act", lambda e: e.activation(out=junk[:, 0:128], in_=p[:, 256:384], func=AF.Square, accum_out=ss[:, 1:2]), r=[p], w=[junk, ss])
                op("act", lambda e: e.activation(out=ss[:, 2:3], in_=ss[:, 0:1], func=AF.Sqrt, bias=epsr[:, 0:1], scale=1.0 / 256), r=[ss, epsr], w=[ss])
                op("act", lambda e: e.activation(out=ss[:, 3:4], in_=ss[:, 1:2], func=AF.Sqrt, bias=epsr[:, 0:1], scale=1.0 / 128), r=[ss, epsr], w=[ss])
                op("dve", lambda e: e.reciprocal(out=ss[:, 2:4], in_=ss[:, 2:4]), r=[ss], w=[ss])
                op("dve", lambda e: e.scalar_tensor_tensor(out=cq[:, 0:256], in0=p[:, 0:256], scalar=ss[:, 2:3], in1=nrow[:, 0:256], op0=ALU.mult, op1=ALU.mult), r=[p, ss, nrow], w=[cq])
                op("dve", lambda e: e.scalar_tensor_tensor(out=cq[:, 256:384], in0=p[:, 256:384], scalar=ss[:, 3:4], in1=nrow[:, 256:384], op0=ALU.mult, op1=ALU.mult), r=[p, ss, nrow], w=[cq])
                p2 = nps()
                for c3 in range(3):
                    op("pe", lambda e: e.transpose(p2[:, c3 * 128:(c3 + 1) * 128], cq[:, c3 * 128:(c3 + 1) * 128], ident()), r=[cq, cm], w=[p2])
                op("act", lambda e: e.activation(out=cT[:, :, tt * 128:(tt + 1) * 128], in_=p2[:, 0:384].rearrange("p (c t) -> p c t", c=3), func=AF.Copy), r=[p2], w=[cT])
            dma("pool", CT[:, t0:t0 + tl].rearrange("(c p) t -> p c t", p=128), cT[:, :, 0:tl], r=[cT])
            p = nps()
            for kc in range(8):
                op("pe", lambda e: e.matmul(p[0:32, 0:tl], wd[:, kc, 384:416], h[:, kc, 0:tl], start=(kc == 0), stop=(kc == 7)), r=[h, wd], w=[p])
            if lat:
                p2 = nps()
                for kc in range(8):
                    op("pe", lambda e: e.matmul(p2[0:32, 0:tl], wd[:, kc, 416:448], h[:, kc, 0:tl], start=(kc == 0), stop=(kc == 7)), r=[h, wd], w=[p2])
                dma("sp", cs[:, 0:tl], rope32[0:32, t0 - C:t0 - C + tl], w=[cs])
                dma("sp", sn[:, 0:tl], rope32[32:64, t0 - C:t0 - C + tl], w=[sn])
                op("dve", lambda e: e.tensor_tensor(out=t1[:, 0:tl], in0=p[0:32, 0:tl], in1=cs[:, 0:tl], op=ALU.mult), r=[p, cs], w=[t1])
                op("dve", lambda e: e.tensor_tensor(out=t2[:, 0:tl], in0=p2[0:32, 0:tl], in1=sn[:, 0:tl], op=ALU.mult), r=[p2, sn], w=[t2])
                op("pool", lambda e: e.tensor_tensor(out=kr[:, 0:tl], in0=t1[:, 0:tl], in1=t2[:, 0:tl], op=ALU.add), r=[t1, t2], w=[kr])
            else:
                op("dve", lambda e: e.tensor_copy(out=kr[:, 0:tl], in_=p[0:32, 0:tl]), r=[p], w=[kr])
            dma("pool", KRT[:, t0:t0 + tl], kr[:, 0:tl], r=[kr])
        kb.pop()
        kb.push()
        cT = kb.sb("cTall", [128, 3, Tn])
        dma("sp", cT[:], CT.ap().rearrange("(c p) t -> p c t", p=128), w=[cT])
        krT = kb.sb("krT", [32, Tn], BF16)
        dma("pool", krT[:], KRT[:, :], w=[krT])
        csb = [kb.sb("csq", [32, 512]) for _ in range(2)]
        snb = [kb.sb("snq", [32, 512]) for _ in range(2)]
        sel = kb.sb("selm", [65, 64])
        op("dve", lambda e: e.memset(sel[:], 0.0), w=[sel])
        op("dve", lambda e: e.memset(sel[64:65, :], 1.0), w=[sel])
        qn = kb.sb("qn", [64, Tn], BF16); qr = kb.sb("qr", [32, Tn], BF16); kn = kb.sb("kn", [64, Tn], BF16)
        Vh = kb.sb("Vh", [128, NT, 65], BF16)
        op("dve", lambda e: e.memset(Vh[:], 1.0), w=[Vh])
        wq = kb.sb("wqh", [128, 2, 128]); wkv = kb.sb("wkvh", [128, 128])
        t1 = kb.sb("t1q", [32, 512]); t2 = kb.sb("t2q", [32, 512])
        pTs = [kb.sb("pTm", [128, 512], BF16) for _ in range(4)]
        Xs = [kb.sb("Xm", [65, 512]) for _ in range(2)]
        rb = [kb.sb("rbm", [64, 512]) for _ in range(2)]
        oo = [kb.sb("oom", [64, 512]) for _ in range(2)]
        scale = 96.0 ** -0.5
        npt = 0
        for hd in range(16):
            dma("sp", wq[:, :, 0:96], ml_wuq[j * 256:(j + 1) * 256, hd * 96:(hd + 1) * 96].rearrange("(c p) n -> p c n", p=128), w=[wq])
            dma("sp", wq[:, :, 96:128], ml_wuq[j * 256:(j + 1) * 256, 1536 + hd * 32:1536 + (hd + 1) * 32].rearrange("(c p) n -> p c n", p=128), w=[wq])
            dma("sp", wkv[:], ml_wukv[j * 128:(j + 1) * 128, hd * 128:(hd + 1) * 128], w=[wkv])
            for gi, (t0, tl) in enumerate(groups()):
                lat = t0 >= C
                p = nps()
                for kc in range(2):
                    op("pe", lambda e: e.matmul(p[0:64, 0:tl], wq[:, kc, 0:64], cT[:, kc, t0:t0 + tl], start=(kc == 0), stop=(kc == 1)), r=[wq, cT], w=[p])
                op("act", lambda e: e.activation(out=qn[:, t0:t0 + tl], in_=p[0:64, 0:tl], func=AF.Copy), r=[p], w=[qn])
                p = nps()
                for kc in range(2):
                    op("pe", lambda e: e.matmul(p[0:32, 0:tl], wq[:, kc, 64:96], cT[:, kc, t0:t0 + tl], start=(kc == 0), stop=(kc == 1)), r=[wq, cT], w=[p])
                if lat:
                    cs = csb[gi % 2]; sn = snb[gi % 2]
                    dma("sp", cs[:, 0:tl], rope32[0:32, t0 - C:t0 - C + tl], w=[cs])
                    dma("sp", sn[:, 0:tl], rope32[32:64, t0 - C:t0 - C + tl], w=[sn])
                    p2 = nps()
                    for kc in range(2):
                        op("pe", lambda e: e.matmul(p2[0:32, 0:tl], wq[:, kc, 96:128], cT[:, kc, t0:t0 + tl], start=(kc == 0), stop=(kc == 1)), r=[wq, cT], w=[p2])
                    op("dve", lambda e: e.tensor_tensor(out=t1[:, 0:tl], in0=p[0:32, 0:tl], in1=cs[:, 0:tl], op=ALU.mult), r=[p, cs], w=[t1])
                    op("dve", lambda e: e.tensor_tensor(out=t2[:, 0:tl], in0=p2[0:32, 0:tl], in1=sn[:, 0:tl], op=ALU.mult), r=[p2, sn], w=[t2])
                    op("pool", lambda e: e.tensor_tensor(out=qr[:, t0:t0 + tl], in0=t1[:, 0:tl], in1=t2[:, 0:tl], op=ALU.add), r=[t1, t2], w=[qr])
                else:
                    op("dve", lambda e: e.tensor_copy(out=qr[:, t0:t0 + tl], in_=p[0:32, 0:tl]), r=[p], w=[qr])
                p = nps()
                op("pe", lambda e: e.matmul(p[0:64, 0:tl], wkv[:, 0:64], cT[:, 2, t0:t0 + tl], start=True, stop=True), r=[wkv, cT], w=[p])
                op("act", lambda e: e.activation(out=kn[:, t0:t0 + tl], in_=p[0:64, 0:tl], func=AF.Copy), r=[p], w=[kn])
                p = nps()
                ntl = tl // 128
                for tt in range(ntl):
                    op("pe", lambda e: e.matmul(p[:, tt * 64:(tt + 1) * 64], cT[:, 2, t0 + tt * 128:t0 + (tt + 1) * 128], wkv[:, 64:128], start=True, stop=True), r=[wkv, cT], w=[p])
                op("dve", lambda e: e.tensor_copy(out=Vh[:, t0 // 128:t0 // 128 + ntl, 0:64], in_=p[:, 0:ntl * 64].rearrange("p (n d) -> p n d", d=64)), r=[p], w=[Vh])
            qgroups = [g for g in groups() if g[0] >= C]
            if need_ctx:
                qgroups = [(0, C)] + qgroups
            for (t0, tl) in qgroups:
                kts = list(range(C // 128)) if t0 < C else list(range(NT))
                po = PS[4 + (npt % 4)]
                for ci, kt in enumerate(kts):
                    pT = pTs[npt % 4]; npt += 1
                    p = nps()
                    op("pe", lambda e: e.matmul(p[:, 0:tl], kn[:, kt * 128:(kt + 1) * 128], qn[:, t0:t0 + tl], start=True, stop=False), r=[kn, qn], w=[p])
                    op("pe", lambda e: e.matmul(p[:, 0:tl], krT[:, kt * 128:(kt + 1) * 128], qr[:, t0:t0 + tl], start=False, stop=True), r=[krT, qr], w=[p])
                    op("act", lambda e: e.activation(out=pT[:, 0:tl], in_=p[:, 0:tl], func=AF.Exp, scale=scale), r=[p], w=[pT])
                    op("pe", lambda e: e.matmul(po[0:65, 0:tl], Vh[:, kt, :], pT[:, 0:tl], start=(ci == 0), stop=(ci == len(kts) - 1)), r=[Vh, pT], w=[po])
                X = Xs[npt % 2]; r_ = rb[npt % 2]; o_ = oo[npt % 2]
                op("act", lambda e: e.activation(out=X[:, 0:tl], in_=po[0:65, 0:tl], func=AF.Copy), r=[po], w=[X])
                p = nps()
                op("pe", lambda e: e.matmul(p[0:64, 0:tl], sel[:], X[:, 0:tl], start=True, stop=True), r=[sel, X], w=[p])
                op("dve", lambda e: e.reciprocal(out=r_[:, 0:tl], in_=p[0:64, 0:tl]), r=[p], w=[r_])
                op("dve", lambda e: e.tensor_tensor(out=o_[:, 0:tl], in0=X[0:64, 0:tl], in1=r_[:, 0:tl], op=ALU.mult), r=[X, r_], w=[o_])
                dma("pool", OT[hd * 64:(hd + 1) * 64, t0:t0 + tl], o_[:, 0:tl], r=[o_])
        kb.pop()
        return ml_w_o[j * D:(j + 1) * D, :], D, None

    Xsrc = xin
    kcount = {0: 0, 1: 0, 2: 0, -1: 0}
    for i, kind in enumerate(kinds):
        last = (i == L - 1)
        need_ctx = not (last and cfg.last_noctx)
        for ex in range(E):
            r0 = (i * E + ex) * D
            kb.dma_bg("pool", WGU16[ex * D:(ex + 1) * D, :], wgu[r0:r0 + D, :])
            kb.dma_bg("pool", WDN16[ex * FF:(ex + 1) * FF, :], wdn[r0:r0 + FF, :])
        kb.dma_bg("pool", BGU16[:, :], bgu[i * E:(i + 1) * E, :])
        kb.dma_bg("pool", BDN16[:, :], bdn[i * E:(i + 1) * E, :])
        phase_mod(i)
        for b in range(NB):
            phase_h(i, b, Xsrc)
            if kind == -1:
                w_o, Dm, b_o = mixer_identity(i, b)
            elif kind == 0:
                w_o, Dm, b_o = mixer_rg(i, b, kcount[0], need_ctx)
            elif kind == 2:
                psn[0] = 4
                w_o, Dm, b_o = mixer_mla(i, b, kcount[2], need_ctx)
                psn[0] = 8
            elif kind == 1:
                psn[0] = 4
                w_o, Dm, b_o = mixer_gqa(i, b, kcount[1], need_ctx)
                psn[0] = 8
            else:
                raise NotImplementedError
            phase_out(i, b, Xsrc, w_o, Dm, b_o, need_ctx)
        phase_moe(i, last)
        kcount[kind] += 1
        Xsrc = XB
    kb.close()
    return kb


def consts():
    cm = np.zeros((128, 416), np.float32)
    cm[:, 0:128] = np.eye(128, dtype=np.float32)
    k = np.arange(128)
    cm[:, 128:256] = (k[:, None] < k[None, :]).astype(np.float32)
    cm[:, 256:384] = 1.0
    cm[:, 384:416] = np.arange(32, dtype=np.float32)[None, :]
    return cm


def rope_table(S, rot_dim):
    rows = S // 64
    row = np.repeat(np.arange(rows, dtype=np.float32), 64)
    col = np.tile(np.arange(64, dtype=np.float32), rows)
    n_freq = rot_dim // 4
    inv = (np.float32(10000.0) ** (-np.arange(n_freq, dtype=np.float32) / np.float32(n_freq))).astype(np.float32)
    ang = np.concatenate([row[:, None] * inv, col[:, None] * inv], axis=-1).astype(np.float32)
    cos = np.cos(ang).astype(np.float32).T
    sin = np.sin(ang).astype(np.float32).T
    return np.ascontiguousarray(np.concatenate([cos, cos, -sin, sin], axis=0))


def prep_inputs(cfg, inp):
    NB, S, kinds = cfg.NB, cfg.S, cfg.kinds
    L = len(kinds)
    f = lambda a: np.ascontiguousarray(np.asarray(a, dtype=np.float32))
    shared = {
        "ada_w": f(inp["ada_w"]).reshape(L * D, 6 * D),
        "ada_b": f(inp["ada_b"]),
        "lnp": f(np.stack([inp["ln1_g"], inp["ln1_b"], inp["ln2_g"], inp["ln2_b"]], axis=1)).reshape(L * 4, D),
        "router_w": f(inp["router_w"]).reshape(L * D, E),
        "router_b": f(inp["router_b"]),
        "wgu": f(np.concatenate([inp["exp_gu_w"][..., 0::2], inp["exp_gu_w"][..., 1::2]], axis=-1)).reshape(L * E * D, 2 * FF),
        "bgu": f(np.concatenate([inp["exp_gu_b"][..., 0::2], inp["exp_gu_b"][..., 1::2]], axis=-1)).reshape(L * E, 2 * FF),
        "wdn": f(inp["exp_down_w"]).reshape(L * E * FF, D),
        "bdn": f(inp["exp_down_b"]).reshape(L * E, D),
        "cmat": consts(),
    }
    if -1 in kinds:
        shared["w_id"] = np.eye(D, dtype=np.float32)
    if 2 in kinds:
        n = kinds.count(2)
        Wd = inp["mla_w_down"][:n]
        def sw16(a):
            sh = a.shape
            return a.reshape(sh[:-1] + (sh[-1] // 32, 2, 16))[..., ::-1, :].reshape(sh)
        shared["ml_wd"] = f(np.concatenate([Wd, sw16(Wd[..., 384:416])], axis=-1)).reshape(n * D, 448)
        shared["ml_norm"] = f(np.concatenate([inp["mla_q_norm"][:n], inp["mla_kv_norm"][:n]], axis=-1))
        Wq = inp["mla_w_uq"][:n].reshape(n, 256, 16, 96)
        shared["ml_wuq"] = f(np.concatenate([Wq.reshape(n, 256, 1536), sw16(np.ascontiguousarray(Wq[..., 64:96])).reshape(n, 256, 512)], axis=-1)).reshape(n * 256, 2048)
        shared["ml_wukv"] = f(inp["mla_w_ukv"][:n]).reshape(n * 128, 2048)
        shared["ml_w_o"] = f(inp["mla_w_o"][:n]).reshape(n * D, D)
        shared["rope32"] = rope_table(S, 32)
    if 1 in kinds:
        n = kinds.count(1)
        W = inp["gqa_w_qkv"][:n]
        Bq = inp["gqa_b_qkv"][:n]
        def sw(a):
            sh = a.shape
            return a.reshape(sh[:-1] + (sh[-1] // 64, 2, 32))[..., ::-1, :].reshape(sh)
        shared["gq_w"] = f(np.concatenate([W[..., 0:1024], sw(W[..., 0:1024]), W[..., 1024:1152], sw(W[..., 1024:1152]), W[..., 1152:1280]], axis=-1)).reshape(n * D, 2432)
        b20 = Bq.reshape(n, 20, 64)
        shared["gq_b64"] = f(np.concatenate([b20.transpose(0, 2, 1), sw(Bq).reshape(n, 20, 64).transpose(0, 2, 1)], axis=-1)).reshape(n * 64, 40)
        shared["gq_bv"] = f(Bq[:, 1152:1280])
        shared["gq_sinks"] = f(inp["gqa_sinks"][:n])
        shared["gq_w_o"] = f(inp["gqa_w_o"][:n]).reshape(n * D, D)
        shared["gq_b_o"] = f(inp["gqa_b_o"][:n])
        shared["rope64"] = rope_table(S, 64)
    if 0 in kinds:
        n_rg = kinds.count(0)
        shared["rg_w_in"] = f(inp["rg_w_in"][:n_rg]).reshape(n_rg * D, 2 * D_RNN)
        shared["rg_p"] = f(np.concatenate([inp["rg_conv_w"][:n_rg], inp["rg_conv_b"][:n_rg, None, :], inp["rg_gate_a_b"][:n_rg], inp["rg_gate_x_b"][:n_rg],
                                           inp["rg_lambda"][:n_rg]], axis=1)).reshape(n_rg * 11, D_RNN)
        shared["rg_ga"] = f(inp["rg_gate_a_w"][:n_rg]).reshape(n_rg * 2 * 6 * 256, 256)
        shared["rg_gx"] = f(inp["rg_gate_x_w"][:n_rg]).reshape(n_rg * 2 * 6 * 256, 256)
        shared["rg_w_out"] = f(inp["rg_w_out"][:n_rg]).reshape(n_rg * D_RNN, D)
    maps = []
    for c in range(cfg.NC):
        bs = slice(c * NB, (c + 1) * NB)
        m = dict(shared)
        m["xin"] = f(np.concatenate([inp["ctx"][bs], inp["x"][bs]], axis=1)).reshape(NB * (C + S), D)
        m["cc"] = f(np.concatenate([inp["c"][bs], inp["c_ctx"][None, :]], axis=0))
        maps.append(m)
    return maps


def run(cfg, inp):
    kb = build(cfg)
    maps = prep_inputs(cfg, inp)
    res = run_bass_kernel_spmd(kb.nc, maps, core_ids=list(range(cfg.NC)))
    outs = [res.results[c]["out"].reshape(cfg.NB, cfg.S, D) for c in range(cfg.NC)]
    return np.concatenate(outs, axis=0)


def kernel(**inputs):
    cfg = Cfg(NB=2, S=4096, kinds=(0, 1, 2, 0), NC=8)
    return run(cfg, inputs).astype(np.float32)
```

```python
import numpy as np
from contextlib import ExitStack
import concourse.bass as bass
import concourse.mybir as mybir
from concourse.bass_utils import run_bass_kernel_spmd

F32 = mybir.dt.float32
I32 = mybir.dt.int32
U32 = mybir.dt.uint32
BF16 = mybir.dt.bfloat16
AF = mybir.ActivationFunctionType
ALU = mybir.AluOpType
AX = mybir.AxisListType

D = 1024
C = 256
E = 32
FF = 1024
D_RNN = 1536
SB = 512
ALPHA = 8.0 ** 0.25
LN_EPS = 1e-5


class Res:
    __slots__ = ("w", "r")

    def __init__(self):
        self.w = None
        self.r = {}


class T:
    def __init__(self, h):
        self.h = h
        self.res = [Res()]

    def __getitem__(self, k):
        return self.h[k]


class KB:
    def __init__(self, n_dma_sems=48):
        self.nc = bass.Bass("TRN2", target_bir_lowering=False)
        nc = self.nc
        self.es = ExitStack()
        self.stacks = []
        self.eng = {"pe": nc.tensor, "act": nc.scalar, "dve": nc.vector, "pool": nc.gpsimd, "sp": nc.sync}
        self.sems = []
        self.esem = {}
        self.cnt = {}
        self.waited = {e: {} for e in self.eng}
        for e in self.eng:
            self.esem[e] = self._newsem("s_" + e)
            self.cnt[e] = 0
        self.dsem = [self._newsem("d%d" % i) for i in range(n_dma_sems)]
        self.duses = [0] * n_dma_sems
        self.drr = 0
        self.ninst = 0
        self.uid = 0
        self.bgsem = [self._newsem("bg%d" % i) for i in range(8)]
        self.bguses = [0] * 8
        self.bgrr = 0

    def _newsem(self, name):
        h = self.es.enter_context(self.nc.semaphore(name))
        self.sems.append(h)
        return len(self.sems) - 1

    def push(self):
        self.stacks.append(ExitStack())

    def pop(self):
        self.barrier()
        self.stacks.pop().close()

    def _ctx(self):
        return self.stacks[-1] if self.stacks else self.es

    def sb(self, name, shape, dtype=F32):
        self.uid += 1
        return T(self._ctx().enter_context(self.nc.sbuf_tensor("%s_%d" % (name, self.uid), list(shape), dtype)))

    def ps(self, name, shape, dtype=F32):
        return T(self.es.enter_context(self.nc.psum_tensor(name, list(shape), dtype)))

    def dram(self, name, shape, dtype=F32, kind="Internal"):
        return self.nc.dram_tensor(name, list(shape), dtype, kind=kind)

    def _wait(self, e, deps):
        eng = self.eng[e]
        best = {}
        for d in deps:
            if d is None:
                continue
            s, v, src = d
            if src == e and e == "pe":
                continue
            if self.waited[e].get(s, 0) >= v:
                continue
            if best.get(s, 0) < v:
                best[s] = v
        for s, v in best.items():
            eng.wait_ge(self.sems[s], v)
            self.waited[e][s] = v

    def _collect(self, r, w):
        deps = []
        for x in r:
            for rs in x.res:
                deps.append(rs.w)
        for x in w:
            for rs in x.res:
                deps.append(rs.w)
                deps.extend(rs.r.values())
        return deps

    def _commit(self, r, w, dep, key):
        for x in r:
            for rs in x.res:
                rs.r[key] = dep
        for x in w:
            for rs in x.res:
                rs.w = dep
                rs.r = {}

    def op(self, e, fn, r=(), w=()):
        self._wait(e, self._collect(r, w))
        inst = fn(self.eng[e])
        self.cnt[e] += 1
        inst.then_inc(self.sems[self.esem[e]], 1)
        dep = (self.esem[e], self.cnt[e], e)
        self._commit(r, w, dep, e)
        self.ninst += 1
        return inst

    def dma(self, q, out, in_, r=(), w=(), fn=None):
        i = self.drr
        self.drr = (i + 1) % len(self.dsem)
        s = self.dsem[i]
        deps = self._collect(r, w)
        deps.append((s, self.duses[i] * 16, "dma"))
        self._wait(q, deps)
        eng = self.eng[q]
        if fn is None:
            inst = eng.dma_start(out=out, in_=in_)
        else:
            inst = fn(eng)
        inst.then_inc(self.sems[s], 16)
        self.duses[i] += 1
        dep = (s, self.duses[i] * 16, "dma")
        self._commit(r, w, dep, ("dma", s))
        self.ninst += 1
        return inst

    def dma_bg(self, q, out, in_):
        i = self.bgrr
        self.bgrr = (i + 1) % len(self.bgsem)
        s = self.bgsem[i]
        self._wait(q, [(s, self.bguses[i] * 16, "dma")])
        self.eng[q].dma_start(out=out, in_=in_).then_inc(self.sems[s], 16)
        self.bguses[i] += 1
        self.ninst += 1

    def wait_bg(self):
        deps = [(s, self.bguses[i] * 16, "dma") for i, s in enumerate(self.bgsem)]
        for e in self.eng:
            self._wait(e, deps)

    def barrier(self):
        deps = [(self.esem[e], self.cnt[e], "x") for e in self.eng]
        deps += [(s, self.duses[i] * 16, "x") for i, s in enumerate(self.dsem)]
        for e in self.eng:
            self._wait(e, [d for d in deps if d[0] != self.esem[e]])

    def close(self):
        self.barrier()
        self.es.close()


class Cfg:
    def __init__(self, NB=2, S=4096, kinds=(0, 1, 2, 0), NC=8, last_noctx=True, debug=False):
        self.debug = debug
        self.NB = NB
        self.S = S
        self.kinds = list(kinds)
        self.NC = NC
        self.last_noctx = last_noctx


def build(cfg):
    NB, S, kinds = cfg.NB, cfg.S, cfg.kinds
    L = len(kinds)
    Tn = C + S
    NT = Tn // 128
    NS = NB + 1
    kb = KB()
    nc = kb.nc
    op, dma = kb.op, kb.dma

    def din(name, shape, dt=F32):
        return kb.dram(name, shape, dt, kind="ExternalInput")

    xin = din("xin", [NB * Tn, D])
    cc = din("cc", [NS, D])
    ada_w = din("ada_w", [L * D, 6 * D])
    ada_b = din("ada_b", [L, 6 * D])
    lnp = din("lnp", [L * 4, D])
    router_w = din("router_w", [L * D, E])
    router_b = din("router_b", [L, E])
    wgu = din("wgu", [L * E * D, 2 * FF])
    bgu = din("bgu", [L * E, 2 * FF])
    wdn = din("wdn", [L * E * FF, D])
    bdn = din("bdn", [L * E, D])
    cmat = din("cmat", [128, 384 + 32])
    w_id = din("w_id", [D, D]) if -1 in kinds else None
    n_rg = max(1, kinds.count(0))
    if 2 in kinds:
        n_ml = kinds.count(2)
        ml_wd = din("ml_wd", [n_ml * D, 448])
        ml_norm = din("ml_norm", [n_ml, 384])
        ml_wuq = din("ml_wuq", [n_ml * 256, 2048])
        ml_wukv = din("ml_wukv", [n_ml * 128, 2048])
        ml_w_o = din("ml_w_o", [n_ml * D, D])
        rope32 = din("rope32", [64, S])
        CT = kb.dram("CT", [384, Tn])
        KRT = kb.dram("KRT", [32, Tn])
    if 1 in kinds:
        n_gq = kinds.count(1)
        gq_w = din("gq_w", [n_gq * D, 2432])
        gq_b64 = din("gq_b64", [n_gq * 64, 40])
        gq_bv = din("gq_bv", [n_gq, 128])
        gq_sinks = din("gq_sinks", [n_gq, 16])
        gq_w_o = din("gq_w_o", [n_gq * D, D])
        gq_b_o = din("gq_b_o", [n_gq, D])
        rope64 = din("rope64", [2 * 64, S])
        QT = kb.dram("QT", [64, 16 * Tn])
        KT = kb.dram("KT", [64, 2 * Tn])
        VA = kb.dram("VA", [Tn, 130])
    if 0 in kinds:
        rg_w_in = din("rg_w_in", [n_rg * D, 2 * D_RNN])
        rg_p = din("rg_p", [n_rg * 11, D_RNN])
        rg_ga = din("rg_ga", [n_rg * 2 * 6 * 256, 256])
        rg_gx = din("rg_gx", [n_rg * 2 * 6 * 256, 256])
        rg_w_out = din("rg_w_out", [n_rg * D_RNN, D])
        PR = kb.dram("PR", [2 * D_RNN, Tn])
    out = kb.dram("out", [NB * S, D], F32, kind="ExternalOutput")

    dk = "ExternalOutput" if cfg.debug else "Internal"
    XA = kb.dram("XA", [NB * Tn, D], F32, dk)
    XB = kb.dram("XB", [NB * Tn, D], F32, dk)
    H2 = kb.dram("H2", [NB * Tn, D], F32, dk)
    HT = kb.dram("HT", [D, Tn], BF16, dk)
    OT = kb.dram("OT", [D_RNN, Tn], BF16, dk)
    MODD = kb.dram("MODD", [NS, 6 * D], F32, dk)
    n_asg_max = NB * Tn * 4
    NBLK = (n_asg_max + E * (SB - 1) + SB - 1) // SB
    NSLOT = NBLK * SB
    WGU16 = kb.dram("WGU16", [E * 128, 8 * 2 * FF], BF16)
    WDN16 = kb.dram("WDN16", [E * 128, 8 * D], BF16)
    BB16 = kb.dram("BB16", [E, 3 * D], BF16)
    XS = kb.dram("XS", [NSLOT, D], F32, dk)
    YS = kb.dram("YS", [NSLOT, D], F32, dk)
    if cfg.debug:
        DBG = {n: kb.dram("dbg_" + n, shp, dt, "ExternalOutput") for n, shp, dt in [
            ("slot4", [128, NB * NT * 4], I32), ("gate4", [128, NB * NT * 4], F32), ("top8", [128, NB * NT * 8], F32),
            ("bef", [128, NBLK], F32), ("runcnt", [128, E], F32), ("excl", [128, E], F32), ("padf", [128, E], F32), ("incl", [128, E], F32), ("posA", [128, NB * NT * E], F32)]}

    cm = kb.sb("cm", [128, 416])
    ident = lambda n=128: cm[0:n, 0:n]
    tri = cm[:, 128:256]
    ones = cm[:, 256:384]
    iota32 = cm[:, 384:416]
    dma("sp", cm[:], cmat[:, :], w=[cm])
    PS = [kb.ps("ps%d" % i, [128, 512]) for i in range(8)]
    psi = [0]
    psn = [8]

    def nps():
        psi[0] = (psi[0] + 1) % psn[0]
        return PS[psi[0]]

    siluT = kb.sb("siluT", [128, 8, NS])
    modT = kb.sb("modT", [128, 48, NS])
    lnrow = kb.sb("lnrow", [128, 4, D])
    epsT = kb.sb("epsT", [128, 1])
    op("dve", lambda e: e.memset(epsT[:], LN_EPS), w=[epsT])

    kb.push()
    ccs = kb.sb("ccs", [NS, D])
    dma("sp", ccs[:], cc[:, :], w=[ccs])
    op("act", lambda e: e.activation(out=ccs[:], in_=ccs[:], func=AF.Silu), r=[ccs], w=[ccs])
    p = nps()
    for c in range(8):
        op("pe", lambda e: e.transpose(p[:, c * NS:(c + 1) * NS], ccs[:, c * 128:(c + 1) * 128], ident(NS)), r=[ccs, cm], w=[p])
    op("dve", lambda e: e.tensor_copy(out=siluT[:].rearrange("p c s -> p (c s)"), in_=p[:, 0:8 * NS]), r=[p], w=[siluT])
    kb.pop()

    def phase_mod(i):
        kb.push()
        aw = [kb.sb("aw", [128, 8, 512]) for _ in range(2)]
        modtm = kb.sb("modtm", [NS, 6 * D])
        adab = kb.sb("adab", [NS, 6 * D])
        for s in range(NS):
            dma("sp", adab[s:s + 1, :], ada_b[i:i + 1, :], w=[adab])
        for g in range(12):
            a = aw[g % 2]
            dma("sp", a[:], ada_w[i * D:(i + 1) * D, g * 512:(g + 1) * 512].rearrange("(c p) n -> p c n", p=128), w=[a])
            p = nps()
            for kc in range(8):
                op("pe", lambda e: e.matmul(p[0:NS, :], siluT[:, kc, :], a[:, kc, :], start=(kc == 0), stop=(kc == 7)),
                   r=[siluT, a], w=[p])
            op("dve", lambda e: e.tensor_tensor(out=modtm[:, g * 512:(g + 1) * 512], in0=p[0:NS, :], in1=adab[:, g * 512:(g + 1) * 512], op=ALU.add),
               r=[p, adab], w=[modtm])
        for k in (1, 4):
            op("dve", lambda e: e.tensor_scalar(out=modtm[:, k * D:(k + 1) * D], in0=modtm[:, k * D:(k + 1) * D], scalar1=1.0, scalar2=None, op0=ALU.add),
               r=[modtm], w=[modtm])
        dma("sp", MODD[:, :], modtm[:], r=[modtm])
        p = nps()
        for j in range(48):
            op("pe", lambda e: e.transpose(p[:, j * NS:(j + 1) * NS], modtm[:, j * 128:(j + 1) * 128], ident(NS)), r=[modtm, cm], w=[p])
        op("dve", lambda e: e.tensor_copy(out=modT[:].rearrange("p c s -> p (c s)"), in_=p[:, 0:48 * NS]), r=[p], w=[modT])
        for k in range(4):
            dma("sp", lnrow[:, k, :], lnp[i * 4 + k, :].partition_broadcast(128), w=[lnrow])
        kb.pop()

    def groups():
        gs = [(0, C)]
        t = C
        while t < Tn:
            gs.append((t, min(512, Tn - t)))
            t += 512
        return gs

    def phase_h(i, b, Xsrc):
        kb.push()
        xt = [kb.sb("xt", [128, D]) for _ in range(8)]
        hT = [kb.sb("hT", [128, 8, 512], BF16) for _ in range(2)]
        n = 0
        for gi, (t0, tl) in enumerate(groups()):
            s = NB if t0 < C else b
            nt = tl // 128
            tiles = []
            for j in range(nt):
                x = xt[n % 8]
                n += 1
                dma("sp", x[:], Xsrc[b * Tn + t0 + j * 128: b * Tn + t0 + (j + 1) * 128, :], w=[x])
                tiles.append(x)
            h = hT[gi % 2]
            for c in range(8):
                p = nps()
                for j in range(nt):
                    x = tiles[j]
                    op("pe", lambda e: e.transpose(p[:, j * 128:(j + 1) * 128], x[:, c * 128:(c + 1) * 128], ident()), r=[x, cm], w=[p])
                op("dve", lambda e: e.tensor_scalar(out=h[:, c, 0:tl], in0=p[:, 0:tl], scalar1=modT[:, 8 + c, s:s + 1], scalar2=modT[:, c, s:s + 1],
                                                    op0=ALU.mult, op1=ALU.add), r=[p, modT], w=[h])
            dma("pool", HT[:, t0:t0 + tl].rearrange("(c p) t -> p c t", p=128), h[:, :, 0:tl], r=[h])
        kb.pop()

    def layernorm(t, k0, st, mv, tmp):
        for h2 in range(2):
            op("dve", lambda e: e.bn_stats(out=st[:, h2, :], in_=t[:, h2 * 512:(h2 + 1) * 512]), r=[t], w=[st])
        op("dve", lambda e: e.bn_aggr(out=mv[:, 0:2], in_=st[:].rearrange("p a b -> p (a b)")), r=[st], w=[mv])
        op("act", lambda e: e.activation(out=mv[:, 2:3], in_=mv[:, 1:2], func=AF.Sqrt, bias=epsT[:, 0:1], scale=1.0), r=[mv, epsT], w=[mv])
        op("dve", lambda e: e.reciprocal(out=mv[:, 3:4], in_=mv[:, 2:3]), r=[mv], w=[mv])
        op("dve", lambda e: e.tensor_scalar(out=t[:], in0=t[:], scalar1=mv[:, 0:1], scalar2=mv[:, 3:4], op0=ALU.subtract, op1=ALU.mult), r=[t, mv], w=[t])
        op("dve", lambda e: e.tensor_tensor(out=t[:], in0=t[:], in1=lnrow[:, k0, :], op=ALU.mult), r=[t, lnrow], w=[t])
        op("dve", lambda e: e.tensor_tensor(out=t[:], in0=t[:], in1=lnrow[:, k0 + 1, :], op=ALU.add), r=[t, lnrow], w=[t])

    NTT = NB * NT
    maskA = kb.sb("maskA", [128, NTT, E])
    top8 = kb.sb("top8", [128, NTT, 8])
    idx8 = kb.sb("idx8", [128, NTT, 8], U32)
    posA = kb.sb("posA", [128, NTT, E])
    runcnt = kb.sb("runcnt", [128, E])
    slot4f = kb.sb("slot4f", [128, NTT, 4])
    slot4 = kb.sb("slot4", [128, NTT, 4], I32)
    gate4 = kb.sb("gate4", [128, NTT, 4])

    def phase_out(i, b, Xsrc, w_o, Dm, b_o, need_ctx):
        kb.push()
        KC = Dm // 128
        wo = kb.sb("wo", [128, KC, D], BF16)
        for kc_ in range(KC):
            dma("pool", wo[:, kc_, :], w_o[kc_ * 128:(kc_ + 1) * 128, :], w=[wo])
        rw = kb.sb("rw", [128, 8, E])
        dma("sp", rw[:], router_w[i * D:(i + 1) * D, :].rearrange("(c p) n -> p c n", p=128), w=[rw])
        rbrow = kb.sb("rbrow", [128, E])
        dma("sp", rbrow[:], router_b[i, :].partition_broadcast(128), w=[rbrow])
        grow = {}
        for s in (NB, b):
            g = kb.sb("grow", [128, 3, D])
            for k, kind in enumerate((2, 4, 3)):
                dma("sp", g[:, k, :], MODD[s, kind * D:(kind + 1) * D].partition_broadcast(128), w=[g])
            grow[s] = g
        borow = None
        if b_o is not None:
            borow = kb.sb("borow", [128, D])
            dma("sp", borow[:], b_o.partition_broadcast(128), w=[borow])
        oTs = [kb.sb("oT", [128, KC, 128], BF16) for _ in range(2)]
        xts = [kb.sb("xt", [128, D]) for _ in range(2)]
        ts = [kb.sb("tt", [128, D]) for _ in range(2)]
        h2s = [kb.sb("h2", [128, D]) for _ in range(2)]
        h2Ts = [kb.sb("h2T", [128, 8, 128]) for _ in range(2)]
        st = kb.sb("st", [128, 2, 6])
        mv = kb.sb("mv", [128, 4])
        lg = kb.sb("lg", [128, E])
        t_start = 0 if need_ctx else C // 128
        for ti in range(t_start, NT):
            s = NB if ti < C // 128 else b
            g = grow[s]
            r0 = b * Tn + ti * 128
            gi = b * NT + ti
            oT = oTs[ti % 2]; x = xts[ti % 2]; t = ts[ti % 2]; h2 = h2s[ti % 2]; h2T = h2Ts[ti % 2]
            dma("sp", oT[:], OT[0:Dm, ti * 128:(ti + 1) * 128].rearrange("(c p) t -> p c t", p=128), w=[oT])
            dma("sp", x[:], Xsrc[r0:r0 + 128, :], w=[x])
            for hf in range(2):
                p = nps()
                for kc in range(KC):
                    op("pe", lambda e: e.matmul(p[:, :], oT[:, kc, :], wo[:, kc, hf * 512:(hf + 1) * 512], start=(kc == 0), stop=(kc == KC - 1)),
                       r=[oT, wo], w=[p])
                sl = slice(hf * 512, (hf + 1) * 512)
                if borow is not None:
                    op("dve", lambda e: e.tensor_tensor(out=t[:, sl], in0=p[:, :], in1=borow[:, sl], op=ALU.add), r=[p, borow], w=[t])
                    op("dve", lambda e: e.tensor_tensor(out=t[:, sl], in0=t[:, sl], in1=g[:, 0, sl], op=ALU.mult), r=[t, g], w=[t])
                else:
                    op("dve", lambda e: e.tensor_tensor(out=t[:, sl], in0=p[:, :], in1=g[:, 0, sl], op=ALU.mult), r=[p, g], w=[t])
            op("dve", lambda e: e.scalar_tensor_tensor(out=t[:], in0=x[:], scalar=ALPHA, in1=t[:], op0=ALU.mult, op1=ALU.add), r=[x, t], w=[t])
            layernorm(t, 0, st, mv, None)
            dma("pool", XA[r0:r0 + 128, :], t[:], r=[t])
            op("pool", lambda e: e.tensor_tensor(out=h2[:], in0=t[:], in1=g[:, 1, :], op=ALU.mult), r=[t, g], w=[h2])
            op("pool", lambda e: e.tensor_tensor(out=h2[:], in0=h2[:], in1=g[:, 2, :], op=ALU.add), r=[h2, g], w=[h2])
            dma("pool", H2[r0:r0 + 128, :], h2[:], r=[h2])
            for half in range(2):
                p = nps()
                for c4 in range(4):
                    c = half * 4 + c4
                    op("pe", lambda e: e.transpose(p[:, c4 * 128:(c4 + 1) * 128], h2[:, c * 128:(c + 1) * 128], ident()), r=[h2, cm], w=[p])
                op("act", lambda e: e.activation(out=h2T[:, half * 4:(half + 1) * 4, :].rearrange("p c t -> p (c t)"), in_=p[:, :], func=AF.Copy),
                   r=[p], w=[h2T])
            p = nps()
            for c in range(8):
                op("pe", lambda e: e.matmul(p[:, 0:E], h2T[:, c, :], rw[:, c, :], start=(c == 0), stop=(c == 7)), r=[h2T, rw], w=[p])
            op("dve", lambda e: e.tensor_tensor(out=lg[:], in0=p[:, 0:E], in1=rbrow[:], op=ALU.add), r=[p, rbrow], w=[lg])
            op("dve", lambda e: e.max(out=top8[:, gi, :], in_=lg[:]), r=[lg], w=[top8])
            op("dve", lambda e: e.max_index(out=idx8[:, gi, :], in_max=top8[:, gi, :], in_values=lg[:]), r=[lg, top8], w=[idx8])
            op("dve", lambda e: e.tensor_scalar(out=maskA[:, gi, :], in0=lg[:], scalar1=top8[:, gi, 3:4], scalar2=None, op0=ALU.is_ge), r=[lg, top8], w=[maskA])
        kb.pop()

    def phase_moe(i, last):
        tiles = []
        for b in range(NB):
            for ti in range(NT):
                if last and cfg.last_noctx and ti < C // 128:
                    continue
                tiles.append((b, ti))
        kb.push()
        op("dve", lambda e: e.memset(runcnt[:], 0.0), w=[runcnt])
        for (b, ti) in tiles:
            gi = b * NT + ti
            p = nps()
            op("pe", lambda e: e.matmul(p[:, 0:E], tri, maskA[:, gi, :], start=True, stop=True), r=[cm, maskA], w=[p])
            op("pe", lambda e: e.matmul(p[:, E:2 * E], ones, maskA[:, gi, :], start=True, stop=True), r=[cm, maskA], w=[p])
            op("dve", lambda e: e.tensor_tensor(out=posA[:, gi, :], in0=p[:, 0:E], in1=runcnt[:], op=ALU.add), r=[p, runcnt], w=[posA])
            op("dve", lambda e: e.tensor_tensor(out=runcnt[:], in0=p[:, E:2 * E], in1=runcnt[:], op=ALU.add), r=[p, runcnt], w=[runcnt])
        padf = kb.sb("padf", [128, E])
        incl = kb.sb("incl", [128, E])
        excl = kb.sb("excl", [128, E])
        zer = kb.sb("zer", [128, E])
        op("dve", lambda e: e.memset(zer[:], 0.0), w=[zer])
        blkpos = kb.sb("blkpos", [128, NBLK])
        tmpb = kb.sb("tmpb", [128, NBLK])
        op("pool", lambda e: e.iota(blkpos[:], pattern=[[SB, NBLK]], base=0, channel_multiplier=0, allow_small_or_imprecise_dtypes=True), w=[blkpos])
        for ex in range(E):
            op("dve", lambda e: e.tensor_scalar(out=tmpb[:], in0=blkpos[:], scalar1=runcnt[:, ex:ex + 1], scalar2=None, op0=ALU.is_lt, op1=ALU.add,
                                                accum_out=padf[:, ex:ex + 1]), r=[blkpos, runcnt], w=[tmpb, padf])
        op("dve", lambda e: e.tensor_scalar(out=padf[:], in0=padf[:], scalar1=float(SB), scalar2=None, op0=ALU.mult), r=[padf], w=[padf])
        op("dve", lambda e: e.tensor_tensor_scan(out=incl[:], data0=padf[:], data1=zer[:], initial=0.0, op0=ALU.add, op1=ALU.add), r=[padf, zer], w=[incl])
        op("dve", lambda e: e.tensor_tensor(out=excl[:], in0=incl[:], in1=padf[:], op=ALU.subtract), r=[incl, padf], w=[excl])
        bef = kb.sb("bef", [128, NBLK])
        op("dve", lambda e: e.memset(bef[:], 0.0), w=[bef])
        for ex in range(E):
            op("dve", lambda e: e.tensor_scalar(out=tmpb[:], in0=blkpos[:], scalar1=incl[:, ex:ex + 1], scalar2=None, op0=ALU.is_ge), r=[blkpos, incl], w=[tmpb])
            op("dve", lambda e: e.tensor_tensor(out=bef[:], in0=bef[:], in1=tmpb[:], op=ALU.add), r=[bef, tmpb], w=[bef])
        op("dve", lambda e: e.tensor_scalar(out=bef[:], in0=bef[:], scalar1=float(E - 1), scalar2=None, op0=ALU.min), r=[bef], w=[bef])
        idxf = kb.sb("idxf", [128, 4])
        oh = kb.sb("oh", [128, 4, E])
        slt = kb.sb("slt", [128, E])
        den = kb.sb("den", [128, 1])
        nb0 = kb.sb("nb0", [128, 1])
        for (b, ti) in tiles:
            gi = b * NT + ti
            op("dve", lambda e: e.tensor_copy(out=idxf[:], in_=idx8[:, gi, 0:4]), r=[idx8], w=[idxf])
            op("dve", lambda e: e.tensor_tensor(out=slt[:], in0=posA[:, gi, :], in1=excl[:], op=ALU.add), r=[posA, excl], w=[slt])
            for k in range(4):
                op("dve", lambda e: e.tensor_scalar(out=oh[:, k, :], in0=iota32, scalar1=idxf[:, k:k + 1], scalar2=None, op0=ALU.is_equal), r=[cm, idxf], w=[oh])
                op("dve", lambda e: e.tensor_tensor(out=oh[:, k, :], in0=oh[:, k, :], in1=slt[:], op=ALU.mult), r=[oh, slt], w=[oh])
            op("dve", lambda e: e.reduce_sum(out=slot4f[:, gi, :], in_=oh[:], axis=AX.X), r=[oh], w=[slot4f])
            op("dve", lambda e: e.tensor_copy(out=slot4[:, gi, :], in_=slot4f[:, gi, :]), r=[slot4f], w=[slot4])
            op("dve", lambda e: e.tensor_scalar(out=nb0[:], in0=top8[:, gi, 0:1], scalar1=-1.0, scalar2=None, op0=ALU.mult), r=[top8], w=[nb0])
            op("act", lambda e: e.activation(out=gate4[:, gi, :], in_=top8[:, gi, 0:4], func=AF.Exp, bias=nb0[:, 0:1], scale=1.0), r=[top8, nb0], w=[gate4])
            op("dve", lambda e: e.reduce_sum(out=den[:], in_=gate4[:, gi, :], axis=AX.X), r=[gate4], w=[den])
            op("dve", lambda e: e.reciprocal(out=den[:], in_=den[:]), r=[den], w=[den])
            op("dve", lambda e: e.tensor_scalar(out=gate4[:, gi, :], in0=gate4[:, gi, :], scalar1=den[:, 0:1], scalar2=None, op0=ALU.mult), r=[gate4, den], w=[gate4])
        if cfg.debug:
            dma("sp", DBG["slot4"][:, :], slot4[:].rearrange("p a b -> p (a b)"), r=[slot4])
            dma("sp", DBG["gate4"][:, :], gate4[:].rearrange("p a b -> p (a b)"), r=[gate4])
            dma("sp", DBG["top8"][:, :], top8[:].rearrange("p a b -> p (a b)"), r=[top8])
            dma("sp", DBG["posA"][:, :], posA[:].rearrange("p a b -> p (a b)"), r=[posA])
            dma("sp", DBG["bef"][:, :], bef[:], r=[bef])
            dma("sp", DBG["runcnt"][:, :], runcnt[:], r=[runcnt])
            dma("sp", DBG["excl"][:, :], excl[:], r=[excl])
            dma("sp", DBG["padf"][:, :], padf[:], r=[padf])
            dma("sp", DBG["incl"][:, :], incl[:], r=[incl])
        kb.push()
        h2s = [kb.sb("h2d", [128, D]) for _ in range(3)]
        for n, (b, ti) in enumerate(tiles):
            gi = b * NT + ti
            r0 = b * Tn + ti * 128
            h2 = h2s[n % 3]
            dma("sp", h2[:], H2[r0:r0 + 128, :], w=[h2])
            for k in range(4):
                dma("pool", None, None, r=[h2, slot4], fn=lambda e: e.indirect_dma_start(
                    out=XS[:, :], out_offset=bass.IndirectOffsetOnAxis(ap=slot4[:, gi, k:k + 1], axis=0), in_=h2[:], in_offset=None))
        kb.pop()
        kb.push()
        wg = [kb.sb("wg", [128, 8, 2 * FF], BF16) for _ in range(2)]
        wd = [kb.sb("wd", [128, 8, D], BF16) for _ in range(2)]
        brow = [kb.sb("brow", [128, 3 * D], BF16) for _ in range(2)]
        onesb = kb.sb("onesb", [1, SB], BF16)
        op("dve", lambda e: e.memset(onesb[:], 1.0), w=[onesb])
        iotap = kb.sb("iotap", [128, 8])
        op("pool", lambda e: e.iota(iotap[:], pattern=[[128, 8]], base=0, channel_multiplier=1, allow_small_or_imprecise_dtypes=True), w=[iotap])
        widx_f = kb.sb("widxf", [128, 9])
        widx = [kb.sb("widx", [128, 9], I32) for _ in range(3)]
        xs = [kb.sb("xs", [128, D]) for _ in range(2)]
        xT = kb.sb("xTm", [128, 8, SB], BF16)
        aT = kb.sb("aTm", [128, 8, SB], BF16)
        gsb = [kb.sb("gsb", [128, SB]) for _ in range(2)]
        sgb = [kb.sb("sgb", [128, SB]) for _ in range(2)]
        usb = [kb.sb("usb", [128, SB]) for _ in range(2)]
        ysb = [kb.sb("ysb", [128, D]) for _ in range(2)]
        be_l = kb.sb("be_l", [128, NBLK])
        op("dve", lambda e: e.tensor_copy(out=be_l[:], in_=bef[:]), r=[bef], w=[be_l])
        kb.wait_bg()

        def load_w(blk):
            wi = widx[blk % 3]
            op("dve", lambda e: e.scalar_tensor_tensor(out=widx_f[:, 0:1], in0=be_l[:, blk:blk + 1], scalar=128.0, in1=iotap[:, 0:1], op0=ALU.mult, op1=ALU.add),
               r=[be_l, iotap], w=[widx_f])
            op("dve", lambda e: e.tensor_copy(out=widx_f[:, 1:2], in_=be_l[:, blk:blk + 1]), r=[be_l], w=[widx_f])
            op("dve", lambda e: e.tensor_copy(out=wi[:, 0:2], in_=widx_f[:, 0:2]), r=[widx_f], w=[wi])
            w1 = wg[blk % 2]; w2 = wd[blk % 2]; br = brow[blk % 2]
            dma("pool", None, None, r=[wi], w=[w1], fn=lambda e: e.indirect_dma_start(
                out=w1[:].rearrange("p c n -> p (c n)"), out_offset=None, in_=WGU16[:, :], in_offset=bass.IndirectOffsetOnAxis(ap=wi[:, 0:1], axis=0)))
            dma("pool", None, None, r=[wi], w=[w2], fn=lambda e: e.indirect_dma_start(
                out=w2[:].rearrange("p c n -> p (c n)"), out_offset=None, in_=WDN16[:, :], in_offset=bass.IndirectOffsetOnAxis(ap=wi[:, 0:1], axis=0)))
            dma("pool", None, None, r=[wi], w=[br], fn=lambda e: e.indirect_dma_start(
                out=br[:, :], out_offset=None, in_=BB16[:, :], in_offset=bass.IndirectOffsetOnAxis(ap=wi[:, 1:2], axis=0)))

        load_w(0)
        nxs = 0
        for blk in range(NBLK):
            if blk + 1 < NBLK:
                load_w(blk + 1)
            w1 = wg[blk % 2]; w2 = wd[blk % 2]; br = brow[blk % 2]
            for j in range(SB // 128):
                x = xs[nxs % 2]; nxs += 1
                dma("sp", x[:], XS[blk * SB + j * 128: blk * SB + (j + 1) * 128, :], w=[x])
                for half in range(2):
                    p = nps()
                    for c4 in range(4):
                        c = half * 4 + c4
                        op("pe", lambda e: e.transpose(p[:, c4 * 128:(c4 + 1) * 128], x[:, c * 128:(c + 1) * 128], ident()), r=[x, cm], w=[p])
                    op("act", lambda e: e.activation(out=xT[:, half * 4:(half + 1) * 4, j * 128:(j + 1) * 128], in_=p[:, :].rearrange("p (c t) -> p c t", c=4), func=AF.Copy),
                       r=[p], w=[xT])
            for fc in range(8):
                g_ = gsb[fc % 2]; s_ = sgb[fc % 2]; u_ = usb[fc % 2]
                pg = nps()
                for kc in range(8):
                    op("pe", lambda e: e.matmul(pg[:, :], w1[:, kc, fc * 128:(fc + 1) * 128], xT[:, kc, :], start=(kc == 0), stop=False), r=[w1, xT], w=[pg])
                op("pe", lambda e: e.matmul(pg[:, :], br[0:1, fc * 128:(fc + 1) * 128], onesb[0:1, :], start=False, stop=True), r=[br, onesb], w=[pg])
                pu = nps()
                for kc in range(8):
                    op("pe", lambda e: e.matmul(pu[:, :], w1[:, kc, FF + fc * 128:FF + (fc + 1) * 128], xT[:, kc, :], start=(kc == 0), stop=False), r=[w1, xT], w=[pu])
                op("pe", lambda e: e.matmul(pu[:, :], br[0:1, FF + fc * 128:FF + (fc + 1) * 128], onesb[0:1, :], start=False, stop=True), r=[br, onesb], w=[pu])
                op("dve", lambda e: e.tensor_scalar(out=g_[:], in0=pg[:, :], scalar1=7.0, scalar2=None, op0=ALU.min), r=[pg], w=[g_])
                op("act", lambda e: e.activation(out=s_[:], in_=g_[:], func=AF.Sigmoid, scale=1.702), r=[g_], w=[s_])
                op("dve", lambda e: e.tensor_scalar(out=u_[:], in0=pu[:, :], scalar1=7.0, scalar2=-7.0, op0=ALU.min, op1=ALU.max), r=[pu], w=[u_])
                op("dve", lambda e: e.scalar_tensor_tensor(out=u_[:], in0=u_[:], scalar=1.0, in1=g_[:], op0=ALU.add, op1=ALU.mult), r=[u_, g_], w=[u_])
                op("pool", lambda e: e.tensor_tensor(out=aT[:, fc, :], in0=u_[:], in1=s_[:], op=ALU.mult), r=[u_, s_], w=[aT])
            for j in range(SB // 128):
                y = ysb[j % 2]
                for hf in range(2):
                    p = nps()
                    for fc in range(8):
                        op("pe", lambda e: e.matmul(p[:, :], aT[:, fc, j * 128:(j + 1) * 128], w2[:, fc, hf * 512:(hf + 1) * 512], start=(fc == 0), stop=False), r=[aT, w2], w=[p])
                    op("pe", lambda e: e.matmul(p[:, :], onesb[0:1, 0:128], br[0:1, 2 * FF + hf * 512:2 * FF + (hf + 1) * 512], start=False, stop=True), r=[br, onesb], w=[p])
                    op("act", lambda e: e.activation(out=y[:, hf * 512:(hf + 1) * 512], in_=p[:, :], func=AF.Copy), r=[p], w=[y])
                dma("sp", YS[blk * SB + j * 128: blk * SB + (j + 1) * 128, :], y[:], r=[y])
        kb.pop()
        kb.push()
        grow = {}
        for s in range(NS):
            g = kb.sb("g2row", [128, D])
            dma("sp", g[:], MODD[s, 5 * D:6 * D].partition_broadcast(128), w=[g])
            grow[s] = g
        yk = [kb.sb("yk", [128, D]) for _ in range(4)]
        accs = [kb.sb("acc", [128, D]) for _ in range(2)]
        xts = [kb.sb("xt2", [128, D]) for _ in range(2)]
        st = kb.sb("st2", [128, 2, 6])
        mv = kb.sb("mv2", [128, 4])
        for n, (b, ti) in enumerate(tiles):
            gi = b * NT + ti
            r0 = b * Tn + ti * 128
            s = NB if ti < C // 128 else b
            acc = accs[n % 2]; x = xts[n % 2]
            dma("sp", x[:], XA[r0:r0 + 128, :], w=[x])
            for k in range(4):
                y = yk[k]
                dma("pool", None, None, r=[slot4], w=[y], fn=lambda e: e.indirect_dma_start(
                    out=y[:], out_offset=None, in_=YS[:, :], in_offset=bass.IndirectOffsetOnAxis(ap=slot4[:, gi, k:k + 1], axis=0)))
                if k == 0:
                    op("dve", lambda e: e.tensor_scalar(out=acc[:], in0=y[:], scalar1=gate4[:, gi, 0:1], scalar2=None, op0=ALU.mult), r=[y, gate4], w=[acc])
                else:
                    op("dve", lambda e: e.scalar_tensor_tensor(out=acc[:], in0=y[:], scalar=gate4[:, gi, k:k + 1], in1=acc[:], op0=ALU.mult, op1=ALU.add), r=[y, gate4, acc], w=[acc])
            op("dve", lambda e: e.tensor_tensor(out=acc[:], in0=acc[:], in1=grow[s][:], op=ALU.mult), r=[acc, grow[s]], w=[acc])
            op("dve", lambda e: e.scalar_tensor_tensor(out=acc[:], in0=x[:], scalar=ALPHA, in1=acc[:], op0=ALU.mult, op1=ALU.add), r=[x, acc], w=[acc])
            layernorm(acc, 2, st, mv, None)
            if last:
                if ti >= C // 128:
                    ro = b * S + (ti - C // 128) * 128
                    dma("sp", out[ro:ro + 128, :], acc[:], r=[acc])
            else:
                dma("sp", XB[r0:r0 + 128, :], acc[:], r=[acc])
        kb.pop()
        kb.pop()

    onesrow_t = kb.sb("onesrow", [1, SB])
    onesrow = onesrow_t
    op("dve", lambda e: e.memset(onesrow_t[:], 1.0), w=[onesrow_t])

    def mixer_identity(i, b):
        kb.push()
        buf = [kb.sb("cp", [128, 8, 512], BF16) for _ in range(2)]
        for gi, (t0, tl) in enumerate(groups()):
            bb = buf[gi % 2]
            dma("sp", bb[:, :, 0:tl], HT[:, t0:t0 + tl].rearrange("(c p) t -> p c t", p=128), w=[bb])
            dma("sp", OT[0:D, t0:t0 + tl].rearrange("(c p) t -> p c t", p=128), bb[:, :, 0:tl], r=[bb])
        kb.pop()
        return w_id[:, :], D, None

    def mixer_rg(i, b, j, need_ctx):
        kb.push()
        win = kb.sb("win", [128, 8, 2 * D_RNN], BF16)
        for kc_ in range(8):
            dma("pool", win[:, kc_, :], rg_w_in[j * D + kc_ * 128:j * D + (kc_ + 1) * 128, :], w=[win])
        hTs = [kb.sb("hTr", [128, 8, 512], BF16) for _ in range(2)]
        pos_ = [kb.sb("pout", [128, 4, 512]) for _ in range(2)]
        n = 0
        for gi, (t0, tl) in enumerate(groups()):
            h = hTs[gi % 2]
            dma("sp", h[:, :, 0:tl], HT[:, t0:t0 + tl].rearrange("(c p) t -> p c t", p=128), w=[h])
            for q in range(6):
                po = pos_[n % 2]; n += 1
                for jj in range(4):
                    jc = q * 4 + jj
                    p = nps()
                    for kc in range(8):
                        op("pe", lambda e: e.matmul(p[:, 0:tl], win[:, kc, jc * 128:(jc + 1) * 128], h[:, kc, 0:tl], start=(kc == 0), stop=(kc == 7)), r=[win, h], w=[p])
                    if jj % 2 == 0:
                        op("act", lambda e: e.activation(out=po[:, jj, 0:tl], in_=p[:, 0:tl], func=AF.Copy), r=[p], w=[po])
                    else:
                        op("dve", lambda e: e.tensor_copy(out=po[:, jj, 0:tl], in_=p[:, 0:tl]), r=[p], w=[po])
                dma("pool", PR[q * 512:(q + 1) * 512, t0:t0 + tl].rearrange("(c p) t -> p c t", p=128), po[:, :, 0:tl], r=[po])
        kb.pop()
        kb.push()
        prow = kb.sb("prow", [11, D_RNN])
        dma("sp", prow[:], rg_p[j * 11:(j + 1) * 11, :], w=[prow])
        chp = kb.sb("chp", [128, 12, 11])
        p = nps()
        for c in range(12):
            op("pe", lambda e: e.transpose(p[:, c * 11:(c + 1) * 11], prow[:, c * 128:(c + 1) * 128], ident(11)), r=[prow, cm], w=[p])
        op("dve", lambda e: e.tensor_copy(out=chp[:].rearrange("p c k -> p (c k)"), in_=p[:, 0:132]), r=[p], w=[chp])
        cdec = kb.sb("cdec", [128, 12, 4])
        op("act", lambda e: e.activation(out=cdec[:, :, 0:2], in_=chp[:, :, 9:11], func=AF.Exp, scale=-1.0), r=[chp], w=[cdec])
        op("act", lambda e: e.activation(out=cdec[:, :, 0:2], in_=cdec[:, :, 0:2], func=AF.Ln, bias=1.0, scale=1.0), r=[cdec], w=[cdec])
        op("dve", lambda e: e.tensor_scalar(out=cdec[:, :, 2:4], in0=cdec[:, :, 0:2], scalar1=-16.0, scalar2=None, op0=ALU.mult), r=[cdec], w=[cdec])
        op("dve", lambda e: e.tensor_scalar(out=cdec[:, :, 0:2], in0=cdec[:, :, 0:2], scalar1=-8.0, scalar2=None, op0=ALU.mult), r=[cdec], w=[cdec])
        xr = kb.sb("xr", [128, Tn])
        xc = kb.sb("xc", [128, 2, Tn])
        gl = kb.sb("gl", [128, Tn])
        aa = kb.sb("aa", [128, Tn])
        uu = kb.sb("uu", [128, Tn])
        hh = kb.sb("hh", [128, Tn])
        hs = kb.sb("hs", [128, Tn])
        gw = kb.sb("gw", [128, 2, 2, 2, 256])
        rr = [kb.sb("rr", [128, 512]) for _ in range(2)]
        ii = [kb.sb("ii", [128, 512]) for _ in range(2)]
        segs = [(0, C), (C, Tn)]
        for nb_ in range(6):
            for d in range(2):
                base = ((j * 2 + d) * 6 + nb_) * 256
                dma("sp", gw[:, d, 0, :, :], rg_ga[base:base + 256, :].rearrange("(c p) n -> p c n", p=128), w=[gw])
                dma("sp", gw[:, d, 1, :, :], rg_gx[base:base + 256, :].rearrange("(c p) n -> p c n", p=128), w=[gw])
            for cc_ in range(2):
                ch = nb_ * 2 + cc_
                dma("sp", xr[:], PR[D_RNN + ch * 128: D_RNN + (ch + 1) * 128, :], w=[xr])
                for (a0, b0) in segs:
                    op("dve", lambda e: e.tensor_scalar(out=xc[:, cc_, a0:b0], in0=xr[:, a0:b0], scalar1=chp[:, ch, 1:2], scalar2=chp[:, ch, 4:5], op0=ALU.mult, op1=ALU.add), r=[xr, chp], w=[xc])
                    op("dve", lambda e: e.scalar_tensor_tensor(out=xc[:, cc_, a0 + 1:b0], in0=xr[:, a0:b0 - 1], scalar=chp[:, ch, 0:1], in1=xc[:, cc_, a0 + 1:b0], op0=ALU.mult, op1=ALU.add), r=[xr, chp, xc], w=[xc])
                    op("dve", lambda e: e.scalar_tensor_tensor(out=xc[:, cc_, a0:b0 - 1], in0=xr[:, a0 + 1:b0], scalar=chp[:, ch, 2:3], in1=xc[:, cc_, a0:b0 - 1], op0=ALU.mult, op1=ALU.add), r=[xr, chp, xc], w=[xc])
                    op("dve", lambda e: e.scalar_tensor_tensor(out=xc[:, cc_, a0:b0 - 2], in0=xr[:, a0 + 2:b0], scalar=chp[:, ch, 3:4], in1=xc[:, cc_, a0:b0 - 2], op0=ALU.mult, op1=ALU.add), r=[xr, chp, xc], w=[xc])
            for oc in range(2):
                ch = nb_ * 2 + oc
                dma("sp", gl[:], PR[ch * 128:(ch + 1) * 128, :], w=[gl])
                for d in range(2):
                    for gi, (t0, tl) in enumerate(groups()):
                        r_ = rr[gi % 2]; i_ = ii[gi % 2]
                        pa = nps()
                        for kc in range(2):
                            op("pe", lambda e: e.matmul(pa[:, 0:tl], gw[:, d, 0, kc, oc * 128:(oc + 1) * 128], xc[:, kc, t0:t0 + tl], start=(kc == 0), stop=(kc == 1)), r=[gw, xc], w=[pa])
                        px = nps()
                        for kc in range(2):
                            op("pe", lambda e: e.matmul(px[:, 0:tl], gw[:, d, 1, kc, oc * 128:(oc + 1) * 128], xc[:, kc, t0:t0 + tl], start=(kc == 0), stop=(kc == 1)), r=[gw, xc], w=[px])
                        op("act", lambda e: e.activation(out=r_[:, 0:tl], in_=pa[:, 0:tl], func=AF.Sigmoid, bias=chp[:, ch, 5 + d:6 + d], scale=1.0), r=[pa, chp], w=[r_])
                        op("act", lambda e: e.activation(out=i_[:, 0:tl], in_=px[:, 0:tl], func=AF.Sigmoid, bias=chp[:, ch, 7 + d:8 + d], scale=1.0), r=[px, chp], w=[i_])
                        op("act", lambda e: e.activation(out=aa[:, t0:t0 + tl], in_=r_[:, 0:tl], func=AF.Exp, scale=cdec[:, ch, d:d + 1]), r=[r_, cdec], w=[aa])
                        op("act", lambda e: e.activation(out=r_[:, 0:tl], in_=r_[:, 0:tl], func=AF.Exp, scale=cdec[:, ch, 2 + d:3 + d]), r=[r_, cdec], w=[r_])
                        op("act", lambda e: e.activation(out=r_[:, 0:tl], in_=r_[:, 0:tl], func=AF.Sqrt, bias=1.0, scale=-1.0), r=[r_], w=[r_])
                        op("dve", lambda e: e.tensor_tensor(out=i_[:, 0:tl], in0=i_[:, 0:tl], in1=xc[:, oc, t0:t0 + tl], op=ALU.mult), r=[i_, xc], w=[i_])
                        op("dve", lambda e: e.tensor_tensor(out=uu[:, t0:t0 + tl], in0=i_[:, 0:tl], in1=r_[:, 0:tl], op=ALU.mult), r=[i_, r_], w=[uu])
                    if d == 0:
                        op("dve", lambda e: e.tensor_tensor_scan(out=hs[:, :], data0=aa[:, :], data1=uu[:, :], initial=0.0, op0=ALU.mult, op1=ALU.add), r=[aa, uu], w=[hs])
                    else:
                        op("dve", lambda e: e.tensor_tensor_scan(out=hh[:, 0:C][:, ::-1], data0=aa[:, 0:C][:, ::-1], data1=uu[:, 0:C][:, ::-1], initial=0.0, op0=ALU.mult, op1=ALU.add), r=[aa, uu], w=[hh])
                        op("dve", lambda e: e.tensor_tensor_scan(out=hh[:, C:Tn][:, ::-1], data0=aa[:, C:Tn][:, ::-1], data1=uu[:, C:Tn][:, ::-1], initial=hh[:, 0:1], op0=ALU.mult, op1=ALU.add), r=[aa, uu, hh], w=[hh])
                        op("pool", lambda e: e.tensor_tensor(out=hs[:], in0=hs[:], in1=hh[:], op=ALU.add), r=[hs, hh], w=[hs])
                op("pool", lambda e: e.tensor_tensor(out=hh[:], in0=gl[:], in1=gl[:], op=ALU.mult), r=[gl], w=[hh])
                op("dve", lambda e: e.tensor_scalar(out=hh[:], in0=hh[:], scalar1=0.044715, scalar2=1.0, op0=ALU.mult, op1=ALU.add), r=[hh], w=[hh])
                op("pool", lambda e: e.tensor_tensor(out=hh[:], in0=hh[:], in1=gl[:], op=ALU.mult), r=[hh, gl], w=[hh])
                op("act", lambda e: e.activation(out=hh[:], in_=hh[:], func=AF.Sigmoid, scale=1.5957691216057308), r=[hh], w=[hh])
                op("pool", lambda e: e.tensor_tensor(out=hh[:], in0=hh[:], in1=gl[:], op=ALU.mult), r=[hh, gl], w=[hh])
                op("dve", lambda e: e.tensor_tensor(out=hs[:], in0=hs[:], in1=hh[:], op=ALU.mult), r=[hs, hh], w=[hs])
                dma("pool", OT[ch * 128:(ch + 1) * 128, :], hs[:], r=[hs])
        kb.pop()
        return rg_w_out[j * D_RNN:(j + 1) * D_RNN, :], D_RNN, None

    def mixer_gqa(i, b, j, need_ctx):
        QT3 = QT.ap().rearrange("p (h t) -> p h t", h=16)
        KT3 = KT.ap().rearrange("p (h t) -> p h t", h=2)
        kb.push()
        w = kb.sb("gqw", [128, 8, 2432], BF16)
        for kc_ in range(8):
            dma("pool", w[:, kc_, :], gq_w[j * D + kc_ * 128:j * D + (kc_ + 1) * 128, :], w=[w])
        b64 = kb.sb("b64", [64, 40])
        dma("sp", b64[:], gq_b64[j * 64:(j + 1) * 64, :], w=[b64])
        bvrow = kb.sb("bvrow", [128, 128])
        dma("sp", bvrow[:], gq_bv[j, :].partition_broadcast(128), w=[bvrow])
        hTs = [kb.sb("hTg", [128, 8, 512], BF16) for _ in range(2)]
        qo = kb.sb("qo", [64, 16, 512])
        ko = kb.sb("ko", [64, 2, 512])
        cs = kb.sb("cs", [64, 512]); sn = kb.sb("sn", [64, 512])
        ta = [kb.sb("ta", [64, 512]) for _ in range(2)]
        tb = [kb.sb("tb", [64, 512]) for _ in range(2)]
        va = [kb.sb("va", [128, 2, 65]) for _ in range(2)]
        for v_ in va:
            op("dve", lambda e: e.memset(v_[:], 1.0), w=[v_])
        n = 0
        for gi, (t0, tl) in enumerate(groups()):
            h = hTs[gi % 2]
            lat = t0 >= C
            dma("sp", h[:, :, 0:tl], HT[:, t0:t0 + tl].rearrange("(c p) t -> p c t", p=128), w=[h])
            if lat:
                dma("sp", cs[:, 0:tl], rope64[0:64, t0 - C:t0 - C + tl], w=[cs])
                dma("sp", sn[:, 0:tl], rope64[64:128, t0 - C:t0 - C + tl], w=[sn])
            for hq in range(18):
                col = hq * 64 if hq < 16 else 2048 + (hq - 16) * 64
                colsw = 1024 + hq * 64 if hq < 16 else 2176 + (hq - 16) * 64
                dst = qo[:, hq, 0:tl] if hq < 16 else ko[:, hq - 16, 0:tl]
                dstT = qo if hq < 16 else ko
                p = nps()
                for kc in range(8):
                    op("pe", lambda e: e.matmul(p[0:64, 0:tl], w[:, kc, col:col + 64], h[:, kc, 0:tl], start=(kc == 0), stop=(kc == 7)), r=[w, h], w=[p])
                if lat:
                    p2 = nps()
                    for kc in range(8):
                        op("pe", lambda e: e.matmul(p2[0:64, 0:tl], w[:, kc, colsw:colsw + 64], h[:, kc, 0:tl], start=(kc == 0), stop=(kc == 7)), r=[w, h], w=[p2])
                    a_ = ta[n % 2]; b_ = tb[n % 2]; n += 1
                    op("dve", lambda e: e.scalar_tensor_tensor(out=a_[:, 0:tl], in0=p[0:64, 0:tl], scalar=b64[:, hq:hq + 1], in1=cs[:, 0:tl], op0=ALU.add, op1=ALU.mult), r=[p, b64, cs], w=[a_])
                    op("dve", lambda e: e.scalar_tensor_tensor(out=b_[:, 0:tl], in0=p2[0:64, 0:tl], scalar=b64[:, 20 + hq:21 + hq], in1=sn[:, 0:tl], op0=ALU.add, op1=ALU.mult), r=[p2, b64, sn], w=[b_])
                    op("pool", lambda e: e.tensor_tensor(out=dst, in0=a_[:, 0:tl], in1=b_[:, 0:tl], op=ALU.add), r=[a_, b_], w=[dstT])
                else:
                    op("dve", lambda e: e.tensor_scalar(out=dst, in0=p[0:64, 0:tl], scalar1=b64[:, hq:hq + 1], scalar2=None, op0=ALU.add), r=[p, b64], w=[dstT])
            dma("pool", QT3[:, :, t0:t0 + tl], qo[:, :, 0:tl], r=[qo])
            dma("pool", KT3[:, :, t0:t0 + tl], ko[:, :, 0:tl], r=[ko])
            for tt in range(tl // 128):
                v_ = va[tt % 2]
                p = nps()
                for kc in range(8):
                    op("pe", lambda e: e.matmul(p[:, 0:128], h[:, kc, tt * 128:(tt + 1) * 128], w[:, kc, 2304:2432], start=(kc == 0), stop=(kc == 7)), r=[w, h], w=[p])
                op("dve", lambda e: e.tensor_tensor(out=v_[:, :, 0:64], in0=p[:, 0:128].rearrange("p (h d) -> p h d", h=2), in1=bvrow[:].rearrange("p (h d) -> p h d", h=2), op=ALU.add), r=[p, bvrow], w=[v_])
                dma("pool", VA[t0 + tt * 128:t0 + (tt + 1) * 128, :], v_[:].rearrange("p h d -> p (h d)"), r=[v_])
        kb.pop()
        kb.push()
        kT = kb.sb("kT", [64, 2, Tn])
        dma("sp", kT[:], KT3[:, :, :], w=[kT])
        V = kb.sb("Vg", [128, NT, 130])
        dma("sp", V[:], VA.ap().rearrange("(n p) c -> p n c", p=128), w=[V])
        esk = kb.sb("esk", [64, 16])
        dma("sp", esk[:], gq_sinks[j, :].partition_broadcast(64), w=[esk])
        op("act", lambda e: e.activation(out=esk[:], in_=esk[:], func=AF.Exp), r=[esk], w=[esk])
        mge = kb.sb("mge", [128, 128])
        mle = kb.sb("mle", [128, 128])
        op("dve", lambda e: e.tensor_scalar(out=mge[:], in0=tri, scalar1=-1.0, scalar2=1.0, op0=ALU.mult, op1=ALU.add), r=[cm], w=[mge])
        op("dve", lambda e: e.tensor_tensor(out=mle[:], in0=tri, in1=ident(), op=ALU.add), r=[cm], w=[mle])
        sel = kb.sb("sel", [65, 64])
        op("dve", lambda e: e.memset(sel[:], 0.0), w=[sel])
        op("dve", lambda e: e.memset(sel[64:65, :], 1.0), w=[sel])
        qs = [kb.sb("qs", [64, 16, 128]) for _ in range(2)]
        pTs = [kb.sb("pT", [128, 512]) for _ in range(4)]
        Xs = [kb.sb("Xo", [65, 512]) for _ in range(2)]
        rb = [kb.sb("rb", [64, 512]) for _ in range(2)]
        oo = [kb.sb("oo", [64, 512]) for _ in range(2)]
        nq = 0
        blocks = []
        if need_ctx:
            blocks += [(tq, [(0, None), (1, None)]) for tq in range(C // 128)]
        nlb = S // 128
        for jq in range(nlb):
            ch = [(0, None), (1, None)]
            if jq > 0:
                ch.append((C // 128 + jq - 1, mge))
            ch.append((C // 128 + jq, None))
            if jq < nlb - 1:
                ch.append((C // 128 + jq + 1, mle))
            blocks.append((C // 128 + jq, ch))
        npt = 0
        for (tq, chunks) in blocks:
            q = qs[nq % 2]; nq += 1
            dma("sp", q[:], QT3[:, :, tq * 128:(tq + 1) * 128], w=[q])
            for kvh in range(2):
                for half in range(2):
                    h0 = kvh * 8 + half * 4
                    po = PS[4 + (npt % 4)]
                    for ci, (kt, msk) in enumerate(chunks):
                        pT = pTs[npt % 4]; npt += 1
                        p = nps()
                        op("pe", lambda e: e.matmul(p[:, :], kT[:, kvh, kt * 128:(kt + 1) * 128], q[:, h0:h0 + 4, :].rearrange("p h t -> p (h t)"), start=True, stop=True), r=[kT, q], w=[p])
                        op("act", lambda e: e.activation(out=pT[:], in_=p[:, :], func=AF.Exp, scale=0.125), r=[p], w=[pT])
                        if msk is not None:
                            for hh in range(4):
                                op("pool" if hh % 2 else "dve", lambda e: e.tensor_tensor(out=pT[:, hh * 128:(hh + 1) * 128], in0=pT[:, hh * 128:(hh + 1) * 128], in1=msk[:], op=ALU.mult), r=[pT, msk], w=[pT])
                        op("pe", lambda e: e.matmul(po[0:65, :], V[:, kt, kvh * 65:(kvh + 1) * 65], pT[:], start=(ci == 0), stop=(ci == len(chunks) - 1)), r=[V, pT], w=[po])
                    X = Xs[npt % 2]; r_ = rb[npt % 2]; o_ = oo[npt % 2]
                    op("act", lambda e: e.activation(out=X[:], in_=po[0:65, :], func=AF.Copy), r=[po], w=[X])
                    p = nps()
                    op("pe", lambda e: e.matmul(p[0:64, :], sel[:], X[:], start=True, stop=True), r=[sel, X], w=[p])
                    for hh in range(4):
                        op("dve", lambda e: e.tensor_scalar(out=r_[:, hh * 128:(hh + 1) * 128], in0=p[0:64, hh * 128:(hh + 1) * 128], scalar1=esk[:, h0 + hh:h0 + hh + 1], scalar2=None, op0=ALU.add), r=[p, esk], w=[r_])
                    op("dve", lambda e: e.reciprocal(out=r_[:], in_=r_[:]), r=[r_], w=[r_])
                    op("dve", lambda e: e.tensor_tensor(out=o_[:], in0=X[0:64, :], in1=r_[:], op=ALU.mult), r=[X, r_], w=[o_])
                    dma("pool", OT[h0 * 64:(h0 + 4) * 64, tq * 128:(tq + 1) * 128].rearrange("(h p) t -> p h t", p=64), o_[:].rearrange("p (h t) -> p h t", h=4), r=[o_])
        kb.pop()
        return gq_w_o[j * D:(j + 1) * D, :], D, gq_b_o[j, :]

    def mixer_mla(i, b, j, need_ctx):
        RMS_EPS = 1e-6
        kb.push()
        wd = kb.sb("mlwd", [128, 8, 448], BF16)
        for kc_ in range(8):
            dma("pool", wd[:, kc_, :], ml_wd[j * D + kc_ * 128:j * D + (kc_ + 1) * 128, :], w=[wd])
        nrow = kb.sb("nrow", [128, 384])
        dma("sp", nrow[:], ml_norm[j, :].partition_broadcast(128), w=[nrow])
        epsr = kb.sb("epsr", [128, 1])
        op("dve", lambda e: e.memset(epsr[:], RMS_EPS), w=[epsr])
        hTs = [kb.sb("hTm", [128, 8, 512], BF16) for _ in range(2)]
        cTs = [kb.sb("cTm", [128, 3, 512]) for _ in range(2)]
        krs = [kb.sb("krm", [32, 512]) for _ in range(2)]
        cs = kb.sb("csm", [32, 512]); sn = kb.sb("snm", [32, 512])
        t1 = kb.sb("t1m", [32, 512]); t2 = kb.sb("t2m", [32, 512])
        cqs = [kb.sb("cqm", [128, 384]) for _ in range(2)]
        junk = kb.sb("junk", [128, 256])
        ss = kb.sb("ssm", [128, 4])
        for gi, (t0, tl) in enumerate(groups()):
            h = hTs[gi % 2]; cT = cTs[gi % 2]; kr = krs[gi % 2]
            lat = t0 >= C
            dma("sp", h[:, :, 0:tl], HT[:, t0:t0 + tl].rearrange("(c p) t -> p c t", p=128), w=[h])
            for tt in range(tl // 128):
                cq = cqs[tt % 2]
                p = nps()
                for kc in range(8):
                    op("pe", lambda e: e.matmul(p[:, 0:384], h[:, kc, tt * 128:(tt + 1) * 128], wd[:, kc, 0:384], start=(kc == 0), stop=(kc == 7)), r=[h, wd], w=[p])
                op("act", lambda e: e.activation(out=junk[:, 0:256], in_=p[:, 0:256], func=AF.Square, accum_out=ss[:, 0:1]), r=[p], w=[junk, ss])
                op("act", lambda e: e.activation(out=junk[:, 0:128], in_=p[:, 256:384], func=AF.Square, accum_out=ss[:, 1:2]), r=[p], w=[junk, ss])
                op("act", lambda e: e.activation(out=ss[:, 2:3], in_=ss[:, 0:1], func=AF.Sqrt, bias=epsr[:, 0:1], scale=1.0 / 256), r=[ss, epsr], w=[ss])
                op("act", lambda e: e.activation(out=ss[:, 3:4], in_=ss[:, 1:2], func=AF.Sqrt, bias=epsr[:, 0:1], scale=1.0 / 128), r=[ss, epsr], w=[ss])
                op("dve", lambda e: e.reciprocal(out=ss[:, 2:4], in_=ss[:, 2:4]), r=[ss], w=[ss])
                op("dve", lambda e: e.scalar_tensor_tensor(out=cq[:, 0:256], in0=p[:, 0:256], scalar=ss[:, 2:3], in1=nrow[:, 0:256], op0=ALU.mult, op1=ALU.mult), r=[p, ss, nrow], w=[cq])
                op("dve", lambda e: e.scalar_tensor_tensor(out=cq[:, 256:384], in0=p[:, 256:384], scalar=ss[:, 3:4], in1=nrow[:, 256:384], op0=ALU.mult, op1=ALU.mult), r=[p, ss, nrow], w=[cq])
                p2 = nps()
                for c3 in range(3):
                    op("pe", lambda e: e.transpose(p2[:, c3 * 128:(c3 + 1) * 128], cq[:, c3 * 128:(c3 + 1) * 128], ident()), r=[cq, cm], w=[p2])
                op("act", lambda e: e.activation(out=cT[:, :, tt * 128:(tt + 1) * 128], in_=p2[:, 0:384].rearrange("p (c t) -> p c t", c=3), func=AF.Copy), r=[p2], w=[cT])
            dma("pool", CT[:, t0:t0 + tl].rearrange("(c p) t -> p c t", p=128), cT[:, :, 0:tl], r=[cT])
            p = nps()
            for kc in range(8):
                op("pe", lambda e: e.matmul(p[0:32, 0:tl], wd[:, kc, 384:416], h[:, kc, 0:tl], start=(kc == 0), stop=(kc == 7)), r=[h, wd], w=[p])
            if lat:
                p2 = nps()
                for kc in range(8):
                    op("pe", lambda e: e.matmul(p2[0:32, 0:tl], wd[:, kc, 416:448], h[:, kc, 0:tl], start=(kc == 0), stop=(kc == 7)), r=[h, wd], w=[p2])
                dma("sp", cs[:, 0:tl], rope32[0:32, t0 - C:t0 - C + tl], w=[cs])
                dma("sp", sn[:, 0:tl], rope32[32:64, t0 - C:t0 - C + tl], w=[sn])
                op("dve", lambda e: e.tensor_tensor(out=t1[:, 0:tl], in0=p[0:32, 0:tl], in1=cs[:, 0:tl], op=ALU.mult), r=[p, cs], w=[t1])
                op("dve", lambda e: e.tensor_tensor(out=t2[:, 0:tl], in0=p2[0:32, 0:tl], in1=sn[:, 0:tl], op=ALU.mult), r=[p2, sn], w=[t2])
                op("pool", lambda e: e.tensor_tensor(out=kr[:, 0:tl], in0=t1[:, 0:tl], in1=t2[:, 0:tl], op=ALU.add), r=[t1, t2], w=[kr])
            else:
                op("dve", lambda e: e.tensor_copy(out=kr[:, 0:tl], in_=p[0:32, 0:tl]), r=[p], w=[kr])
            dma("pool", KRT[:, t0:t0 + tl], kr[:, 0:tl], r=[kr])
        kb.pop()
        kb.push()
        cT = kb.sb("cTall", [128, 3, Tn])
        dma("sp", cT[:], CT.ap().rearrange("(c p) t -> p c t", p=128), w=[cT])
        krT = kb.sb("krT", [32, Tn], BF16)
        dma("pool", krT[:], KRT[:, :], w=[krT])
        csb = [kb.sb("csq", [32, 512]) for _ in range(2)]
        snb = [kb.sb("snq", [32, 512]) for _ in range(2)]
        sel = kb.sb("selm", [65, 64])
        op("dve", lambda e: e.memset(sel[:], 0.0), w=[sel])
        op("dve", lambda e: e.memset(sel[64:65, :], 1.0), w=[sel])
        qn = kb.sb("qn", [64, Tn], BF16); qr = kb.sb("qr", [32, Tn], BF16); kn = kb.sb("kn", [64, Tn], BF16)
        Vh = kb.sb("Vh", [128, NT, 65], BF16)
        op("dve", lambda e: e.memset(Vh[:], 1.0), w=[Vh])
        wq = kb.sb("wqh", [128, 2, 128]); wkv = kb.sb("wkvh", [128, 128])
        t1 = kb.sb("t1q", [32, 512]); t2 = kb.sb("t2q", [32, 512])
        pTs = [kb.sb("pTm", [128, 512], BF16) for _ in range(4)]
        Xs = [kb.sb("Xm", [65, 512]) for _ in range(2)]
        rb = [kb.sb("rbm", [64, 512]) for _ in range(2)]
        oo = [kb.sb("oom", [64, 512]) for _ in range(2)]
        scale = 96.0 ** -0.5
        npt = 0
        for hd in range(16):
            dma("sp", wq[:, :, 0:96], ml_wuq[j * 256:(j + 1) * 256, hd * 96:(hd + 1) * 96].rearrange("(c p) n -> p c n", p=128), w=[wq])
            dma("sp", wq[:, :, 96:128], ml_wuq[j * 256:(j + 1) * 256, 1536 + hd * 32:1536 + (hd + 1) * 32].rearrange("(c p) n -> p c n", p=128), w=[wq])
            dma("sp", wkv[:], ml_wukv[j * 128:(j + 1) * 128, hd * 128:(hd + 1) * 128], w=[wkv])
            for gi, (t0, tl) in enumerate(groups()):
                lat = t0 >= C
                p = nps()
                for kc in range(2):
                    op("pe", lambda e: e.matmul(p[0:64, 0:tl], wq[:, kc, 0:64], cT[:, kc, t0:t0 + tl], start=(kc == 0), stop=(kc == 1)), r=[wq, cT], w=[p])
                op("act", lambda e: e.activation(out=qn[:, t0:t0 + tl], in_=p[0:64, 0:tl], func=AF.Copy), r=[p], w=[qn])
                p = nps()
                for kc in range(2):
                    op("pe", lambda e: e.matmul(p[0:32, 0:tl], wq[:, kc, 64:96], cT[:, kc, t0:t0 + tl], start=(kc == 0), stop=(kc == 1)), r=[wq, cT], w=[p])
                if lat:
                    cs = csb[gi % 2]; sn = snb[gi % 2]
                    dma("sp", cs[:, 0:tl], rope32[0:32, t0 - C:t0 - C + tl], w=[cs])
                    dma("sp", sn[:, 0:tl], rope32[32:64, t0 - C:t0 - C + tl], w=[sn])
                    p2 = nps()
                    for kc in range(2):
                        op("pe", lambda e: e.matmul(p2[0:32, 0:tl], wq[:, kc, 96:128], cT[:, kc, t0:t0 + tl], start=(kc == 0), stop=(kc == 1)), r=[wq, cT], w=[p2])
                    op("dve", lambda e: e.tensor_tensor(out=t1[:, 0:tl], in0=p[0:32, 0:tl], in1=cs[:, 0:tl], op=ALU.mult), r=[p, cs], w=[t1])
                    op("dve", lambda e: e.tensor_tensor(out=t2[:, 0:tl], in0=p2[0:32, 0:tl], in1=sn[:, 0:tl], op=ALU.mult), r=[p2, sn], w=[t2])
                    op("pool", lambda e: e.tensor_tensor(out=qr[:, t0:t0 + tl], in0=t1[:, 0:tl], in1=t2[:, 0:tl], op=ALU.add), r=[t1, t2], w=[qr])
                else:
                    op("dve", lambda e: e.tensor_copy(out=qr[:, t0:t0 + tl], in_=p[0:32, 0:tl]), r=[p], w=[qr])
                p = nps()
                op("pe", lambda e: e.matmul(p[0:64, 0:tl], wkv[:, 0:64], cT[:, 2, t0:t0 + tl], start=True, stop=True), r=[wkv, cT], w=[p])
                op("act", lambda e: e.activation(out=kn[:, t0:t0 + tl], in_=p[0:64, 0:tl], func=AF.Copy), r=[p], w=[kn])
                p = nps()
                ntl = tl // 128
                for tt in range(ntl):
                    op("pe", lambda e: e.matmul(p[:, tt * 64:(tt + 1) * 64], cT[:, 2, t0 + tt * 128:t0 + (tt + 1) * 128], wkv[:, 64:128], start=True, stop=True), r=[wkv, cT], w=[p])
                op("dve", lambda e: e.tensor_copy(out=Vh[:, t0 // 128:t0 // 128 + ntl, 0:64], in_=p[:, 0:ntl * 64].rearrange("p (n d) -> p n d", d=64)), r=[p], w=[Vh])
            qgroups = [g for g in groups() if g[0] >= C]
            if need_ctx:
                qgroups = [(0, C)] + qgroups
            for (t0, tl) in qgroups:
                kts = list(range(C // 128)) if t0 < C else list(range(NT))
                po = PS[4 + (npt % 4)]
                for ci, kt in enumerate(kts):
                    pT = pTs[npt % 4]; npt += 1
                    p = nps()
                    op("pe", lambda e: e.matmul(p[:, 0:tl], kn[:, kt * 128:(kt + 1) * 128], qn[:, t0:t0 + tl], start=True, stop=False), r=[kn, qn], w=[p])
                    op("pe", lambda e: e.matmul(p[:, 0:tl], krT[:, kt * 128:(kt + 1) * 128], qr[:, t0:t0 + tl], start=False, stop=True), r=[krT, qr], w=[p])
                    op("act", lambda e: e.activation(out=pT[:, 0:tl], in_=p[:, 0:tl], func=AF.Exp, scale=scale), r=[p], w=[pT])
                    op("pe", lambda e: e.matmul(po[0:65, 0:tl], Vh[:, kt, :], pT[:, 0:tl], start=(ci == 0), stop=(ci == len(kts) - 1)), r=[Vh, pT], w=[po])
                X = Xs[npt % 2]; r_ = rb[npt % 2]; o_ = oo[npt % 2]
                op("act", lambda e: e.activation(out=X[:, 0:tl], in_=po[0:65, 0:tl], func=AF.Copy), r=[po], w=[X])
                p = nps()
                op("pe", lambda e: e.matmul(p[0:64, 0:tl], sel[:], X[:, 0:tl], start=True, stop=True), r=[sel, X], w=[p])
                op("dve", lambda e: e.reciprocal(out=r_[:, 0:tl], in_=p[0:64, 0:tl]), r=[p], w=[r_])
                op("dve", lambda e: e.tensor_tensor(out=o_[:, 0:tl], in0=X[0:64, 0:tl], in1=r_[:, 0:tl], op=ALU.mult), r=[X, r_], w=[o_])
                dma("pool", OT[hd * 64:(hd + 1) * 64, t0:t0 + tl], o_[:, 0:tl], r=[o_])
        kb.pop()
        return ml_w_o[j * D:(j + 1) * D, :], D, None

    Xsrc = xin
    kcount = {0: 0, 1: 0, 2: 0, -1: 0}
    for i, kind in enumerate(kinds):
        last = (i == L - 1)
        need_ctx = not (last and cfg.last_noctx)
        for ex in range(E):
            r0 = (i * E + ex) * D
            kb.dma_bg("pool", WGU16[ex * 128:(ex + 1) * 128, :].rearrange("p (c n) -> p c n", c=8), wgu[r0:r0 + D, :].rearrange("(c p) n -> p c n", p=128))
            kb.dma_bg("pool", WDN16[ex * 128:(ex + 1) * 128, :].rearrange("p (c n) -> p c n", c=8), wdn[r0:r0 + FF, :].rearrange("(c p) n -> p c n", p=128))
        kb.dma_bg("pool", BB16[:, 0:2 * FF], bgu[i * E:(i + 1) * E, :])
        kb.dma_bg("pool", BB16[:, 2 * FF:3 * FF], bdn[i * E:(i + 1) * E, :])
        phase_mod(i)
        for b in range(NB):
            phase_h(i, b, Xsrc)
            if kind == -1:
                w_o, Dm, b_o = mixer_identity(i, b)
            elif kind == 0:
                w_o, Dm, b_o = mixer_rg(i, b, kcount[0], need_ctx)
            elif kind == 2:
                psn[0] = 4
                w_o, Dm, b_o = mixer_mla(i, b, kcount[2], need_ctx)
                psn[0] = 8
            elif kind == 1:
                psn[0] = 4
                w_o, Dm, b_o = mixer_gqa(i, b, kcount[1], need_ctx)
                psn[0] = 8
            else:
                raise NotImplementedError
            phase_out(i, b, Xsrc, w_o, Dm, b_o, need_ctx)
        phase_moe(i, last)
        kcount[kind] += 1
        Xsrc = XB
    kb.close()
    return kb


def consts():
    cm = np.zeros((128, 416), np.float32)
    cm[:, 0:128] = np.eye(128, dtype=np.float32)
    k = np.arange(128)
    cm[:, 128:256] = (k[:, None] < k[None, :]).astype(np.float32)
    cm[:, 256:384] = 1.0
    cm[:, 384:416] = np.arange(32, dtype=np.float32)[None, :]
    return cm


def rope_table(S, rot_dim):
    rows = S // 64
    row = np.repeat(np.arange(rows, dtype=np.float32), 64)
    col = np.tile(np.arange(64, dtype=np.float32), rows)
    n_freq = rot_dim // 4
    inv = (np.float32(10000.0) ** (-np.arange(n_freq, dtype=np.float32) / np.float32(n_freq))).astype(np.float32)
    ang = np.concatenate([row[:, None] * inv, col[:, None] * inv], axis=-1).astype(np.float32)
    cos = np.cos(ang).astype(np.float32).T
    sin = np.sin(ang).astype(np.float32).T
    return np.ascontiguousarray(np.concatenate([cos, cos, -sin, sin], axis=0))


def prep_inputs(cfg, inp):
    NB, S, kinds = cfg.NB, cfg.S, cfg.kinds
    L = len(kinds)
    f = lambda a: np.ascontiguousarray(np.asarray(a, dtype=np.float32))
    shared = {
        "ada_w": f(inp["ada_w"]).reshape(L * D, 6 * D),
        "ada_b": f(inp["ada_b"]),
        "lnp": f(np.stack([inp["ln1_g"], inp["ln1_b"], inp["ln2_g"], inp["ln2_b"]], axis=1)).reshape(L * 4, D),
        "router_w": f(inp["router_w"]).reshape(L * D, E),
        "router_b": f(inp["router_b"]),
        "wgu": f(np.concatenate([inp["exp_gu_w"][..., 0::2], inp["exp_gu_w"][..., 1::2]], axis=-1)).reshape(L * E * D, 2 * FF),
        "bgu": f(np.concatenate([inp["exp_gu_b"][..., 0::2], inp["exp_gu_b"][..., 1::2]], axis=-1)).reshape(L * E, 2 * FF),
        "wdn": f(inp["exp_down_w"]).reshape(L * E * FF, D),
        "bdn": f(inp["exp_down_b"]).reshape(L * E, D),
        "cmat": consts(),
    }
    if -1 in kinds:
        shared["w_id"] = np.eye(D, dtype=np.float32)
    if 2 in kinds:
        n = kinds.count(2)
        Wd = inp["mla_w_down"][:n]
        def sw16(a):
            sh = a.shape
            return a.reshape(sh[:-1] + (sh[-1] // 32, 2, 16))[..., ::-1, :].reshape(sh)
        shared["ml_wd"] = f(np.concatenate([Wd, sw16(Wd[..., 384:416])], axis=-1)).reshape(n * D, 448)
        shared["ml_norm"] = f(np.concatenate([inp["mla_q_norm"][:n], inp["mla_kv_norm"][:n]], axis=-1))
        Wq = inp["mla_w_uq"][:n].reshape(n, 256, 16, 96)
        shared["ml_wuq"] = f(np.concatenate([Wq.reshape(n, 256, 1536), sw16(np.ascontiguousarray(Wq[..., 64:96])).reshape(n, 256, 512)], axis=-1)).reshape(n * 256, 2048)
        shared["ml_wukv"] = f(inp["mla_w_ukv"][:n]).reshape(n * 128, 2048)
        shared["ml_w_o"] = f(inp["mla_w_o"][:n]).reshape(n * D, D)
        shared["rope32"] = rope_table(S, 32)
    if 1 in kinds:
        n = kinds.count(1)
        W = inp["gqa_w_qkv"][:n]
        Bq = inp["gqa_b_qkv"][:n]
        def sw(a):
            sh = a.shape
            return a.reshape(sh[:-1] + (sh[-1] // 64, 2, 32))[..., ::-1, :].reshape(sh)
        shared["gq_w"] = f(np.concatenate([W[..., 0:1024], sw(W[..., 0:1024]), W[..., 1024:1152], sw(W[..., 1024:1152]), W[..., 1152:1280]], axis=-1)).reshape(n * D, 2432)
        b20 = Bq.reshape(n, 20, 64)
        shared["gq_b64"] = f(np.concatenate([b20.transpose(0, 2, 1), sw(Bq).reshape(n, 20, 64).transpose(0, 2, 1)], axis=-1)).reshape(n * 64, 40)
        shared["gq_bv"] = f(Bq[:, 1152:1280])
        shared["gq_sinks"] = f(inp["gqa_sinks"][:n])
        shared["gq_w_o"] = f(inp["gqa_w_o"][:n]).reshape(n * D, D)
        shared["gq_b_o"] = f(inp["gqa_b_o"][:n])
        shared["rope64"] = rope_table(S, 64)
    if 0 in kinds:
        n_rg = kinds.count(0)
        shared["rg_w_in"] = f(inp["rg_w_in"][:n_rg]).reshape(n_rg * D, 2 * D_RNN)
        shared["rg_p"] = f(np.concatenate([inp["rg_conv_w"][:n_rg], inp["rg_conv_b"][:n_rg, None, :], inp["rg_gate_a_b"][:n_rg], inp["rg_gate_x_b"][:n_rg],
                                           inp["rg_lambda"][:n_rg]], axis=1)).reshape(n_rg * 11, D_RNN)
        shared["rg_ga"] = f(inp["rg_gate_a_w"][:n_rg]).reshape(n_rg * 2 * 6 * 256, 256)
        shared["rg_gx"] = f(inp["rg_gate_x_w"][:n_rg]).reshape(n_rg * 2 * 6 * 256, 256)
        shared["rg_w_out"] = f(inp["rg_w_out"][:n_rg]).reshape(n_rg * D_RNN, D)
    maps = []
    for c in range(cfg.NC):
        bs = slice(c * NB, (c + 1) * NB)
        m = dict(shared)
        m["xin"] = f(np.concatenate([inp["ctx"][bs], inp["x"][bs]], axis=1)).reshape(NB * (C + S), D)
        m["cc"] = f(np.concatenate([inp["c"][bs], inp["c_ctx"][None, :]], axis=0))
        maps.append(m)
    return maps


def run(cfg, inp):
    kb = build(cfg)
    maps = prep_inputs(cfg, inp)
    res = run_bass_kernel_spmd(kb.nc, maps, core_ids=list(range(cfg.NC)))
    outs = [res.results[c]["out"].reshape(cfg.NB, cfg.S, D) for c in range(cfg.NC)]
    return np.concatenate(outs, axis=0)


def kernel(**inputs):
    cfg = Cfg(NB=2, S=4096, kinds=(0, 1, 2, 0), NC=8)
    return run(cfg, inputs).astype(np.float32)
```

```python
import numpy as np
from contextlib import ExitStack
import concourse.bass as bass
import concourse.mybir as mybir
from concourse.bass_utils import run_bass_kernel_spmd

F32 = mybir.dt.float32
I32 = mybir.dt.int32
U32 = mybir.dt.uint32
BF16 = mybir.dt.bfloat16
AF = mybir.ActivationFunctionType
ALU = mybir.AluOpType
AX = mybir.AxisListType

D = 1024
C = 256
E = 32
FF = 1024
D_RNN = 1536
SB = 512
ALPHA = 8.0 ** 0.25
LN_EPS = 1e-5


class Res:
    __slots__ = ("w", "r")

    def __init__(self):
        self.w = None
        self.r = {}


class T:
    def __init__(self, h):
        self.h = h
        self.res = [Res()]

    def __getitem__(self, k):
        return self.h[k]


class KB:
    def __init__(self, n_dma_sems=48):
        self.nc = bass.Bass("TRN2", target_bir_lowering=False)
        nc = self.nc
        self.es = ExitStack()
        self.stacks = []
        self.eng = {"pe": nc.tensor, "act": nc.scalar, "dve": nc.vector, "pool": nc.gpsimd, "sp": nc.sync}
        self.sems = []
        self.esem = {}
        self.cnt = {}
        self.waited = {e: {} for e in self.eng}
        for e in self.eng:
            self.esem[e] = self._newsem("s_" + e)
            self.cnt[e] = 0
        self.dsem = [self._newsem("d%d" % i) for i in range(n_dma_sems)]
        self.duses = [0] * n_dma_sems
        self.drr = 0
        self.ninst = 0
        self.uid = 0
        self.bgsem = [self._newsem("bg%d" % i) for i in range(8)]
        self.bguses = [0] * 8
        self.bgrr = 0

    def _newsem(self, name):
        h = self.es.enter_context(self.nc.semaphore(name))
        self.sems.append(h)
        return len(self.sems) - 1

    def push(self):
        self.stacks.append(ExitStack())

    def pop(self):
        self.barrier()
        self.stacks.pop().close()

    def _ctx(self):
        return self.stacks[-1] if self.stacks else self.es

    def sb(self, name, shape, dtype=F32):
        self.uid += 1
        return T(self._ctx().enter_context(self.nc.sbuf_tensor("%s_%d" % (name, self.uid), list(shape), dtype)))

    def ps(self, name, shape, dtype=F32):
        return T(self.es.enter_context(self.nc.psum_tensor(name, list(shape), dtype)))

    def dram(self, name, shape, dtype=F32, kind="Internal"):
        return self.nc.dram_tensor(name, list(shape), dtype, kind=kind)

    def _wait(self, e, deps):
        eng = self.eng[e]
        best = {}
        for d in deps:
            if d is None:
                continue
            s, v, src = d
            if src == e and e == "pe":
                continue
            if self.waited[e].get(s, 0) >= v:
                continue
            if best.get(s, 0) < v:
                best[s] = v
        for s, v in best.items():
            eng.wait_ge(self.sems[s], v)
            self.waited[e][s] = v

    def _collect(self, r, w):
        deps = []
        for x in r:
            for rs in x.res:
                deps.append(rs.w)
        for x in w:
            for rs in x.res:
                deps.append(rs.w)
                deps.extend(rs.r.values())
        return deps

    def _commit(self, r, w, dep, key):
        for x in r:
            for rs in x.res:
                rs.r[key] = dep
        for x in w:
            for rs in x.res:
                rs.w = dep
                rs.r = {}

    def op(self, e, fn, r=(), w=()):
        self._wait(e, self._collect(r, w))
        inst = fn(self.eng[e])
        self.cnt[e] += 1
        inst.then_inc(self.sems[self.esem[e]], 1)
        dep = (self.esem[e], self.cnt[e], e)
        self._commit(r, w, dep, e)
        self.ninst += 1
        return inst

    def dma(self, q, out, in_, r=(), w=(), fn=None):
        i = self.drr
        self.drr = (i + 1) % len(self.dsem)
        s = self.dsem[i]
        deps = self._collect(r, w)
        deps.append((s, self.duses[i] * 16, "dma"))
        self._wait(q, deps)
        eng = self.eng[q]
        if fn is None:
            inst = eng.dma_start(out=out, in_=in_)
        else:
            inst = fn(eng)
        inst.then_inc(self.sems[s], 16)
        self.duses[i] += 1
        dep = (s, self.duses[i] * 16, "dma")
        self._commit(r, w, dep, ("dma", s))
        self.ninst += 1
        return inst

    def dma_bg(self, q, out, in_):
        i = self.bgrr
        self.bgrr = (i + 1) % len(self.bgsem)
        s = self.bgsem[i]
        self._wait(q, [(s, self.bguses[i] * 16, "dma")])
        self.eng[q].dma_start(out=out, in_=in_).then_inc(self.sems[s], 16)
        self.bguses[i] += 1
        self.ninst += 1

    def wait_bg(self):
        deps = [(s, self.bguses[i] * 16, "dma") for i, s in enumerate(self.bgsem)]
        for e in self.eng:
            self._wait(e, deps)

    def barrier(self):
        deps = [(self.esem[e], self.cnt[e], "x") for e in self.eng]
        deps += [(s, self.duses[i] * 16, "x") for i, s in enumerate(self.dsem)]
        for e in self.eng:
            self._wait(e, [d for d in deps if d[0] != self.esem[e]])

    def close(self):
        self.barrier()
        self.es.close()


class Cfg:
    def __init__(self, NB=2, S=4096, kinds=(0, 1, 2, 0), NC=8, last_noctx=True, debug=False):
        self.debug = debug
        self.NB = NB
        self.S = S
        self.kinds = list(kinds)
        self.NC = NC
        self.last_noctx = last_noctx


def build(cfg):
    NB, S, kinds = cfg.NB, cfg.S, cfg.kinds
    L = len(kinds)
    Tn = C + S
    NT = Tn // 128
    NS = NB + 1
    kb = KB()
    nc = kb.nc
    op, dma = kb.op, kb.dma

    def din(name, shape, dt=F32):
        return kb.dram(name, shape, dt, kind="ExternalInput")

    xin = din("xin", [NB * Tn, D])
    cc = din("cc", [NS, D])
    ada_w = din("ada_w", [L * D, 6 * D])
    ada_b = din("ada_b", [L, 6 * D])
    lnp = din("lnp", [L * 4, D])
    router_w = din("router_w", [L * D, E])
    router_b = din("router_b", [L, E])
    wgu = din("wgu", [L * E * D, 2 * FF])
    bgu = din("bgu", [L * E, 2 * FF])
    wdn = din("wdn", [L * E * FF, D])
    bdn = din("bdn", [L * E, D])
    cmat = din("cmat", [128, 384 + 32])
    w_id = din("w_id", [D, D]) if -1 in kinds else None
    n_rg = max(1, kinds.count(0))
    if 2 in kinds:
        n_ml = kinds.count(2)
        ml_wd = din("ml_wd", [n_ml * D, 448])
        ml_norm = din("ml_norm", [n_ml, 384])
        ml_wuq = din("ml_wuq", [n_ml * 256, 2048])
        ml_wukv = din("ml_wukv", [n_ml * 128, 2048])
        ml_w_o = din("ml_w_o", [n_ml * D, D])
        rope32 = din("rope32", [64, S])
        CT = kb.dram("CT", [384, Tn])
        KRT = kb.dram("KRT", [32, Tn])
    if 1 in kinds:
        n_gq = kinds.count(1)
        gq_w = din("gq_w", [n_gq * D, 2432])
        gq_b64 = din("gq_b64", [n_gq * 64, 40])
        gq_bv = din("gq_bv", [n_gq, 128])
        gq_sinks = din("gq_sinks", [n_gq, 16])
        gq_w_o = din("gq_w_o", [n_gq * D, D])
        gq_b_o = din("gq_b_o", [n_gq, D])
        rope64 = din("rope64", [2 * 64, S])
        QT = kb.dram("QT", [64, 16 * Tn])
        KT = kb.dram("KT", [64, 2 * Tn])
        VA = kb.dram("VA", [Tn, 130])
    if 0 in kinds:
        rg_w_in = din("rg_w_in", [n_rg * D, 2 * D_RNN])
        rg_p = din("rg_p", [n_rg * 11, D_RNN])
        rg_ga = din("rg_ga", [n_rg * 2 * 6 * 256, 256])
        rg_gx = din("rg_gx", [n_rg * 2 * 6 * 256, 256])
        rg_w_out = din("rg_w_out", [n_rg * D_RNN, D])
        PR = kb.dram("PR", [2 * D_RNN, Tn])
    out = kb.dram("out", [NB * S, D], F32, kind="ExternalOutput")

    dk = "ExternalOutput" if cfg.debug else "Internal"
    XA = kb.dram("XA", [NB * Tn, D], F32, dk)
    XB = kb.dram("XB", [NB * Tn, D], F32, dk)
    H2 = kb.dram("H2", [NB * Tn, D], F32, dk)
    HT = kb.dram("HT", [D, Tn], BF16, dk)
    OT = kb.dram("OT", [D_RNN, Tn], BF16, dk)
    MODD = kb.dram("MODD", [NS, 6 * D], F32, dk)
    n_asg_max = NB * Tn * 4
    NBLK = (n_asg_max + E * (SB - 1) + SB - 1) // SB
    NSLOT = NBLK * SB
    WGU16 = kb.dram("WGU16", [E * 128, 8 * 2 * FF], BF16)
    WDN16 = kb.dram("WDN16", [E * 128, 8 * D], BF16)
    BB16 = kb.dram("BB16", [E, 3 * D], BF16)
    XS = kb.dram("XS", [NSLOT, D], F32, dk)
    YS = kb.dram("YS", [NSLOT, D], F32, dk)
    if cfg.debug:
        DBG = {n: kb.dram("dbg_" + n, shp, dt, "ExternalOutput") for n, shp, dt in [
            ("slot4", [128, NB * NT * 4], I32), ("gate4", [128, NB * NT * 4], F32), ("top8", [128, NB * NT * 8], F32),
            ("bef", [128, NBLK], F32), ("runcnt", [128, E], F32), ("excl", [128, E], F32), ("padf", [128, E], F32), ("incl", [128, E], F32), ("posA", [128, NB * NT * E], F32)]}

    cm = kb.sb("cm", [128, 416])
    ident = lambda n=128: cm[0:n, 0:n]
    tri = cm[:, 128:256]
    ones = cm[:, 256:384]
    iota32 = cm[:, 384:416]
    dma("sp", cm[:], cmat[:, :], w=[cm])
    PS = [kb.ps("ps%d" % i, [128, 512]) for i in range(8)]
    psi = [0]
    psn = [8]

    def nps():
        psi[0] = (psi[0] + 1) % psn[0]
        return PS[psi[0]]

    siluT = kb.sb("siluT", [128, 8, NS])
    modT = kb.sb("modT", [128, 48, NS])
    lnrow = kb.sb("lnrow", [128, 4, D])
    epsT = kb.sb("epsT", [128, 1])
    op("dve", lambda e: e.memset(epsT[:], LN_EPS), w=[epsT])

    kb.push()
    ccs = kb.sb("ccs", [NS, D])
    dma("sp", ccs[:], cc[:, :], w=[ccs])
    op("act", lambda e: e.activation(out=ccs[:], in_=ccs[:], func=AF.Silu), r=[ccs], w=[ccs])
    p = nps()
    for c in range(8):
        op("pe", lambda e: e.transpose(p[:, c * NS:(c + 1) * NS], ccs[:, c * 128:(c + 1) * 128], ident(NS)), r=[ccs, cm], w=[p])
    op("dve", lambda e: e.tensor_copy(out=siluT[:].rearrange("p c s -> p (c s)"), in_=p[:, 0:8 * NS]), r=[p], w=[siluT])
    kb.pop()

    def phase_mod(i):
        kb.push()
        aw = [kb.sb("aw", [128, 8, 512]) for _ in range(2)]
        modtm = kb.sb("modtm", [NS, 6 * D])
        adab = kb.sb("adab", [NS, 6 * D])
        for s in range(NS):
            dma("sp", adab[s:s + 1, :], ada_b[i:i + 1, :], w=[adab])
        for g in range(12):
            a = aw[g % 2]
            dma("sp", a[:], ada_w[i * D:(i + 1) * D, g * 512:(g + 1) * 512].rearrange("(c p) n -> p c n", p=128), w=[a])
            p = nps()
            for kc in range(8):
                op("pe", lambda e: e.matmul(p[0:NS, :], siluT[:, kc, :], a[:, kc, :], start=(kc == 0), stop=(kc == 7)),
                   r=[siluT, a], w=[p])
            op("dve", lambda e: e.tensor_tensor(out=modtm[:, g * 512:(g + 1) * 512], in0=p[0:NS, :], in1=adab[:, g * 512:(g + 1) * 512], op=ALU.add),
               r=[p, adab], w=[modtm])
        for k in (1, 4):
            op("dve", lambda e: e.tensor_scalar(out=modtm[:, k * D:(k + 1) * D], in0=modtm[:, k * D:(k + 1) * D], scalar1=1.0, scalar2=None, op0=ALU.add),
               r=[modtm], w=[modtm])
        dma("sp", MODD[:, :], modtm[:], r=[modtm])
        p = nps()
        for j in range(48):
            op("pe", lambda e: e.transpose(p[:, j * NS:(j + 1) * NS], modtm[:, j * 128:(j + 1) * 128], ident(NS)), r=[modtm, cm], w=[p])
        op("dve", lambda e: e.tensor_copy(out=modT[:].rearrange("p c s -> p (c s)"), in_=p[:, 0:48 * NS]), r=[p], w=[modT])
        for k in range(4):
            dma("sp", lnrow[:, k, :], lnp[i * 4 + k, :].partition_broadcast(128), w=[lnrow])
        kb.pop()

    def groups():
        gs = [(0, C)]
        t = C
        while t < Tn:
            gs.append((t, min(512, Tn - t)))
            t += 512
        return gs

    def phase_h(i, b, Xsrc):
        kb.push()
        xt = [kb.sb("xt", [128, D]) for _ in range(8)]
        hT = [kb.sb("hT", [128, 8, 512], BF16) for _ in range(2)]
        n = 0
        for gi, (t0, tl) in enumerate(groups()):
            s = NB if t0 < C else b
            nt = tl // 128
            tiles = []
            for j in range(nt):
                x = xt[n % 8]
                n += 1
                dma("sp", x[:], Xsrc[b * Tn + t0 + j * 128: b * Tn + t0 + (j + 1) * 128, :], w=[x])
                tiles.append(x)
            h = hT[gi % 2]
            for c in range(8):
                p = nps()
                for j in range(nt):
                    x = tiles[j]
                    op("pe", lambda e: e.transpose(p[:, j * 128:(j + 1) * 128], x[:, c * 128:(c + 1) * 128], ident()), r=[x, cm], w=[p])
                op("dve", lambda e: e.tensor_scalar(out=h[:, c, 0:tl], in0=p[:, 0:tl], scalar1=modT[:, 8 + c, s:s + 1], scalar2=modT[:, c, s:s + 1],
                                                    op0=ALU.mult, op1=ALU.add), r=[p, modT], w=[h])
            dma("pool", HT[:, t0:t0 + tl].rearrange("(c p) t -> p c t", p=128), h[:, :, 0:tl], r=[h])
        kb.pop()

    def layernorm(t, k0, st, mv, tmp):
        for h2 in range(2):
            op("dve", lambda e: e.bn_stats(out=st[:, h2, :], in_=t[:, h2 * 512:(h2 + 1) * 512]), r=[t], w=[st])
        op("dve", lambda e: e.bn_aggr(out=mv[:, 0:2], in_=st[:].rearrange("p a b -> p (a b)")), r=[st], w=[mv])
        op("act", lambda e: e.activation(out=mv[:, 2:3], in_=mv[:, 1:2], func=AF.Sqrt, bias=epsT[:, 0:1], scale=1.0), r=[mv, epsT], w=[mv])
        op("dve", lambda e: e.reciprocal(out=mv[:, 3:4], in_=mv[:, 2:3]), r=[mv], w=[mv])
        op("dve", lambda e: e.tensor_scalar(out=t[:], in0=t[:], scalar1=mv[:, 0:1], scalar2=mv[:, 3:4], op0=ALU.subtract, op1=ALU.mult), r=[t, mv], w=[t])
        op("dve", lambda e: e.tensor_tensor(out=t[:], in0=t[:], in1=lnrow[:, k0, :], op=ALU.mult), r=[t, lnrow], w=[t])
        op("dve", lambda e: e.tensor_tensor(out=t[:], in0=t[:], in1=lnrow[:, k0 + 1, :], op=ALU.add), r=[t, lnrow], w=[t])

    NTT = NB * NT
    maskA = kb.sb("maskA", [128, NTT, E])
    top8 = kb.sb("top8", [128, NTT, 8])
    idx8 = kb.sb("idx8", [128, NTT, 8], U32)
    posA = kb.sb("posA", [128, NTT, E])
    runcnt = kb.sb("runcnt", [128, E])
    slot4f = kb.sb("slot4f", [128, NTT, 4])
    slot4 = kb.sb("slot4", [128, NTT, 4], I32)
    gate4 = kb.sb("gate4", [128, NTT, 4])

    def phase_out(i, b, Xsrc, w_o, Dm, b_o, need_ctx):
        kb.push()
        KC = Dm // 128
        wo = kb.sb("wo", [128, KC, D], BF16)
        for kc_ in range(KC):
            dma("pool", wo[:, kc_, :], w_o[kc_ * 128:(kc_ + 1) * 128, :], w=[wo])
        rw = kb.sb("rw", [128, 8, E])
        dma("sp", rw[:], router_w[i * D:(i + 1) * D, :].rearrange("(c p) n -> p c n", p=128), w=[rw])
        rbrow = kb.sb("rbrow", [128, E])
        dma("sp", rbrow[:], router_b[i, :].partition_broadcast(128), w=[rbrow])
        grow = {}
        for s in (NB, b):
            g = kb.sb("grow", [128, 3, D])
            for k, kind in enumerate((2, 4, 3)):
                dma("sp", g[:, k, :], MODD[s, kind * D:(kind + 1) * D].partition_broadcast(128), w=[g])
            grow[s] = g
        borow = None
        if b_o is not None:
            borow = kb.sb("borow", [128, D])
            dma("sp", borow[:], b_o.partition_broadcast(128), w=[borow])
        oTs = [kb.sb("oT", [128, KC, 128], BF16) for _ in range(3)]
        xts = [kb.sb("xt", [128, D]) for _ in range(3)]
        ts = [kb.sb("tt", [128, D]) for _ in range(3)]
        h2s = [kb.sb("h2", [128, D]) for _ in range(3)]
        h2Ts = [kb.sb("h2T", [128, 8, 128]) for _ in range(3)]
        st = kb.sb("st", [128, 2, 6])
        mv = kb.sb("mv", [128, 4])
        lg = kb.sb("lg", [128, E])
        t_start = 0 if need_ctx else C // 128
        for ti in range(t_start, NT):
            s = NB if ti < C // 128 else b
            g = grow[s]
            r0 = b * Tn + ti * 128
            gi = b * NT + ti
            oT = oTs[ti % 3]; x = xts[ti % 3]; t = ts[ti % 3]; h2 = h2s[ti % 3]; h2T = h2Ts[ti % 3]
            dma("sp", oT[:], OT[0:Dm, ti * 128:(ti + 1) * 128].rearrange("(c p) t -> p c t", p=128), w=[oT])
            dma("sp", x[:], Xsrc[r0:r0 + 128, :], w=[x])
            for hf in range(2):
                p = nps()
                for kc in range(KC):
                    op("pe", lambda e: e.matmul(p[:, :], oT[:, kc, :], wo[:, kc, hf * 512:(hf + 1) * 512], start=(kc == 0), stop=(kc == KC - 1)),
                       r=[oT, wo], w=[p])
                sl = slice(hf * 512, (hf + 1) * 512)
                if borow is not None:
                    op("dve", lambda e: e.tensor_tensor(out=t[:, sl], in0=p[:, :], in1=borow[:, sl], op=ALU.add), r=[p, borow], w=[t])
                    op("dve", lambda e: e.tensor_tensor(out=t[:, sl], in0=t[:, sl], in1=g[:, 0, sl], op=ALU.mult), r=[t, g], w=[t])
                else:
                    op("dve", lambda e: e.tensor_tensor(out=t[:, sl], in0=p[:, :], in1=g[:, 0, sl], op=ALU.mult), r=[p, g], w=[t])
            op("dve", lambda e: e.scalar_tensor_tensor(out=t[:], in0=x[:], scalar=ALPHA, in1=t[:], op0=ALU.mult, op1=ALU.add), r=[x, t], w=[t])
            layernorm(t, 0, st, mv, None)
            dma("pool", XA[r0:r0 + 128, :], t[:], r=[t])
            op("pool", lambda e: e.tensor_tensor(out=h2[:], in0=t[:], in1=g[:, 1, :], op=ALU.mult), r=[t, g], w=[h2])
            op("pool", lambda e: e.tensor_tensor(out=h2[:], in0=h2[:], in1=g[:, 2, :], op=ALU.add), r=[h2, g], w=[h2])
            dma("pool", H2[r0:r0 + 128, :], h2[:], r=[h2])
            for half in range(2):
                p = nps()
                for c4 in range(4):
                    c = half * 4 + c4
                    op("pe", lambda e: e.transpose(p[:, c4 * 128:(c4 + 1) * 128], h2[:, c * 128:(c + 1) * 128], ident()), r=[h2, cm], w=[p])
                op("act", lambda e: e.activation(out=h2T[:, half * 4:(half + 1) * 4, :].rearrange("p c t -> p (c t)"), in_=p[:, :], func=AF.Copy),
                   r=[p], w=[h2T])
            p = nps()
            for c in range(8):
                op("pe", lambda e: e.matmul(p[:, 0:E], h2T[:, c, :], rw[:, c, :], start=(c == 0), stop=(c == 7)), r=[h2T, rw], w=[p])
            op("dve", lambda e: e.tensor_tensor(out=lg[:], in0=p[:, 0:E], in1=rbrow[:], op=ALU.add), r=[p, rbrow], w=[lg])
            op("dve", lambda e: e.max(out=top8[:, gi, :], in_=lg[:]), r=[lg], w=[top8])
            op("dve", lambda e: e.max_index(out=idx8[:, gi, :], in_max=top8[:, gi, :], in_values=lg[:]), r=[lg, top8], w=[idx8])
            op("dve", lambda e: e.tensor_scalar(out=maskA[:, gi, :], in0=lg[:], scalar1=top8[:, gi, 3:4], scalar2=None, op0=ALU.is_ge), r=[lg, top8], w=[maskA])
        kb.pop()

    def phase_moe(i, last):
        tiles = []
        for b in range(NB):
            for ti in range(NT):
                if last and cfg.last_noctx and ti < C // 128:
                    continue
                tiles.append((b, ti))
        kb.push()
        op("dve", lambda e: e.memset(runcnt[:], 0.0), w=[runcnt])
        for (b, ti) in tiles:
            gi = b * NT + ti
            p = nps()
            op("pe", lambda e: e.matmul(p[:, 0:E], tri, maskA[:, gi, :], start=True, stop=True), r=[cm, maskA], w=[p])
            op("pe", lambda e: e.matmul(p[:, E:2 * E], ones, maskA[:, gi, :], start=True, stop=True), r=[cm, maskA], w=[p])
            op("dve", lambda e: e.tensor_tensor(out=posA[:, gi, :], in0=p[:, 0:E], in1=runcnt[:], op=ALU.add), r=[p, runcnt], w=[posA])
            op("dve", lambda e: e.tensor_tensor(out=runcnt[:], in0=p[:, E:2 * E], in1=runcnt[:], op=ALU.add), r=[p, runcnt], w=[runcnt])
        padf = kb.sb("padf", [128, E])
        incl = kb.sb("incl", [128, E])
        excl = kb.sb("excl", [128, E])
        zer = kb.sb("zer", [128, E])
        op("dve", lambda e: e.memset(zer[:], 0.0), w=[zer])
        blkpos = kb.sb("blkpos", [128, NBLK])
        tmpb = kb.sb("tmpb", [128, NBLK])
        op("pool", lambda e: e.iota(blkpos[:], pattern=[[SB, NBLK]], base=0, channel_multiplier=0, allow_small_or_imprecise_dtypes=True), w=[blkpos])
        for ex in range(E):
            op("dve", lambda e: e.tensor_scalar(out=tmpb[:], in0=blkpos[:], scalar1=runcnt[:, ex:ex + 1], scalar2=None, op0=ALU.is_lt, op1=ALU.add,
                                                accum_out=padf[:, ex:ex + 1]), r=[blkpos, runcnt], w=[tmpb, padf])
        op("dve", lambda e: e.tensor_scalar(out=padf[:], in0=padf[:], scalar1=float(SB), scalar2=None, op0=ALU.mult), r=[padf], w=[padf])
        op("dve", lambda e: e.tensor_tensor_scan(out=incl[:], data0=padf[:], data1=zer[:], initial=0.0, op0=ALU.add, op1=ALU.add), r=[padf, zer], w=[incl])
        op("dve", lambda e: e.tensor_tensor(out=excl[:], in0=incl[:], in1=padf[:], op=ALU.subtract), r=[incl, padf], w=[excl])
        bef = kb.sb("bef", [128, NBLK])
        op("dve", lambda e: e.memset(bef[:], 0.0), w=[bef])
        for ex in range(E):
            op("dve", lambda e: e.tensor_scalar(out=tmpb[:], in0=blkpos[:], scalar1=incl[:, ex:ex + 1], scalar2=None, op0=ALU.is_ge), r=[blkpos, incl], w=[tmpb])
            op("dve", lambda e: e.tensor_tensor(out=bef[:], in0=bef[:], in1=tmpb[:], op=ALU.add), r=[bef, tmpb], w=[bef])
        op("dve", lambda e: e.tensor_scalar(out=bef[:], in0=bef[:], scalar1=float(E - 1), scalar2=None, op0=ALU.min), r=[bef], w=[bef])
        idxf = kb.sb("idxf", [128, 4])
        oh = kb.sb("oh", [128, 4, E])
        slt = kb.sb("slt", [128, E])
        den = kb.sb("den", [128, 1])
        nb0 = kb.sb("nb0", [128, 1])
        for (b, ti) in tiles:
            gi = b * NT + ti
            op("dve", lambda e: e.tensor_copy(out=idxf[:], in_=idx8[:, gi, 0:4]), r=[idx8], w=[idxf])
            op("dve", lambda e: e.tensor_tensor(out=slt[:], in0=posA[:, gi, :], in1=excl[:], op=ALU.add), r=[posA, excl], w=[slt])
            for k in range(4):
                op("dve", lambda e: e.tensor_scalar(out=oh[:, k, :], in0=iota32, scalar1=idxf[:, k:k + 1], scalar2=None, op0=ALU.is_equal), r=[cm, idxf], w=[oh])
                op("dve", lambda e: e.tensor_tensor(out=oh[:, k, :], in0=oh[:, k, :], in1=slt[:], op=ALU.mult), r=[oh, slt], w=[oh])
            op("dve", lambda e: e.reduce_sum(out=slot4f[:, gi, :], in_=oh[:], axis=AX.X), r=[oh], w=[slot4f])
            op("dve", lambda e: e.tensor_copy(out=slot4[:, gi, :], in_=slot4f[:, gi, :]), r=[slot4f], w=[slot4])
            op("dve", lambda e: e.tensor_scalar(out=nb0[:], in0=top8[:, gi, 0:1], scalar1=-1.0, scalar2=None, op0=ALU.mult), r=[top8], w=[nb0])
            op("act", lambda e: e.activation(out=gate4[:, gi, :], in_=top8[:, gi, 0:4], func=AF.Exp, bias=nb0[:, 0:1], scale=1.0), r=[top8, nb0], w=[gate4])
            op("dve", lambda e: e.reduce_sum(out=den[:], in_=gate4[:, gi, :], axis=AX.X), r=[gate4], w=[den])
            op("dve", lambda e: e.reciprocal(out=den[:], in_=den[:]), r=[den], w=[den])
            op("dve", lambda e: e.tensor_scalar(out=gate4[:, gi, :], in0=gate4[:, gi, :], scalar1=den[:, 0:1], scalar2=None, op0=ALU.mult), r=[gate4, den], w=[gate4])
        if cfg.debug:
            dma("sp", DBG["slot4"][:, :], slot4[:].rearrange("p a b -> p (a b)"), r=[slot4])
            dma("sp", DBG["gate4"][:, :], gate4[:].rearrange("p a b -> p (a b)"), r=[gate4])
            dma("sp", DBG["top8"][:, :], top8[:].rearrange("p a b -> p (a b)"), r=[top8])
            dma("sp", DBG["posA"][:, :], posA[:].rearrange("p a b -> p (a b)"), r=[posA])
            dma("sp", DBG["bef"][:, :], bef[:], r=[bef])
            dma("sp", DBG["runcnt"][:, :], runcnt[:], r=[runcnt])
            dma("sp", DBG["excl"][:, :], excl[:], r=[excl])
            dma("sp", DBG["padf"][:, :], padf[:], r=[padf])
            dma("sp", DBG["incl"][:, :], incl[:], r=[incl])
        kb.push()
        h2s = [kb.sb("h2d", [128, D]) for _ in range(3)]
        for n, (b, ti) in enumerate(tiles):
            gi = b * NT + ti
            r0 = b * Tn + ti * 128
            h2 = h2s[n % 3]
            dma("sp", h2[:], H2[r0:r0 + 128, :], w=[h2])
            for k in range(4):
                dma("pool", None, None, r=[h2, slot4], fn=lambda e: e.indirect_dma_start(
                    out=XS[:, :], out_offset=bass.IndirectOffsetOnAxis(ap=slot4[:, gi, k:k + 1], axis=0), in_=h2[:], in_offset=None))
        kb.pop()
        kb.push()
        wg = [kb.sb("wg", [128, 8, 2 * FF], BF16) for _ in range(2)]
        wd = [kb.sb("wd", [128, 8, D], BF16) for _ in range(2)]
        brow = [kb.sb("brow", [128, 3 * D], BF16) for _ in range(2)]
        onesb = kb.sb("onesb", [1, SB], BF16)
        op("dve", lambda e: e.memset(onesb[:], 1.0), w=[onesb])
        iotap = kb.sb("iotap", [128, 8])
        op("pool", lambda e: e.iota(iotap[:], pattern=[[128, 8]], base=0, channel_multiplier=1, allow_small_or_imprecise_dtypes=True), w=[iotap])
        widx_f = kb.sb("widxf", [128, 9])
        widx = [kb.sb("widx", [128, 9], I32) for _ in range(3)]
        xs = [kb.sb("xs", [128, D]) for _ in range(3)]
        xT = kb.sb("xTm", [128, 8, SB], BF16)
        aT = kb.sb("aTm", [128, 8, SB], BF16)
        gsb = [kb.sb("gsb", [128, SB]) for _ in range(2)]
        sgb = [kb.sb("sgb", [128, SB]) for _ in range(2)]
        usb = [kb.sb("usb", [128, SB]) for _ in range(2)]
        ysb = [kb.sb("ysb", [128, D]) for _ in range(2)]
        be_l = kb.sb("be_l", [128, NBLK])
        op("dve", lambda e: e.tensor_copy(out=be_l[:], in_=bef[:]), r=[bef], w=[be_l])
        kb.wait_bg()

        def load_w(blk):
            wi = widx[blk % 3]
            op("dve", lambda e: e.scalar_tensor_tensor(out=widx_f[:, 0:1], in0=be_l[:, blk:blk + 1], scalar=128.0, in1=iotap[:, 0:1], op0=ALU.mult, op1=ALU.add),
               r=[be_l, iotap], w=[widx_f])
            op("dve", lambda e: e.tensor_copy(out=widx_f[:, 1:2], in_=be_l[:, blk:blk + 1]), r=[be_l], w=[widx_f])
            op("dve", lambda e: e.tensor_copy(out=wi[:, 0:2], in_=widx_f[:, 0:2]), r=[widx_f], w=[wi])
            w1 = wg[blk % 2]; w2 = wd[blk % 2]; br = brow[blk % 2]
            dma("pool", None, None, r=[wi], w=[w1], fn=lambda e: e.indirect_dma_start(
                out=w1[:].rearrange("p c n -> p (c n)"), out_offset=None, in_=WGU16[:, :], in_offset=bass.IndirectOffsetOnAxis(ap=wi[:, 0:1], axis=0)))
            dma("pool", None, None, r=[wi], w=[w2], fn=lambda e: e.indirect_dma_start(
                out=w2[:].rearrange("p c n -> p (c n)"), out_offset=None, in_=WDN16[:, :], in_offset=bass.IndirectOffsetOnAxis(ap=wi[:, 0:1], axis=0)))
            dma("pool", None, None, r=[wi], w=[br], fn=lambda e: e.indirect_dma_start(
                out=br[:, :], out_offset=None, in_=BB16[:, :], in_offset=bass.IndirectOffsetOnAxis(ap=wi[:, 1:2], axis=0)))

        load_w(0)
        nxs = 0
        for blk in range(NBLK):
            if blk + 1 < NBLK:
                load_w(blk + 1)
            w1 = wg[blk % 2]; w2 = wd[blk % 2]; br = brow[blk % 2]
            for j in range(SB // 128):
                x = xs[nxs % 3]; nxs += 1
                dma("sp", x[:], XS[blk * SB + j * 128: blk * SB + (j + 1) * 128, :], w=[x])
                for half in range(2):
                    p = nps()
                    for c4 in range(4):
                        c = half * 4 + c4
                        op("pe", lambda e: e.transpose(p[:, c4 * 128:(c4 + 1) * 128], x[:, c * 128:(c + 1) * 128], ident()), r=[x, cm], w=[p])
                    op("act", lambda e: e.activation(out=xT[:, half * 4:(half + 1) * 4, j * 128:(j + 1) * 128], in_=p[:, :].rearrange("p (c t) -> p c t", c=4), func=AF.Copy),
                       r=[p], w=[xT])
            for fc in range(8):
                g_ = gsb[fc % 2]; s_ = sgb[fc % 2]; u_ = usb[fc % 2]
                pg = nps()
                for kc in range(8):
                    op("pe", lambda e: e.matmul(pg[:, :], w1[:, kc, fc * 128:(fc + 1) * 128], xT[:, kc, :], start=(kc == 0), stop=False), r=[w1, xT], w=[pg])
                op("pe", lambda e: e.matmul(pg[:, :], br[0:1, fc * 128:(fc + 1) * 128], onesb[0:1, :], start=False, stop=True), r=[br, onesb], w=[pg])
                pu = nps()
                for kc in range(8):
                    op("pe", lambda e: e.matmul(pu[:, :], w1[:, kc, FF + fc * 128:FF + (fc + 1) * 128], xT[:, kc, :], start=(kc == 0), stop=False), r=[w1, xT], w=[pu])
                op("pe", lambda e: e.matmul(pu[:, :], br[0:1, FF + fc * 128:FF + (fc + 1) * 128], onesb[0:1, :], start=False, stop=True), r=[br, onesb], w=[pu])
                op("dve", lambda e: e.tensor_scalar(out=g_[:], in0=pg[:, :], scalar1=7.0, scalar2=None, op0=ALU.min), r=[pg], w=[g_])
                op("act", lambda e: e.activation(out=s_[:], in_=g_[:], func=AF.Sigmoid, scale=1.702), r=[g_], w=[s_])
                op("dve", lambda e: e.tensor_scalar(out=u_[:], in0=pu[:, :], scalar1=7.0, scalar2=-7.0, op0=ALU.min, op1=ALU.max), r=[pu], w=[u_])
                op("dve", lambda e: e.scalar_tensor_tensor(out=u_[:], in0=u_[:], scalar=1.0, in1=g_[:], op0=ALU.add, op1=ALU.mult), r=[u_, g_], w=[u_])
                op("pool", lambda e: e.tensor_tensor(out=aT[:, fc, :], in0=u_[:], in1=s_[:], op=ALU.mult), r=[u_, s_], w=[aT])
            for j in range(SB // 128):
                y = ysb[j % 2]
                for hf in range(2):
                    p = nps()
                    for fc in range(8):
                        op("pe", lambda e: e.matmul(p[:, :], aT[:, fc, j * 128:(j + 1) * 128], w2[:, fc, hf * 512:(hf + 1) * 512], start=(fc == 0), stop=False), r=[aT, w2], w=[p])
                    op("pe", lambda e: e.matmul(p[:, :], onesb[0:1, 0:128], br[0:1, 2 * FF + hf * 512:2 * FF + (hf + 1) * 512], start=False, stop=True), r=[br, onesb], w=[p])
                    op("act", lambda e: e.activation(out=y[:, hf * 512:(hf + 1) * 512], in_=p[:, :], func=AF.Copy), r=[p], w=[y])
                dma("sp", YS[blk * SB + j * 128: blk * SB + (j + 1) * 128, :], y[:], r=[y])
        kb.pop()
        kb.push()
        grow = {}
        for s in range(NS):
            g = kb.sb("g2row", [128, D])
            dma("sp", g[:], MODD[s, 5 * D:6 * D].partition_broadcast(128), w=[g])
            grow[s] = g
        yk = [kb.sb("yk", [128, D]) for _ in range(8)]
        accs = [kb.sb("acc", [128, D]) for _ in range(3)]
        xts = [kb.sb("xt2", [128, D]) for _ in range(3)]
        st = kb.sb("st2", [128, 2, 6])
        mv = kb.sb("mv2", [128, 4])
        for n, (b, ti) in enumerate(tiles):
            gi = b * NT + ti
            r0 = b * Tn + ti * 128
            s = NB if ti < C // 128 else b
            acc = accs[n % 3]; x = xts[n % 3]
            dma("sp", x[:], XA[r0:r0 + 128, :], w=[x])
            for k in range(4):
                y = yk[(n % 2) * 4 + k]
                dma("pool", None, None, r=[slot4], w=[y], fn=lambda e: e.indirect_dma_start(
                    out=y[:], out_offset=None, in_=YS[:, :], in_offset=bass.IndirectOffsetOnAxis(ap=slot4[:, gi, k:k + 1], axis=0)))
                if k == 0:
                    op("dve", lambda e: e.tensor_scalar(out=acc[:], in0=y[:], scalar1=gate4[:, gi, 0:1], scalar2=None, op0=ALU.mult), r=[y, gate4], w=[acc])
                else:
                    op("dve", lambda e: e.scalar_tensor_tensor(out=acc[:], in0=y[:], scalar=gate4[:, gi, k:k + 1], in1=acc[:], op0=ALU.mult, op1=ALU.add), r=[y, gate4, acc], w=[acc])
            op("dve", lambda e: e.tensor_tensor(out=acc[:], in0=acc[:], in1=grow[s][:], op=ALU.mult), r=[acc, grow[s]], w=[acc])
            op("dve", lambda e: e.scalar_tensor_tensor(out=acc[:], in0=x[:], scalar=ALPHA, in1=acc[:], op0=ALU.mult, op1=ALU.add), r=[x, acc], w=[acc])
            layernorm(acc, 2, st, mv, None)
            if last:
                if ti >= C // 128:
                    ro = b * S + (ti - C // 128) * 128
                    dma("sp", out[ro:ro + 128, :], acc[:], r=[acc])
            else:
                dma("sp", XB[r0:r0 + 128, :], acc[:], r=[acc])
        kb.pop()
        kb.pop()

    onesrow_t = kb.sb("onesrow", [1, SB])
    onesrow = onesrow_t
    op("dve", lambda e: e.memset(onesrow_t[:], 1.0), w=[onesrow_t])

    def mixer_identity(i, b):
        kb.push()
        buf = [kb.sb("cp", [128, 8, 512], BF16) for _ in range(2)]
        for gi, (t0, tl) in enumerate(groups()):
            bb = buf[gi % 2]
            dma("sp", bb[:, :, 0:tl], HT[:, t0:t0 + tl].rearrange("(c p) t -> p c t", p=128), w=[bb])
            dma("sp", OT[0:D, t0:t0 + tl].rearrange("(c p) t -> p c t", p=128), bb[:, :, 0:tl], r=[bb])
        kb.pop()
        return w_id[:, :], D, None

    def mixer_rg(i, b, j, need_ctx):
        kb.push()
        win = kb.sb("win", [128, 8, 2 * D_RNN], BF16)
        for kc_ in range(8):
            dma("pool", win[:, kc_, :], rg_w_in[j * D + kc_ * 128:j * D + (kc_ + 1) * 128, :], w=[win])
        hTs = [kb.sb("hTr", [128, 8, 512], BF16) for _ in range(2)]
        pos_ = [kb.sb("pout", [128, 4, 512]) for _ in range(2)]
        n = 0
        for gi, (t0, tl) in enumerate(groups()):
            h = hTs[gi % 2]
            dma("sp", h[:, :, 0:tl], HT[:, t0:t0 + tl].rearrange("(c p) t -> p c t", p=128), w=[h])
            for q in range(6):
                po = pos_[n % 2]; n += 1
                for jj in range(4):
                    jc = q * 4 + jj
                    p = nps()
                    for kc in range(8):
                        op("pe", lambda e: e.matmul(p[:, 0:tl], win[:, kc, jc * 128:(jc + 1) * 128], h[:, kc, 0:tl], start=(kc == 0), stop=(kc == 7)), r=[win, h], w=[p])
                    if jj % 2 == 0:
                        op("act", lambda e: e.activation(out=po[:, jj, 0:tl], in_=p[:, 0:tl], func=AF.Copy), r=[p], w=[po])
                    else:
                        op("dve", lambda e: e.tensor_copy(out=po[:, jj, 0:tl], in_=p[:, 0:tl]), r=[p], w=[po])
                dma("pool", PR[q * 512:(q + 1) * 512, t0:t0 + tl].rearrange("(c p) t -> p c t", p=128), po[:, :, 0:tl], r=[po])
        kb.pop()
        kb.push()
        prow = kb.sb("prow", [11, D_RNN])
        dma("sp", prow[:], rg_p[j * 11:(j + 1) * 11, :], w=[prow])
        chp = kb.sb("chp", [128, 12, 11])
        p = nps()
        for c in range(12):
            op("pe", lambda e: e.transpose(p[:, c * 11:(c + 1) * 11], prow[:, c * 128:(c + 1) * 128], ident(11)), r=[prow, cm], w=[p])
        op("dve", lambda e: e.tensor_copy(out=chp[:].rearrange("p c k -> p (c k)"), in_=p[:, 0:132]), r=[p], w=[chp])
        cdec = kb.sb("cdec", [128, 12, 4])
        op("act", lambda e: e.activation(out=cdec[:, :, 0:2], in_=chp[:, :, 9:11], func=AF.Exp, scale=-1.0), r=[chp], w=[cdec])
        op("act", lambda e: e.activation(out=cdec[:, :, 0:2], in_=cdec[:, :, 0:2], func=AF.Ln, bias=1.0, scale=1.0), r=[cdec], w=[cdec])
        op("dve", lambda e: e.tensor_scalar(out=cdec[:, :, 2:4], in0=cdec[:, :, 0:2], scalar1=-16.0, scalar2=None, op0=ALU.mult), r=[cdec], w=[cdec])
        op("dve", lambda e: e.tensor_scalar(out=cdec[:, :, 0:2], in0=cdec[:, :, 0:2], scalar1=-8.0, scalar2=None, op0=ALU.mult), r=[cdec], w=[cdec])
        xr = kb.sb("xr", [128, Tn])
        xc = kb.sb("xc", [128, 2, Tn])
        gl = kb.sb("gl", [128, Tn])
        aa = kb.sb("aa", [128, Tn])
        uu = kb.sb("uu", [128, Tn])
        hh = kb.sb("hh", [128, Tn])
        hs = kb.sb("hs", [128, Tn])
        gw = kb.sb("gw", [128, 2, 2, 2, 256])
        rr = [kb.sb("rr", [128, 512]) for _ in range(2)]
        ii = [kb.sb("ii", [128, 512]) for _ in range(2)]
        segs = [(0, C), (C, Tn)]
        for nb_ in range(6):
            for d in range(2):
                base = ((j * 2 + d) * 6 + nb_) * 256
                dma("sp", gw[:, d, 0, :, :], rg_ga[base:base + 256, :].rearrange("(c p) n -> p c n", p=128), w=[gw])
                dma("sp", gw[:, d, 1, :, :], rg_gx[base:base + 256, :].rearrange("(c p) n -> p c n", p=128), w=[gw])
            for cc_ in range(2):
                ch = nb_ * 2 + cc_
                dma("sp", xr[:], PR[D_RNN + ch * 128: D_RNN + (ch + 1) * 128, :], w=[xr])
                for (a0, b0) in segs:
                    op("dve", lambda e: e.tensor_scalar(out=xc[:, cc_, a0:b0], in0=xr[:, a0:b0], scalar1=chp[:, ch, 1:2], scalar2=chp[:, ch, 4:5], op0=ALU.mult, op1=ALU.add), r=[xr, chp], w=[xc])
                    op("dve", lambda e: e.scalar_tensor_tensor(out=xc[:, cc_, a0 + 1:b0], in0=xr[:, a0:b0 - 1], scalar=chp[:, ch, 0:1], in1=xc[:, cc_, a0 + 1:b0], op0=ALU.mult, op1=ALU.add), r=[xr, chp, xc], w=[xc])
                    op("dve", lambda e: e.scalar_tensor_tensor(out=xc[:, cc_, a0:b0 - 1], in0=xr[:, a0 + 1:b0], scalar=chp[:, ch, 2:3], in1=xc[:, cc_, a0:b0 - 1], op0=ALU.mult, op1=ALU.add), r=[xr, chp, xc], w=[xc])
                    op("dve", lambda e: e.scalar_tensor_tensor(out=xc[:, cc_, a0:b0 - 2], in0=xr[:, a0 + 2:b0], scalar=chp[:, ch, 3:4], in1=xc[:, cc_, a0:b0 - 2], op0=ALU.mult, op1=ALU.add), r=[xr, chp, xc], w=[xc])
            for oc in range(2):
                ch = nb_ * 2 + oc
                dma("sp", gl[:], PR[ch * 128:(ch + 1) * 128, :], w=[gl])
                for d in range(2):
                    for gi, (t0, tl) in enumerate(groups()):
                        r_ = rr[gi % 2]; i_ = ii[gi % 2]
                        pa = nps()
                        for kc in range(2):
                            op("pe", lambda e: e.matmul(pa[:, 0:tl], gw[:, d, 0, kc, oc * 128:(oc + 1) * 128], xc[:, kc, t0:t0 + tl], start=(kc == 0), stop=(kc == 1)), r=[gw, xc], w=[pa])
                        px = nps()
                        for kc in range(2):
                            op("pe", lambda e: e.matmul(px[:, 0:tl], gw[:, d, 1, kc, oc * 128:(oc + 1) * 128], xc[:, kc, t0:t0 + tl], start=(kc == 0), stop=(kc == 1)), r=[gw, xc], w=[px])
                        op("act", lambda e: e.activation(out=r_[:, 0:tl], in_=pa[:, 0:tl], func=AF.Sigmoid, bias=chp[:, ch, 5 + d:6 + d], scale=1.0), r=[pa, chp], w=[r_])
                        op("act", lambda e: e.activation(out=i_[:, 0:tl], in_=px[:, 0:tl], func=AF.Sigmoid, bias=chp[:, ch, 7 + d:8 + d], scale=1.0), r=[px, chp], w=[i_])
                        op("act", lambda e: e.activation(out=aa[:, t0:t0 + tl], in_=r_[:, 0:tl], func=AF.Exp, scale=cdec[:, ch, d:d + 1]), r=[r_, cdec], w=[aa])
                        op("act", lambda e: e.activation(out=r_[:, 0:tl], in_=r_[:, 0:tl], func=AF.Exp, scale=cdec[:, ch, 2 + d:3 + d]), r=[r_, cdec], w=[r_])
                        op("act", lambda e: e.activation(out=r_[:, 0:tl], in_=r_[:, 0:tl], func=AF.Sqrt, bias=1.0, scale=-1.0), r=[r_], w=[r_])
                        op("dve", lambda e: e.tensor_tensor(out=i_[:, 0:tl], in0=i_[:, 0:tl], in1=xc[:, oc, t0:t0 + tl], op=ALU.mult), r=[i_, xc], w=[i_])
                        op("dve", lambda e: e.tensor_tensor(out=uu[:, t0:t0 + tl], in0=i_[:, 0:tl], in1=r_[:, 0:tl], op=ALU.mult), r=[i_, r_], w=[uu])
                    if d == 0:
                        op("dve", lambda e: e.tensor_tensor_scan(out=hs[:, :], data0=aa[:, :], data1=uu[:, :], initial=0.0, op0=ALU.mult, op1=ALU.add), r=[aa, uu], w=[hs])
                    else:
                        op("dve", lambda e: e.tensor_tensor_scan(out=hh[:, 0:C][:, ::-1], data0=aa[:, 0:C][:, ::-1], data1=uu[:, 0:C][:, ::-1], initial=0.0, op0=ALU.mult, op1=ALU.add), r=[aa, uu], w=[hh])
                        op("dve", lambda e: e.tensor_tensor_scan(out=hh[:, C:Tn][:, ::-1], data0=aa[:, C:Tn][:, ::-1], data1=uu[:, C:Tn][:, ::-1], initial=hh[:, 0:1], op0=ALU.mult, op1=ALU.add), r=[aa, uu, hh], w=[hh])
                        op("pool", lambda e: e.tensor_tensor(out=hs[:], in0=hs[:], in1=hh[:], op=ALU.add), r=[hs, hh], w=[hs])
                op("pool", lambda e: e.tensor_tensor(out=hh[:], in0=gl[:], in1=gl[:], op=ALU.mult), r=[gl], w=[hh])
                op("dve", lambda e: e.tensor_scalar(out=hh[:], in0=hh[:], scalar1=0.044715, scalar2=1.0, op0=ALU.mult, op1=ALU.add), r=[hh], w=[hh])
                op("pool", lambda e: e.tensor_tensor(out=hh[:], in0=hh[:], in1=gl[:], op=ALU.mult), r=[hh, gl], w=[hh])
                op("act", lambda e: e.activation(out=hh[:], in_=hh[:], func=AF.Sigmoid, scale=1.5957691216057308), r=[hh], w=[hh])
                op("pool", lambda e: e.tensor_tensor(out=hh[:], in0=hh[:], in1=gl[:], op=ALU.mult), r=[hh, gl], w=[hh])
                op("dve", lambda e: e.tensor_tensor(out=hs[:], in0=hs[:], in1=hh[:], op=ALU.mult), r=[hs, hh], w=[hs])
                dma("pool", OT[ch * 128:(ch + 1) * 128, :], hs[:], r=[hs])
        kb.pop()
        return rg_w_out[j * D_RNN:(j + 1) * D_RNN, :], D_RNN, None

    def mixer_gqa(i, b, j, need_ctx):
        QT3 = QT.ap().rearrange("p (h t) -> p h t", h=16)
        KT3 = KT.ap().rearrange("p (h t) -> p h t", h=2)
        kb.push()
        w = kb.sb("gqw", [128, 8, 2432], BF16)
        for kc_ in range(8):
            dma("pool", w[:, kc_, :], gq_w[j * D + kc_ * 128:j * D + (kc_ + 1) * 128, :], w=[w])
        b64 = kb.sb("b64", [64, 40])
        dma("sp", b64[:], gq_b64[j * 64:(j + 1) * 64, :], w=[b64])
        bvrow = kb.sb("bvrow", [128, 128])
        dma("sp", bvrow[:], gq_bv[j, :].partition_broadcast(128), w=[bvrow])
        hTs = [kb.sb("hTg", [128, 8, 512], BF16) for _ in range(2)]
        qo = kb.sb("qo", [64, 16, 512])
        ko = kb.sb("ko", [64, 2, 512])
        cs = kb.sb("cs", [64, 512]); sn = kb.sb("sn", [64, 512])
        ta = [kb.sb("ta", [64, 512]) for _ in range(2)]
        tb = [kb.sb("tb", [64, 512]) for _ in range(2)]
        va = [kb.sb("va", [128, 2, 65]) for _ in range(2)]
        for v_ in va:
            op("dve", lambda e: e.memset(v_[:], 1.0), w=[v_])
        n = 0
        for gi, (t0, tl) in enumerate(groups()):
            h = hTs[gi % 2]
            lat = t0 >= C
            dma("sp", h[:, :, 0:tl], HT[:, t0:t0 + tl].rearrange("(c p) t -> p c t", p=128), w=[h])
            if lat:
                dma("sp", cs[:, 0:tl], rope64[0:64, t0 - C:t0 - C + tl], w=[cs])
                dma("sp", sn[:, 0:tl], rope64[64:128, t0 - C:t0 - C + tl], w=[sn])
            for hq in range(18):
                col = hq * 64 if hq < 16 else 2048 + (hq - 16) * 64
                colsw = 1024 + hq * 64 if hq < 16 else 2176 + (hq - 16) * 64
                dst = qo[:, hq, 0:tl] if hq < 16 else ko[:, hq - 16, 0:tl]
                dstT = qo if hq < 16 else ko
                p = nps()
                for kc in range(8):
                    op("pe", lambda e: e.matmul(p[0:64, 0:tl], w[:, kc, col:col + 64], h[:, kc, 0:tl], start=(kc == 0), stop=(kc == 7)), r=[w, h], w=[p])
                if lat:
                    p2 = nps()
                    for kc in range(8):
                        op("pe", lambda e: e.matmul(p2[0:64, 0:tl], w[:, kc, colsw:colsw + 64], h[:, kc, 0:tl], start=(kc == 0), stop=(kc == 7)), r=[w, h], w=[p2])
                    a_ = ta[n % 2]; b_ = tb[n % 2]; n += 1
                    op("dve", lambda e: e.scalar_tensor_tensor(out=a_[:, 0:tl], in0=p[0:64, 0:tl], scalar=b64[:, hq:hq + 1], in1=cs[:, 0:tl], op0=ALU.add, op1=ALU.mult), r=[p, b64, cs], w=[a_])
                    op("dve", lambda e: e.scalar_tensor_tensor(out=b_[:, 0:tl], in0=p2[0:64, 0:tl], scalar=b64[:, 20 + hq:21 + hq], in1=sn[:, 0:tl], op0=ALU.add, op1=ALU.mult), r=[p2, b64, sn], w=[b_])
                    op("pool", lambda e: e.tensor_tensor(out=dst, in0=a_[:, 0:tl], in1=b_[:, 0:tl], op=ALU.add), r=[a_, b_], w=[dstT])
                else:
                    op("dve", lambda e: e.tensor_scalar(out=dst, in0=p[0:64, 0:tl], scalar1=b64[:, hq:hq + 1], scalar2=None, op0=ALU.add), r=[p, b64], w=[dstT])
            dma("pool", QT3[:, :, t0:t0 + tl], qo[:, :, 0:tl], r=[qo])
            dma("pool", KT3[:, :, t0:t0 + tl], ko[:, :, 0:tl], r=[ko])
            for tt in range(tl // 128):
                v_ = va[tt % 2]
                p = nps()
                for kc in range(8):
                    op("pe", lambda e: e.matmul(p[:, 0:128], h[:, kc, tt * 128:(tt + 1) * 128], w[:, kc, 2304:2432], start=(kc == 0), stop=(kc == 7)), r=[w, h], w=[p])
                op("dve", lambda e: e.tensor_tensor(out=v_[:, :, 0:64], in0=p[:, 0:128].rearrange("p (h d) -> p h d", h=2), in1=bvrow[:].rearrange("p (h d) -> p h d", h=2), op=ALU.add), r=[p, bvrow], w=[v_])
                dma("pool", VA[t0 + tt * 128:t0 + (tt + 1) * 128, :], v_[:].rearrange("p h d -> p (h d)"), r=[v_])
        kb.pop()
        kb.push()
        kT = kb.sb("kT", [64, 2, Tn])
        dma("sp", kT[:], KT3[:, :, :], w=[kT])
        V = kb.sb("Vg", [128, NT, 130])
        dma("sp", V[:], VA.ap().rearrange("(n p) c -> p n c", p=128), w=[V])
        esk = kb.sb("esk", [64, 16])
        dma("sp", esk[:], gq_sinks[j, :].partition_broadcast(64), w=[esk])
        op("act", lambda e: e.activation(out=esk[:], in_=esk[:], func=AF.Exp), r=[esk], w=[esk])
        mge = kb.sb("mge", [128, 128])
        mle = kb.sb("mle", [128, 128])
        op("dve", lambda e: e.tensor_scalar(out=mge[:], in0=tri, scalar1=-1.0, scalar2=1.0, op0=ALU.mult, op1=ALU.add), r=[cm], w=[mge])
        op("dve", lambda e: e.tensor_tensor(out=mle[:], in0=tri, in1=ident(), op=ALU.add), r=[cm], w=[mle])
        sel = kb.sb("sel", [65, 64])
        op("dve", lambda e: e.memset(sel[:], 0.0), w=[sel])
        op("dve", lambda e: e.memset(sel[64:65, :], 1.0), w=[sel])
        qs = [kb.sb("qs", [64, 16, 128]) for _ in range(2)]
        pTs = [kb.sb("pT", [128, 512]) for _ in range(4)]
        Xs = [kb.sb("Xo", [65, 512]) for _ in range(2)]
        rb = [kb.sb("rb", [64, 512]) for _ in range(2)]
        oo = [kb.sb("oo", [64, 512]) for _ in range(2)]
        nq = 0
        blocks = []
        if need_ctx:
            blocks += [(tq, [(0, None), (1, None)]) for tq in range(C // 128)]
        nlb = S // 128
        for jq in range(nlb):
            ch = [(0, None), (1, None)]
            if jq > 0:
                ch.append((C // 128 + jq - 1, mge))
            ch.append((C // 128 + jq, None))
            if jq < nlb - 1:
                ch.append((C // 128 + jq + 1, mle))
            blocks.append((C // 128 + jq, ch))
        npt = 0
        for (tq, chunks) in blocks:
            q = qs[nq % 2]; nq += 1
            dma("sp", q[:], QT3[:, :, tq * 128:(tq + 1) * 128], w=[q])
            for kvh in range(2):
                for half in range(2):
                    h0 = kvh * 8 + half * 4
                    po = PS[4 + (npt % 4)]
                    for ci, (kt, msk) in enumerate(chunks):
                        pT = pTs[npt % 4]; npt += 1
                        p = nps()
                        op("pe", lambda e: e.matmul(p[:, :], kT[:, kvh, kt * 128:(kt + 1) * 128], q[:, h0:h0 + 4, :].rearrange("p h t -> p (h t)"), start=True, stop=True), r=[kT, q], w=[p])
                        op("act", lambda e: e.activation(out=pT[:], in_=p[:, :], func=AF.Exp, scale=0.125), r=[p], w=[pT])
                        if msk is not None:
                            for hh in range(4):
                                op("pool" if hh % 2 else "dve", lambda e: e.tensor_tensor(out=pT[:, hh * 128:(hh + 1) * 128], in0=pT[:, hh * 128:(hh + 1) * 128], in1=msk[:], op=ALU.mult), r=[pT, msk], w=[pT])
                        op("pe", lambda e: e.matmul(po[0:65, :], V[:, kt, kvh * 65:(kvh + 1) * 65], pT[:], start=(ci == 0), stop=(ci == len(chunks) - 1)), r=[V, pT], w=[po])
                    X = Xs[npt % 2]; r_ = rb[npt % 2]; o_ = oo[npt % 2]
                    op("act", lambda e: e.activation(out=X[:], in_=po[0:65, :], func=AF.Copy), r=[po], w=[X])
                    p = nps()
                    op("pe", lambda e: e.matmul(p[0:64, :], sel[:], X[:], start=True, stop=True), r=[sel, X], w=[p])
                    for hh in range(4):
                        op("dve", lambda e: e.tensor_scalar(out=r_[:, hh * 128:(hh + 1) * 128], in0=p[0:64, hh * 128:(hh + 1) * 128], scalar1=esk[:, h0 + hh:h0 + hh + 1], scalar2=None, op0=ALU.add), r=[p, esk], w=[r_])
                    op("dve", lambda e: e.reciprocal(out=r_[:], in_=r_[:]), r=[r_], w=[r_])
                    op("dve", lambda e: e.tensor_tensor(out=o_[:], in0=X[0:64, :], in1=r_[:], op=ALU.mult), r=[X, r_], w=[o_])
                    dma("pool", OT[h0 * 64:(h0 + 4) * 64, tq * 128:(tq + 1) * 128].rearrange("(h p) t -> p h t", p=64), o_[:].rearrange("p (h t) -> p h t", h=4), r=[o_])
        kb.pop()
        return gq_w_o[j * D:(j + 1) * D, :], D, gq_b_o[j, :]

    def mixer_mla(i, b, j, need_ctx):
        RMS_EPS = 1e-6
        kb.push()
        wd = kb.sb("mlwd", [128, 8, 448], BF16)
        for kc_ in range(8):
            dma("pool", wd[:, kc_, :], ml_wd[j * D + kc_ * 128:j * D + (kc_ + 1) * 128, :], w=[wd])
        nrow = kb.sb("nrow", [128, 384])
        dma("sp", nrow[:], ml_norm[j, :].partition_broadcast(128), w=[nrow])
        epsr = kb.sb("epsr", [128, 1])
        op("dve", lambda e: e.memset(epsr[:], RMS_EPS), w=[epsr])
        hTs = [kb.sb("hTm", [128, 8, 512], BF16) for _ in range(2)]
        cTs = [kb.sb("cTm", [128, 3, 512]) for _ in range(2)]
        krs = [kb.sb("krm", [32, 512]) for _ in range(2)]
        cs = kb.sb("csm", [32, 512]); sn = kb.sb("snm", [32, 512])
        t1 = kb.sb("t1m", [32, 512]); t2 = kb.sb("t2m", [32, 512])
        cqs = [kb.sb("cqm", [128, 384]) for _ in range(2)]
        junk = kb.sb("junk", [128, 256])
        ss = kb.sb("ssm", [128, 4])
        for gi, (t0, tl) in enumerate(groups()):
            h = hTs[gi % 2]; cT = cTs[gi % 2]; kr = krs[gi % 2]
            lat = t0 >= C
            dma("sp", h[:, :, 0:tl], HT[:, t0:t0 + tl].rearrange("(c p) t -> p c t", p=128), w=[h])
            for tt in range(tl // 128):
                cq = cqs[tt % 2]
                p = nps()
                for kc in range(8):
                    op("pe", lambda e: e.matmul(p[:, 0:384], h[:, kc, tt * 128:(tt + 1) * 128], wd[:, kc, 0:384], start=(kc == 0), stop=(kc == 7)), r=[h, wd], w=[p])
                op("act", lambda e: e.activation(out=junk[:, 0:256], in_=p[:, 0:256], func=AF.Square, accum_out=ss[:, 0:1]), r=[p], w=[junk, ss])
                op("act", lambda e: e.activation(out=junk[:, 0:128], in_=p[:, 256:384], func=AF.Square, accum_out=ss[:, 1:2]), r=[p], w=[junk, ss])
                op("act", lambda e: e.activation(out=ss[:, 2:3], in_=ss[:, 0:1], func=AF.Sqrt, bias=epsr[:, 0:1], scale=1.0 / 256), r=[ss, epsr], w=[ss])
                op("act", lambda e: e.activation(out=ss[:, 3:4], in_=ss[:, 1:2], func=AF.Sqrt, bias=epsr[:, 0:1], scale=1.0 / 128), r=[ss, epsr], w=[ss])
                op("dve", lambda e: e.reciprocal(out=ss[:, 2:4], in_=ss[:, 2:4]), r=[ss], w=[ss])
                op("dve", lambda e: e.scalar_tensor_tensor(out=cq[:, 0:256], in0=p[:, 0:256], scalar=ss[:, 2:3], in1=nrow[:, 0:256], op0=ALU.mult, op1=ALU.mult), r=[p, ss, nrow], w=[cq])
                op("dve", lambda e: e.scalar_tensor_tensor(out=cq[:, 256:384], in0=p[:, 256:384], scalar=ss[:, 3:4], in1=nrow[:, 256:384], op0=ALU.mult, op1=ALU.mult), r=[p, ss, nrow], w=[cq])
                p2 = nps()
                for c3 in range(3):
                    op("pe", lambda e: e.transpose(p2[:, c3 * 128:(c3 + 1) * 128], cq[:, c3 * 128:(c3 + 1) * 128], ident()), r=[cq, cm], w=[p2])
                op("act", lambda e: e.activation(out=cT[:, :, tt * 128:(tt + 1) * 128], in_=p2[:, 0:384].rearrange("p (c t) -> p c t", c=3), func=AF.Copy), r=[p2], w=[cT])
            dma("pool", CT[:, t0:t0 + tl].rearrange("(c p) t -> p c t", p=128), cT[:, :, 0:tl], r=[cT])
            p = nps()
            for kc in range(8):
                op("pe", lambda e: e.matmul(p[0:32, 0:tl], wd[:, kc, 384:416], h[:, kc, 0:tl], start=(kc == 0), stop=(kc == 7)), r=[h, wd], w=[p])
            if lat:
                p2 = nps()
                for kc in range(8):
                    op("pe", lambda e: e.matmul(p2[0:32, 0:tl], wd[:, kc, 416:448], h[:, kc, 0:tl], start=(kc == 0), stop=(kc == 7)), r=[h, wd], w=[p2])
                dma("sp", cs[:, 0:tl], rope32[0:32, t0 - C:t0 - C + tl], w=[cs])
                dma("sp", sn[:, 0:tl], rope32[32:64, t0 - C:t0 - C + tl], w=[sn])
                op("dve", lambda e: e.tensor_tensor(out=t1[:, 0:tl], in0=p[0:32, 0:tl], in1=cs[:, 0:tl], op=ALU.mult), r=[p, cs], w=[t1])
                op("dve", lambda e: e.tensor_tensor(out=t2[:, 0:tl], in0=p2[0:32, 0:tl], in1=sn[:, 0:tl], op=ALU.mult), r=[p2, sn], w=[t2])
                op("pool", lambda e: e.tensor_tensor(out=kr[:, 0:tl], in0=t1[:, 0:tl], in1=t2[:, 0:tl], op=ALU.add), r=[t1, t2], w=[kr])
            else:
                op("dve", lambda e: e.tensor_copy(out=kr[:, 0:tl], in_=p[0:32, 0:tl]), r=[p], w=[kr])
            dma("pool", KRT[:, t0:t0 + tl], kr[:, 0:tl], r=[kr])
        kb.pop()
        kb.push()
        cT = kb.sb("cTall", [128, 3, Tn])
        dma("sp", cT[:], CT.ap().rearrange("(c p) t -> p c t", p=128), w=[cT])
        krT = kb.sb("krT", [32, Tn], BF16)
        dma("pool", krT[:], KRT[:, :], w=[krT])
        csb = [kb.sb("csq", [32, 512]) for _ in range(2)]
        snb = [kb.sb("snq", [32, 512]) for _ in range(2)]
        sel = kb.sb("selm", [65, 64])
        op("dve", lambda e: e.memset(sel[:], 0.0), w=[sel])
        op("dve", lambda e: e.memset(sel[64:65, :], 1.0), w=[sel])
        qn = kb.sb("qn", [64, Tn], BF16); qr = kb.sb("qr", [32, Tn], BF16); kn = kb.sb("kn", [64, Tn], BF16)
        Vh = kb.sb("Vh", [128, NT, 65], BF16)
        op("dve", lambda e: e.memset(Vh[:], 1.0), w=[Vh])
        wq = kb.sb("wqh", [128, 2, 128]); wkv = kb.sb("wkvh", [128, 128])
        t1 = kb.sb("t1q", [32, 512]); t2 = kb.sb("t2q", [32, 512])
        pTs = [kb.sb("pTm", [128, 512], BF16) for _ in range(4)]
        Xs = [kb.sb("Xm", [65, 512]) for _ in range(2)]
        rb = [kb.sb("rbm", [64, 512]) for _ in range(2)]
        oo = [kb.sb("oom", [64, 512]) for _ in range(2)]
        scale = 96.0 ** -0.5
        npt = 0
        for hd in range(16):
            dma("sp", wq[:, :, 0:96], ml_wuq[j * 256:(j + 1) * 256, hd * 96:(hd + 1) * 96].rearrange("(c p) n -> p c n", p=128), w=[wq])
            dma("sp", wq[:, :, 96:128], ml_wuq[j * 256:(j + 1) * 256, 1536 + hd * 32:1536 + (hd + 1) * 32].rearrange("(c p) n -> p c n", p=128), w=[wq])
            dma("sp", wkv[:], ml_wukv[j * 128:(j + 1) * 128, hd * 128:(hd + 1) * 128], w=[wkv])
            for gi, (t0, tl) in enumerate(groups()):
                lat = t0 >= C
                p = nps()
                for kc in range(2):
                    op("pe", lambda e: e.matmul(p[0:64, 0:tl], wq[:, kc, 0:64], cT[:, kc, t0:t0 + tl], start=(kc == 0), stop=(kc == 1)), r=[wq, cT], w=[p])
                op("act", lambda e: e.activation(out=qn[:, t0:t0 + tl], in_=p[0:64, 0:tl], func=AF.Copy), r=[p], w=[qn])
                p = nps()
                for kc in range(2):
                    op("pe", lambda e: e.matmul(p[0:32, 0:tl], wq[:, kc, 64:96], cT[:, kc, t0:t0 + tl], start=(kc == 0), stop=(kc == 1)), r=[wq, cT], w=[p])
                if lat:
                    cs = csb[gi % 2]; sn = snb[gi % 2]
                    dma("sp", cs[:, 0:tl], rope32[0:32, t0 - C:t0 - C + tl], w=[cs])
                    dma("sp", sn[:, 0:tl], rope32[32:64, t0 - C:t0 - C + tl], w=[sn])
                    p2 = nps()
                    for kc in range(2):
                        op("pe", lambda e: e.matmul(p2[0:32, 0:tl], wq[:, kc, 96:128], cT[:, kc, t0:t0 + tl], start=(kc == 0), stop=(kc == 1)), r=[wq, cT], w=[p2])
                    op("dve", lambda e: e.tensor_tensor(out=t1[:, 0:tl], in0=p[0:32, 0:tl], in1=cs[:, 0:tl], op=ALU.mult), r=[p, cs], w=[t1])
                    op("dve", lambda e: e.tensor_tensor(out=t2[:, 0:tl], in0=p2[0:32, 0:tl], in1=sn[:, 0:tl], op=ALU.mult), r=[p2, sn], w=[t2])
                    op("pool", lambda e: e.tensor_tensor(out=qr[:, t0:t0 + tl], in0=t1[:, 0:tl], in1=t2[:, 0:tl], op=ALU.add), r=[t1, t2], w=[qr])
                else:
                    op("dve", lambda e: e.tensor_copy(out=qr[:, t0:t0 + tl], in_=p[0:32, 0:tl]), r=[p], w=[qr])
                p = nps()
                op("pe", lambda e: e.matmul(p[0:64, 0:tl], wkv[:, 0:64], cT[:, 2, t0:t0 + tl], start=True, stop=True), r=[wkv, cT], w=[p])
                op("act", lambda e: e.activation(out=kn[:, t0:t0 + tl], in_=p[0:64, 0:tl], func=AF.Copy), r=[p], w=[kn])
                p = nps()
                ntl = tl // 128
                for tt in range(ntl):
                    op("pe", lambda e: e.matmul(p[:, tt * 64:(tt + 1) * 64], cT[:, 2, t0 + tt * 128:t0 + (tt + 1) * 128], wkv[:, 64:128], start=True, stop=True), r=[wkv, cT], w=[p])
                op("dve", lambda e: e.tensor_copy(out=Vh[:, t0 // 128:t0 // 128 + ntl, 0:64], in_=p[:, 0:ntl * 64].rearrange("p (n d) -> p n d", d=64)), r=[p], w=[Vh])
            qgroups = [g for g in groups() if g[0] >= C]
            if need_ctx:
                qgroups = [(0, C)] + qgroups
            for (t0, tl) in qgroups:
                kts = list(range(C // 128)) if t0 < C else list(range(NT))
                po = PS[4 + (npt % 4)]
                for ci, kt in enumerate(kts):
                    pT = pTs[npt % 4]; npt += 1
                    p = nps()
                    op("pe", lambda e: e.matmul(p[:, 0:tl], kn[:, kt * 128:(kt + 1) * 128], qn[:, t0:t0 + tl], start=True, stop=False), r=[kn, qn], w=[p])
                    op("pe", lambda e: e.matmul(p[:, 0:tl], krT[:, kt * 128:(kt + 1) * 128], qr[:, t0:t0 + tl], start=False, stop=True), r=[krT, qr], w=[p])
                    op("act", lambda e: e.activation(out=pT[:, 0:tl], in_=p[:, 0:tl], func=AF.Exp, scale=scale), r=[p], w=[pT])
                    op("pe", lambda e: e.matmul(po[0:65, 0:tl], Vh[:, kt, :], pT[:, 0:tl], start=(ci == 0), stop=(ci == len(kts) - 1)), r=[Vh, pT], w=[po])
                X = Xs[npt % 2]; r_ = rb[npt % 2]; o_ = oo[npt % 2]
                op("act", lambda e: e.activation(out=X[:, 0:tl], in_=po[0:65, 0:tl], func=AF.Copy), r=[po], w=[X])
                p = nps()
                op("pe", lambda e: e.matmul(p[0:64, 0:tl], sel[:], X[:, 0:tl], start=True, stop=True), r=[sel, X], w=[p])
                op("dve", lambda e: e.reciprocal(out=r_[:, 0:tl], in_=p[0:64, 0:tl]), r=[p], w=[r_])
                op("dve", lambda e: e.tensor_tensor(out=o_[:, 0:tl], in0=X[0:64, 0:tl], in1=r_[:, 0:tl], op=ALU.mult), r=[X, r_], w=[o_])
                dma("pool", OT[hd * 64:(hd + 1) * 64, t0:t0 + tl], o_[:, 0:tl], r=[o_])
        kb.pop()
        return ml_w_o[j * D:(j + 1) * D, :], D, None

    Xsrc = xin
    kcount = {0: 0, 1: 0, 2: 0, -1: 0}
    for i, kind in enumerate(kinds):
        last = (i == L - 1)
        need_ctx = not (last and cfg.last_noctx)
        for ex in range(E):
            r0 = (i * E + ex) * D
            kb.dma_bg("pool", WGU16[ex * 128:(ex + 1) * 128, :].rearrange("p (c n) -> p c n", c=8), wgu[r0:r0 + D, :].rearrange("(c p) n -> p c n", p=128))
            kb.dma_bg("pool", WDN16[ex * 128:(ex + 1) * 128, :].rearrange("p (c n) -> p c n", c=8), wdn[r0:r0 + FF, :].rearrange("(c p) n -> p c n", p=128))
        kb.dma_bg("pool", BB16[:, 0:2 * FF], bgu[i * E:(i + 1) * E, :])
        kb.dma_bg("pool", BB16[:, 2 * FF:3 * FF], bdn[i * E:(i + 1) * E, :])
        phase_mod(i)
        for b in range(NB):
            phase_h(i, b, Xsrc)
            if kind == -1:
                w_o, Dm, b_o = mixer_identity(i, b)
            elif kind == 0:
                w_o, Dm, b_o = mixer_rg(i, b, kcount[0], need_ctx)
            elif kind == 2:
                psn[0] = 4
                w_o, Dm, b_o = mixer_mla(i, b, kcount[2], need_ctx)
                psn[0] = 8
            elif kind == 1:
                psn[0] = 4
                w_o, Dm, b_o = mixer_gqa(i, b, kcount[1], need_ctx)
                psn[0] = 8
            else:
                raise NotImplementedError
            phase_out(i, b, Xsrc, w_o, Dm, b_o, need_ctx)
        phase_moe(i, last)
        kcount[kind] += 1
        Xsrc = XB
    kb.close()
    return kb


def consts():
    cm = np.zeros((128, 416), np.float32)
    cm[:, 0:128] = np.eye(128, dtype=np.float32)
    k = np.arange(128)
    cm[:, 128:256] = (k[:, None] < k[None, :]).astype(np.float32)
    cm[:, 256:384] = 1.0
    cm[:, 384:416] = np.arange(32, dtype=np.float32)[None, :]
    return cm


def rope_table(S, rot_dim):
    rows = S // 64
    row = np.repeat(np.arange(rows, dtype=np.float32), 64)
    col = np.tile(np.arange(64, dtype=np.float32), rows)
    n_freq = rot_dim // 4
    inv = (np.float32(10000.0) ** (-np.arange(n_freq, dtype=np.float32) / np.float32(n_freq))).astype(np.float32)
    ang = np.concatenate([row[:, None] * inv, col[:, None] * inv], axis=-1).astype(np.float32)
    cos = np.cos(ang).astype(np.float32).T
    sin = np.sin(ang).astype(np.float32).T
    return np.ascontiguousarray(np.concatenate([cos, cos, -sin, sin], axis=0))


def prep_inputs(cfg, inp):
    NB, S, kinds = cfg.NB, cfg.S, cfg.kinds
    L = len(kinds)
    f = lambda a: np.ascontiguousarray(np.asarray(a, dtype=np.float32))
    shared = {
        "ada_w": f(inp["ada_w"]).reshape(L * D, 6 * D),
        "ada_b": f(inp["ada_b"]),
        "lnp": f(np.stack([inp["ln1_g"], inp["ln1_b"], inp["ln2_g"], inp["ln2_b"]], axis=1)).reshape(L * 4, D),
        "router_w": f(inp["router_w"]).reshape(L * D, E),
        "router_b": f(inp["router_b"]),
        "wgu": f(np.concatenate([inp["exp_gu_w"][..., 0::2], inp["exp_gu_w"][..., 1::2]], axis=-1)).reshape(L * E * D, 2 * FF),
        "bgu": f(np.concatenate([inp["exp_gu_b"][..., 0::2], inp["exp_gu_b"][..., 1::2]], axis=-1)).reshape(L * E, 2 * FF),
        "wdn": f(inp["exp_down_w"]).reshape(L * E * FF, D),
        "bdn": f(inp["exp_down_b"]).reshape(L * E, D),
        "cmat": consts(),
    }
    if -1 in kinds:
        shared["w_id"] = np.eye(D, dtype=np.float32)
    if 2 in kinds:
        n = kinds.count(2)
        Wd = inp["mla_w_down"][:n]
        def sw16(a):
            sh = a.shape
            return a.reshape(sh[:-1] + (sh[-1] // 32, 2, 16))[..., ::-1, :].reshape(sh)
        shared["ml_wd"] = f(np.concatenate([Wd, sw16(Wd[..., 384:416])], axis=-1)).reshape(n * D, 448)
        shared["ml_norm"] = f(np.concatenate([inp["mla_q_norm"][:n], inp["mla_kv_norm"][:n]], axis=-1))
        Wq = inp["mla_w_uq"][:n].reshape(n, 256, 16, 96)
        shared["ml_wuq"] = f(np.concatenate([Wq.reshape(n, 256, 1536), sw16(np.ascontiguousarray(Wq[..., 64:96])).reshape(n, 256, 512)], axis=-1)).reshape(n * 256, 2048)
        shared["ml_wukv"] = f(inp["mla_w_ukv"][:n]).reshape(n * 128, 2048)
        shared["ml_w_o"] = f(inp["mla_w_o"][:n]).reshape(n * D, D)
        shared["rope32"] = rope_table(S, 32)
    if 1 in kinds:
        n = kinds.count(1)
        W = inp["gqa_w_qkv"][:n]
        Bq = inp["gqa_b_qkv"][:n]
        def sw(a):
            sh = a.shape
            return a.reshape(sh[:-1] + (sh[-1] // 64, 2, 32))[..., ::-1, :].reshape(sh)
        shared["gq_w"] = f(np.concatenate([W[..., 0:1024], sw(W[..., 0:1024]), W[..., 1024:1152], sw(W[..., 1024:1152]), W[..., 1152:1280]], axis=-1)).reshape(n * D, 2432)
        b20 = Bq.reshape(n, 20, 64)
        shared["gq_b64"] = f(np.concatenate([b20.transpose(0, 2, 1), sw(Bq).reshape(n, 20, 64).transpose(0, 2, 1)], axis=-1)).reshape(n * 64, 40)
        shared["gq_bv"] = f(Bq[:, 1152:1280])
        shared["gq_sinks"] = f(inp["gqa_sinks"][:n])
        shared["gq_w_o"] = f(inp["gqa_w_o"][:n]).reshape(n * D, D)
        shared["gq_b_o"] = f(inp["gqa_b_o"][:n])
        shared["rope64"] = rope_table(S, 64)
    if 0 in kinds:
        n_rg = kinds.count(0)
        shared["rg_w_in"] = f(inp["rg_w_in"][:n_rg]).reshape(n_rg * D, 2 * D_RNN)
        shared["rg_p"] = f(np.concatenate([inp["rg_conv_w"][:n_rg], inp["rg_conv_b"][:n_rg, None, :], inp["rg_gate_a_b"][:n_rg], inp["rg_gate_x_b"][:n_rg],
                                           inp["rg_lambda"][:n_rg]], axis=1)).reshape(n_rg * 11, D_RNN)
        shared["rg_ga"] = f(inp["rg_gate_a_w"][:n_rg]).reshape(n_rg * 2 * 6 * 256, 256)
        shared["rg_gx"] = f(inp["rg_gate_x_w"][:n_rg]).reshape(n_rg * 2 * 6 * 256, 256)
        shared["rg_w_out"] = f(inp["rg_w_out"][:n_rg]).reshape(n_rg * D_RNN, D)
    maps = []
    for c in range(cfg.NC):
        bs = slice(c * NB, (c + 1) * NB)
        m = dict(shared)
        m["xin"] = f(np.concatenate([inp["ctx"][bs], inp["x"][bs]], axis=1)).reshape(NB * (C + S), D)
        m["cc"] = f(np.concatenate([inp["c"][bs], inp["c_ctx"][None, :]], axis=0))
        maps.append(m)
    return maps


def run(cfg, inp):
    kb = build(cfg)
    maps = prep_inputs(cfg, inp)
    res = run_bass_kernel_spmd(kb.nc, maps, core_ids=list(range(cfg.NC)))
    outs = [res.results[c]["out"].reshape(cfg.NB, cfg.S, D) for c in range(cfg.NC)]
    return np.concatenate(outs, axis=0)


def kernel(**inputs):
    cfg = Cfg(NB=2, S=4096, kinds=(0, 1, 2, 0), NC=8)
    return run(cfg, inputs).astype(np.float32)
```
